# Optimizing a Trainium2 kernel written in Bass

```python
import jax, jax.numpy as jnp
from jax import lax
import numpy as np

D_MODEL = 1024
BATCH = 4
SEQ = 8192
DEPTH = 1

N_META = 16
ATTN_HEADS = 4
ATTN_HEAD_DIM = 128
ATTN_W = ATTN_HEADS * ATTN_HEAD_DIM
ROPE_THETA = 500000.0
ROPE_FRACTION = 4
IDX_HEADS = 8
IDX_HEAD_DIM = 64
INDEX_TOPK = 256
CONV_CH = D_MODEL // 2
CONV_GROUPS = 4
CONV_WIDTH = 31
MIX_W = ATTN_W + CONV_CH
D_FF = 4 * D_MODEL
Q_BLOCK = 128
NORM_EPS = 1e-5
NEG = -1e30
IN_SIZES = (ATTN_W, ATTN_W, ATTN_W, IDX_HEADS * IDX_HEAD_DIM, IDX_HEAD_DIM, IDX_HEADS, CONV_CH, CONV_CH)
D_IN = sum(IN_SIZES)

kernel_name = "hymba_dsa_conformer_hybrid"


def rms_norm(x, g):
    xf = x.astype(jnp.float32)
    y = xf * lax.rsqrt(jnp.mean(xf * xf, axis=-1, keepdims=True) + NORM_EPS)
    return (y * g.astype(jnp.float32)).astype(x.dtype)


def rope_tables(T, rot_dim):
    inv = ROPE_THETA ** (-jnp.arange(0, rot_dim, 2, dtype=jnp.float32) / rot_dim)
    ang = jnp.arange(T, dtype=jnp.float32)[:, None] * inv[None, :]
    return jnp.cos(ang), jnp.sin(ang)


def partial_rope(x, rot_dim):
    T = x.shape[1]
    cos, sin = rope_tables(T, rot_dim)
    c = cos[None, :, None, :].astype(x.dtype)
    s = sin[None, :, None, :].astype(x.dtype)
    half = rot_dim // 2
    x1, x2, rest = x[..., :half], x[..., half:rot_dim], x[..., rot_dim:]
    return jnp.concatenate([x1 * c - x2 * s, x2 * c + x1 * s, rest], axis=-1)


def dsa_block(q, iq, iw, qpos, k, v, ik, k_top):
    B, Qb, H, dh = q.shape
    T = k.shape[1]
    logits = jnp.einsum('bqhd,bsd->bqhs', iq.astype(jnp.float32), ik.astype(jnp.float32)) * (IDX_HEAD_DIM ** -0.5)
    score = jnp.einsum('bqhs,bqh->bqs', jax.nn.relu(logits), iw.astype(jnp.float32))
    causal = jnp.arange(T)[None, :] <= qpos[:, None]
    score = jnp.where(causal[None], score, NEG)
    _, idx = lax.top_k(score, k_top)
    valid = idx <= qpos[None, :, None]
    gather = jax.vmap(lambda a, i: a[i])
    ks = gather(k, idx)
    vs = gather(v, idx)
    att = jnp.einsum('bqhd,bqkhd->bhqk', q, ks).astype(jnp.float32) * (ATTN_HEAD_DIM ** -0.5)
    att = jnp.where(valid[:, None], att, NEG)
    p = jax.nn.softmax(att, axis=-1).astype(v.dtype)
    o = jnp.einsum('bhqk,bqkhd->bqhd', p, vs)
    return o.reshape(B, Qb, H * dh)


def conformer_conv(a, g, conv_w, conv_b, ln_g, ln_b):
    u = a * jax.nn.sigmoid(g)
    C = u.shape[-1]
    y = lax.conv_general_dilated(
        u, conv_w[:, None, :].astype(u.dtype), window_strides=(1,),
        padding=[(CONV_WIDTH - 1, 0)], dimension_numbers=('NWC', 'WIO', 'NWC'),
        feature_group_count=C)
    y = y + conv_b.astype(u.dtype)
    B, T, _ = y.shape
    yf = y.astype(jnp.float32).reshape(B, T, CONV_GROUPS, C // CONV_GROUPS)
    mu = jnp.mean(yf, axis=-1, keepdims=True)
    var = jnp.mean(jnp.square(yf - mu), axis=-1, keepdims=True)
    yn = ((yf - mu) * lax.rsqrt(var + NORM_EPS)).reshape(B, T, C)
    yn = yn * ln_g.astype(jnp.float32) + ln_b.astype(jnp.float32)
    return jax.nn.silu(yn).astype(u.dtype)


def setup_inputs(seed: int = 0) -> dict:
    key = jax.random.key(seed)
    ks = jax.random.split(key, 13)
    f32 = jnp.float32
    return {
        "x": jax.random.normal(ks[0], (BATCH, SEQ, D_MODEL), f32),
        "meta_tokens": jax.random.normal(ks[1], (N_META, D_MODEL), f32),
        "attn_norm_g": 1.0 + 0.01 * jax.random.normal(ks[2], (DEPTH, D_MODEL), f32),
        "w_in": jax.random.normal(ks[3], (DEPTH, D_MODEL, D_IN), f32) * D_MODEL ** -0.5,
        "conv_w": jax.random.normal(ks[4], (DEPTH, CONV_WIDTH, CONV_CH), f32) * CONV_WIDTH ** -0.5,
        "conv_b": 0.01 * jax.random.normal(ks[5], (DEPTH, CONV_CH), f32),
        "conv_norm_g": 1.0 + 0.01 * jax.random.normal(ks[6], (DEPTH, CONV_CH), f32),
        "conv_norm_b": 0.01 * jax.random.normal(ks[7], (DEPTH, CONV_CH), f32),
        "w_out": jax.random.normal(ks[8], (DEPTH, MIX_W, D_MODEL), f32) * MIX_W ** -0.5,
        "mlp_norm_g": 1.0 + 0.01 * jax.random.normal(ks[9], (DEPTH, D_MODEL), f32),
        "w_up": jax.random.normal(ks[10], (DEPTH, D_MODEL, D_FF), f32) * D_MODEL ** -0.5,
        "w_down": jax.random.normal(ks[11], (DEPTH, D_FF, D_MODEL), f32) * D_FF ** -0.5,
        "final_norm_g": 1.0 + 0.01 * jax.random.normal(ks[12], (D_MODEL,), f32),
    }


def reference(x, meta_tokens, attn_norm_g, w_in, conv_w, conv_b, conv_norm_g, conv_norm_b,
              w_out, mlp_norm_g, w_up, w_down, final_norm_g):
    B, S, D = x.shape
    h = jnp.concatenate([jnp.broadcast_to(meta_tokens[None].astype(x.dtype), (B, N_META, D)), x], axis=1)
    T = h.shape[1]
    k_top = min(INDEX_TOPK, S // 4)
    n_blocks = S // Q_BLOCK
    offsets = [0]
    for sz in IN_SIZES:
        offsets.append(offsets[-1] + sz)

    for l in range(DEPTH):
        xn = rms_norm(h, attn_norm_g[l])
        proj = jnp.einsum('btd,de->bte', xn, w_in[l])
        q, k, v, iq, ik, iw, ga, gg = [proj[..., offsets[i]:offsets[i + 1]] for i in range(len(IN_SIZES))]
        q = partial_rope(q.reshape(B, T, ATTN_HEADS, ATTN_HEAD_DIM), ATTN_HEAD_DIM // ROPE_FRACTION)
        k = partial_rope(k.reshape(B, T, ATTN_HEADS, ATTN_HEAD_DIM), ATTN_HEAD_DIM // ROPE_FRACTION)
        v = v.reshape(B, T, ATTN_HEADS, ATTN_HEAD_DIM)
        iq = partial_rope(iq.reshape(B, T, IDX_HEADS, IDX_HEAD_DIM), IDX_HEAD_DIM // ROPE_FRACTION)
        ik = partial_rope(ik[:, :, None, :], IDX_HEAD_DIM // ROPE_FRACTION)[:, :, 0, :]
        iw = iw * (IDX_HEADS ** -0.5)

        o_meta = dsa_block(q[:, :N_META], iq[:, :N_META], iw[:, :N_META],
                           jnp.arange(N_META), k, v, ik, k_top)

        def to_blocks(a):
            a = a[:, N_META:].reshape((B, n_blocks, Q_BLOCK) + a.shape[2:])
            return jnp.moveaxis(a, 1, 0)

        qpos_r = N_META + jnp.arange(S).reshape(n_blocks, Q_BLOCK)
        o_real = lax.map(
            lambda xs: dsa_block(xs[0], xs[1], xs[2], xs[3], k, v, ik, k_top),
            (to_blocks(q), to_blocks(iq), to_blocks(iw), qpos_r))
        o_real = jnp.moveaxis(o_real, 0, 1).reshape(B, S, ATTN_W)
        attn_out = jnp.concatenate([o_meta, o_real], axis=1)

        conv_out = conformer_conv(ga, gg, conv_w[l], conv_b[l], conv_norm_g[l], conv_norm_b[l])

        mixed = jnp.concatenate([attn_out, conv_out], axis=-1)
        h = h + jnp.einsum('bte,ed->btd', mixed, w_out[l])

        hn = rms_norm(h, mlp_norm_g[l])
        u = jnp.square(jax.nn.relu(jnp.einsum('btd,df->btf', hn, w_up[l])))
        h = h + jnp.einsum('btf,fd->btd', u, w_down[l])

    out = rms_norm(h, final_norm_g)
    return out[:, N_META:]
```

```python
import numpy as np
from contextlib import ExitStack
import concourse.bass as bass
import concourse.mybir as mybir
from concourse.bass_utils import run_bass_kernel_spmd

F32 = mybir.dt.float32
BF16 = mybir.dt.bfloat16
ALU = mybir.AluOpType
AF = mybir.ActivationFunctionType
AX = mybir.AxisListType

D = 1024
NMETA = 16
DIN = 3144
DFF = 4096
EPS = 1e-5
KTOP = 256
NBIS = 22
ENGS = ("pe", "act", "dve", "pool", "sp")
SEM_CH = 30000
NDMA_SEM = 12


class Op:
    __slots__ = ("eng", "fn", "deps", "dma", "signals", "sig", "dma_idx")


class Sched:
    def __init__(self):
        self.ops = {e: [] for e in ENGS}
        self.lastw = {}
        self.readers = {}
        self.ndma = {e: 0 for e in ENGS}

    def op(self, eng, fn, reads=(), writes=(), dma=False):
        o = Op()
        o.eng = eng
        o.fn = fn
        o.dma = dma
        o.signals = False
        o.sig = None
        deps = {}
        xr = [k for k in reads if isinstance(k, tuple) and k[0] in ("pg", "psc", "pacc")]
        if xr:
            writes = list(writes) + [k for k in xr if k not in writes]
        for k in reads:
            w = self.lastw.get(k)
            if w is not None:
                deps[id(w)] = (w, True)
        for k in writes:
            w = self.lastw.get(k)
            if w is not None and id(w) not in deps:
                deps[id(w)] = (w, False)
            for r in self.readers.get(k, ()):
                if id(r) not in deps:
                    deps[id(r)] = (r, False)
        o.deps = []
        for d, raw in deps.values():
            if d.eng == eng and not d.dma:
                if eng == "pe":
                    continue
                if not raw and not dma:
                    continue
            d.signals = True
            o.deps.append(d)
        if dma:
            o.dma_idx = self.ndma[eng]
            self.ndma[eng] += 1
            o.signals = True
        for k in reads:
            self.readers.setdefault(k, []).append(o)
        for k in writes:
            self.lastw[k] = o
            self.readers[k] = []
        self.ops[eng].append(o)
        return o

    def emit(self, nc, stack):
        nsig = {}
        for e in ENGS:
            n = 0
            for o in self.ops[e]:
                if o.signals and not o.dma:
                    o.sig = n
                    n += 1
            nsig[e] = n
        csem = {}
        for e in ENGS:
            nch = (nsig[e] + SEM_CH - 1) // SEM_CH
            csem[e] = [stack.enter_context(nc.semaphore(f"c_{e}_{i}")) for i in range(nch)]
        dsem = {}
        for e in ENGS:
            if self.ndma[e]:
                dsem[e] = [stack.enter_context(nc.semaphore(f"d_{e}_{i}")) for i in range(NDMA_SEM)]

        def target(d):
            if d.dma:
                return dsem[d.eng][d.dma_idx % NDMA_SEM], 16 * (d.dma_idx // NDMA_SEM + 1)
            return csem[d.eng][d.sig // SEM_CH], d.sig % SEM_CH + 1

        block = stack.enter_context(nc.Block())

        def run(e):
            def body(eng):
                waited = {}
                for o in self.ops[e]:
                    tg = [target(d) for d in o.deps]
                    if o.dma and o.dma_idx >= NDMA_SEM:
                        tg.append((dsem[e][o.dma_idx % NDMA_SEM], 16 * (o.dma_idx // NDMA_SEM)))
                    for s, v in tg:
                        key = id(s)
                        if waited.get(key, 0) < v:
                            eng.wait_ge(s, v)
                            waited[key] = v
                    ins = o.fn(eng)
                    if o.signals:
                        if o.dma:
                            s, _ = target(o)
                            ins.then_inc(s, 16)
                        else:
                            ins.then_inc(csem[e][o.sig // SEM_CH], 1)
                if self.ndma[e]:
                    n = self.ndma[e]
                    for i in range(max(0, n - NDMA_SEM), n):
                        eng.wait_ge(dsem[e][i % NDMA_SEM], 16 * (i // NDMA_SEM + 1))
            return body

        for e, reg in (("pe", block.tensor), ("act", block.scalar), ("dve", block.vector),
                       ("pool", block.gpsimd), ("sp", block.sync)):
            if self.ops[e]:
                reg(run(e))


class Pool:
    def __init__(self, tiles, name):
        self.tiles = tiles
        self.name = name
        self.i = 0

    def get(self):
        j = self.i % len(self.tiles)
        self.i += 1
        return self.tiles[j], (self.name, j)


def build(S):
    T = NMETA + S
    NCH = S // 512
    NSLOT = NCH // 2
    NKT = 1 + S // 128
    nc = bass.Bass("TRN2", target_bir_lowering=False)

    def din(name, shape, dt=F32):
        return nc.dram_tensor(name, shape, dt, kind="ExternalInput").ap()

    xall = din("xall", [T, D])
    xown = din("xown", [NSLOT, 544, D])
    ropek = din("ropek", [T, 512])
    ropeq = din("ropeq", [NSLOT, 512, 512])
    qrel_d = din("qrel", [128, NSLOT * 8])
    w_in = din("w_in", [D, DIN])
    w_out = din("w_out", [D, D])
    w_up = din("w_up", [D, DFF])
    w_down = din("w_down", [DFF, D])
    attn_g = din("attn_g", [D])
    mlp_g = din("mlp_g", [D])
    final_g = din("final_g", [D])
    conv_w = din("conv_w", [31, 512])
    conv_b = din("conv_b", [512])
    cn_g = din("cn_g", [512])
    cn_b = din("cn_b", [512])
    iota_d = din("iota", [128, 512])
    pow2_d = din("pow2", [128, NBIS])
    e32_d = din("e32", [128, 32])
    g4_d = din("g4", [128, 4])
    dm_d = din("dm", [128, 4 * 128])
    out_d = nc.dram_tensor("out", [NSLOT * 512, D], F32, kind="ExternalOutput").ap()

    k_scr = nc.dram_tensor("k_scr", [4, 128, T], BF16).ap()
    v_scr = nc.dram_tensor("v_scr", [T, 512], BF16).ap()

    import os
    KSTOP = float(os.environ.get('KSTOP', '99'))
    S_ = Sched()
    stage = [0]

    def op(*a, **k):
        if stage[0] <= KSTOP:
            return S_.op(*a, **k)
        return None
    uid = [0]

    def sb(shape, dt, name=None):
        uid[0] += 1
        return nc.alloc_sbuf_tensor(f"{name or 't'}{uid[0]}", shape, dt)

    def mkpool(n, shape, dt, name):
        return Pool([sb(shape, dt, name) for _ in range(n)], name)

    identf = sb([128, 128], F32, "identf")
    ident = sb([128, 128], BF16, "ident")
    op("pool", lambda e: e.memset(identf[:], 0.0), writes=["identf"])
    op("pool", lambda e: e.affine_select(out=identf[:], in_=identf[:], pattern=[[-1, 128]],
                                         compare_op=ALU.not_equal, fill=1.0, base=0, channel_multiplier=1),
       reads=["identf"], writes=["identf"])
    op("dve", lambda e: e.tensor_copy(out=ident[:], in_=identf[:]), reads=["identf"], writes=["ident"])

    def load_const(name, shape, src, dt=F32):
        t = sb(shape, dt, name)
        op("sp", lambda e: e.dma_start(out=t[:], in_=src, allow_slow_non_contiguous=True), writes=[name], dma=True)
        return t

    iota = load_const("iota", [128, 512], iota_d)
    pow2 = load_const("pow2", [128, NBIS], pow2_d)
    e32 = load_const("e32", [128, 32], e32_d)
    g4 = load_const("g4", [128, 4], g4_d)
    qrel = load_const("qrelc", [128, NSLOT * 8], qrel_d)
    gA = load_const("gA", [128, 8], attn_g.rearrange("(c p) -> p c", p=128))
    gM = load_const("gM", [128, 8], mlp_g.rearrange("(c p) -> p c", p=128))
    def bcast_row(name, src, n):
        t = sb([128, n], F32, name)
        op("sp", lambda e: e.dma_start(out=t[:], in_=src.partition_broadcast(128)), writes=[name], dma=True)
        return t
    gF = bcast_row("gF", final_g, D)
    cb_bc = bcast_row("cb_bc", conv_b, 512)
    cg_bc = bcast_row("cg_bc", cn_g, 512)
    cnb_bc = bcast_row("cnb_bc", cn_b, 512)
    cwT = sb([128, 4, 31], F32, "cwT")
    for cc_ in range(4):
        op("sp", lambda e, cc_=cc_: e.dma_start(out=cwT[:, cc_, :], in_=conv_w[:, cc_ * 128:(cc_ + 1) * 128].rearrange("j p -> p j"),
                                                 allow_slow_non_contiguous=True), writes=["cwT"], dma=True)
    dmb = sb([128, 512], BF16, "dmb")
    op("pool", lambda e: e.dma_start(out=dmb[:], in_=dm_d), writes=["dmb"], dma=True)
    wik_t = sb([128, 8, 64], BF16, "wik")
    op("pool", lambda e: e.dma_start(out=wik_t[:, :, :], in_=w_in[:, 2048:2112].rearrange("(c p) n -> p c n", p=128)), writes=["wik"], dma=True)
    wiw_t = sb([128, 8, 8], BF16, "wiw")
    op("pool", lambda e: e.dma_start(out=wiw_t[:, :, :], in_=w_in[:, 2112:2120].rearrange("(c p) n -> p c n", p=128), allow_slow_non_contiguous=True),
       writes=["wiw"], dma=True)

    WIN_KEYS = []
    WOUT_KEYS = []
    WUP_KEYS = []
    WDOWN_KEYS = []
    win_b, wout_b, wup_b, wdown_b = w_in, w_out, w_up, w_down

    ikT = sb([128, T], BF16, "ikT")
    scores = sb([128, T], F32, "scores")
    wbuf = mkpool(4, [128, 8, 512], BF16, "wbuf")
    xt_pool = mkpool(3, [128, D], F32, "xt")
    xn_pool = mkpool(1, [128, D], BF16, "xn")
    xnT_pool = mkpool(2, [128, 8, 128], BF16, "xnT")
    rope_pool = mkpool(2, [128, 256], F32, "rope")
    st_pool = mkpool(4, [128, 8], F32, "stat")
    tm_pool = mkpool(3, [128, 512], BF16, "tm")
    rt_pool = mkpool(2, [128, 256], F32, "rt")
    jk_pool = mkpool(2, [128, 512], F32, "jk")
    bigjunk = sb([128, T], mybir.dt.uint8, "bigjunk")
    pg = Pool([nc.alloc_psum_tensor(f"pg{i}", [128, 512], F32) for i in range(4)], "pg")
    psc = [nc.alloc_psum_tensor(f"psc{i}", [128, 512], F32) for i in range(2)]
    pacc = [nc.alloc_psum_tensor(f"pacc{i}", [128, 512], F32) for i in range(2)]

    def pg_bf16(t):
        return t.bitcast(BF16) if hasattr(t, "bitcast") else None

    def rmsnorm_T(x_t, x_k, rows, gcol, xnT, xnT_k):
        st, st_k = st_pool.get()
        jk, jk_k = jk_pool.get()
        xn, xn_k = xn_pool.get()
        op("act", lambda e: e.activation(out=jk[0:rows, :], in_=x_t[0:rows, 0:512], func=AF.Square,
                                         accum_out=st[0:rows, 0:1]), reads=[x_k], writes=[jk_k, (st_k, 0)])
        op("act", lambda e: e.activation(out=jk[0:rows, :], in_=x_t[0:rows, 512:1024], func=AF.Square,
                                         accum_out=st[0:rows, 1:2]), reads=[x_k], writes=[jk_k, (st_k, 1)])
        op("dve", lambda e: e.tensor_tensor(out=st[0:rows, 2:3], in0=st[0:rows, 0:1], in1=st[0:rows, 1:2], op=ALU.add),
           reads=[(st_k, 0), (st_k, 1)], writes=[(st_k, 2)])
        op("dve", lambda e: e.tensor_scalar(out=st[0:rows, 3:4], in0=st[0:rows, 2:3], scalar1=1.0 / D, scalar2=EPS,
                                            op0=ALU.mult, op1=ALU.add), reads=[(st_k, 2)], writes=[(st_k, 3)])
        op("act", lambda e: e.activation(out=st[0:rows, 4:5], in_=st[0:rows, 3:4], func=AF.Sqrt),
           reads=[(st_k, 3)], writes=[(st_k, 4)])
        op("dve", lambda e: e.reciprocal(out=st[0:rows, 5:6], in_=st[0:rows, 4:5]), reads=[(st_k, 4)], writes=[(st_k, 5)])
        op("dve", lambda e: e.tensor_scalar(out=xn[0:rows, :], in0=x_t[0:rows, :], scalar1=st[0:rows, 5:6], scalar2=None,
                                            op0=ALU.mult), reads=[x_k, (st_k, 5)], writes=[xn_k])
        pt, pt_k = pg.get()
        ptb = pt[:].bitcast(BF16)
        for dc in range(8):
            op("pe", lambda e, dc=dc: e.transpose(out=ptb[:, dc * 128:dc * 128 + rows], in_=xn[0:rows, dc * 128:(dc + 1) * 128],
                                                  identity=ident[0:rows, 0:rows]),
               reads=[xn_k, "ident"], writes=[pt_k])
        op("dve", lambda e: e.tensor_tensor(out=xnT[:, :, 0:rows],
                                            in0=ptb.rearrange("p (c t) -> p c t", c=8)[:, :, 0:rows],
                                            in1=gcol[:, :].unsqueeze(2).to_broadcast([128, 8, rows]), op=ALU.mult),
           reads=[pt_k], writes=[xnT_k])
        return (st, st_k)

    def load_w(src_ap, keys):
        w, w_k = wbuf.get()
        ncols = src_ap.shape[1]
        op("pool", lambda e: e.dma_start(out=w[:, :, 0:ncols], in_=src_ap.rearrange("(c p) n -> p c n", p=128)),
           reads=keys, writes=[w_k], dma=True)
        return w, w_k

    def proj_tm(xnT, xnT_k, rows, w, w_k, ncols, ps, ps_k, pcol=0):
        for dc in range(8):
            op("pe", lambda e, dc=dc: e.matmul(ps[0:rows, pcol:pcol + ncols], lhsT=xnT[:, dc, 0:rows], rhs=w[:, dc, 0:ncols],
                                               start=(dc == 0), stop=(dc == 7)),
               reads=[xnT_k, w_k], writes=[ps_k])


    def rope_tm(ps, ps_k, rows, nh, hd, half, rp, rp_k, roff, dst, dst_k, dcol=0):
        r2 = 2 * half
        n = nh * hd
        xf, xf_k = jk_pool.get()
        op("act", lambda e: e.copy(out=xf[0:rows, 0:n], in_=ps[0:rows, 0:n]), reads=[ps_k], writes=[xf_k])
        op("act", lambda e: e.copy(out=dst[0:rows, dcol:dcol + n], in_=ps[0:rows, 0:n]), reads=[ps_k], writes=[dst_k])
        rt, rt_k = rt_pool.get()
        pv = xf[0:rows, 0:n].rearrange("p (h d) -> p h d", h=nh)
        A = rt[0:rows, 0:nh * r2].rearrange("p (h d) -> p h d", h=nh)
        Bm = rt[0:rows, 128:128 + nh * r2].rearrange("p (h d) -> p h d", h=nh)
        cc_ = rp[0:rows, roff:roff + nh * r2].rearrange("p (h d) -> p h d", h=nh)
        ss_ = rp[0:rows, roff + 128:roff + 128 + nh * r2].rearrange("p (h d) -> p h d", h=nh)
        op("dve", lambda e: e.tensor_tensor(out=A, in0=pv[:, :, 0:r2], in1=cc_, op=ALU.mult), reads=[xf_k, rp_k], writes=[(rt_k, 0)])
        op("dve", lambda e: e.tensor_tensor(out=Bm, in0=pv[:, :, 0:r2], in1=ss_, op=ALU.mult), reads=[xf_k, rp_k], writes=[(rt_k, 1)])
        dv = dst[0:rows, dcol:dcol + n].rearrange("p (h d) -> p h d", h=nh)
        op("dve", lambda e: e.tensor_tensor(out=dv[:, :, 0:half], in0=A[:, :, 0:half], in1=Bm[:, :, half:r2], op=ALU.subtract),
           reads=[(rt_k, 0), (rt_k, 1), dst_k], writes=[dst_k])
        op("dve", lambda e: e.tensor_tensor(out=dv[:, :, half:r2], in0=A[:, :, half:r2], in1=Bm[:, :, 0:half], op=ALU.add),
           reads=[(rt_k, 0), (rt_k, 1), dst_k], writes=[dst_k])

    stage[0] = 1
    wk, wk_k = load_w(win_b[:, 512:1024], WIN_KEYS)
    wv, wv_k = load_w(win_b[:, 1024:1536], WIN_KEYS)
    wik, wik_k = wik_t, "wik"
    stage[0] = 1
    for kt in range(min(NKT, int(os.environ.get('KT_MAX', '999')))):
        rows = NMETA if kt == 0 else 128
        p0 = 0 if kt == 0 else NMETA + 128 * (kt - 1)
        xt, xt_k = xt_pool.get()
        op("sp", lambda e, xt=xt, p0=p0, rows=rows: e.dma_start(out=xt[0:rows, :], in_=xall[p0:p0 + rows, :]), writes=[xt_k], dma=True)
        rp, rp_k = rope_pool.get()
        op("sp", lambda e, rp=rp, p0=p0, rows=rows: e.dma_start(out=rp[0:rows, :], in_=ropek[p0:p0 + rows, 0:256]), writes=[rp_k], dma=True)
        rpi, rpi_k = rope_pool.get()
        op("sp", lambda e, rpi=rpi, p0=p0, rows=rows: e.dma_start(out=rpi[0:rows, :], in_=ropek[p0:p0 + rows, 256:512]), writes=[rpi_k], dma=True)
        xnT, xnT_k = xnT_pool.get()
        stage[0] = 1.1
        rmsnorm_T(xt, xt_k, rows, gA, xnT, xnT_k)
        stage[0] = 1.2
        ps, ps_k = pg.get()
        proj_tm(xnT, xnT_k, rows, wk, wk_k, 512, ps, ps_k)
        stage[0] = 1.25
        kb, kb_k = tm_pool.get()
        rope_tm(ps, ps_k, rows, 4, 128, 16, rp, rp_k, 0, kb, kb_k)
        stage[0] = 1.3
        pt, pt_k = pg.get()
        ptb = pt[:].bitcast(BF16)
        for h in range(4):
            op("pe", lambda e, h=h, ptb=ptb, kb=kb, rows=rows: e.transpose(out=ptb[:, h * 128:h * 128 + rows], in_=kb[0:rows, h * 128:(h + 1) * 128],
                                                                         identity=ident[0:rows, 0:rows]), reads=[kb_k, "ident"], writes=[pt_k])
        kTt, kTt_k = tm_pool.get()
        op("act", lambda e, kTt=kTt, ptb=ptb: e.copy(out=kTt[:, :], in_=ptb[:, 0:512]), reads=[pt_k], writes=[kTt_k])
        op("sp", lambda e, kTt=kTt, p0=p0, rows=rows: e.dma_start(
            out=k_scr[:, :, p0:p0 + rows].rearrange("h p t -> p h t"),
            in_=kTt[:, :].rearrange("p (h t) -> p h t", h=4)[:, :, 0:rows]),
           reads=[kTt_k], writes=[("k_scr", kt)], dma=True)
        stage[0] = 1.4
        ps, ps_k = pg.get()
        proj_tm(xnT, xnT_k, rows, wv, wv_k, 512, ps, ps_k)
        vb, vb_k = tm_pool.get()
        op("act", lambda e, vb=vb, ps=ps, rows=rows: e.copy(out=vb[0:rows, :], in_=ps[0:rows, :]), reads=[ps_k], writes=[vb_k])
        op("sp", lambda e, vb=vb, p0=p0, rows=rows: e.dma_start(out=v_scr[p0:p0 + rows, :], in_=vb[0:rows, :]),
           reads=[vb_k], writes=[("v_scr", kt)], dma=True)
        stage[0] = 1.5
        ps, ps_k = pg.get()
        proj_tm(xnT, xnT_k, rows, wik, wik_k, 64, ps, ps_k)
        ib, ib_k = tm_pool.get()
        rope_tm(ps, ps_k, rows, 1, 64, 8, rpi, rpi_k, 0, ib, ib_k)
        op("dve", lambda e, ib=ib, rows=rows: e.tensor_copy(out=ib[0:rows, 64:128], in_=ib[0:rows, 0:64]), reads=[ib_k], writes=[ib_k])
        pt, pt_k = pg.get()
        ptb = pt[:].bitcast(BF16)
        op("pe", lambda e, ptb=ptb, ib=ib, rows=rows: e.transpose(out=ptb[:, 0:rows], in_=ib[0:rows, 0:128], identity=ident[0:rows, 0:rows]),
           reads=[ib_k, "ident"], writes=[pt_k])
        op("act", lambda e, ptb=ptb, p0=p0, rows=rows: e.copy(out=ikT[:, p0:p0 + rows], in_=ptb[:, 0:rows]), reads=[pt_k], writes=[("ikT", kt)])

    stage[0] = 2
    qT = sb([128, 4, 512], BF16, "qT")
    iqT = sb([128, 4, 512], BF16, "iqT")
    iwt = sb([128, 4, 8], F32, "iwt")
    uT = sb([128, 4, 544], BF16, "uT")
    mixed = sb([128, 4, D], BF16, "mixed")
    ysb = sb([128, 4, 512], F32, "ysb")
    Dcc = sb([128, 31, 128], BF16, "Dcc")
    negm_pool = mkpool(2, [128, 512], F32, "negm")
    sel = sb([128, 8, 128], BF16, "sel")
    lw = sb([128, 2, 128], BF16, "lw")
    w2 = sb([128, 8], F32, "w2")
    g4b = sb([128, 4], BF16, "g4b")
    op("dve", lambda e: e.tensor_copy(out=g4b[:], in_=g4[:]), reads=["g4"], writes=["g4b"])
    bis = sb([128, 8 + NBIS], F32, "bis")
    top8 = sb([128, 8], F32, "top8")
    cst = sb([128, 4, 6], F32, "cst")
    cmv = sb([128, 4, 2], F32, "cmv")
    crs = sb([128, 4], F32, "crs")
    R_pool = mkpool(3, [128, 512], BF16, "R")
    PT_pool = mkpool(2, [128, 512], BF16, "PT")
    mb_pool = mkpool(2, [128, 512], BF16, "mb")
    GK = 4
    kv_pool = [(sb([128, 4, GK * 128], BF16, "kbuf"), sb([128, GK, 4, 129], BF16, "vbuf")) for _ in range(2)]
    for i_, (kb_, vb_) in enumerate(kv_pool):
        op("pool", lambda e, vb_=vb_: e.memset(vb_[:, :, :, 128:129], 1.0), writes=[("vbuf", i_)])
    kmeta = sb([128, 4, NMETA], BF16, "kmeta")
    vmeta = sb([128, 4, 129], BF16, "vmeta")
    op("pool", lambda e: e.memset(vmeta[:, :, 128:129], 1.0), writes=["vmeta"])
    op("sp", lambda e: e.dma_start(out=kmeta[:, :, :], in_=k_scr[:, :, 0:NMETA].rearrange("h p t -> p h t")),
       reads=[("k_scr", 0)], writes=["kmeta"], dma=True)
    op("sp", lambda e: e.dma_start(out=vmeta[0:NMETA, :, 0:128], in_=v_scr[0:NMETA, :].rearrange("p (h d) -> p h d", h=4)),
       reads=[("v_scr", 0), "vmeta"], writes=["vmeta"], dma=True)
    mT = sb([128, 8, 128], BF16, "mT")
    hnT = sb([128, 8, 256], BF16, "hnT")
    uf_pool = mkpool(2, [128, 256], BF16, "uf")
    rden = sb([128, 4], F32, "rden")

    SC = 0.125 * (8 ** -0.5)
    kv_ctr = [0]

    for sl in range(NSLOT):
        E = NMETA + 512 * (2 * sl + 2)
        W0 = E - 1024
        NCHK = 2 * sl + 2
        wq, wq_k = load_w(win_b[:, 0:512], WIN_KEYS)
        wiq, wiq_k = load_w(win_b[:, 1536:2048], WIN_KEYS)
        wga, wga_k = load_w(win_b[:, 2120:2632], WIN_KEYS)
        wgg, wgg_k = load_w(win_b[:, 2632:3144], WIN_KEYS)
        for ti in range(5):
            rows = 32 if ti == 0 else 128
            r0 = 0 if ti == 0 else 32 + 128 * (ti - 1)
            xt, xt_k = xt_pool.get()
            op("sp", lambda e, xt=xt, r0=r0, rows=rows, sl=sl: e.dma_start(out=xt[0:rows, :], in_=xown[sl, r0:r0 + rows, :]),
               writes=[xt_k], dma=True)
            xnT, xnT_k = xnT_pool.get()
            rmsnorm_T(xt, xt_k, rows, gA, xnT, xnT_k)
            if ti > 0:
                tb = ti - 1
                rp, rp_k = rope_pool.get()
                op("sp", lambda e, rp=rp, tb=tb, sl=sl: e.dma_start(out=rp[:, :], in_=ropeq[sl, 128 * tb:128 * (tb + 1), 0:256]), writes=[rp_k], dma=True)
                ps, ps_k = pg.get()
                proj_tm(xnT, xnT_k, 128, wq, wq_k, 512, ps, ps_k)
                qb, qb_k = tm_pool.get()
                rope_tm(ps, ps_k, 128, 4, 128, 16, rp, rp_k, 0, qb, qb_k)
                pt, pt_k = pg.get()
                ptb = pt[:].bitcast(BF16)
                for h in range(4):
                    op("pe", lambda e, h=h, ptb=ptb, qb=qb: e.transpose(out=ptb[:, h * 128:(h + 1) * 128], in_=qb[:, h * 128:(h + 1) * 128], identity=ident[:, :]),
                       reads=[qb_k, "ident"], writes=[pt_k])
                op("act", lambda e, ptb=ptb, tb=tb: e.copy(out=qT[:, :, tb * 128:(tb + 1) * 128], in_=ptb[:, 0:512].rearrange("p (h t) -> p h t", h=4)),
                   reads=[pt_k], writes=[("qT", tb)])
                rpi, rpi_k = rope_pool.get()
                op("sp", lambda e, rpi=rpi, tb=tb, sl=sl: e.dma_start(out=rpi[:, :], in_=ropeq[sl, 128 * tb:128 * (tb + 1), 256:512]), writes=[rpi_k], dma=True)
                ps, ps_k = pg.get()
                proj_tm(xnT, xnT_k, 128, wiq, wiq_k, 512, ps, ps_k)
                ib, ib_k = tm_pool.get()
                rope_tm(ps, ps_k, 128, 8, 64, 8, rpi, rpi_k, 0, ib, ib_k)
                pt, pt_k = pg.get()
                ptb = pt[:].bitcast(BF16)
                for hp in range(4):
                    op("pe", lambda e, hp=hp, ptb=ptb, ib=ib: e.transpose(out=ptb[:, hp * 128:(hp + 1) * 128], in_=ib[:, hp * 128:(hp + 1) * 128], identity=ident[:, :]),
                       reads=[ib_k, "ident"], writes=[pt_k])
                op("act", lambda e, ptb=ptb, tb=tb: e.copy(out=iqT[:, tb, :].rearrange("p (g h i) -> p h g i", g=4, h=4, i=32),
                                                           in_=ptb[:, 0:512].rearrange("p (h g i) -> p h g i", h=4, g=4, i=32)),
                   reads=[pt_k], writes=[("iqT", tb)])
                ps, ps_k = pg.get()
                proj_tm(xnT, xnT_k, 128, wiw_t, "wiw", 8, ps, ps_k)
                op("dve", lambda e, ps=ps, tb=tb: e.tensor_scalar(out=iwt[:, tb, :], in0=ps[:, 0:8], scalar1=SC, scalar2=None, op0=ALU.mult),
                   reads=[ps_k], writes=[("iwt", tb)])
            psa, psa_k = pg.get()
            proj_tm(xnT, xnT_k, rows, wga, wga_k, 512, psa, psa_k)
            psg, psg_k = pg.get()
            proj_tm(xnT, xnT_k, rows, wgg, wgg_k, 512, psg, psg_k)
            jk, jk_k = jk_pool.get()
            op("act", lambda e, jk=jk, psg=psg, rows=rows: e.activation(out=jk[0:rows, :], in_=psg[0:rows, :], func=AF.Sigmoid),
               reads=[psg_k], writes=[jk_k])
            ub, ub_k = tm_pool.get()
            op("dve", lambda e, ub=ub, psa=psa, jk=jk, rows=rows: e.tensor_tensor(out=ub[0:rows, :], in0=psa[0:rows, :], in1=jk[0:rows, :], op=ALU.mult),
               reads=[psa_k, jk_k], writes=[ub_k])
            pt, pt_k = pg.get()
            ptb = pt[:].bitcast(BF16)
            for cc in range(4):
                op("pe", lambda e, cc=cc, ptb=ptb, ub=ub, rows=rows: e.transpose(out=ptb[:, cc * 128:cc * 128 + rows], in_=ub[0:rows, cc * 128:(cc + 1) * 128],
                                                                              identity=ident[0:rows, 0:rows]),
                   reads=[ub_k, "ident"], writes=[pt_k])
            op("act", lambda e, ptb=ptb, r0=r0, rows=rows: e.copy(out=uT[:, :, r0:r0 + rows], in_=ptb[:, 0:512].rearrange("p (c t) -> p c t", c=4)[:, :, 0:rows]),
               reads=[pt_k], writes=[("uT", ti)])
        stage[0] = 3
        UTK = [("uT", ti) for ti in range(5)]
        for cc in range(4):
            op("dve", lambda e, cc=cc: e.tensor_tensor(out=Dcc[:, :, :], in0=identf[:, :].unsqueeze(1).to_broadcast([128, 31, 128]),
                                                       in1=cwT[:, cc, :].unsqueeze(2).to_broadcast([128, 31, 128]), op=ALU.mult),
               reads=["identf", "cwT"], writes=["Dcc"])
            for tb in range(4):
                ps, ps_k = pg.get()
                for j in range(31):
                    c0 = 2 + tb * 128 + j
                    op("pe", lambda e, ps=ps, cc=cc, c0=c0, j=j: e.matmul(ps[:, 0:128], lhsT=uT[:, cc, c0:c0 + 128], rhs=Dcc[:, j, :], start=(j == 0), stop=(j == 30)),
                       reads=UTK + ["Dcc"], writes=[ps_k])
                op("act", lambda e, ps=ps, tb=tb, cc=cc: e.copy(out=ysb[:, tb, cc * 128:(cc + 1) * 128], in_=ps[:, 0:128]),
                   reads=[ps_k], writes=[("ysb", tb)])
        for tb in range(4):
            yk = ("ysb", tb)
            y = ysb[:, tb, :]
            op("dve", lambda e, y=y: e.tensor_tensor(out=y, in0=y, in1=cb_bc[:, :], op=ALU.add), reads=[yk, "cb_bc"], writes=[yk])
            for g in range(4):
                op("dve", lambda e, y=y, g=g: e.bn_stats(out=cst[:, g, :], in_=y[:, g * 128:(g + 1) * 128]), reads=[yk], writes=[("cst", g)])
                op("dve", lambda e, g=g: e.bn_aggr(out=cmv[:, g, :], in_=cst[:, g, :]), reads=[("cst", g)], writes=[("cmv", g)])
            CM = [("cmv", g) for g in range(4)]
            op("dve", lambda e: e.tensor_scalar(out=crs[:, :], in0=cmv[:, :, 1], scalar1=EPS, scalar2=None, op0=ALU.add), reads=CM, writes=["crs"])
            op("act", lambda e: e.activation(out=crs[:, :], in_=crs[:, :], func=AF.Sqrt), reads=["crs"], writes=["crs"])
            op("dve", lambda e: e.reciprocal(out=crs[:, :], in_=crs[:, :]), reads=["crs"], writes=["crs"])
            for g in range(4):
                op("dve", lambda e, y=y, g=g: e.tensor_scalar(out=y[:, g * 128:(g + 1) * 128], in0=y[:, g * 128:(g + 1) * 128],
                                                            scalar1=cmv[:, g, 0:1], scalar2=crs[:, g:g + 1], op0=ALU.subtract, op1=ALU.mult),
                   reads=[yk, "crs"] + CM, writes=[yk])
            op("dve", lambda e, y=y: e.tensor_tensor(out=y, in0=y, in1=cg_bc[:, :], op=ALU.mult), reads=[yk, "cg_bc"], writes=[yk])
            op("dve", lambda e, y=y: e.tensor_tensor(out=y, in0=y, in1=cnb_bc[:, :], op=ALU.add), reads=[yk, "cnb_bc"], writes=[yk])
            op("act", lambda e, y=y, tb=tb: e.activation(out=mixed[:, tb, 512:1024], in_=y, func=AF.Silu), reads=[yk], writes=[("mixC", tb)])

        stage[0] = 4
        for tb in range(4):
            for h in range(8):
                par, hp = h % 2, h // 2
                op("pool", lambda e, h=h, par=par, hp=hp, tb=tb: e.tensor_scalar(out=lw[:, par, hp * 32:(hp + 1) * 32], in0=e32[:, :], scalar1=iwt[:, tb, h:h + 1],
                                                                                 scalar2=None, op0=ALU.mult),
                   reads=["e32", ("iwt", tb)], writes=[("lw", h)])
            LWK = [("lw", h) for h in range(8)]
            ps, ps_k = pg.get()
            for par in range(2):
                op("pe", lambda e, ps=ps, par=par: e.matmul(ps[:, par * 4:(par + 1) * 4], lhsT=lw[:, par, :], rhs=g4b[:, :], start=True, stop=True),
                   reads=LWK + ["g4b"], writes=[ps_k])
            op("dve", lambda e, ps=ps: e.tensor_copy(out=w2[:, :], in_=ps[:, 0:8]), reads=[ps_k], writes=["w2"])
            for idx in range(8):
                g = idx % 4
                op("pool", lambda e, idx=idx, g=g: e.tensor_scalar(out=sel[:, idx, :], in0=dmb[:, g * 128:(g + 1) * 128], scalar1=w2[:, idx:idx + 1],
                                                                   scalar2=None, op0=ALU.mult),
                   reads=["dmb", "w2"], writes=[("sel", idx)])
            chunks = [(0, NMETA)] + [(NMETA + 512 * m, 512) for m in range(NCHK)]
            SCK = []
            wcnt = 0
            for ci, (c0, cn) in enumerate(chunks):
                ikk = [("ikT", 0)] if ci == 0 else [("ikT", 1 + 4 * (ci - 1) + q) for q in range(4)]
                sc = psc[ci % 2]
                sc_k = ("psc", ci % 2)
                pend = None
                items = [(g, par) for par in range(2) for g in range(4)]
                for k, (g, par) in enumerate(items):
                    ps, ps_k = pg.get()
                    t0 = tb * 128 + g * 32
                    op("pe", lambda e, ps=ps, par=par, tb=tb, g=g, c0=c0, cn=cn: e.matmul(ps[:, 0:cn], lhsT=iqT[par * 64:(par + 1) * 64, tb, g * 128:(g + 1) * 128],
                                                                                    rhs=ikT[par * 64:(par + 1) * 64, c0:c0 + cn], start=True, stop=True),
                       reads=[("iqT", tb)] + ikk, writes=[ps_k])
                    R, R_k = R_pool.get()
                    if k % 2 == 0:
                        op("act", lambda e, R=R, ps=ps, cn=cn: e.activation(out=R[:, 0:cn], in_=ps[:, 0:cn], func=AF.Relu), reads=[ps_k], writes=[R_k])
                    else:
                        op("dve", lambda e, R=R, ps=ps, cn=cn: e.tensor_scalar(out=R[:, 0:cn], in0=ps[:, 0:cn], scalar1=0.0, scalar2=None, op0=ALU.max),
                           reads=[ps_k], writes=[R_k])
                    if pend is not None:
                        pk, pR, pR_k, pidx = pend
                        op("pe", lambda e, sc=sc, pidx=pidx, pR=pR, cn=cn, pk=pk: e.matmul(sc[:, 0:cn], lhsT=sel[:, pidx, :], rhs=pR[:, 0:cn], start=(pk == 0), stop=False),
                           reads=[("sel", pidx), pR_k], writes=[sc_k])
                    pend = (k, R, R_k, par * 4 + g)
                pk, pR, pR_k, pidx = pend
                op("pe", lambda e, sc=sc, pidx=pidx, pR=pR, cn=cn: e.matmul(sc[:, 0:cn], lhsT=sel[:, pidx, :], rhs=pR[:, 0:cn], start=False, stop=True),
                   reads=[("sel", pidx), pR_k], writes=[sc_k])
                sk = ("sc", ci)
                SCK.append(sk)
                if c0 >= W0 and ci > 0:
                    col = (sl * 4 + tb) * 2 + wcnt
                    negm, negm_k = negm_pool.get()
                    op("dve", lambda e, col=col, negm=negm: e.tensor_scalar(out=negm[:, :], in0=iota[:, :], scalar1=qrel[:, col:col + 1], scalar2=-1e30,
                                                                          op0=ALU.is_gt, op1=ALU.mult), reads=["iota", "qrelc"], writes=[negm_k])
                    op("dve", lambda e, sc=sc, c0=c0, negm=negm: e.tensor_tensor(out=scores[:, c0:c0 + 512], in0=sc[:, :], in1=negm[:, :], op=ALU.add),
                       reads=[sc_k, negm_k], writes=[sk])
                    jk, jk_k = jk_pool.get()
                    op("dve", lambda e, sc=sc, jk=jk, negm=negm: e.tensor_tensor(out=jk[:, :], in0=sc[:, :], in1=negm[:, :], op=ALU.subtract),
                       reads=[sc_k, negm_k], writes=[jk_k])
                    op("dve", lambda e, jk=jk, wcnt=wcnt: e.tensor_reduce(out=bis[:, 5 + wcnt:6 + wcnt], in_=jk[:, :], axis=AX.X, op=ALU.min),
                       reads=[jk_k], writes=[("bis", 5 + wcnt)])
                    wcnt += 1
                else:
                    op("act", lambda e, sc=sc, c0=c0, cn=cn: e.copy(out=scores[:, c0:c0 + cn], in_=sc[:, 0:cn]), reads=[sc_k], writes=[sk])
            assert wcnt == 2
            stage[0] = 5
            op("dve", lambda e, E=E: e.max(out=top8[:, :], in_=scores[:, 0:E]), reads=SCK, writes=["top8"])
            op("dve", lambda e, W0=W0: e.tensor_reduce(out=bis[:, 7:8], in_=scores[:, 0:W0], axis=AX.X, op=ALU.min), reads=SCK, writes=[("bis", 7)])
            op("dve", lambda e: e.tensor_reduce(out=bis[:, 0:1], in_=bis[:, 5:8], axis=AX.X, op=ALU.min),
               reads=[("bis", 5), ("bis", 6), ("bis", 7)], writes=[("bis", 0)])
            op("dve", lambda e: e.tensor_tensor(out=bis[:, 1:2], in0=top8[:, 0:1], in1=bis[:, 0:1], op=ALU.subtract),
               reads=["top8", ("bis", 0)], writes=[("bis", 1)])
            op("dve", lambda e: e.tensor_scalar(out=bis[:, 8:8 + NBIS], in0=pow2[:, :], scalar1=bis[:, 1:2], scalar2=None, op0=ALU.mult),
               reads=["pow2", ("bis", 1)], writes=["bisW"])
            for k in range(NBIS):
                op("dve", lambda e, k=k: e.tensor_tensor(out=bis[:, 2:3], in0=bis[:, 8 + k:9 + k], in1=bis[:, 0:1], op=ALU.add),
                   reads=["bisW", ("bis", 0)], writes=[("bis", 2)])
                op("dve", lambda e, E=E: e.tensor_scalar(out=bigjunk[:, 0:E], in0=scores[:, 0:E], scalar1=bis[:, 2:3], scalar2=None,
                                                        op0=ALU.is_ge, op1=ALU.add, accum_out=bis[:, 3:4]),
                   reads=SCK + [("bis", 2)], writes=["bigjunk", ("bis", 3)])
                op("dve", lambda e, k=k: e.tensor_scalar(out=bis[:, 4:5], in0=bis[:, 3:4], scalar1=KTOP - 0.5, scalar2=bis[:, 8 + k:9 + k],
                                                        op0=ALU.is_ge, op1=ALU.mult), reads=[("bis", 3), "bisW"], writes=[("bis", 4)])
                op("dve", lambda e: e.tensor_tensor(out=bis[:, 0:1], in0=bis[:, 0:1], in1=bis[:, 4:5], op=ALU.add),
                   reads=[("bis", 0), ("bis", 4)], writes=[("bis", 0)])
            stage[0] = 6
            nkb_total = 1 + 4 * NCHK
            kbi = 0
            for ci, (c0, cn) in enumerate(chunks):
                mbt, mb_k = mb_pool.get()
                op("dve", lambda e, mbt=mbt, c0=c0, cn=cn: e.tensor_scalar(out=mbt[:, 0:cn], in0=scores[:, c0:c0 + cn], scalar1=bis[:, 0:1], scalar2=-30000.0,
                                                                          op0=ALU.is_lt, op1=ALU.mult), reads=[SCK[ci], ("bis", 0)], writes=[mb_k])
                if ci == 0:
                    kbuf, vbuf, kvk, vvk = kmeta, vmeta, "kmeta", "vmeta"
                    nk = 1
                else:
                    kvn = kv_ctr[0] % 2
                    kv_ctr[0] += 1
                    kbuf, vbuf = kv_pool[kvn]
                    kvk, vvk = ("kbuf", kvn), ("vbuf", kvn)
                    tl = [1 + 4 * (ci - 1) + q for q in range(4)]
                    op("sp", lambda e, kbuf=kbuf, c0=c0: e.dma_start(out=kbuf[:, :, :], in_=k_scr[:, :, c0:c0 + 512].rearrange("h p t -> p h t")),
                       reads=[("k_scr", t) for t in tl], writes=[kvk], dma=True)
                    for g_ in range(4):
                        op("sp", lambda e, vbuf=vbuf, c0=c0, g_=g_: e.dma_start(out=vbuf[:, g_, :, 0:128],
                                                                         in_=v_scr[c0 + g_ * 128:c0 + (g_ + 1) * 128, :].rearrange("p (h d) -> p h d", h=4)),
                           reads=[("v_scr", t) for t in tl], writes=[vvk], dma=True)
                    nk = 4
                for kb in range(nk):
                    ks = NMETA if ci == 0 else 128
                    st, st_k = pg.get()
                    for h in range(4):
                        op("pe", lambda e, st=st, h=h, kbuf=kbuf, kb=kb, ks=ks, tb=tb: e.matmul(st[0:ks, h * 128:(h + 1) * 128], lhsT=kbuf[:, h, kb * 128:kb * 128 + ks],
                                                                                             rhs=qT[:, h, tb * 128:(tb + 1) * 128], start=True, stop=False),
                           reads=[kvk, ("qT", tb)], writes=[st_k])
                        op("pe", lambda e, st=st, h=h, mbt=mbt, kb=kb, ks=ks: e.matmul(st[0:ks, h * 128:(h + 1) * 128], lhsT=mbt[:, kb * 128:kb * 128 + ks],
                                                                                     rhs=ident[:, :], start=False, stop=True),
                           reads=[mb_k, "ident"], writes=[st_k])
                    PT, PT_k = PT_pool.get()
                    op("act", lambda e, PT=PT, st=st, ks=ks: e.activation(out=PT[0:ks, :], in_=st[0:ks, :], func=AF.Exp, scale=128 ** -0.5),
                       reads=[st_k], writes=[PT_k])
                    for h in range(4):
                        acc = pacc[h // 2]
                        a0 = (h % 2) * 256
                        rhs = vbuf[0:ks, h, :] if ci == 0 else vbuf[:, kb, h, :]
                        op("pe", lambda e, acc=acc, a0=a0, PT=PT, h=h, ks=ks, rhs=rhs, kbi=kbi: e.matmul(acc[:, a0:a0 + 129], lhsT=PT[0:ks, h * 128:(h + 1) * 128], rhs=rhs,
                                                                                                   start=(kbi == 0 and h % 2 == 0), stop=(kbi == nkb_total - 1),
                                                                                                   skip_group_check=True),
                           reads=[PT_k, vvk], writes=[("pacc", h // 2)])
                    kbi += 1
            for h in range(4):
                acc = pacc[h // 2]
                a0 = (h % 2) * 256
                op("dve", lambda e, acc=acc, a0=a0, h=h: e.reciprocal(out=rden[:, h:h + 1], in_=acc[:, a0 + 128:a0 + 129]),
                   reads=[("pacc", h // 2)], writes=[("rden", h)])
                op("dve", lambda e, acc=acc, a0=a0, h=h, tb=tb: e.tensor_scalar(out=mixed[:, tb, h * 128:(h + 1) * 128], in0=acc[:, a0:a0 + 128], scalar1=rden[:, h:h + 1],
                                                                              scalar2=None, op0=ALU.mult),
                   reads=[("pacc", h // 2), ("rden", h)], writes=[("mixA", tb, h)])

        stage[0] = 7
        for pair in range(2):
            h1s = []
            for t2 in range(2):
                tb = 2 * pair + t2
                pt, pt_k = pg.get()
                ptb = pt[:].bitcast(BF16)
                for ec in range(8):
                    op("pe", lambda e, ptb=ptb, ec=ec, tb=tb: e.transpose(out=ptb[:, ec * 128:(ec + 1) * 128], in_=mixed[:, tb, ec * 128:(ec + 1) * 128], identity=ident[:, :]),
                       reads=[("mixA", tb, h) for h in range(4)] + [("mixC", tb), "ident"], writes=[pt_k])
                op("act", lambda e, ptb=ptb: e.copy(out=mT[:, :, :], in_=ptb[:, :].rearrange("p (c t) -> p c t", c=8)), reads=[pt_k], writes=["mT"])
                xt, xt_k = xt_pool.get()
                op("sp", lambda e, xt=xt, sl=sl, tb=tb: e.dma_start(out=xt[:, :], in_=xown[sl, 32 + tb * 128:32 + (tb + 1) * 128, :]), writes=[xt_k], dma=True)
                h1, h1_k = xt, xt_k
                for half in range(2):
                    wo, wo_k = load_w(wout_b[:, half * 512:(half + 1) * 512], WOUT_KEYS)
                    for ec in range(8):
                        op("pe", lambda e, half=half, ec=ec, wo=wo: e.matmul(psc[half][:, :], lhsT=mT[:, ec, :], rhs=wo[:, ec, :], start=(ec == 0), stop=(ec == 7)),
                           reads=["mT", wo_k], writes=[("psc", half)])
                    op("dve", lambda e, half=half, h1=h1, xt=xt: e.tensor_tensor(out=h1[:, half * 512:(half + 1) * 512], in0=psc[half][:, :],
                                                                               in1=xt[:, half * 512:(half + 1) * 512], op=ALU.add),
                       reads=[("psc", half), xt_k], writes=[h1_k])
                rmsnorm_T(h1, h1_k, 128, gM, hnT[:, :, t2 * 128:(t2 + 1) * 128], ("hnT", t2))
                h1s.append((h1, h1_k, tb))
            accs = [[(psc[0], ("psc", 0)), (psc[1], ("psc", 1))], [(pacc[0], ("pacc", 0)), (pacc[1], ("pacc", 1))]]
            for fg in range(8):
                wu, wu_k = load_w(wup_b[:, fg * 512:(fg + 1) * 512], WUP_KEYS)
                wd, wd_k = wbuf.get()
                wdv = wd[:, :, :].rearrange("p a b -> p (a b)").rearrange("p (f n) -> p f n", f=4)
                op("pool", lambda e, wdv=wdv, fg=fg: e.dma_start(out=wdv, in_=wdown_b[fg * 512:(fg + 1) * 512, :].rearrange("(f p) n -> p f n", p=128)),
                   reads=WDOWN_KEYS, writes=[wd_k], dma=True)
                for fc in range(4):
                    ps, ps_k = pg.get()
                    for dc in range(8):
                        op("pe", lambda e, ps=ps, wu=wu, dc=dc, fc=fc: e.matmul(ps[:, 0:256], lhsT=wu[:, dc, fc * 128:(fc + 1) * 128], rhs=hnT[:, dc, :],
                                                                             start=(dc == 0), stop=(dc == 7)),
                           reads=[wu_k, ("hnT", 0), ("hnT", 1)], writes=[ps_k])
                    uf, uf_k = uf_pool.get()
                    op("act", lambda e, uf=uf, ps=ps: e.activation(out=uf[:, :], in_=ps[:, 0:256], func=AF.Relu), reads=[ps_k], writes=[uf_k])
                    op("pool", lambda e, uf=uf: e.tensor_tensor(out=uf[:, :], in0=uf[:, :], in1=uf[:, :], op=ALU.mult), reads=[uf_k], writes=[uf_k])
                    first = (fg == 0 and fc == 0)
                    last = (fg == 7 and fc == 3)
                    for t2 in range(2):
                        for half in range(2):
                            acc, acc_k = accs[t2][half]
                            op("pe", lambda e, acc=acc, uf=uf, t2=t2, half=half, wdv=wdv, fc=fc, first=first, last=last: e.matmul(
                                acc[:, :], lhsT=uf[:, t2 * 128:(t2 + 1) * 128], rhs=wdv[:, fc, half * 512:(half + 1) * 512], start=first, stop=last),
                               reads=[uf_k, wd_k], writes=[acc_k])
            for t2 in range(2):
                h1, h1_k, tb = h1s[t2]
                for half in range(2):
                    acc, acc_k = accs[t2][half]
                    op("dve", lambda e, acc=acc, h1=h1, half=half: e.tensor_tensor(out=h1[:, half * 512:(half + 1) * 512], in0=acc[:, :],
                                                                                 in1=h1[:, half * 512:(half + 1) * 512], op=ALU.add),
                       reads=[acc_k, h1_k], writes=[h1_k])
                st, st_k = st_pool.get()
                jk, jk_k = jk_pool.get()
                op("act", lambda e, jk=jk, h1=h1, st=st: e.activation(out=jk[:, :], in_=h1[:, 0:512], func=AF.Square, accum_out=st[:, 0:1]),
                   reads=[h1_k], writes=[jk_k, (st_k, 0)])
                op("act", lambda e, jk=jk, h1=h1, st=st: e.activation(out=jk[:, :], in_=h1[:, 512:1024], func=AF.Square, accum_out=st[:, 1:2]),
                   reads=[h1_k], writes=[jk_k, (st_k, 1)])
                op("dve", lambda e, st=st: e.tensor_tensor(out=st[:, 2:3], in0=st[:, 0:1], in1=st[:, 1:2], op=ALU.add), reads=[(st_k, 0), (st_k, 1)], writes=[(st_k, 2)])
                op("dve", lambda e, st=st: e.tensor_scalar(out=st[:, 3:4], in0=st[:, 2:3], scalar1=1.0 / D, scalar2=EPS, op0=ALU.mult, op1=ALU.add),
                   reads=[(st_k, 2)], writes=[(st_k, 3)])
                op("act", lambda e, st=st: e.activation(out=st[:, 4:5], in_=st[:, 3:4], func=AF.Sqrt), reads=[(st_k, 3)], writes=[(st_k, 4)])
                op("dve", lambda e, st=st: e.reciprocal(out=st[:, 5:6], in_=st[:, 4:5]), reads=[(st_k, 4)], writes=[(st_k, 5)])
                op("dve", lambda e, h1=h1, st=st: e.scalar_tensor_tensor(out=h1[:, :], in0=h1[:, :], scalar=st[:, 5:6], in1=gF[:, :], op0=ALU.mult, op1=ALU.mult),
                   reads=[h1_k, (st_k, 5), "gF"], writes=[h1_k])
                r0 = sl * 512 + tb * 128
                op("sp", lambda e, h1=h1, r0=r0: e.dma_start(out=out_d[r0:r0 + 128, :], in_=h1[:, :]), reads=[h1_k], writes=[("out", r0)], dma=True)

    if os.environ.get("KDEBUG"):
        print("sbuf remaining", nc.sbuf_bytes_remaining, "ops", {e: len(S_.ops[e]) for e in ENGS})
    with ExitStack() as stack:
        S_.emit(nc, stack)
    return nc


def _rope_tab(pos):
    pos = np.asarray(pos, np.float32)
    out = np.zeros((len(pos), 512), np.float32)
    inv = (np.float32(500000.0) ** (-np.arange(0, 32, 2, dtype=np.float32) / np.float32(32))).astype(np.float32)
    ang = pos[:, None] * inv[None, :]
    c, s = np.cos(ang).astype(np.float32), np.sin(ang).astype(np.float32)
    out[:, 0:128] = np.tile(np.concatenate([c, c], 1), (1, 4))
    out[:, 128:256] = np.tile(np.concatenate([s, s], 1), (1, 4))
    inv = (np.float32(500000.0) ** (-np.arange(0, 16, 2, dtype=np.float32) / np.float32(16))).astype(np.float32)
    ang = pos[:, None] * inv[None, :]
    c, s = np.cos(ang).astype(np.float32), np.sin(ang).astype(np.float32)
    out[:, 256:384] = np.tile(np.concatenate([c, c], 1), (1, 8))
    out[:, 384:512] = np.tile(np.concatenate([s, s], 1), (1, 8))
    return out


def _chunk_of(sl, half):
    return 2 * sl + (sl % 2) if half == 0 else 2 * sl + 1 - (sl % 2)


_NC_CACHE = {}


def kernel(x, meta_tokens, attn_norm_g, w_in, conv_w, conv_b, conv_norm_g, conv_norm_b,
           w_out, mlp_norm_g, w_up, w_down, final_norm_g):
    x = np.asarray(x, np.float32)
    B, S, _ = x.shape
    T = NMETA + S
    NSLOT = S // 1024
    if S not in _NC_CACHE:
        _NC_CACHE[S] = build(S)
    nc = _NC_CACHE[S]
    f = lambda a: np.ascontiguousarray(np.asarray(a, np.float32))
    meta = f(meta_tokens)
    ropek = _rope_tab(np.arange(T))
    p = np.arange(128)
    consts = {
        "iota": np.tile(np.arange(512, dtype=np.float32)[None, :], (128, 1)),
        "pow2": np.tile((2.0 ** -(np.arange(NBIS) + 1.0)).astype(np.float32)[None, :], (128, 1)),
        "e32": (np.arange(32)[None, :] == (p % 32)[:, None]).astype(np.float32),
        "g4": (np.arange(4)[None, :] == (p // 32)[:, None]).astype(np.float32),
        "dm": np.concatenate([(np.arange(128)[None, :] == (32 * g + p % 32)[:, None]).astype(np.float32) for g in range(4)], axis=1),
    }
    shared = {
        "w_in": f(w_in[0]), "w_out": f(w_out[0]), "w_up": f(w_up[0]), "w_down": f(w_down[0]),
        "attn_g": f(attn_norm_g[0]), "mlp_g": f(mlp_norm_g[0]), "final_g": f(final_norm_g),
        "conv_w": f(conv_w[0]), "conv_b": f(conv_b[0]), "cn_g": f(conv_norm_g[0]), "cn_b": f(conv_norm_b[0]),
        "ropek": ropek,
    }
    shared.update(consts)
    in_maps = []
    for core in range(8):
        b, half = core // 2, core % 2
        hall = np.concatenate([meta, x[b]], axis=0)
        xown = np.zeros((NSLOT, 544, D), np.float32)
        ropeq = np.zeros((NSLOT, 512, 512), np.float32)
        qrel = np.zeros((128, NSLOT * 8), np.float32)
        for sl in range(NSLOT):
            c = _chunk_of(sl, half)
            p0 = NMETA + 512 * c
            lo = p0 - 32
            src_lo = max(lo, 0)
            xown[sl, src_lo - lo:, :] = hall[src_lo:p0 + 512]
            ropeq[sl] = ropek[p0:p0 + 512]
            w0 = NMETA + 1024 * sl
            for tb in range(4):
                for wc in range(2):
                    qrel[:, (sl * 4 + tb) * 2 + wc] = (p0 + tb * 128 + p) - w0 - 512 * wc
        m = dict(shared)
        m.update({"xall": hall, "xown": xown, "ropeq": ropeq, "qrel": qrel})
        in_maps.append(m)
    res = run_bass_kernel_spmd(nc, in_maps, core_ids=list(range(8)))
    out = np.zeros((B, S, D), np.float32)
    for core in range(8):
        b, half = core // 2, core % 2
        o = res.results[core]["out"]
        for sl in range(NSLOT):
            c = _chunk_of(sl, half)
            out[b, 512 * c:512 * (c + 1)] = o[sl * 512:(sl + 1) * 512]
    return out
```

```python
import numpy as np
from contextlib import ExitStack
import concourse.bass as bass
import concourse.mybir as mybir
from concourse.bass_utils import run_bass_kernel_spmd

F32 = mybir.dt.float32
BF16 = mybir.dt.bfloat16
ALU = mybir.AluOpType
AF = mybir.ActivationFunctionType
AX = mybir.AxisListType

D = 1024
NMETA = 16
DIN = 3144
DFF = 4096
EPS = 1e-5
KTOP = 256
NBIS = 22
ENGS = ("pe", "act", "dve", "pool", "sp")
SEM_CH = 30000
NDMA_SEM = 12


class Op:
    __slots__ = ("eng", "fn", "deps", "dma", "signals", "sig", "dma_idx")


class Sched:
    def __init__(self):
        self.ops = {e: [] for e in ENGS}
        self.lastw = {}
        self.readers = {}
        self.ndma = {e: 0 for e in ENGS}

    def op(self, eng, fn, reads=(), writes=(), dma=False):
        o = Op()
        o.eng = eng
        o.fn = fn
        o.dma = dma
        o.signals = False
        o.sig = None
        deps = {}
        xr = [k for k in reads if isinstance(k, tuple) and k[0] in ("pg", "psc", "pacc")]
        if xr:
            writes = list(writes) + [k for k in xr if k not in writes]
        for k in reads:
            w = self.lastw.get(k)
            if w is not None:
                deps[id(w)] = (w, True)
        for k in writes:
            w = self.lastw.get(k)
            if w is not None and id(w) not in deps:
                deps[id(w)] = (w, False)
            for r in self.readers.get(k, ()):
                if id(r) not in deps:
                    deps[id(r)] = (r, False)
        o.deps = []
        for d, raw in deps.values():
            if d.eng == eng and not d.dma:
                if eng == "pe":
                    continue
                if not raw and not dma:
                    continue
            d.signals = True
            o.deps.append(d)
        if dma:
            o.dma_idx = self.ndma[eng]
            self.ndma[eng] += 1
            o.signals = True
        for k in reads:
            self.readers.setdefault(k, []).append(o)
        for k in writes:
            self.lastw[k] = o
            self.readers[k] = []
        self.ops[eng].append(o)
        return o

    def emit(self, nc, stack):
        nsig = {}
        for e in ENGS:
            n = 0
            for o in self.ops[e]:
                if o.signals and not o.dma:
                    o.sig = n
                    n += 1
            nsig[e] = n
        csem = {}
        for e in ENGS:
            nch = (nsig[e] + SEM_CH - 1) // SEM_CH
            csem[e] = [stack.enter_context(nc.semaphore(f"c_{e}_{i}")) for i in range(nch)]
        dsem = {}
        for e in ENGS:
            if self.ndma[e]:
                dsem[e] = [stack.enter_context(nc.semaphore(f"d_{e}_{i}")) for i in range(NDMA_SEM)]

        def target(d):
            if d.dma:
                return dsem[d.eng][d.dma_idx % NDMA_SEM], 16 * (d.dma_idx // NDMA_SEM + 1)
            return csem[d.eng][d.sig // SEM_CH], d.sig % SEM_CH + 1

        block = stack.enter_context(nc.Block())

        def run(e):
            def body(eng):
                waited = {}
                for o in self.ops[e]:
                    tg = [target(d) for d in o.deps]
                    if o.dma and o.dma_idx >= NDMA_SEM:
                        tg.append((dsem[e][o.dma_idx % NDMA_SEM], 16 * (o.dma_idx // NDMA_SEM)))
                    for s, v in tg:
                        key = id(s)
                        if waited.get(key, 0) < v:
                            eng.wait_ge(s, v)
                            waited[key] = v
                    ins = o.fn(eng)
                    if o.signals:
                        if o.dma:
                            s, _ = target(o)
                            ins.then_inc(s, 16)
                        else:
                            ins.then_inc(csem[e][o.sig // SEM_CH], 1)
                if self.ndma[e]:
                    n = self.ndma[e]
                    for i in range(max(0, n - NDMA_SEM), n):
                        eng.wait_ge(dsem[e][i % NDMA_SEM], 16 * (i // NDMA_SEM + 1))
            return body

        for e, reg in (("pe", block.tensor), ("act", block.scalar), ("dve", block.vector),
                       ("pool", block.gpsimd), ("sp", block.sync)):
            if self.ops[e]:
                reg(run(e))


class Pool:
    def __init__(self, tiles, name, off=0):
        self.tiles = tiles
        self.name = name
        self.i = 0
        self.off = off

    def get(self):
        j = self.i % len(self.tiles)
        self.i += 1
        return self.tiles[j], (self.name, j + self.off)


def build(S):
    T = NMETA + S
    NCH = S // 512
    NSLOT = NCH // 2
    NKT = 1 + S // 128
    nc = bass.Bass("TRN2", target_bir_lowering=False)

    def din(name, shape, dt=F32):
        return nc.dram_tensor(name, shape, dt, kind="ExternalInput").ap()

    xall = din("xall", [T, D])
    xown = din("xown", [NSLOT, 544, D])
    ropek = din("ropek", [T, 512])
    ropeq = din("ropeq", [NSLOT, 512, 512])
    qrel_d = din("qrel", [128, NSLOT * 8])
    w_in = din("w_in", [D, DIN])
    w_out = din("w_out", [D, D])
    w_up = din("w_up", [D, DFF])
    w_down = din("w_down", [DFF, D])
    attn_g = din("attn_g", [D])
    mlp_g = din("mlp_g", [D])
    final_g = din("final_g", [D])
    conv_w = din("conv_w", [31, 512])
    conv_b = din("conv_b", [512])
    cn_g = din("cn_g", [512])
    cn_b = din("cn_b", [512])
    iota_d = din("iota", [128, 512])
    pow2_d = din("pow2", [128, NBIS])
    e32_d = din("e32", [128, 32])
    g4_d = din("g4", [128, 4])
    dm_d = din("dm", [128, 4 * 128])
    out_d = nc.dram_tensor("out", [NSLOT * 512, D], F32, kind="ExternalOutput").ap()

    k_scr = nc.dram_tensor("k_scr", [4, 128, T], BF16).ap()
    v_scr = nc.dram_tensor("v_scr", [T, 512], BF16).ap()

    import os
    KSTOP = float(os.environ.get('KSTOP', '99'))
    S_ = Sched()
    stage = [0]

    def op(*a, **k):
        if stage[0] <= KSTOP:
            return S_.op(*a, **k)
        return None
    uid = [0]

    def sb(shape, dt, name=None):
        uid[0] += 1
        return nc.alloc_sbuf_tensor(f"{name or 't'}{uid[0]}", shape, dt)

    def mkpool(n, shape, dt, name):
        return Pool([sb(shape, dt, name) for _ in range(n)], name)

    identf = sb([128, 128], F32, "identf")
    ident = sb([128, 128], BF16, "ident")
    op("pool", lambda e: e.memset(identf[:], 0.0), writes=["identf"])
    op("pool", lambda e: e.affine_select(out=identf[:], in_=identf[:], pattern=[[-1, 128]],
                                         compare_op=ALU.not_equal, fill=1.0, base=0, channel_multiplier=1),
       reads=["identf"], writes=["identf"])
    op("dve", lambda e: e.tensor_copy(out=ident[:], in_=identf[:]), reads=["identf"], writes=["ident"])

    def load_const(name, shape, src, dt=F32):
        t = sb(shape, dt, name)
        op("sp", lambda e: e.dma_start(out=t[:], in_=src, allow_slow_non_contiguous=True), writes=[name], dma=True)
        return t

    iota = load_const("iota", [128, 512], iota_d)
    pow2 = load_const("pow2", [128, NBIS], pow2_d)
    e32 = load_const("e32", [128, 32], e32_d)
    g4 = load_const("g4", [128, 4], g4_d)
    qrel = load_const("qrelc", [128, NSLOT * 8], qrel_d)
    gA = load_const("gA", [128, 8], attn_g.rearrange("(c p) -> p c", p=128))
    gM = load_const("gM", [128, 8], mlp_g.rearrange("(c p) -> p c", p=128))
    def bcast_row(name, src, n):
        t = sb([128, n], F32, name)
        op("sp", lambda e: e.dma_start(out=t[:], in_=src.partition_broadcast(128)), writes=[name], dma=True)
        return t
    gF = bcast_row("gF", final_g, D)
    cb_bc = bcast_row("cb_bc", conv_b, 512)
    cg_bc = bcast_row("cg_bc", cn_g, 512)
    cnb_bc = bcast_row("cnb_bc", cn_b, 512)
    cwT = sb([128, 4, 31], F32, "cwT")
    for cc_ in range(4):
        op("sp", lambda e, cc_=cc_: e.dma_start(out=cwT[:, cc_, :], in_=conv_w[:, cc_ * 128:(cc_ + 1) * 128].rearrange("j p -> p j"),
                                                 allow_slow_non_contiguous=True), writes=["cwT"], dma=True)
    dmb = sb([128, 512], BF16, "dmb")
    op("pool", lambda e: e.dma_start(out=dmb[:], in_=dm_d), writes=["dmb"], dma=True)
    wik_t = sb([128, 8, 64], BF16, "wik")
    op("pool", lambda e: e.dma_start(out=wik_t[:, :, :], in_=w_in[:, 2048:2112].rearrange("(c p) n -> p c n", p=128)), writes=["wik"], dma=True)
    wiw_t = sb([128, 8, 8], BF16, "wiw")
    op("pool", lambda e: e.dma_start(out=wiw_t[:, :, :], in_=w_in[:, 2112:2120].rearrange("(c p) n -> p c n", p=128), allow_slow_non_contiguous=True),
       writes=["wiw"], dma=True)

    win_b = nc.dram_tensor("win_b", [D, DIN], BF16).ap()
    wout_b = nc.dram_tensor("wout_b", [D, D], BF16).ap()
    wup_b = nc.dram_tensor("wup_b", [D, DFF], BF16).ap()
    wdown_b = nc.dram_tensor("wdown_b", [DFF, D], BF16).ap()

    ikT = sb([128, T], BF16, "ikT")
    scores = sb([128, T], F32, "scores")
    wbuf = mkpool(4, [128, 8, 512], BF16, "wbuf")
    xt_pool = mkpool(3, [128, D], F32, "xt")
    xn_pool = mkpool(1, [128, D], BF16, "xn")
    xnT_pool = mkpool(2, [128, 8, 128], BF16, "xnT")
    rope_pool = mkpool(2, [128, 256], F32, "rope")
    st_pool = mkpool(4, [128, 8], F32, "stat")
    tm_pool = mkpool(3, [128, 512], BF16, "tm")
    rt_pool = mkpool(2, [128, 256], F32, "rt")
    jk_pool = mkpool(2, [128, 512], F32, "jk")
    bigjunk = sb([128, T], mybir.dt.uint8, "bigjunk")
    pg = Pool([nc.alloc_psum_tensor(f"pg{i}", [128, 512], F32) for i in range(4)], "pg")
    psc = [nc.alloc_psum_tensor(f"psc{i}", [128, 512], F32) for i in range(2)]
    pacc = [nc.alloc_psum_tensor(f"pacc{i}", [128, 512], F32) for i in range(2)]

    def pg_bf16(t):
        return t.bitcast(BF16) if hasattr(t, "bitcast") else None

    def rmsnorm_T(x_t, x_k, rows, gcol, xnT, xnT_k):
        st, st_k = st_pool.get()
        jk, jk_k = jk_pool.get()
        xn, xn_k = xn_pool.get()
        op("act", lambda e: e.activation(out=jk[0:rows, :], in_=x_t[0:rows, 0:512], func=AF.Square,
                                         accum_out=st[0:rows, 0:1]), reads=[x_k], writes=[jk_k, (st_k, 0)])
        op("act", lambda e: e.activation(out=jk[0:rows, :], in_=x_t[0:rows, 512:1024], func=AF.Square,
                                         accum_out=st[0:rows, 1:2]), reads=[x_k], writes=[jk_k, (st_k, 1)])
        op("dve", lambda e: e.tensor_tensor(out=st[0:rows, 2:3], in0=st[0:rows, 0:1], in1=st[0:rows, 1:2], op=ALU.add),
           reads=[(st_k, 0), (st_k, 1)], writes=[(st_k, 2)])
        op("dve", lambda e: e.tensor_scalar(out=st[0:rows, 3:4], in0=st[0:rows, 2:3], scalar1=1.0 / D, scalar2=EPS,
                                            op0=ALU.mult, op1=ALU.add), reads=[(st_k, 2)], writes=[(st_k, 3)])
        op("act", lambda e: e.activation(out=st[0:rows, 4:5], in_=st[0:rows, 3:4], func=AF.Sqrt),
           reads=[(st_k, 3)], writes=[(st_k, 4)])
        op("dve", lambda e: e.reciprocal(out=st[0:rows, 5:6], in_=st[0:rows, 4:5]), reads=[(st_k, 4)], writes=[(st_k, 5)])
        op("dve", lambda e: e.tensor_scalar(out=xn[0:rows, :], in0=x_t[0:rows, :], scalar1=st[0:rows, 5:6], scalar2=None,
                                            op0=ALU.mult), reads=[x_k, (st_k, 5)], writes=[xn_k])
        pt, pt_k = pg.get()
        ptb = pt[:].bitcast(BF16)
        for dc in range(8):
            op("pe", lambda e, dc=dc: e.transpose(out=ptb[:, dc * 128:dc * 128 + rows], in_=xn[0:rows, dc * 128:(dc + 1) * 128],
                                                  identity=ident[0:rows, 0:rows]),
               reads=[xn_k, "ident"], writes=[pt_k])
        op("dve", lambda e: e.tensor_tensor(out=xnT[:, :, 0:rows],
                                            in0=ptb.rearrange("p (c t) -> p c t", c=8)[:, :, 0:rows],
                                            in1=gcol[:, :].unsqueeze(2).to_broadcast([128, 8, rows]), op=ALU.mult),
           reads=[pt_k], writes=[xnT_k])
        return (st, st_k)

    def wviews(kind, i, w):
        if kind == "down":
            v = w[:, :, :].rearrange("p a b -> p (a b)").rearrange("p (f n) -> p f n", f=4)
            r = lambda a: a[i * 512:(i + 1) * 512, :].rearrange("(f p) n -> p f n", p=128)
            return v, r(w_down), r(wdown_b)
        src, dst = {"in": (w_in, win_b), "out": (w_out, wout_b), "up": (w_up, wup_b)}[kind]
        r = lambda a: a[:, i:i + 512].rearrange("(c p) n -> p c n", p=128) if kind == "in" else a[:, i * 512:(i + 1) * 512].rearrange("(c p) n -> p c n", p=128)
        return w[:, :, :], r(src), r(dst)

    def load_w(kind, i, cast=False):
        w, w_k = wbuf.get()
        v, src, scr = wviews(kind, i, w)
        if cast:
            op("pool", lambda e: e.dma_start(out=v, in_=src), writes=[w_k], dma=True)
        else:
            op("sp", lambda e: e.dma_start(out=v, in_=scr), reads=[("wcv", kind, i)], writes=[w_k], dma=True)
        return (v if kind == "down" else w), w_k

    def proj_tm(xnT, xnT_k, rows, w, w_k, ncols, ps, ps_k, pcol=0):
        for dc in range(8):
            op("pe", lambda e, dc=dc: e.matmul(ps[0:rows, pcol:pcol + ncols], lhsT=xnT[:, dc, 0:rows], rhs=w[:, dc, 0:ncols],
                                               start=(dc == 0), stop=(dc == 7)),
               reads=[xnT_k, w_k], writes=[ps_k])


    def rope_tm(ps, ps_k, rows, nh, hd, half, rp, rp_k, roff, dst, dst_k, dcol=0):
        r2 = 2 * half
        n = nh * hd
        xf, xf_k = jk_pool.get()
        op("act", lambda e: e.copy(out=xf[0:rows, 0:n], in_=ps[0:rows, 0:n]), reads=[ps_k], writes=[xf_k])
        op("act", lambda e: e.copy(out=dst[0:rows, dcol:dcol + n], in_=ps[0:rows, 0:n]), reads=[ps_k], writes=[dst_k])
        rt, rt_k = rt_pool.get()
        pv = xf[0:rows, 0:n].rearrange("p (h d) -> p h d", h=nh)
        A = rt[0:rows, 0:nh * r2].rearrange("p (h d) -> p h d", h=nh)
        Bm = rt[0:rows, 128:128 + nh * r2].rearrange("p (h d) -> p h d", h=nh)
        cc_ = rp[0:rows, roff:roff + nh * r2].rearrange("p (h d) -> p h d", h=nh)
        ss_ = rp[0:rows, roff + 128:roff + 128 + nh * r2].rearrange("p (h d) -> p h d", h=nh)
        op("dve", lambda e: e.tensor_tensor(out=A, in0=pv[:, :, 0:r2], in1=cc_, op=ALU.mult), reads=[xf_k, rp_k], writes=[(rt_k, 0)])
        op("dve", lambda e: e.tensor_tensor(out=Bm, in0=pv[:, :, 0:r2], in1=ss_, op=ALU.mult), reads=[xf_k, rp_k], writes=[(rt_k, 1)])
        dv = dst[0:rows, dcol:dcol + n].rearrange("p (h d) -> p h d", h=nh)
        op("dve", lambda e: e.tensor_tensor(out=dv[:, :, 0:half], in0=A[:, :, 0:half], in1=Bm[:, :, half:r2], op=ALU.subtract),
           reads=[(rt_k, 0), (rt_k, 1), dst_k], writes=[dst_k])
        op("dve", lambda e: e.tensor_tensor(out=dv[:, :, half:r2], in0=A[:, :, half:r2], in1=Bm[:, :, 0:half], op=ALU.add),
           reads=[(rt_k, 0), (rt_k, 1), dst_k], writes=[dst_k])

    stage[0] = 1
    wk, wk_k = load_w("in", 512, cast=True)
    wv, wv_k = load_w("in", 1024, cast=True)
    cvt_pool = Pool(wbuf.tiles[2:4], "wbuf", off=2)
    for kind, idxs in (("in", (0, 1536, 2120, 2632)), ("out", (0, 1)), ("up", tuple(range(8))), ("down", tuple(range(8)))):
        for i in idxs:
            w, w_k = cvt_pool.get()
            v, src, scr = wviews(kind, i, w)
            op("pool", lambda e, v=v, src=src: e.dma_start(out=v, in_=src), writes=[w_k], dma=True)
            op("pool", lambda e, v=v, scr=scr: e.dma_start(out=scr, in_=v), reads=[w_k], writes=[("wcv", kind, i)], dma=True)
    wbuf.i = 2
    wik, wik_k = wik_t, "wik"
    stage[0] = 1
    for kt in range(min(NKT, int(os.environ.get('KT_MAX', '999')))):
        rows = NMETA if kt == 0 else 128
        p0 = 0 if kt == 0 else NMETA + 128 * (kt - 1)
        xt, xt_k = xt_pool.get()
        op("sp", lambda e, xt=xt, p0=p0, rows=rows: e.dma_start(out=xt[0:rows, :], in_=xall[p0:p0 + rows, :]), writes=[xt_k], dma=True)
        rp, rp_k = rope_pool.get()
        op("sp", lambda e, rp=rp, p0=p0, rows=rows: e.dma_start(out=rp[0:rows, :], in_=ropek[p0:p0 + rows, 0:256]), writes=[rp_k], dma=True)
        rpi, rpi_k = rope_pool.get()
        op("sp", lambda e, rpi=rpi, p0=p0, rows=rows: e.dma_start(out=rpi[0:rows, :], in_=ropek[p0:p0 + rows, 256:512]), writes=[rpi_k], dma=True)
        xnT, xnT_k = xnT_pool.get()
        stage[0] = 1.1
        rmsnorm_T(xt, xt_k, rows, gA, xnT, xnT_k)
        stage[0] = 1.2
        ps, ps_k = pg.get()
        proj_tm(xnT, xnT_k, rows, wk, wk_k, 512, ps, ps_k)
        stage[0] = 1.25
        kb, kb_k = tm_pool.get()
        rope_tm(ps, ps_k, rows, 4, 128, 16, rp, rp_k, 0, kb, kb_k)
        stage[0] = 1.3
        pt, pt_k = pg.get()
        ptb = pt[:].bitcast(BF16)
        for h in range(4):
            op("pe", lambda e, h=h, ptb=ptb, kb=kb, rows=rows: e.transpose(out=ptb[:, h * 128:h * 128 + rows], in_=kb[0:rows, h * 128:(h + 1) * 128],
                                                                         identity=ident[0:rows, 0:rows]), reads=[kb_k, "ident"], writes=[pt_k])
        kTt, kTt_k = tm_pool.get()
        op("act", lambda e, kTt=kTt, ptb=ptb: e.copy(out=kTt[:, :], in_=ptb[:, 0:512]), reads=[pt_k], writes=[kTt_k])
        op("sp", lambda e, kTt=kTt, p0=p0, rows=rows: e.dma_start(
            out=k_scr[:, :, p0:p0 + rows].rearrange("h p t -> p h t"),
            in_=kTt[:, :].rearrange("p (h t) -> p h t", h=4)[:, :, 0:rows]),
           reads=[kTt_k], writes=[("k_scr", kt)], dma=True)
        stage[0] = 1.4
        ps, ps_k = pg.get()
        proj_tm(xnT, xnT_k, rows, wv, wv_k, 512, ps, ps_k)
        vb, vb_k = tm_pool.get()
        op("act", lambda e, vb=vb, ps=ps, rows=rows: e.copy(out=vb[0:rows, :], in_=ps[0:rows, :]), reads=[ps_k], writes=[vb_k])
        op("sp", lambda e, vb=vb, p0=p0, rows=rows: e.dma_start(out=v_scr[p0:p0 + rows, :], in_=vb[0:rows, :]),
           reads=[vb_k], writes=[("v_scr", kt)], dma=True)
        stage[0] = 1.5
        ps, ps_k = pg.get()
        proj_tm(xnT, xnT_k, rows, wik, wik_k, 64, ps, ps_k)
        ib, ib_k = tm_pool.get()
        rope_tm(ps, ps_k, rows, 1, 64, 8, rpi, rpi_k, 0, ib, ib_k)
        op("dve", lambda e, ib=ib, rows=rows: e.tensor_copy(out=ib[0:rows, 64:128], in_=ib[0:rows, 0:64]), reads=[ib_k], writes=[ib_k])
        pt, pt_k = pg.get()
        ptb = pt[:].bitcast(BF16)
        op("pe", lambda e, ptb=ptb, ib=ib, rows=rows: e.transpose(out=ptb[:, 0:rows], in_=ib[0:rows, 0:128], identity=ident[0:rows, 0:rows]),
           reads=[ib_k, "ident"], writes=[pt_k])
        op("act", lambda e, ptb=ptb, p0=p0, rows=rows: e.copy(out=ikT[:, p0:p0 + rows], in_=ptb[:, 0:rows]), reads=[pt_k], writes=[("ikT", kt)])

    stage[0] = 2
    qT = sb([128, 4, 512], BF16, "qT")
    iqT = sb([128, 4, 512], BF16, "iqT")
    iwt = sb([128, 4, 8], F32, "iwt")
    uT = sb([128, 4, 544], BF16, "uT")
    mixed = sb([128, 4, D], BF16, "mixed")
    ysb = sb([128, 4, 512], F32, "ysb")
    Dcc = sb([128, 31, 128], BF16, "Dcc")
    negm_pool = mkpool(2, [128, 512], F32, "negm")
    sel = sb([128, 8, 128], BF16, "sel")
    lw = sb([128, 2, 128], BF16, "lw")
    w2 = sb([128, 8], F32, "w2")
    g4b = sb([128, 4], BF16, "g4b")
    op("dve", lambda e: e.tensor_copy(out=g4b[:], in_=g4[:]), reads=["g4"], writes=["g4b"])
    bis = sb([128, 8 + NBIS], F32, "bis")
    top8 = sb([128, 8], F32, "top8")
    cst = sb([128, 4, 6], F32, "cst")
    cmv = sb([128, 4, 2], F32, "cmv")
    crs = sb([128, 4], F32, "crs")
    R_pool = mkpool(3, [128, 512], BF16, "R")
    PT_pool = mkpool(2, [128, 512], BF16, "PT")
    mb_pool = mkpool(2, [128, 512], BF16, "mb")
    GK = 4
    kv_pool = [(sb([128, 4, GK * 128], BF16, "kbuf"), sb([128, GK, 4, 129], BF16, "vbuf")) for _ in range(2)]
    for i_, (kb_, vb_) in enumerate(kv_pool):
        op("pool", lambda e, vb_=vb_: e.memset(vb_[:, :, :, 128:129], 1.0), writes=[("vbuf", i_)])
    kmeta = sb([128, 4, NMETA], BF16, "kmeta")
    vmeta = sb([128, 4, 129], BF16, "vmeta")
    op("pool", lambda e: e.memset(vmeta[:, :, 128:129], 1.0), writes=["vmeta"])
    op("sp", lambda e: e.dma_start(out=kmeta[:, :, :], in_=k_scr[:, :, 0:NMETA].rearrange("h p t -> p h t")),
       reads=[("k_scr", 0)], writes=["kmeta"], dma=True)
    op("sp", lambda e: e.dma_start(out=vmeta[0:NMETA, :, 0:128], in_=v_scr[0:NMETA, :].rearrange("p (h d) -> p h d", h=4)),
       reads=[("v_scr", 0), "vmeta"], writes=["vmeta"], dma=True)
    mT = sb([128, 8, 128], BF16, "mT")
    hnT = sb([128, 8, 256], BF16, "hnT")
    uf_pool = mkpool(2, [128, 256], BF16, "uf")
    rden = sb([128, 4], F32, "rden")

    SC = 0.125 * (8 ** -0.5)
    kv_ctr = [0]

    for sl in range(NSLOT):
        E = NMETA + 512 * (2 * sl + 2)
        W0 = E - 1024
        NCHK = 2 * sl + 2
        wq, wq_k = load_w("in", 0)
        wiq, wiq_k = load_w("in", 1536)
        wga, wga_k = load_w("in", 2120)
        wgg, wgg_k = load_w("in", 2632)
        plan = []
        for pair_ in range(2):
            plan += [("out", 0), ("out", 1)]
            for fg_ in range(8):
                plan += [("up", fg_), ("down", fg_)]
        wfifo = []

        def issue_next():
            if plan:
                kind_, i_ = plan.pop(0)
                wfifo.append(load_w(kind_, i_))
        for ti in range(5):
            rows = 32 if ti == 0 else 128
            r0 = 0 if ti == 0 else 32 + 128 * (ti - 1)
            xt, xt_k = xt_pool.get()
            op("sp", lambda e, xt=xt, r0=r0, rows=rows, sl=sl: e.dma_start(out=xt[0:rows, :], in_=xown[sl, r0:r0 + rows, :]),
               writes=[xt_k], dma=True)
            xnT, xnT_k = xnT_pool.get()
            rmsnorm_T(xt, xt_k, rows, gA, xnT, xnT_k)
            if ti > 0:
                tb = ti - 1
                rp, rp_k = rope_pool.get()
                op("sp", lambda e, rp=rp, tb=tb, sl=sl: e.dma_start(out=rp[:, :], in_=ropeq[sl, 128 * tb:128 * (tb + 1), 0:256]), writes=[rp_k], dma=True)
                ps, ps_k = pg.get()
                proj_tm(xnT, xnT_k, 128, wq, wq_k, 512, ps, ps_k)
                qb, qb_k = tm_pool.get()
                rope_tm(ps, ps_k, 128, 4, 128, 16, rp, rp_k, 0, qb, qb_k)
                pt, pt_k = pg.get()
                ptb = pt[:].bitcast(BF16)
                for h in range(4):
                    op("pe", lambda e, h=h, ptb=ptb, qb=qb: e.transpose(out=ptb[:, h * 128:(h + 1) * 128], in_=qb[:, h * 128:(h + 1) * 128], identity=ident[:, :]),
                       reads=[qb_k, "ident"], writes=[pt_k])
                op("act", lambda e, ptb=ptb, tb=tb: e.copy(out=qT[:, :, tb * 128:(tb + 1) * 128], in_=ptb[:, 0:512].rearrange("p (h t) -> p h t", h=4)),
                   reads=[pt_k], writes=[("qT", tb)])
                rpi, rpi_k = rope_pool.get()
                op("sp", lambda e, rpi=rpi, tb=tb, sl=sl: e.dma_start(out=rpi[:, :], in_=ropeq[sl, 128 * tb:128 * (tb + 1), 256:512]), writes=[rpi_k], dma=True)
                ps, ps_k = pg.get()
                proj_tm(xnT, xnT_k, 128, wiq, wiq_k, 512, ps, ps_k)
                ib, ib_k = tm_pool.get()
                rope_tm(ps, ps_k, 128, 8, 64, 8, rpi, rpi_k, 0, ib, ib_k)
                pt, pt_k = pg.get()
                ptb = pt[:].bitcast(BF16)
                for hp in range(4):
                    op("pe", lambda e, hp=hp, ptb=ptb, ib=ib: e.transpose(out=ptb[:, hp * 128:(hp + 1) * 128], in_=ib[:, hp * 128:(hp + 1) * 128], identity=ident[:, :]),
                       reads=[ib_k, "ident"], writes=[pt_k])
                op("act", lambda e, ptb=ptb, tb=tb: e.copy(out=iqT[:, tb, :].rearrange("p (g h i) -> p h g i", g=4, h=4, i=32),
                                                           in_=ptb[:, 0:512].rearrange("p (h g i) -> p h g i", h=4, g=4, i=32)),
                   reads=[pt_k], writes=[("iqT", tb)])
                ps, ps_k = pg.get()
                proj_tm(xnT, xnT_k, 128, wiw_t, "wiw", 8, ps, ps_k)
                op("dve", lambda e, ps=ps, tb=tb: e.tensor_scalar(out=iwt[:, tb, :], in0=ps[:, 0:8], scalar1=SC, scalar2=None, op0=ALU.mult),
                   reads=[ps_k], writes=[("iwt", tb)])
            psa, psa_k = pg.get()
            proj_tm(xnT, xnT_k, rows, wga, wga_k, 512, psa, psa_k)
            psg, psg_k = pg.get()
            proj_tm(xnT, xnT_k, rows, wgg, wgg_k, 512, psg, psg_k)
            jk, jk_k = jk_pool.get()
            op("act", lambda e, jk=jk, psg=psg, rows=rows: e.activation(out=jk[0:rows, :], in_=psg[0:rows, :], func=AF.Sigmoid),
               reads=[psg_k], writes=[jk_k])
            ub, ub_k = tm_pool.get()
            op("dve", lambda e, ub=ub, psa=psa, jk=jk, rows=rows: e.tensor_tensor(out=ub[0:rows, :], in0=psa[0:rows, :], in1=jk[0:rows, :], op=ALU.mult),
               reads=[psa_k, jk_k], writes=[ub_k])
            pt, pt_k = pg.get()
            ptb = pt[:].bitcast(BF16)
            for cc in range(4):
                op("pe", lambda e, cc=cc, ptb=ptb, ub=ub, rows=rows: e.transpose(out=ptb[:, cc * 128:cc * 128 + rows], in_=ub[0:rows, cc * 128:(cc + 1) * 128],
                                                                              identity=ident[0:rows, 0:rows]),
                   reads=[ub_k, "ident"], writes=[pt_k])
            op("act", lambda e, ptb=ptb, r0=r0, rows=rows: e.copy(out=uT[:, :, r0:r0 + rows], in_=ptb[:, 0:512].rearrange("p (c t) -> p c t", c=4)[:, :, 0:rows]),
               reads=[pt_k], writes=[("uT", ti)])
        stage[0] = 3
        UTK = [("uT", ti) for ti in range(5)]
        for cc in range(4):
            op("dve", lambda e, cc=cc: e.tensor_tensor(out=Dcc[:, :, :], in0=identf[:, :].unsqueeze(1).to_broadcast([128, 31, 128]),
                                                       in1=cwT[:, cc, :].unsqueeze(2).to_broadcast([128, 31, 128]), op=ALU.mult),
               reads=["identf", "cwT"], writes=["Dcc"])
            for tb in range(4):
                ps, ps_k = pg.get()
                for j in range(31):
                    c0 = 2 + tb * 128 + j
                    op("pe", lambda e, ps=ps, cc=cc, c0=c0, j=j: e.matmul(ps[:, 0:128], lhsT=uT[:, cc, c0:c0 + 128], rhs=Dcc[:, j, :], start=(j == 0), stop=(j == 30)),
                       reads=UTK + ["Dcc"], writes=[ps_k])
                op("act", lambda e, ps=ps, tb=tb, cc=cc: e.copy(out=ysb[:, tb, cc * 128:(cc + 1) * 128], in_=ps[:, 0:128]),
                   reads=[ps_k], writes=[("ysb", tb)])
        for tb in range(4):
            yk = ("ysb", tb)
            y = ysb[:, tb, :]
            op("dve", lambda e, y=y: e.tensor_tensor(out=y, in0=y, in1=cb_bc[:, :], op=ALU.add), reads=[yk, "cb_bc"], writes=[yk])
            for g in range(4):
                op("dve", lambda e, y=y, g=g: e.bn_stats(out=cst[:, g, :], in_=y[:, g * 128:(g + 1) * 128]), reads=[yk], writes=[("cst", g)])
                op("dve", lambda e, g=g: e.bn_aggr(out=cmv[:, g, :], in_=cst[:, g, :]), reads=[("cst", g)], writes=[("cmv", g)])
            CM = [("cmv", g) for g in range(4)]
            op("dve", lambda e: e.tensor_scalar(out=crs[:, :], in0=cmv[:, :, 1], scalar1=EPS, scalar2=None, op0=ALU.add), reads=CM, writes=["crs"])
            op("act", lambda e: e.activation(out=crs[:, :], in_=crs[:, :], func=AF.Sqrt), reads=["crs"], writes=["crs"])
            op("dve", lambda e: e.reciprocal(out=crs[:, :], in_=crs[:, :]), reads=["crs"], writes=["crs"])
            for g in range(4):
                op("dve", lambda e, y=y, g=g: e.tensor_scalar(out=y[:, g * 128:(g + 1) * 128], in0=y[:, g * 128:(g + 1) * 128],
                                                            scalar1=cmv[:, g, 0:1], scalar2=crs[:, g:g + 1], op0=ALU.subtract, op1=ALU.mult),
                   reads=[yk, "crs"] + CM, writes=[yk])
            op("dve", lambda e, y=y: e.tensor_tensor(out=y, in0=y, in1=cg_bc[:, :], op=ALU.mult), reads=[yk, "cg_bc"], writes=[yk])
            op("dve", lambda e, y=y: e.tensor_tensor(out=y, in0=y, in1=cnb_bc[:, :], op=ALU.add), reads=[yk, "cnb_bc"], writes=[yk])
            op("act", lambda e, y=y, tb=tb: e.activation(out=mixed[:, tb, 512:1024], in_=y, func=AF.Silu), reads=[yk], writes=[("mixC", tb)])

        stage[0] = 4
        for tb in range(4):
            if tb == 3:
                for _ in range(4):
                    issue_next()
            for h in range(8):
                par, hp = h % 2, h // 2
                op("pool", lambda e, h=h, par=par, hp=hp, tb=tb: e.tensor_scalar(out=lw[:, par, hp * 32:(hp + 1) * 32], in0=e32[:, :], scalar1=iwt[:, tb, h:h + 1],
                                                                                 scalar2=None, op0=ALU.mult),
                   reads=["e32", ("iwt", tb)], writes=[("lw", h)])
            LWK = [("lw", h) for h in range(8)]
            ps, ps_k = pg.get()
            for par in range(2):
                op("pe", lambda e, ps=ps, par=par: e.matmul(ps[:, par * 4:(par + 1) * 4], lhsT=lw[:, par, :], rhs=g4b[:, :], start=True, stop=True),
                   reads=LWK + ["g4b"], writes=[ps_k])
            op("dve", lambda e, ps=ps: e.tensor_copy(out=w2[:, :], in_=ps[:, 0:8]), reads=[ps_k], writes=["w2"])
            for idx in range(8):
                g = idx % 4
                op("pool", lambda e, idx=idx, g=g: e.tensor_scalar(out=sel[:, idx, :], in0=dmb[:, g * 128:(g + 1) * 128], scalar1=w2[:, idx:idx + 1],
                                                                   scalar2=None, op0=ALU.mult),
                   reads=["dmb", "w2"], writes=[("sel", idx)])
            chunks = [(0, NMETA)] + [(NMETA + 512 * m, 512) for m in range(NCHK)]
            SCK = []
            wcnt = 0
            for ci, (c0, cn) in enumerate(chunks):
                ikk = [("ikT", 0)] if ci == 0 else [("ikT", 1 + 4 * (ci - 1) + q) for q in range(4)]
                sc = psc[ci % 2]
                sc_k = ("psc", ci % 2)
                pend = None
                items = [(g, par) for par in range(2) for g in range(4)]
                for k, (g, par) in enumerate(items):
                    ps, ps_k = pg.get()
                    t0 = tb * 128 + g * 32
                    op("pe", lambda e, ps=ps, par=par, tb=tb, g=g, c0=c0, cn=cn: e.matmul(ps[:, 0:cn], lhsT=iqT[par * 64:(par + 1) * 64, tb, g * 128:(g + 1) * 128],
                                                                                    rhs=ikT[par * 64:(par + 1) * 64, c0:c0 + cn], start=True, stop=True),
                       reads=[("iqT", tb)] + ikk, writes=[ps_k])
                    R, R_k = R_pool.get()
                    if k % 2 == 0:
                        op("act", lambda e, R=R, ps=ps, cn=cn: e.activation(out=R[:, 0:cn], in_=ps[:, 0:cn], func=AF.Relu), reads=[ps_k], writes=[R_k])
                    else:
                        op("dve", lambda e, R=R, ps=ps, cn=cn: e.tensor_scalar(out=R[:, 0:cn], in0=ps[:, 0:cn], scalar1=0.0, scalar2=None, op0=ALU.max),
                           reads=[ps_k], writes=[R_k])
                    if pend is not None:
                        pk, pR, pR_k, pidx = pend
                        op("pe", lambda e, sc=sc, pidx=pidx, pR=pR, cn=cn, pk=pk: e.matmul(sc[:, 0:cn], lhsT=sel[:, pidx, :], rhs=pR[:, 0:cn], start=(pk == 0), stop=False),
                           reads=[("sel", pidx), pR_k], writes=[sc_k])
                    pend = (k, R, R_k, par * 4 + g)
                pk, pR, pR_k, pidx = pend
                op("pe", lambda e, sc=sc, pidx=pidx, pR=pR, cn=cn: e.matmul(sc[:, 0:cn], lhsT=sel[:, pidx, :], rhs=pR[:, 0:cn], start=False, stop=True),
                   reads=[("sel", pidx), pR_k], writes=[sc_k])
                sk = ("sc", ci)
                SCK.append(sk)
                if c0 >= W0 and ci > 0:
                    col = (sl * 4 + tb) * 2 + wcnt
                    negm, negm_k = negm_pool.get()
                    op("dve", lambda e, col=col, negm=negm: e.tensor_scalar(out=negm[:, :], in0=iota[:, :], scalar1=qrel[:, col:col + 1], scalar2=-1e30,
                                                                          op0=ALU.is_gt, op1=ALU.mult), reads=["iota", "qrelc"], writes=[negm_k])
                    op("dve", lambda e, sc=sc, c0=c0, negm=negm: e.tensor_tensor(out=scores[:, c0:c0 + 512], in0=sc[:, :], in1=negm[:, :], op=ALU.add),
                       reads=[sc_k, negm_k], writes=[sk])
                    jk, jk_k = jk_pool.get()
                    op("dve", lambda e, sc=sc, jk=jk, negm=negm: e.tensor_tensor(out=jk[:, :], in0=sc[:, :], in1=negm[:, :], op=ALU.subtract),
                       reads=[sc_k, negm_k], writes=[jk_k])
                    op("dve", lambda e, jk=jk, wcnt=wcnt: e.tensor_reduce(out=bis[:, 5 + wcnt:6 + wcnt], in_=jk[:, :], axis=AX.X, op=ALU.min),
                       reads=[jk_k], writes=[("bis", 5 + wcnt)])
                    wcnt += 1
                else:
                    op("act", lambda e, sc=sc, c0=c0, cn=cn: e.copy(out=scores[:, c0:c0 + cn], in_=sc[:, 0:cn]), reads=[sc_k], writes=[sk])
            assert wcnt == 2
            stage[0] = 5
            op("dve", lambda e, E=E: e.max(out=top8[:, :], in_=scores[:, 0:E]), reads=SCK, writes=["top8"])
            op("dve", lambda e, W0=W0: e.tensor_reduce(out=bis[:, 7:8], in_=scores[:, 0:W0], axis=AX.X, op=ALU.min), reads=SCK, writes=[("bis", 7)])
            op("dve", lambda e: e.tensor_reduce(out=bis[:, 0:1], in_=bis[:, 5:8], axis=AX.X, op=ALU.min),
               reads=[("bis", 5), ("bis", 6), ("bis", 7)], writes=[("bis", 0)])
            op("dve", lambda e: e.tensor_tensor(out=bis[:, 1:2], in0=top8[:, 0:1], in1=bis[:, 0:1], op=ALU.subtract),
               reads=["top8", ("bis", 0)], writes=[("bis", 1)])
            op("dve", lambda e: e.tensor_scalar(out=bis[:, 8:8 + NBIS], in0=pow2[:, :], scalar1=bis[:, 1:2], scalar2=None, op0=ALU.mult),
               reads=["pow2", ("bis", 1)], writes=["bisW"])
            for k in range(NBIS):
                op("dve", lambda e, k=k: e.tensor_tensor(out=bis[:, 2:3], in0=bis[:, 8 + k:9 + k], in1=bis[:, 0:1], op=ALU.add),
                   reads=["bisW", ("bis", 0)], writes=[("bis", 2)])
                op("dve", lambda e, E=E: e.tensor_scalar(out=bigjunk[:, 0:E], in0=scores[:, 0:E], scalar1=bis[:, 2:3], scalar2=None,
                                                        op0=ALU.is_ge, op1=ALU.add, accum_out=bis[:, 3:4]),
                   reads=SCK + [("bis", 2)], writes=["bigjunk", ("bis", 3)])
                op("dve", lambda e, k=k: e.tensor_scalar(out=bis[:, 4:5], in0=bis[:, 3:4], scalar1=KTOP - 0.5, scalar2=bis[:, 8 + k:9 + k],
                                                        op0=ALU.is_ge, op1=ALU.mult), reads=[("bis", 3), "bisW"], writes=[("bis", 4)])
                op("dve", lambda e: e.tensor_tensor(out=bis[:, 0:1], in0=bis[:, 0:1], in1=bis[:, 4:5], op=ALU.add),
                   reads=[("bis", 0), ("bis", 4)], writes=[("bis", 0)])
            stage[0] = 6
            nkb_total = 1 + 4 * NCHK
            kbi = 0
            for ci, (c0, cn) in enumerate(chunks):
                mbt, mb_k = mb_pool.get()
                op("dve", lambda e, mbt=mbt, c0=c0, cn=cn: e.tensor_scalar(out=mbt[:, 0:cn], in0=scores[:, c0:c0 + cn], scalar1=bis[:, 0:1], scalar2=-30000.0,
                                                                          op0=ALU.is_lt, op1=ALU.mult), reads=[SCK[ci], ("bis", 0)], writes=[mb_k])
                if ci == 0:
                    kbuf, vbuf, kvk, vvk = kmeta, vmeta, "kmeta", "vmeta"
                    nk = 1
                else:
                    kvn = kv_ctr[0] % 2
                    kv_ctr[0] += 1
                    kbuf, vbuf = kv_pool[kvn]
                    kvk, vvk = ("kbuf", kvn), ("vbuf", kvn)
                    tl = [1 + 4 * (ci - 1) + q for q in range(4)]
                    op("sp", lambda e, kbuf=kbuf, c0=c0: e.dma_start(out=kbuf[:, :, :], in_=k_scr[:, :, c0:c0 + 512].rearrange("h p t -> p h t")),
                       reads=[("k_scr", t) for t in tl], writes=[kvk], dma=True)
                    for g_ in range(4):
                        op("sp", lambda e, vbuf=vbuf, c0=c0, g_=g_: e.dma_start(out=vbuf[:, g_, :, 0:128],
                                                                         in_=v_scr[c0 + g_ * 128:c0 + (g_ + 1) * 128, :].rearrange("p (h d) -> p h d", h=4)),
                           reads=[("v_scr", t) for t in tl], writes=[vvk], dma=True)
                    nk = 4
                for kb in range(nk):
                    ks = NMETA if ci == 0 else 128
                    st, st_k = pg.get()
                    for h in range(4):
                        op("pe", lambda e, st=st, h=h, kbuf=kbuf, kb=kb, ks=ks, tb=tb: e.matmul(st[0:ks, h * 128:(h + 1) * 128], lhsT=kbuf[:, h, kb * 128:kb * 128 + ks],
                                                                                             rhs=qT[:, h, tb * 128:(tb + 1) * 128], start=True, stop=False),
                           reads=[kvk, ("qT", tb)], writes=[st_k])
                        op("pe", lambda e, st=st, h=h, mbt=mbt, kb=kb, ks=ks: e.matmul(st[0:ks, h * 128:(h + 1) * 128], lhsT=mbt[:, kb * 128:kb * 128 + ks],
                                                                                     rhs=ident[:, :], start=False, stop=True),
                           reads=[mb_k, "ident"], writes=[st_k])
                    PT, PT_k = PT_pool.get()
                    op("act", lambda e, PT=PT, st=st, ks=ks: e.activation(out=PT[0:ks, :], in_=st[0:ks, :], func=AF.Exp, scale=128 ** -0.5),
                       reads=[st_k], writes=[PT_k])
                    for h in range(4):
                        acc = pacc[h // 2]
                        a0 = (h % 2) * 256
                        rhs = vbuf[0:ks, h, :] if ci == 0 else vbuf[:, kb, h, :]
                        op("pe", lambda e, acc=acc, a0=a0, PT=PT, h=h, ks=ks, rhs=rhs, kbi=kbi: e.matmul(acc[:, a0:a0 + 129], lhsT=PT[0:ks, h * 128:(h + 1) * 128], rhs=rhs,
                                                                                                   start=(kbi == 0 and h % 2 == 0), stop=(kbi == nkb_total - 1),
                                                                                                   skip_group_check=True),
                           reads=[PT_k, vvk], writes=[("pacc", h // 2)])
                    kbi += 1
            for h in range(4):
                acc = pacc[h // 2]
                a0 = (h % 2) * 256
                op("dve", lambda e, acc=acc, a0=a0, h=h: e.reciprocal(out=rden[:, h:h + 1], in_=acc[:, a0 + 128:a0 + 129]),
                   reads=[("pacc", h // 2)], writes=[("rden", h)])
                op("dve", lambda e, acc=acc, a0=a0, h=h, tb=tb: e.tensor_scalar(out=mixed[:, tb, h * 128:(h + 1) * 128], in0=acc[:, a0:a0 + 128], scalar1=rden[:, h:h + 1],
                                                                              scalar2=None, op0=ALU.mult),
                   reads=[("pacc", h // 2), ("rden", h)], writes=[("mixA", tb, h)])

        stage[0] = 7
        for pair in range(2):
            h1s = []
            while len(wfifo) < 2:
                issue_next()
            wos = [wfifo.pop(0), wfifo.pop(0)]
            for t2 in range(2):
                tb = 2 * pair + t2
                pt, pt_k = pg.get()
                ptb = pt[:].bitcast(BF16)
                for ec in range(8):
                    op("pe", lambda e, ptb=ptb, ec=ec, tb=tb: e.transpose(out=ptb[:, ec * 128:(ec + 1) * 128], in_=mixed[:, tb, ec * 128:(ec + 1) * 128], identity=ident[:, :]),
                       reads=[("mixA", tb, h) for h in range(4)] + [("mixC", tb), "ident"], writes=[pt_k])
                op("act", lambda e, ptb=ptb: e.copy(out=mT[:, :, :], in_=ptb[:, :].rearrange("p (c t) -> p c t", c=8)), reads=[pt_k], writes=["mT"])
                xt, xt_k = xt_pool.get()
                op("sp", lambda e, xt=xt, sl=sl, tb=tb: e.dma_start(out=xt[:, :], in_=xown[sl, 32 + tb * 128:32 + (tb + 1) * 128, :]), writes=[xt_k], dma=True)
                h1, h1_k = xt, xt_k
                for half in range(2):
                    wo, wo_k = wos[half]
                    for ec in range(8):
                        op("pe", lambda e, half=half, ec=ec, wo=wo: e.matmul(psc[half][:, :], lhsT=mT[:, ec, :], rhs=wo[:, ec, :], start=(ec == 0), stop=(ec == 7)),
                           reads=["mT", wo_k], writes=[("psc", half)])
                    op("dve", lambda e, half=half, h1=h1, xt=xt: e.tensor_tensor(out=h1[:, half * 512:(half + 1) * 512], in0=psc[half][:, :],
                                                                               in1=xt[:, half * 512:(half + 1) * 512], op=ALU.add),
                       reads=[("psc", half), xt_k], writes=[h1_k])
                rmsnorm_T(h1, h1_k, 128, gM, hnT[:, :, t2 * 128:(t2 + 1) * 128], ("hnT", t2))
                h1s.append((h1, h1_k, tb))
            issue_next()
            issue_next()
            accs = [[(psc[0], ("psc", 0)), (psc[1], ("psc", 1))], [(pacc[0], ("pacc", 0)), (pacc[1], ("pacc", 1))]]
            for fg in range(8):
                while len(wfifo) < 2:
                    issue_next()
                wu, wu_k = wfifo.pop(0)
                wdv, wd_k = wfifo.pop(0)
                for fc in range(4):
                    ps, ps_k = pg.get()
                    for dc in range(8):
                        op("pe", lambda e, ps=ps, wu=wu, dc=dc, fc=fc: e.matmul(ps[:, 0:256], lhsT=wu[:, dc, fc * 128:(fc + 1) * 128], rhs=hnT[:, dc, :],
                                                                             start=(dc == 0), stop=(dc == 7)),
                           reads=[wu_k, ("hnT", 0), ("hnT", 1)], writes=[ps_k])
                    uf, uf_k = uf_pool.get()
                    op("act", lambda e, uf=uf, ps=ps: e.activation(out=uf[:, :], in_=ps[:, 0:256], func=AF.Relu), reads=[ps_k], writes=[uf_k])
                    op("pool", lambda e, uf=uf: e.tensor_tensor(out=uf[:, :], in0=uf[:, :], in1=uf[:, :], op=ALU.mult), reads=[uf_k], writes=[uf_k])
                    first = (fg == 0 and fc == 0)
                    last = (fg == 7 and fc == 3)
                    for t2 in range(2):
                        for half in range(2):
                            acc, acc_k = accs[t2][half]
                            op("pe", lambda e, acc=acc, uf=uf, t2=t2, half=half, wdv=wdv, fc=fc, first=first, last=last: e.matmul(
                                acc[:, :], lhsT=uf[:, t2 * 128:(t2 + 1) * 128], rhs=wdv[:, fc, half * 512:(half + 1) * 512], start=first, stop=last),
                               reads=[uf_k, wd_k], writes=[acc_k])
                issue_next()
                issue_next()
            for t2 in range(2):
                h1, h1_k, tb = h1s[t2]
                for half in range(2):
                    acc, acc_k = accs[t2][half]
                    op("dve", lambda e, acc=acc, h1=h1, half=half: e.tensor_tensor(out=h1[:, half * 512:(half + 1) * 512], in0=acc[:, :],
                                                                                 in1=h1[:, half * 512:(half + 1) * 512], op=ALU.add),
                       reads=[acc_k, h1_k], writes=[h1_k])
                st, st_k = st_pool.get()
                jk, jk_k = jk_pool.get()
                op("act", lambda e, jk=jk, h1=h1, st=st: e.activation(out=jk[:, :], in_=h1[:, 0:512], func=AF.Square, accum_out=st[:, 0:1]),
                   reads=[h1_k], writes=[jk_k, (st_k, 0)])
                op("act", lambda e, jk=jk, h1=h1, st=st: e.activation(out=jk[:, :], in_=h1[:, 512:1024], func=AF.Square, accum_out=st[:, 1:2]),
                   reads=[h1_k], writes=[jk_k, (st_k, 1)])
                op("dve", lambda e, st=st: e.tensor_tensor(out=st[:, 2:3], in0=st[:, 0:1], in1=st[:, 1:2], op=ALU.add), reads=[(st_k, 0), (st_k, 1)], writes=[(st_k, 2)])
                op("dve", lambda e, st=st: e.tensor_scalar(out=st[:, 3:4], in0=st[:, 2:3], scalar1=1.0 / D, scalar2=EPS, op0=ALU.mult, op1=ALU.add),
                   reads=[(st_k, 2)], writes=[(st_k, 3)])
                op("act", lambda e, st=st: e.activation(out=st[:, 4:5], in_=st[:, 3:4], func=AF.Sqrt), reads=[(st_k, 3)], writes=[(st_k, 4)])
                op("dve", lambda e, st=st: e.reciprocal(out=st[:, 5:6], in_=st[:, 4:5]), reads=[(st_k, 4)], writes=[(st_k, 5)])
                op("dve", lambda e, h1=h1, st=st: e.scalar_tensor_tensor(out=h1[:, :], in0=h1[:, :], scalar=st[:, 5:6], in1=gF[:, :], op0=ALU.mult, op1=ALU.mult),
                   reads=[h1_k, (st_k, 5), "gF"], writes=[h1_k])
                r0 = sl * 512 + tb * 128
                op("sp", lambda e, h1=h1, r0=r0: e.dma_start(out=out_d[r0:r0 + 128, :], in_=h1[:, :]), reads=[h1_k], writes=[("out", r0)], dma=True)

    if os.environ.get("KDEBUG"):
        print("sbuf remaining", nc.sbuf_bytes_remaining, "ops", {e: len(S_.ops[e]) for e in ENGS})
    with ExitStack() as stack:
        S_.emit(nc, stack)
    return nc


def _rope_tab(pos):
    pos = np.asarray(pos, np.float32)
    out = np.zeros((len(pos), 512), np.float32)
    inv = (np.float32(500000.0) ** (-np.arange(0, 32, 2, dtype=np.float32) / np.float32(32))).astype(np.float32)
    ang = pos[:, None] * inv[None, :]
    c, s = np.cos(ang).astype(np.float32), np.sin(ang).astype(np.float32)
    out[:, 0:128] = np.tile(np.concatenate([c, c], 1), (1, 4))
    out[:, 128:256] = np.tile(np.concatenate([s, s], 1), (1, 4))
    inv = (np.float32(500000.0) ** (-np.arange(0, 16, 2, dtype=np.float32) / np.float32(16))).astype(np.float32)
    ang = pos[:, None] * inv[None, :]
    c, s = np.cos(ang).astype(np.float32), np.sin(ang).astype(np.float32)
    out[:, 256:384] = np.tile(np.concatenate([c, c], 1), (1, 8))
    out[:, 384:512] = np.tile(np.concatenate([s, s], 1), (1, 8))
    return out


def _chunk_of(sl, half):
    return 2 * sl + (sl % 2) if half == 0 else 2 * sl + 1 - (sl % 2)


_NC_CACHE = {}


def kernel(x, meta_tokens, attn_norm_g, w_in, conv_w, conv_b, conv_norm_g, conv_norm_b,
           w_out, mlp_norm_g, w_up, w_down, final_norm_g):
    x = np.asarray(x, np.float32)
    B, S, _ = x.shape
    T = NMETA + S
    NSLOT = S // 1024
    if S not in _NC_CACHE:
        _NC_CACHE[S] = build(S)
    nc = _NC_CACHE[S]
    f = lambda a: np.ascontiguousarray(np.asarray(a, np.float32))
    meta = f(meta_tokens)
    ropek = _rope_tab(np.arange(T))
    p = np.arange(128)
    consts = {
        "iota": np.tile(np.arange(512, dtype=np.float32)[None, :], (128, 1)),
        "pow2": np.tile((2.0 ** -(np.arange(NBIS) + 1.0)).astype(np.float32)[None, :], (128, 1)),
        "e32": (np.arange(32)[None, :] == (p % 32)[:, None]).astype(np.float32),
        "g4": (np.arange(4)[None, :] == (p // 32)[:, None]).astype(np.float32),
        "dm": np.concatenate([(np.arange(128)[None, :] == (32 * g + p % 32)[:, None]).astype(np.float32) for g in range(4)], axis=1),
    }
    shared = {
        "w_in": f(w_in[0]), "w_out": f(w_out[0]), "w_up": f(w_up[0]), "w_down": f(w_down[0]),
        "attn_g": f(attn_norm_g[0]), "mlp_g": f(mlp_norm_g[0]), "final_g": f(final_norm_g),
        "conv_w": f(conv_w[0]), "conv_b": f(conv_b[0]), "cn_g": f(conv_norm_g[0]), "cn_b": f(conv_norm_b[0]),
        "ropek": ropek,
    }
    shared.update(consts)
    in_maps = []
    for core in range(8):
        b, half = core // 2, core % 2
        hall = np.concatenate([meta, x[b]], axis=0)
        xown = np.zeros((NSLOT, 544, D), np.float32)
        ropeq = np.zeros((NSLOT, 512, 512), np.float32)
        qrel = np.zeros((128, NSLOT * 8), np.float32)
        for sl in range(NSLOT):
            c = _chunk_of(sl, half)
            p0 = NMETA + 512 * c
            lo = p0 - 32
            src_lo = max(lo, 0)
            xown[sl, src_lo - lo:, :] = hall[src_lo:p0 + 512]
            ropeq[sl] = ropek[p0:p0 + 512]
            w0 = NMETA + 1024 * sl
            for tb in range(4):
                for wc in range(2):
                    qrel[:, (sl * 4 + tb) * 2 + wc] = (p0 + tb * 128 + p) - w0 - 512 * wc
        m = dict(shared)
        m.update({"xall": hall, "xown": xown, "ropeq": ropeq, "qrel": qrel})
        in_maps.append(m)
    res = run_bass_kernel_spmd(nc, in_maps, core_ids=list(range(8)))
    out = np.zeros((B, S, D), np.float32)
    for core in range(8):
        b, half = core // 2, core % 2
        o = res.results[core]["out"]
        for sl in range(NSLOT):
            c = _chunk_of(sl, half)
            out[b, 512 * c:512 * (c + 1)] = o[sl * 512:(sl + 1) * 512]
    return out
```

```python
import numpy as np
from contextlib import ExitStack
import concourse.bass as bass
import concourse.mybir as mybir
from concourse.bass_utils import run_bass_kernel_spmd

F32 = mybir.dt.float32
BF16 = mybir.dt.bfloat16
ALU = mybir.AluOpType
AF = mybir.ActivationFunctionType
AX = mybir.AxisListType

D = 1024
NMETA = 16
DIN = 3144
DFF = 4096
EPS = 1e-5
KTOP = 256
NBIS = 20
ENGS = ("pe", "act", "dve", "pool", "sp")
SEM_CH = 30000
NDMA_SEM = 12


class Op:
    __slots__ = ("eng", "fn", "deps", "dma", "signals", "sig", "dma_idx")


class Sched:
    def __init__(self):
        self.ops = {e: [] for e in ENGS}
        self.lastw = {}
        self.readers = {}
        self.ndma = {e: 0 for e in ENGS}

    def op(self, eng, fn, reads=(), writes=(), dma=False):
        o = Op()
        o.eng = eng
        o.fn = fn
        o.dma = dma
        o.signals = False
        o.sig = None
        deps = {}
        xr = [k for k in reads if isinstance(k, tuple) and k[0] in ("pg", "psc", "pacc")]
        if xr:
            writes = list(writes) + [k for k in xr if k not in writes]
        for k in reads:
            w = self.lastw.get(k)
            if w is not None:
                deps[id(w)] = (w, True)
        for k in writes:
            w = self.lastw.get(k)
            if w is not None and id(w) not in deps:
                deps[id(w)] = (w, False)
            for r in self.readers.get(k, ()):
                if id(r) not in deps:
                    deps[id(r)] = (r, False)
        o.deps = []
        for d, raw in deps.values():
            if d.eng == eng and not d.dma:
                if eng == "pe":
                    continue
                if not raw and not dma:
                    continue
            d.signals = True
            o.deps.append(d)
        if dma:
            o.dma_idx = self.ndma[eng]
            self.ndma[eng] += 1
            o.signals = True
        for k in reads:
            self.readers.setdefault(k, []).append(o)
        for k in writes:
            self.lastw[k] = o
            self.readers[k] = []
        self.ops[eng].append(o)
        return o

    def emit(self, nc, stack):
        nsig = {}
        for e in ENGS:
            n = 0
            for o in self.ops[e]:
                if o.signals and not o.dma:
                    o.sig = n
                    n += 1
            nsig[e] = n
        csem = {}
        for e in ENGS:
            nch = (nsig[e] + SEM_CH - 1) // SEM_CH
            csem[e] = [stack.enter_context(nc.semaphore(f"c_{e}_{i}")) for i in range(nch)]
        dsem = {}
        for e in ENGS:
            if self.ndma[e]:
                dsem[e] = [stack.enter_context(nc.semaphore(f"d_{e}_{i}")) for i in range(NDMA_SEM)]

        def target(d):
            if d.dma:
                return dsem[d.eng][d.dma_idx % NDMA_SEM], 16 * (d.dma_idx // NDMA_SEM + 1)
            return csem[d.eng][d.sig // SEM_CH], d.sig % SEM_CH + 1

        block = stack.enter_context(nc.Block())

        def run(e):
            def body(eng):
                waited = {}
                for o in self.ops[e]:
                    tg = [target(d) for d in o.deps]
                    if o.dma and o.dma_idx >= NDMA_SEM:
                        tg.append((dsem[e][o.dma_idx % NDMA_SEM], 16 * (o.dma_idx // NDMA_SEM)))
                    for s, v in tg:
                        key = id(s)
                        if waited.get(key, 0) < v:
                            eng.wait_ge(s, v)
                            waited[key] = v
                    ins = o.fn(eng)
                    if o.signals:
                        if o.dma:
                            s, _ = target(o)
                            ins.then_inc(s, 16)
                        else:
                            ins.then_inc(csem[e][o.sig // SEM_CH], 1)
                if self.ndma[e]:
                    n = self.ndma[e]
                    for i in range(max(0, n - NDMA_SEM), n):
                        eng.wait_ge(dsem[e][i % NDMA_SEM], 16 * (i // NDMA_SEM + 1))
            return body

        for e, reg in (("pe", block.tensor), ("act", block.scalar), ("dve", block.vector),
                       ("pool", block.gpsimd), ("sp", block.sync)):
            if self.ops[e]:
                reg(run(e))


class Pool:
    def __init__(self, tiles, name, off=0):
        self.tiles = tiles
        self.name = name
        self.i = 0
        self.off = off

    def get(self):
        j = self.i % len(self.tiles)
        self.i += 1
        return self.tiles[j], (self.name, j + self.off)


def build(S):
    T = NMETA + S
    NCH = S // 512
    NSLOT = NCH // 2
    NKT = 1 + S // 128
    nc = bass.Bass("TRN2", target_bir_lowering=False)

    def din(name, shape, dt=F32):
        return nc.dram_tensor(name, shape, dt, kind="ExternalInput").ap()

    xall = din("xall", [T, D])
    xown = din("xown", [NSLOT, 544, D])
    ropek = din("ropek", [T, 512])
    ropeq = din("ropeq", [NSLOT, 512, 512])
    qrel_d = din("qrel", [128, NSLOT * 8])
    w_in = din("w_in", [D, DIN])
    w_out = din("w_out", [D, D])
    w_up = din("w_up", [D, DFF])
    w_down = din("w_down", [DFF, D])
    attn_g = din("attn_g", [D])
    mlp_g = din("mlp_g", [D])
    final_g = din("final_g", [D])
    conv_w = din("conv_w", [31, 512])
    conv_b = din("conv_b", [512])
    cn_g = din("cn_g", [512])
    cn_b = din("cn_b", [512])
    iota_d = din("iota", [128, 512])
    pow2_d = din("pow2", [128, NBIS])
    e32_d = din("e32", [128, 32])
    g4_d = din("g4", [128, 4])
    dm_d = din("dm", [128, 4 * 128])
    out_d = nc.dram_tensor("out", [NSLOT * 512, D], F32, kind="ExternalOutput").ap()

    k_scr = nc.dram_tensor("k_scr", [4, 128, T], BF16).ap()
    v_scr = nc.dram_tensor("v_scr", [T, 512], BF16).ap()

    import os
    KSTOP = float(os.environ.get('KSTOP', '99'))
    S_ = Sched()
    stage = [0]

    def op(*a, **k):
        if stage[0] <= KSTOP:
            return S_.op(*a, **k)
        return None
    uid = [0]

    def sb(shape, dt, name=None):
        uid[0] += 1
        return nc.alloc_sbuf_tensor(f"{name or 't'}{uid[0]}", shape, dt)

    def mkpool(n, shape, dt, name):
        return Pool([sb(shape, dt, name) for _ in range(n)], name)

    identf = sb([128, 128], F32, "identf")
    ident = sb([128, 128], BF16, "ident")
    op("pool", lambda e: e.memset(identf[:], 0.0), writes=["identf"])
    op("pool", lambda e: e.affine_select(out=identf[:], in_=identf[:], pattern=[[-1, 128]],
                                         compare_op=ALU.not_equal, fill=1.0, base=0, channel_multiplier=1),
       reads=["identf"], writes=["identf"])
    op("dve", lambda e: e.tensor_copy(out=ident[:], in_=identf[:]), reads=["identf"], writes=["ident"])

    def load_const(name, shape, src, dt=F32):
        t = sb(shape, dt, name)
        op("sp", lambda e: e.dma_start(out=t[:], in_=src, allow_slow_non_contiguous=True), writes=[name], dma=True)
        return t

    iota = load_const("iota", [128, 512], iota_d)
    pow2 = load_const("pow2", [128, NBIS], pow2_d)
    e32 = load_const("e32", [128, 32], e32_d)
    g4 = load_const("g4", [128, 4], g4_d)
    qrel = load_const("qrelc", [128, NSLOT * 8], qrel_d)
    gA = load_const("gA", [128, 8], attn_g.rearrange("(c p) -> p c", p=128))
    gM = load_const("gM", [128, 8], mlp_g.rearrange("(c p) -> p c", p=128))
    def bcast_row(name, src, n):
        t = sb([128, n], F32, name)
        op("sp", lambda e: e.dma_start(out=t[:], in_=src.partition_broadcast(128)), writes=[name], dma=True)
        return t
    gF = bcast_row("gF", final_g, D)
    cb_bc = bcast_row("cb_bc", conv_b, 512)
    cg_bc = bcast_row("cg_bc", cn_g, 512)
    cnb_bc = bcast_row("cnb_bc", cn_b, 512)
    cwT = sb([128, 4, 31], F32, "cwT")
    for cc_ in range(4):
        op("sp", lambda e, cc_=cc_: e.dma_start(out=cwT[:, cc_, :], in_=conv_w[:, cc_ * 128:(cc_ + 1) * 128].rearrange("j p -> p j"),
                                                 allow_slow_non_contiguous=True), writes=["cwT"], dma=True)
    dmb = sb([128, 512], BF16, "dmb")
    op("pool", lambda e: e.dma_start(out=dmb[:], in_=dm_d), writes=["dmb"], dma=True)
    wik_t = sb([128, 8, 64], BF16, "wik")
    op("pool", lambda e: e.dma_start(out=wik_t[:, :, :], in_=w_in[:, 2048:2112].rearrange("(c p) n -> p c n", p=128)), writes=["wik"], dma=True)
    wiw_t = sb([128, 8, 8], BF16, "wiw")
    op("pool", lambda e: e.dma_start(out=wiw_t[:, :, :], in_=w_in[:, 2112:2120].rearrange("(c p) n -> p c n", p=128), allow_slow_non_contiguous=True),
       writes=["wiw"], dma=True)

    win_b = nc.dram_tensor("win_b", [D, DIN], BF16).ap()
    wout_b = nc.dram_tensor("wout_b", [D, D], BF16).ap()
    wup_b = nc.dram_tensor("wup_b", [D, DFF], BF16).ap()
    wdown_b = nc.dram_tensor("wdown_b", [DFF, D], BF16).ap()

    ikT = sb([128, T], BF16, "ikT")
    scores = sb([128, T], F32, "scores")
    wbuf = mkpool(4, [128, 8, 512], BF16, "wbuf")
    xt_pool = mkpool(3, [128, D], F32, "xt")
    xn_pool = mkpool(1, [128, D], BF16, "xn")
    xnT_pool = mkpool(2, [128, 8, 128], BF16, "xnT")
    rope_pool = mkpool(2, [128, 256], F32, "rope")
    st_pool = mkpool(4, [128, 8], F32, "stat")
    tm_pool = mkpool(3, [128, 512], BF16, "tm")
    rt_pool = mkpool(2, [128, 256], F32, "rt")
    jk_pool = mkpool(2, [128, 512], F32, "jk")
    bigjunk = sb([128, T], mybir.dt.uint8, "bigjunk")
    pg = Pool([nc.alloc_psum_tensor(f"pg{i}", [128, 512], F32) for i in range(4)], "pg")
    psc = [nc.alloc_psum_tensor(f"psc{i}", [128, 512], F32) for i in range(2)]
    pacc = [nc.alloc_psum_tensor(f"pacc{i}", [128, 512], F32) for i in range(2)]

    def pg_bf16(t):
        return t.bitcast(BF16) if hasattr(t, "bitcast") else None

    def rmsnorm_T(x_t, x_k, rows, gcol, xnT, xnT_k):
        st, st_k = st_pool.get()
        jk, jk_k = jk_pool.get()
        xn, xn_k = xn_pool.get()
        op("act", lambda e: e.activation(out=jk[0:rows, :], in_=x_t[0:rows, 0:512], func=AF.Square,
                                         accum_out=st[0:rows, 0:1]), reads=[x_k], writes=[jk_k, (st_k, 0)])
        op("act", lambda e: e.activation(out=jk[0:rows, :], in_=x_t[0:rows, 512:1024], func=AF.Square,
                                         accum_out=st[0:rows, 1:2]), reads=[x_k], writes=[jk_k, (st_k, 1)])
        op("dve", lambda e: e.tensor_tensor(out=st[0:rows, 2:3], in0=st[0:rows, 0:1], in1=st[0:rows, 1:2], op=ALU.add),
           reads=[(st_k, 0), (st_k, 1)], writes=[(st_k, 2)])
        op("dve", lambda e: e.tensor_scalar(out=st[0:rows, 3:4], in0=st[0:rows, 2:3], scalar1=1.0 / D, scalar2=EPS,
                                            op0=ALU.mult, op1=ALU.add), reads=[(st_k, 2)], writes=[(st_k, 3)])
        op("act", lambda e: e.activation(out=st[0:rows, 4:5], in_=st[0:rows, 3:4], func=AF.Sqrt),
           reads=[(st_k, 3)], writes=[(st_k, 4)])
        op("dve", lambda e: e.reciprocal(out=st[0:rows, 5:6], in_=st[0:rows, 4:5]), reads=[(st_k, 4)], writes=[(st_k, 5)])
        op("dve", lambda e: e.tensor_scalar(out=xn[0:rows, :], in0=x_t[0:rows, :], scalar1=st[0:rows, 5:6], scalar2=None,
                                            op0=ALU.mult), reads=[x_k, (st_k, 5)], writes=[xn_k])
        pt, pt_k = pg.get()
        ptb = pt[:].bitcast(BF16)
        for dc in range(8):
            op("pe", lambda e, dc=dc: e.transpose(out=ptb[:, dc * 128:dc * 128 + rows], in_=xn[0:rows, dc * 128:(dc + 1) * 128],
                                                  identity=ident[0:rows, 0:rows]),
               reads=[xn_k, "ident"], writes=[pt_k])
        op("dve", lambda e: e.tensor_tensor(out=xnT[:, :, 0:rows],
                                            in0=ptb.rearrange("p (c t) -> p c t", c=8)[:, :, 0:rows],
                                            in1=gcol[:, :].unsqueeze(2).to_broadcast([128, 8, rows]), op=ALU.mult),
           reads=[pt_k], writes=[xnT_k])
        return (st, st_k)

    def wviews(kind, i, w):
        if kind == "down":
            v = w[:, :, :].rearrange("p a b -> p (a b)").rearrange("p (f n) -> p f n", f=4)
            r = lambda a: a[i * 512:(i + 1) * 512, :].rearrange("(f p) n -> p f n", p=128)
            return v, r(w_down), r(wdown_b)
        src, dst = {"in": (w_in, win_b), "out": (w_out, wout_b), "up": (w_up, wup_b)}[kind]
        r = lambda a: a[:, i:i + 512].rearrange("(c p) n -> p c n", p=128) if kind == "in" else a[:, i * 512:(i + 1) * 512].rearrange("(c p) n -> p c n", p=128)
        return w[:, :, :], r(src), r(dst)

    def load_w(kind, i, cast=False):
        w, w_k = wbuf.get()
        v, src, scr = wviews(kind, i, w)
        if cast:
            op("pool", lambda e: e.dma_start(out=v, in_=src), writes=[w_k], dma=True)
        else:
            op("sp", lambda e: e.dma_start(out=v, in_=scr), reads=[("wcv", kind, i)], writes=[w_k], dma=True)
        return (v if kind == "down" else w), w_k

    def proj_tm(xnT, xnT_k, rows, w, w_k, ncols, ps, ps_k, pcol=0):
        for dc in range(8):
            op("pe", lambda e, dc=dc: e.matmul(ps[0:rows, pcol:pcol + ncols], lhsT=xnT[:, dc, 0:rows], rhs=w[:, dc, 0:ncols],
                                               start=(dc == 0), stop=(dc == 7)),
               reads=[xnT_k, w_k], writes=[ps_k])


    def rope_tm(ps, ps_k, rows, nh, hd, half, rp, rp_k, roff, dst, dst_k, dcol=0):
        r2 = 2 * half
        n = nh * hd
        xf, xf_k = jk_pool.get()
        op("act", lambda e: e.copy(out=xf[0:rows, 0:n], in_=ps[0:rows, 0:n]), reads=[ps_k], writes=[xf_k])
        op("act", lambda e: e.copy(out=dst[0:rows, dcol:dcol + n], in_=ps[0:rows, 0:n]), reads=[ps_k], writes=[dst_k])
        rt, rt_k = rt_pool.get()
        pv = xf[0:rows, 0:n].rearrange("p (h d) -> p h d", h=nh)
        A = rt[0:rows, 0:nh * r2].rearrange("p (h d) -> p h d", h=nh)
        Bm = rt[0:rows, 128:128 + nh * r2].rearrange("p (h d) -> p h d", h=nh)
        cc_ = rp[0:rows, roff:roff + nh * r2].rearrange("p (h d) -> p h d", h=nh)
        ss_ = rp[0:rows, roff + 128:roff + 128 + nh * r2].rearrange("p (h d) -> p h d", h=nh)
        op("dve", lambda e: e.tensor_tensor(out=A, in0=pv[:, :, 0:r2], in1=cc_, op=ALU.mult), reads=[xf_k, rp_k], writes=[(rt_k, 0)])
        op("dve", lambda e: e.tensor_tensor(out=Bm, in0=pv[:, :, 0:r2], in1=ss_, op=ALU.mult), reads=[xf_k, rp_k], writes=[(rt_k, 1)])
        dv = dst[0:rows, dcol:dcol + n].rearrange("p (h d) -> p h d", h=nh)
        op("dve", lambda e: e.tensor_tensor(out=dv[:, :, 0:half], in0=A[:, :, 0:half], in1=Bm[:, :, half:r2], op=ALU.subtract),
           reads=[(rt_k, 0), (rt_k, 1), dst_k], writes=[dst_k])
        op("dve", lambda e: e.tensor_tensor(out=dv[:, :, half:r2], in0=A[:, :, half:r2], in1=Bm[:, :, 0:half], op=ALU.add),
           reads=[(rt_k, 0), (rt_k, 1), dst_k], writes=[dst_k])

    stage[0] = 1
    wk, wk_k = load_w("in", 512, cast=True)
    wv, wv_k = load_w("in", 1024, cast=True)
    cvt_pool = Pool(wbuf.tiles[2:4], "wbuf", off=2)
    for kind, idxs in (("in", (0, 1536, 2120, 2632)), ("out", (0, 1)), ("up", tuple(range(8))), ("down", tuple(range(8)))):
        for i in idxs:
            w, w_k = cvt_pool.get()
            v, src, scr = wviews(kind, i, w)
            op("pool", lambda e, v=v, src=src: e.dma_start(out=v, in_=src), writes=[w_k], dma=True)
            op("pool", lambda e, v=v, scr=scr: e.dma_start(out=scr, in_=v), reads=[w_k], writes=[("wcv", kind, i)], dma=True)
    wbuf.i = 2
    wik, wik_k = wik_t, "wik"
    stage[0] = 1
    for kt in range(min(NKT, int(os.environ.get('KT_MAX', '999')))):
        rows = NMETA if kt == 0 else 128
        p0 = 0 if kt == 0 else NMETA + 128 * (kt - 1)
        xt, xt_k = xt_pool.get()
        op("sp", lambda e, xt=xt, p0=p0, rows=rows: e.dma_start(out=xt[0:rows, :], in_=xall[p0:p0 + rows, :]), writes=[xt_k], dma=True)
        rp, rp_k = rope_pool.get()
        op("sp", lambda e, rp=rp, p0=p0, rows=rows: e.dma_start(out=rp[0:rows, :], in_=ropek[p0:p0 + rows, 0:256]), writes=[rp_k], dma=True)
        rpi, rpi_k = rope_pool.get()
        op("sp", lambda e, rpi=rpi, p0=p0, rows=rows: e.dma_start(out=rpi[0:rows, :], in_=ropek[p0:p0 + rows, 256:512]), writes=[rpi_k], dma=True)
        xnT, xnT_k = xnT_pool.get()
        stage[0] = 1.1
        rmsnorm_T(xt, xt_k, rows, gA, xnT, xnT_k)
        stage[0] = 1.2
        ps, ps_k = pg.get()
        proj_tm(xnT, xnT_k, rows, wk, wk_k, 512, ps, ps_k)
        stage[0] = 1.25
        kb, kb_k = tm_pool.get()
        rope_tm(ps, ps_k, rows, 4, 128, 16, rp, rp_k, 0, kb, kb_k)
        stage[0] = 1.3
        pt, pt_k = pg.get()
        ptb = pt[:].bitcast(BF16)
        for h in range(4):
            op("pe", lambda e, h=h, ptb=ptb, kb=kb, rows=rows: e.transpose(out=ptb[:, h * 128:h * 128 + rows], in_=kb[0:rows, h * 128:(h + 1) * 128],
                                                                         identity=ident[0:rows, 0:rows]), reads=[kb_k, "ident"], writes=[pt_k])
        kTt, kTt_k = tm_pool.get()
        op("act", lambda e, kTt=kTt, ptb=ptb: e.copy(out=kTt[:, :], in_=ptb[:, 0:512]), reads=[pt_k], writes=[kTt_k])
        op("sp", lambda e, kTt=kTt, p0=p0, rows=rows: e.dma_start(
            out=k_scr[:, :, p0:p0 + rows].rearrange("h p t -> p h t"),
            in_=kTt[:, :].rearrange("p (h t) -> p h t", h=4)[:, :, 0:rows]),
           reads=[kTt_k], writes=[("k_scr", kt)], dma=True)
        stage[0] = 1.4
        ps, ps_k = pg.get()
        proj_tm(xnT, xnT_k, rows, wv, wv_k, 512, ps, ps_k)
        vb, vb_k = tm_pool.get()
        op("act", lambda e, vb=vb, ps=ps, rows=rows: e.copy(out=vb[0:rows, :], in_=ps[0:rows, :]), reads=[ps_k], writes=[vb_k])
        op("sp", lambda e, vb=vb, p0=p0, rows=rows: e.dma_start(out=v_scr[p0:p0 + rows, :], in_=vb[0:rows, :]),
           reads=[vb_k], writes=[("v_scr", kt)], dma=True)
        stage[0] = 1.5
        ps, ps_k = pg.get()
        proj_tm(xnT, xnT_k, rows, wik, wik_k, 64, ps, ps_k)
        ib, ib_k = tm_pool.get()
        rope_tm(ps, ps_k, rows, 1, 64, 8, rpi, rpi_k, 0, ib, ib_k)
        op("dve", lambda e, ib=ib, rows=rows: e.tensor_copy(out=ib[0:rows, 64:128], in_=ib[0:rows, 0:64]), reads=[ib_k], writes=[ib_k])
        pt, pt_k = pg.get()
        ptb = pt[:].bitcast(BF16)
        op("pe", lambda e, ptb=ptb, ib=ib, rows=rows: e.transpose(out=ptb[:, 0:rows], in_=ib[0:rows, 0:128], identity=ident[0:rows, 0:rows]),
           reads=[ib_k, "ident"], writes=[pt_k])
        op("act", lambda e, ptb=ptb, p0=p0, rows=rows: e.copy(out=ikT[:, p0:p0 + rows], in_=ptb[:, 0:rows]), reads=[pt_k], writes=[("ikT", kt)])

    stage[0] = 2
    qT = sb([128, 4, 512], BF16, "qT")
    iqT = sb([128, 4, 512], BF16, "iqT")
    iwt = sb([128, 4, 8], F32, "iwt")
    uT = sb([128, 4, 544], BF16, "uT")
    mixed = sb([128, 4, D], BF16, "mixed")
    ysb = sb([128, 4, 512], F32, "ysb")
    Dcc = sb([128, 31, 128], BF16, "Dcc")
    negm_pool = mkpool(2, [128, 512], F32, "negm")
    sel = sb([128, 8, 128], BF16, "sel")
    lw = sb([128, 2, 128], BF16, "lw")
    w2 = sb([128, 8], F32, "w2")
    g4b = sb([128, 4], BF16, "g4b")
    op("dve", lambda e: e.tensor_copy(out=g4b[:], in_=g4[:]), reads=["g4"], writes=["g4b"])
    bis = sb([128, 8 + NBIS], F32, "bis")
    top8 = sb([128, 8], F32, "top8")
    cst = sb([128, 4, 6], F32, "cst")
    cmv = sb([128, 4, 2], F32, "cmv")
    crs = sb([128, 4], F32, "crs")
    R_pool = mkpool(3, [128, 512], BF16, "R")
    PT_pool = mkpool(2, [128, 512], BF16, "PT")
    mb_pool = mkpool(2, [128, 512], BF16, "mb")
    GK = 4
    kv_pool = [(sb([128, 4, GK * 128], BF16, "kbuf"), sb([128, GK, 4, 129], BF16, "vbuf")) for _ in range(2)]
    for i_, (kb_, vb_) in enumerate(kv_pool):
        op("pool", lambda e, vb_=vb_: e.memset(vb_[:, :, :, 128:129], 1.0), writes=[("vbuf", i_)])
    kmeta = sb([128, 4, NMETA], BF16, "kmeta")
    vmeta = sb([128, 4, 129], BF16, "vmeta")
    op("pool", lambda e: e.memset(vmeta[:, :, 128:129], 1.0), writes=["vmeta"])
    op("sp", lambda e: e.dma_start(out=kmeta[:, :, :], in_=k_scr[:, :, 0:NMETA].rearrange("h p t -> p h t")),
       reads=[("k_scr", 0)], writes=["kmeta"], dma=True)
    op("sp", lambda e: e.dma_start(out=vmeta[0:NMETA, :, 0:128], in_=v_scr[0:NMETA, :].rearrange("p (h d) -> p h d", h=4)),
       reads=[("v_scr", 0), "vmeta"], writes=["vmeta"], dma=True)
    mT = sb([128, 8, 128], BF16, "mT")
    hnT = sb([128, 8, 256], BF16, "hnT")
    uf_pool = mkpool(2, [128, 256], BF16, "uf")
    rden = sb([128, 4], F32, "rden")

    SC = 0.125 * (8 ** -0.5)
    kv_ctr = [0]

    for sl in range(NSLOT):
        E = NMETA + 512 * (2 * sl + 2)
        W0 = E - 1024
        NCHK = 2 * sl + 2
        wq, wq_k = load_w("in", 0)
        wiq, wiq_k = load_w("in", 1536)
        wga, wga_k = load_w("in", 2120)
        wgg, wgg_k = load_w("in", 2632)
        plan = []
        for pair_ in range(2):
            plan += [("out", 0), ("out", 1)]
            for fg_ in range(8):
                plan += [("up", fg_), ("down", fg_)]
        wfifo = []

        def issue_next():
            if plan:
                kind_, i_ = plan.pop(0)
                wfifo.append(load_w(kind_, i_))
        for ti in range(5):
            rows = 32 if ti == 0 else 128
            r0 = 0 if ti == 0 else 32 + 128 * (ti - 1)
            xt, xt_k = xt_pool.get()
            op("sp", lambda e, xt=xt, r0=r0, rows=rows, sl=sl: e.dma_start(out=xt[0:rows, :], in_=xown[sl, r0:r0 + rows, :]),
               writes=[xt_k], dma=True)
            xnT, xnT_k = xnT_pool.get()
            rmsnorm_T(xt, xt_k, rows, gA, xnT, xnT_k)
            if ti > 0:
                tb = ti - 1
                rp, rp_k = rope_pool.get()
                op("sp", lambda e, rp=rp, tb=tb, sl=sl: e.dma_start(out=rp[:, :], in_=ropeq[sl, 128 * tb:128 * (tb + 1), 0:256]), writes=[rp_k], dma=True)
                ps, ps_k = pg.get()
                proj_tm(xnT, xnT_k, 128, wq, wq_k, 512, ps, ps_k)
                qb, qb_k = tm_pool.get()
                rope_tm(ps, ps_k, 128, 4, 128, 16, rp, rp_k, 0, qb, qb_k)
                pt, pt_k = pg.get()
                ptb = pt[:].bitcast(BF16)
                for h in range(4):
                    op("pe", lambda e, h=h, ptb=ptb, qb=qb: e.transpose(out=ptb[:, h * 128:(h + 1) * 128], in_=qb[:, h * 128:(h + 1) * 128], identity=ident[:, :]),
                       reads=[qb_k, "ident"], writes=[pt_k])
                op("act", lambda e, ptb=ptb, tb=tb: e.copy(out=qT[:, :, tb * 128:(tb + 1) * 128], in_=ptb[:, 0:512].rearrange("p (h t) -> p h t", h=4)),
                   reads=[pt_k], writes=[("qT", tb)])
                rpi, rpi_k = rope_pool.get()
                op("sp", lambda e, rpi=rpi, tb=tb, sl=sl: e.dma_start(out=rpi[:, :], in_=ropeq[sl, 128 * tb:128 * (tb + 1), 256:512]), writes=[rpi_k], dma=True)
                ps, ps_k = pg.get()
                proj_tm(xnT, xnT_k, 128, wiq, wiq_k, 512, ps, ps_k)
                ib, ib_k = tm_pool.get()
                rope_tm(ps, ps_k, 128, 8, 64, 8, rpi, rpi_k, 0, ib, ib_k)
                pt, pt_k = pg.get()
                ptb = pt[:].bitcast(BF16)
                for hp in range(4):
                    op("pe", lambda e, hp=hp, ptb=ptb, ib=ib: e.transpose(out=ptb[:, hp * 128:(hp + 1) * 128], in_=ib[:, hp * 128:(hp + 1) * 128], identity=ident[:, :]),
                       reads=[ib_k, "ident"], writes=[pt_k])
                op("act", lambda e, ptb=ptb, tb=tb: e.copy(out=iqT[:, tb, :].rearrange("p (g h i) -> p h g i", g=4, h=4, i=32),
                                                           in_=ptb[:, 0:512].rearrange("p (h g i) -> p h g i", h=4, g=4, i=32)),
                   reads=[pt_k], writes=[("iqT", tb)])
                ps, ps_k = pg.get()
                proj_tm(xnT, xnT_k, 128, wiw_t, "wiw", 8, ps, ps_k)
                op("dve", lambda e, ps=ps, tb=tb: e.tensor_scalar(out=iwt[:, tb, :], in0=ps[:, 0:8], scalar1=SC, scalar2=None, op0=ALU.mult),
                   reads=[ps_k], writes=[("iwt", tb)])
            psa, psa_k = pg.get()
            proj_tm(xnT, xnT_k, rows, wga, wga_k, 512, psa, psa_k)
            psg, psg_k = pg.get()
            proj_tm(xnT, xnT_k, rows, wgg, wgg_k, 512, psg, psg_k)
            jk, jk_k = jk_pool.get()
            op("act", lambda e, jk=jk, psg=psg, rows=rows: e.activation(out=jk[0:rows, :], in_=psg[0:rows, :], func=AF.Sigmoid),
               reads=[psg_k], writes=[jk_k])
            ub, ub_k = tm_pool.get()
            op("dve", lambda e, ub=ub, psa=psa, jk=jk, rows=rows: e.tensor_tensor(out=ub[0:rows, :], in0=psa[0:rows, :], in1=jk[0:rows, :], op=ALU.mult),
               reads=[psa_k, jk_k], writes=[ub_k])
            pt, pt_k = pg.get()
            ptb = pt[:].bitcast(BF16)
            for cc in range(4):
                op("pe", lambda e, cc=cc, ptb=ptb, ub=ub, rows=rows: e.transpose(out=ptb[:, cc * 128:cc * 128 + rows], in_=ub[0:rows, cc * 128:(cc + 1) * 128],
                                                                              identity=ident[0:rows, 0:rows]),
                   reads=[ub_k, "ident"], writes=[pt_k])
            op("act", lambda e, ptb=ptb, r0=r0, rows=rows: e.copy(out=uT[:, :, r0:r0 + rows], in_=ptb[:, 0:512].rearrange("p (c t) -> p c t", c=4)[:, :, 0:rows]),
               reads=[pt_k], writes=[("uT", ti)])
        stage[0] = 3
        UTK = [("uT", ti) for ti in range(5)]
        for cc in range(4):
            op("dve", lambda e, cc=cc: e.tensor_tensor(out=Dcc[:, :, :], in0=identf[:, :].unsqueeze(1).to_broadcast([128, 31, 128]),
                                                       in1=cwT[:, cc, :].unsqueeze(2).to_broadcast([128, 31, 128]), op=ALU.mult),
               reads=["identf", "cwT"], writes=["Dcc"])
            for tb in range(4):
                ps, ps_k = pg.get()
                for j in range(31):
                    c0 = 2 + tb * 128 + j
                    op("pe", lambda e, ps=ps, cc=cc, c0=c0, j=j: e.matmul(ps[:, 0:128], lhsT=uT[:, cc, c0:c0 + 128], rhs=Dcc[:, j, :], start=(j == 0), stop=(j == 30)),
                       reads=UTK + ["Dcc"], writes=[ps_k])
                op("act", lambda e, ps=ps, tb=tb, cc=cc: e.copy(out=ysb[:, tb, cc * 128:(cc + 1) * 128], in_=ps[:, 0:128]),
                   reads=[ps_k], writes=[("ysb", tb)])
        for tb in range(4):
            yk = ("ysb", tb)
            y = ysb[:, tb, :]
            op("dve", lambda e, y=y: e.tensor_tensor(out=y, in0=y, in1=cb_bc[:, :], op=ALU.add), reads=[yk, "cb_bc"], writes=[yk])
            for g in range(4):
                op("dve", lambda e, y=y, g=g: e.bn_stats(out=cst[:, g, :], in_=y[:, g * 128:(g + 1) * 128]), reads=[yk], writes=[("cst", g)])
                op("dve", lambda e, g=g: e.bn_aggr(out=cmv[:, g, :], in_=cst[:, g, :]), reads=[("cst", g)], writes=[("cmv", g)])
            CM = [("cmv", g) for g in range(4)]
            op("dve", lambda e: e.tensor_scalar(out=crs[:, :], in0=cmv[:, :, 1], scalar1=EPS, scalar2=None, op0=ALU.add), reads=CM, writes=["crs"])
            op("act", lambda e: e.activation(out=crs[:, :], in_=crs[:, :], func=AF.Sqrt), reads=["crs"], writes=["crs"])
            op("dve", lambda e: e.reciprocal(out=crs[:, :], in_=crs[:, :]), reads=["crs"], writes=["crs"])
            for g in range(4):
                op("dve", lambda e, y=y, g=g: e.tensor_scalar(out=y[:, g * 128:(g + 1) * 128], in0=y[:, g * 128:(g + 1) * 128],
                                                            scalar1=cmv[:, g, 0:1], scalar2=crs[:, g:g + 1], op0=ALU.subtract, op1=ALU.mult),
                   reads=[yk, "crs"] + CM, writes=[yk])
            op("dve", lambda e, y=y: e.tensor_tensor(out=y, in0=y, in1=cg_bc[:, :], op=ALU.mult), reads=[yk, "cg_bc"], writes=[yk])
            op("dve", lambda e, y=y: e.tensor_tensor(out=y, in0=y, in1=cnb_bc[:, :], op=ALU.add), reads=[yk, "cnb_bc"], writes=[yk])
            op("act", lambda e, y=y, tb=tb: e.activation(out=mixed[:, tb, 512:1024], in_=y, func=AF.Silu), reads=[yk], writes=[("mixC", tb)])

        stage[0] = 4
        for tb in range(4):
            if tb == 3:
                for _ in range(4):
                    issue_next()
            for h in range(8):
                par, hp = h % 2, h // 2
                op("pool", lambda e, h=h, par=par, hp=hp, tb=tb: e.tensor_scalar(out=lw[:, par, hp * 32:(hp + 1) * 32], in0=e32[:, :], scalar1=iwt[:, tb, h:h + 1],
                                                                                 scalar2=None, op0=ALU.mult),
                   reads=["e32", ("iwt", tb)], writes=[("lw", h)])
            LWK = [("lw", h) for h in range(8)]
            ps, ps_k = pg.get()
            for par in range(2):
                op("pe", lambda e, ps=ps, par=par: e.matmul(ps[:, par * 4:(par + 1) * 4], lhsT=lw[:, par, :], rhs=g4b[:, :], start=True, stop=True),
                   reads=LWK + ["g4b"], writes=[ps_k])
            op("dve", lambda e, ps=ps: e.tensor_copy(out=w2[:, :], in_=ps[:, 0:8]), reads=[ps_k], writes=["w2"])
            for idx in range(8):
                g = idx % 4
                op("pool", lambda e, idx=idx, g=g: e.tensor_scalar(out=sel[:, idx, :], in0=dmb[:, g * 128:(g + 1) * 128], scalar1=w2[:, idx:idx + 1],
                                                                   scalar2=None, op0=ALU.mult),
                   reads=["dmb", "w2"], writes=[("sel", idx)])
            chunks = [(0, NMETA)] + [(NMETA + 512 * m, 512) for m in range(NCHK)]
            SCK = []
            wcnt = 0
            for ci, (c0, cn) in enumerate(chunks):
                ikk = [("ikT", 0)] if ci == 0 else [("ikT", 1 + 4 * (ci - 1) + q) for q in range(4)]
                sc = psc[ci % 2]
                sc_k = ("psc", ci % 2)
                pend = None
                items = [(g, par) for par in range(2) for g in range(4)]
                for k, (g, par) in enumerate(items):
                    ps, ps_k = pg.get()
                    t0 = tb * 128 + g * 32
                    op("pe", lambda e, ps=ps, par=par, tb=tb, g=g, c0=c0, cn=cn: e.matmul(ps[:, 0:cn], lhsT=iqT[par * 64:(par + 1) * 64, tb, g * 128:(g + 1) * 128],
                                                                                    rhs=ikT[par * 64:(par + 1) * 64, c0:c0 + cn], start=True, stop=True),
                       reads=[("iqT", tb)] + ikk, writes=[ps_k])
                    R, R_k = R_pool.get()
                    if k % 2 == 0:
                        op("act", lambda e, R=R, ps=ps, cn=cn: e.activation(out=R[:, 0:cn], in_=ps[:, 0:cn], func=AF.Relu), reads=[ps_k], writes=[R_k])
                    else:
                        op("dve", lambda e, R=R, ps=ps, cn=cn: e.tensor_scalar(out=R[:, 0:cn], in0=ps[:, 0:cn], scalar1=0.0, scalar2=None, op0=ALU.max),
                           reads=[ps_k], writes=[R_k])
                    if pend is not None:
                        pk, pR, pR_k, pidx = pend
                        op("pe", lambda e, sc=sc, pidx=pidx, pR=pR, cn=cn, pk=pk: e.matmul(sc[:, 0:cn], lhsT=sel[:, pidx, :], rhs=pR[:, 0:cn], start=(pk == 0), stop=False),
                           reads=[("sel", pidx), pR_k], writes=[sc_k])
                    pend = (k, R, R_k, par * 4 + g)
                pk, pR, pR_k, pidx = pend
                op("pe", lambda e, sc=sc, pidx=pidx, pR=pR, cn=cn: e.matmul(sc[:, 0:cn], lhsT=sel[:, pidx, :], rhs=pR[:, 0:cn], start=False, stop=True),
                   reads=[("sel", pidx), pR_k], writes=[sc_k])
                sk = ("sc", ci)
                SCK.append(sk)
                if c0 >= W0 and ci > 0:
                    col = (sl * 4 + tb) * 2 + wcnt
                    negm, negm_k = negm_pool.get()
                    op("dve", lambda e, col=col, negm=negm: e.tensor_scalar(out=negm[:, :], in0=iota[:, :], scalar1=qrel[:, col:col + 1], scalar2=-1e30,
                                                                          op0=ALU.is_gt, op1=ALU.mult), reads=["iota", "qrelc"], writes=[negm_k])
                    op("dve", lambda e, sc=sc, c0=c0, negm=negm: e.tensor_tensor(out=scores[:, c0:c0 + 512], in0=sc[:, :], in1=negm[:, :], op=ALU.add),
                       reads=[sc_k, negm_k], writes=[sk])
                    jk, jk_k = jk_pool.get()
                    op("dve", lambda e, sc=sc, jk=jk, negm=negm: e.tensor_tensor(out=jk[:, :], in0=sc[:, :], in1=negm[:, :], op=ALU.subtract),
                       reads=[sc_k, negm_k], writes=[jk_k])
                    op("dve", lambda e, jk=jk, wcnt=wcnt: e.tensor_reduce(out=bis[:, 5 + wcnt:6 + wcnt], in_=jk[:, :], axis=AX.X, op=ALU.min),
                       reads=[jk_k], writes=[("bis", 5 + wcnt)])
                    wcnt += 1
                else:
                    op("act", lambda e, sc=sc, c0=c0, cn=cn: e.copy(out=scores[:, c0:c0 + cn], in_=sc[:, 0:cn]), reads=[sc_k], writes=[sk])
            assert wcnt == 2
            stage[0] = 5
            nD = max(1, int(round(0.45 * NCHK)))
            ED = NMETA + 512 * nD
            EA = E - ED
            SCK_D = SCK[0:1 + nD]
            SCK_A = SCK[1 + nD:]
            op("dve", lambda e, E=E: e.max(out=top8[:, :], in_=scores[:, 0:E]), reads=SCK, writes=["top8"])
            op("dve", lambda e, W0=W0: e.tensor_reduce(out=bis[:, 7:8], in_=scores[:, 0:W0], axis=AX.X, op=ALU.min), reads=SCK, writes=[("bis", 7)])
            op("dve", lambda e: e.tensor_reduce(out=bis[:, 0:1], in_=bis[:, 5:8], axis=AX.X, op=ALU.min),
               reads=[("bis", 5), ("bis", 6), ("bis", 7)], writes=[("bis", 0)])
            op("dve", lambda e: e.tensor_tensor(out=bis[:, 1:2], in0=top8[:, 0:1], in1=bis[:, 0:1], op=ALU.subtract),
               reads=["top8", ("bis", 0)], writes=[("bis", 1)])
            op("dve", lambda e: e.tensor_scalar(out=bis[:, 8:8 + NBIS], in0=pow2[:, :], scalar1=bis[:, 1:2], scalar2=None, op0=ALU.mult),
               reads=["pow2", ("bis", 1)], writes=["bisW"])
            op("dve", lambda e: e.tensor_tensor(out=bis[:, 2:3], in0=bis[:, 8:9], in1=bis[:, 0:1], op=ALU.add),
               reads=["bisW", ("bis", 0)], writes=[("bis", 2)])
            for k in range(NBIS):
                op("dve", lambda e, ED=ED: e.tensor_scalar(out=bigjunk[:, 0:ED], in0=scores[:, 0:ED], scalar1=bis[:, 2:3], scalar2=None,
                                                          op0=ALU.is_ge, op1=ALU.add, accum_out=bis[:, 3:4]),
                   reads=SCK_D + [("bis", 2)], writes=["bigjunkD", ("bis", 3)])
                op("act", lambda e, ED=ED, E=E: e.activation(out=bigjunk[:, ED:E].bitcast(mybir.dt.int8), in_=scores[:, ED:E], func=AF.Sign,
                                                            bias=bis[:, 2:3], scale=-1.0, accum_out=bis[:, 5:6]),
                   reads=SCK_A + [("bis", 2)], writes=["bigjunkA", ("bis", 5)])
                op("dve", lambda e: e.scalar_tensor_tensor(out=bis[:, 6:7], in0=bis[:, 3:4], scalar=2.0, in1=bis[:, 5:6], op0=ALU.mult, op1=ALU.subtract),
                   reads=[("bis", 3), ("bis", 5)], writes=[("bis", 6)])
                op("dve", lambda e, k=k, EA=EA: e.tensor_scalar(out=bis[:, 4:5], in0=bis[:, 6:7], scalar1=float(2 * KTOP - 1 - EA), scalar2=bis[:, 8 + k:9 + k],
                                                               op0=ALU.is_ge, op1=ALU.mult), reads=[("bis", 6), "bisW"], writes=[("bis", 4)])
                if k < NBIS - 1:
                    op("dve", lambda e, k=k: e.scalar_tensor_tensor(out=bis[:, 2:3], in0=bis[:, 4:5], scalar=bis[:, 9 + k:10 + k], in1=bis[:, 2:3],
                                                                  op0=ALU.subtract, op1=ALU.add),
                       reads=[("bis", 4), "bisW", ("bis", 2)], writes=[("bis", 2)])
                else:
                    op("dve", lambda e, k=k: e.scalar_tensor_tensor(out=bis[:, 0:1], in0=bis[:, 4:5], scalar=bis[:, 8 + k:9 + k], in1=bis[:, 2:3],
                                                                  op0=ALU.subtract, op1=ALU.add),
                       reads=[("bis", 4), "bisW", ("bis", 2)], writes=[("bis", 0)])
            stage[0] = 6
            nkb_total = 1 + 4 * NCHK
            kblist = []
            for ci, (c0, cn) in enumerate(chunks):
                for kb in range(1 if ci == 0 else 4):
                    kblist.append((ci, c0, cn, kb))
            cstate = {}

            def chunk_setup(ci, c0, cn):
                mbt, mb_k = mb_pool.get()
                op("dve", lambda e, mbt=mbt, c0=c0, cn=cn: e.tensor_scalar(out=mbt[:, 0:cn], in0=scores[:, c0:c0 + cn], scalar1=bis[:, 0:1], scalar2=-30000.0,
                                                                          op0=ALU.is_lt, op1=ALU.mult), reads=[SCK[ci], ("bis", 0)], writes=[mb_k])
                if ci == 0:
                    cstate[ci] = (mbt, mb_k, kmeta, vmeta, "kmeta", "vmeta")
                    return
                kvn = kv_ctr[0] % 2
                kv_ctr[0] += 1
                kbuf, vbuf = kv_pool[kvn]
                kvk, vvk = ("kbuf", kvn), ("vbuf", kvn)
                tl = [1 + 4 * (ci - 1) + q for q in range(4)]
                op("sp", lambda e, kbuf=kbuf, c0=c0: e.dma_start(out=kbuf[:, :, :], in_=k_scr[:, :, c0:c0 + 512].rearrange("h p t -> p h t")),
                   reads=[("k_scr", t) for t in tl], writes=[kvk], dma=True)
                for g_ in range(4):
                    op("sp", lambda e, vbuf=vbuf, c0=c0, g_=g_: e.dma_start(out=vbuf[:, g_, :, 0:128],
                                                                     in_=v_scr[c0 + g_ * 128:c0 + (g_ + 1) * 128, :].rearrange("p (h d) -> p h d", h=4)),
                       reads=[("v_scr", t) for t in tl], writes=[vvk], dma=True)
                cstate[ci] = (mbt, mb_k, kbuf, vbuf, kvk, vvk)

            def emit_S(i):
                ci, c0, cn, kb = kblist[i]
                if kb == 0:
                    chunk_setup(ci, c0, cn)
                mbt, mb_k, kbuf, vbuf, kvk, vvk = cstate[ci]
                ks = NMETA if ci == 0 else 128
                st, st_k = pg.get()
                for h in range(4):
                    op("pe", lambda e, st=st, h=h, kbuf=kbuf, kb=kb, ks=ks, tb=tb: e.matmul(st[0:ks, h * 128:(h + 1) * 128], lhsT=kbuf[:, h, kb * 128:kb * 128 + ks],
                                                                                         rhs=qT[:, h, tb * 128:(tb + 1) * 128], start=True, stop=False),
                       reads=[kvk, ("qT", tb)], writes=[st_k])
                    op("pe", lambda e, st=st, h=h, mbt=mbt, kb=kb, ks=ks: e.matmul(st[0:ks, h * 128:(h + 1) * 128], lhsT=mbt[:, kb * 128:kb * 128 + ks],
                                                                                 rhs=ident[:, :], start=False, stop=True),
                       reads=[mb_k, "ident"], writes=[st_k])
                PT, PT_k = PT_pool.get()
                op("act", lambda e, PT=PT, st=st, ks=ks: e.activation(out=PT[0:ks, :], in_=st[0:ks, :], func=AF.Exp, scale=128 ** -0.5),
                   reads=[st_k], writes=[PT_k])
                return (PT, PT_k)

            def emit_P(i, PTs):
                ci, c0, cn, kb = kblist[i]
                mbt, mb_k, kbuf, vbuf, kvk, vvk = cstate[ci]
                ks = NMETA if ci == 0 else 128
                PT, PT_k = PTs
                for h in range(4):
                    acc = pacc[h // 2]
                    a0 = (h % 2) * 256
                    rhs = vbuf[0:ks, h, :] if ci == 0 else vbuf[:, kb, h, :]
                    op("pe", lambda e, acc=acc, a0=a0, PT=PT, h=h, ks=ks, rhs=rhs, i=i: e.matmul(acc[:, a0:a0 + 129], lhsT=PT[0:ks, h * 128:(h + 1) * 128], rhs=rhs,
                                                                                           start=(i == 0 and h % 2 == 0), stop=(i == nkb_total - 1),
                                                                                           skip_group_check=True),
                       reads=[PT_k, vvk], writes=[("pacc", h // 2)])

            prev = emit_S(0)
            for i in range(nkb_total):
                nxt = emit_S(i + 1) if i + 1 < nkb_total else None
                emit_P(i, prev)
                prev = nxt
            for h in range(4):
                acc = pacc[h // 2]
                a0 = (h % 2) * 256
                op("dve", lambda e, acc=acc, a0=a0, h=h: e.reciprocal(out=rden[:, h:h + 1], in_=acc[:, a0 + 128:a0 + 129]),
                   reads=[("pacc", h // 2)], writes=[("rden", h)])
                op("dve", lambda e, acc=acc, a0=a0, h=h, tb=tb: e.tensor_scalar(out=mixed[:, tb, h * 128:(h + 1) * 128], in0=acc[:, a0:a0 + 128], scalar1=rden[:, h:h + 1],
                                                                              scalar2=None, op0=ALU.mult),
                   reads=[("pacc", h // 2), ("rden", h)], writes=[("mixA", tb, h)])

        stage[0] = 7
        for pair in range(2):
            h1s = []
            while len(wfifo) < 2:
                issue_next()
            wos = [wfifo.pop(0), wfifo.pop(0)]
            for t2 in range(2):
                tb = 2 * pair + t2
                pt, pt_k = pg.get()
                ptb = pt[:].bitcast(BF16)
                for ec in range(8):
                    op("pe", lambda e, ptb=ptb, ec=ec, tb=tb: e.transpose(out=ptb[:, ec * 128:(ec + 1) * 128], in_=mixed[:, tb, ec * 128:(ec + 1) * 128], identity=ident[:, :]),
                       reads=[("mixA", tb, h) for h in range(4)] + [("mixC", tb), "ident"], writes=[pt_k])
                op("act", lambda e, ptb=ptb: e.copy(out=mT[:, :, :], in_=ptb[:, :].rearrange("p (c t) -> p c t", c=8)), reads=[pt_k], writes=["mT"])
                xt, xt_k = xt_pool.get()
                op("sp", lambda e, xt=xt, sl=sl, tb=tb: e.dma_start(out=xt[:, :], in_=xown[sl, 32 + tb * 128:32 + (tb + 1) * 128, :]), writes=[xt_k], dma=True)
                h1, h1_k = xt, xt_k
                for half in range(2):
                    wo, wo_k = wos[half]
                    for ec in range(8):
                        op("pe", lambda e, half=half, ec=ec, wo=wo: e.matmul(psc[half][:, :], lhsT=mT[:, ec, :], rhs=wo[:, ec, :], start=(ec == 0), stop=(ec == 7)),
                           reads=["mT", wo_k], writes=[("psc", half)])
                    op("dve", lambda e, half=half, h1=h1, xt=xt: e.tensor_tensor(out=h1[:, half * 512:(half + 1) * 512], in0=psc[half][:, :],
                                                                               in1=xt[:, half * 512:(half + 1) * 512], op=ALU.add),
                       reads=[("psc", half), xt_k], writes=[h1_k])
                rmsnorm_T(h1, h1_k, 128, gM, hnT[:, :, t2 * 128:(t2 + 1) * 128], ("hnT", t2))
                h1s.append((h1, h1_k, tb))
            issue_next()
            issue_next()
            accs = [[(psc[0], ("psc", 0)), (psc[1], ("psc", 1))], [(pacc[0], ("pacc", 0)), (pacc[1], ("pacc", 1))]]
            for fg in range(8):
                while len(wfifo) < 2:
                    issue_next()
                wu, wu_k = wfifo.pop(0)
                wdv, wd_k = wfifo.pop(0)
                for fc in range(4):
                    ps, ps_k = pg.get()
                    for dc in range(8):
                        op("pe", lambda e, ps=ps, wu=wu, dc=dc, fc=fc: e.matmul(ps[:, 0:256], lhsT=wu[:, dc, fc * 128:(fc + 1) * 128], rhs=hnT[:, dc, :],
                                                                             start=(dc == 0), stop=(dc == 7)),
                           reads=[wu_k, ("hnT", 0), ("hnT", 1)], writes=[ps_k])
                    uf, uf_k = uf_pool.get()
                    op("act", lambda e, uf=uf, ps=ps: e.activation(out=uf[:, :], in_=ps[:, 0:256], func=AF.Relu), reads=[ps_k], writes=[uf_k])
                    op("pool", lambda e, uf=uf: e.tensor_tensor(out=uf[:, :], in0=uf[:, :], in1=uf[:, :], op=ALU.mult), reads=[uf_k], writes=[uf_k])
                    first = (fg == 0 and fc == 0)
                    last = (fg == 7 and fc == 3)
                    for t2 in range(2):
                        for half in range(2):
                            acc, acc_k = accs[t2][half]
                            op("pe", lambda e, acc=acc, uf=uf, t2=t2, half=half, wdv=wdv, fc=fc, first=first, last=last: e.matmul(
                                acc[:, :], lhsT=uf[:, t2 * 128:(t2 + 1) * 128], rhs=wdv[:, fc, half * 512:(half + 1) * 512], start=first, stop=last),
                               reads=[uf_k, wd_k], writes=[acc_k])
                issue_next()
                issue_next()
            for t2 in range(2):
                h1, h1_k, tb = h1s[t2]
                for half in range(2):
                    acc, acc_k = accs[t2][half]
                    op("dve", lambda e, acc=acc, h1=h1, half=half: e.tensor_tensor(out=h1[:, half * 512:(half + 1) * 512], in0=acc[:, :],
                                                                                 in1=h1[:, half * 512:(half + 1) * 512], op=ALU.add),
                       reads=[acc_k, h1_k], writes=[h1_k])
                st, st_k = st_pool.get()
                jk, jk_k = jk_pool.get()
                op("act", lambda e, jk=jk, h1=h1, st=st: e.activation(out=jk[:, :], in_=h1[:, 0:512], func=AF.Square, accum_out=st[:, 0:1]),
                   reads=[h1_k], writes=[jk_k, (st_k, 0)])
                op("act", lambda e, jk=jk, h1=h1, st=st: e.activation(out=jk[:, :], in_=h1[:, 512:1024], func=AF.Square, accum_out=st[:, 1:2]),
                   reads=[h1_k], writes=[jk_k, (st_k, 1)])
                op("dve", lambda e, st=st: e.tensor_tensor(out=st[:, 2:3], in0=st[:, 0:1], in1=st[:, 1:2], op=ALU.add), reads=[(st_k, 0), (st_k, 1)], writes=[(st_k, 2)])
                op("dve", lambda e, st=st: e.tensor_scalar(out=st[:, 3:4], in0=st[:, 2:3], scalar1=1.0 / D, scalar2=EPS, op0=ALU.mult, op1=ALU.add),
                   reads=[(st_k, 2)], writes=[(st_k, 3)])
                op("act", lambda e, st=st: e.activation(out=st[:, 4:5], in_=st[:, 3:4], func=AF.Sqrt), reads=[(st_k, 3)], writes=[(st_k, 4)])
                op("dve", lambda e, st=st: e.reciprocal(out=st[:, 5:6], in_=st[:, 4:5]), reads=[(st_k, 4)], writes=[(st_k, 5)])
                op("dve", lambda e, h1=h1, st=st: e.scalar_tensor_tensor(out=h1[:, :], in0=h1[:, :], scalar=st[:, 5:6], in1=gF[:, :], op0=ALU.mult, op1=ALU.mult),
                   reads=[h1_k, (st_k, 5), "gF"], writes=[h1_k])
                r0 = sl * 512 + tb * 128
                op("sp", lambda e, h1=h1, r0=r0: e.dma_start(out=out_d[r0:r0 + 128, :], in_=h1[:, :]), reads=[h1_k], writes=[("out", r0)], dma=True)

    if os.environ.get("KDEBUG"):
        print("sbuf remaining", nc.sbuf_bytes_remaining, "ops", {e: len(S_.ops[e]) for e in ENGS})
    with ExitStack() as stack:
        S_.emit(nc, stack)
    return nc


def _rope_tab(pos):
    pos = np.asarray(pos, np.float32)
    out = np.zeros((len(pos), 512), np.float32)
    inv = (np.float32(500000.0) ** (-np.arange(0, 32, 2, dtype=np.float32) / np.float32(32))).astype(np.float32)
    ang = pos[:, None] * inv[None, :]
    c, s = np.cos(ang).astype(np.float32), np.sin(ang).astype(np.float32)
    out[:, 0:128] = np.tile(np.concatenate([c, c], 1), (1, 4))
    out[:, 128:256] = np.tile(np.concatenate([s, s], 1), (1, 4))
    inv = (np.float32(500000.0) ** (-np.arange(0, 16, 2, dtype=np.float32) / np.float32(16))).astype(np.float32)
    ang = pos[:, None] * inv[None, :]
    c, s = np.cos(ang).astype(np.float32), np.sin(ang).astype(np.float32)
    out[:, 256:384] = np.tile(np.concatenate([c, c], 1), (1, 8))
    out[:, 384:512] = np.tile(np.concatenate([s, s], 1), (1, 8))
    return out


def _chunk_of(sl, half):
    return 2 * sl + (sl % 2) if half == 0 else 2 * sl + 1 - (sl % 2)


_NC_CACHE = {}


def kernel(x, meta_tokens, attn_norm_g, w_in, conv_w, conv_b, conv_norm_g, conv_norm_b,
           w_out, mlp_norm_g, w_up, w_down, final_norm_g):
    x = np.asarray(x, np.float32)
    B, S, _ = x.shape
    T = NMETA + S
    NSLOT = S // 1024
    if S not in _NC_CACHE:
        _NC_CACHE[S] = build(S)
    nc = _NC_CACHE[S]
    f = lambda a: np.ascontiguousarray(np.asarray(a, np.float32))
    meta = f(meta_tokens)
    ropek = _rope_tab(np.arange(T))
    p = np.arange(128)
    consts = {
        "iota": np.tile(np.arange(512, dtype=np.float32)[None, :], (128, 1)),
        "pow2": np.tile((2.0 ** -(np.arange(NBIS) + 1.0)).astype(np.float32)[None, :], (128, 1)),
        "e32": (np.arange(32)[None, :] == (p % 32)[:, None]).astype(np.float32),
        "g4": (np.arange(4)[None, :] == (p // 32)[:, None]).astype(np.float32),
        "dm": np.concatenate([(np.arange(128)[None, :] == (32 * g + p % 32)[:, None]).astype(np.float32) for g in range(4)], axis=1),
    }
    shared = {
        "w_in": f(w_in[0]), "w_out": f(w_out[0]), "w_up": f(w_up[0]), "w_down": f(w_down[0]),
        "attn_g": f(attn_norm_g[0]), "mlp_g": f(mlp_norm_g[0]), "final_g": f(final_norm_g),
        "conv_w": f(conv_w[0]), "conv_b": f(conv_b[0]), "cn_g": f(conv_norm_g[0]), "cn_b": f(conv_norm_b[0]),
        "ropek": ropek,
    }
    shared.update(consts)
    in_maps = []
    for core in range(8):
        b, half = core // 2, core % 2
        hall = np.concatenate([meta, x[b]], axis=0)
        xown = np.zeros((NSLOT, 544, D), np.float32)
        ropeq = np.zeros((NSLOT, 512, 512), np.float32)
        qrel = np.zeros((128, NSLOT * 8), np.float32)
        for sl in range(NSLOT):
            c = _chunk_of(sl, half)
            p0 = NMETA + 512 * c
            lo = p0 - 32
            src_lo = max(lo, 0)
            xown[sl, src_lo - lo:, :] = hall[src_lo:p0 + 512]
            ropeq[sl] = ropek[p0:p0 + 512]
            w0 = NMETA + 1024 * sl
            for tb in range(4):
                for wc in range(2):
                    qrel[:, (sl * 4 + tb) * 2 + wc] = (p0 + tb * 128 + p) - w0 - 512 * wc
        m = dict(shared)
        m.update({"xall": hall, "xown": xown, "ropeq": ropeq, "qrel": qrel})
        in_maps.append(m)
    res = run_bass_kernel_spmd(nc, in_maps, core_ids=list(range(8)))
    out = np.zeros((B, S, D), np.float32)
    for core in range(8):
        b, half = core // 2, core % 2
        o = res.results[core]["out"]
        for sl in range(NSLOT):
            c = _chunk_of(sl, half)
            out[b, 512 * c:512 * (c + 1)] = o[sl * 512:(sl + 1) * 512]
    return out
```

```python
import numpy as np
from contextlib import ExitStack
import concourse.bass as bass
import concourse.mybir as mybir
from concourse.bass_utils import run_bass_kernel_spmd

F32 = mybir.dt.float32
BF16 = mybir.dt.bfloat16
ALU = mybir.AluOpType
AF = mybir.ActivationFunctionType
AX = mybir.AxisListType

D = 1024
NMETA = 16
DIN = 3144
DFF = 4096
EPS = 1e-5
KTOP = 256
NBIS = 20
ENGS = ("pe", "act", "dve", "pool", "sp")
SEM_CH = 30000
NDMA_SEM = 12


class Op:
    __slots__ = ("eng", "fn", "deps", "dma", "signals", "sig", "dma_idx")


class Sched:
    def __init__(self):
        self.ops = {e: [] for e in ENGS}
        self.lastw = {}
        self.readers = {}
        self.ndma = {e: 0 for e in ENGS}

    def op(self, eng, fn, reads=(), writes=(), dma=False):
        o = Op()
        o.eng = eng
        o.fn = fn
        o.dma = dma
        o.signals = False
        o.sig = None
        deps = {}
        xr = [k for k in reads if isinstance(k, tuple) and k[0] in ("pg", "psc", "pacc")]
        if xr:
            writes = list(writes) + [k for k in xr if k not in writes]
        for k in reads:
            w = self.lastw.get(k)
            if w is not None:
                deps[id(w)] = (w, True)
        for k in writes:
            w = self.lastw.get(k)
            if w is not None and id(w) not in deps:
                deps[id(w)] = (w, False)
            for r in self.readers.get(k, ()):
                if id(r) not in deps:
                    deps[id(r)] = (r, False)
        o.deps = []
        for d, raw in deps.values():
            if d.eng == eng and not d.dma:
                if eng == "pe":
                    continue
                if not raw and not dma:
                    continue
            d.signals = True
            o.deps.append(d)
        if dma:
            o.dma_idx = self.ndma[eng]
            self.ndma[eng] += 1
            o.signals = True
        for k in reads:
            self.readers.setdefault(k, []).append(o)
        for k in writes:
            self.lastw[k] = o
            self.readers[k] = []
        self.ops[eng].append(o)
        return o

    def emit(self, nc, stack):
        nsig = {}
        for e in ENGS:
            n = 0
            for o in self.ops[e]:
                if o.signals and not o.dma:
                    o.sig = n
                    n += 1
            nsig[e] = n
        csem = {}
        for e in ENGS:
            nch = (nsig[e] + SEM_CH - 1) // SEM_CH
            csem[e] = [stack.enter_context(nc.semaphore(f"c_{e}_{i}")) for i in range(nch)]
        dsem = {}
        for e in ENGS:
            if self.ndma[e]:
                dsem[e] = [stack.enter_context(nc.semaphore(f"d_{e}_{i}")) for i in range(NDMA_SEM)]

        def target(d):
            if d.dma:
                return dsem[d.eng][d.dma_idx % NDMA_SEM], 16 * (d.dma_idx // NDMA_SEM + 1)
            return csem[d.eng][d.sig // SEM_CH], d.sig % SEM_CH + 1

        block = stack.enter_context(nc.Block())

        def run(e):
            def body(eng):
                waited = {}
                for o in self.ops[e]:
                    tg = [target(d) for d in o.deps]
                    if o.dma and o.dma_idx >= NDMA_SEM:
                        tg.append((dsem[e][o.dma_idx % NDMA_SEM], 16 * (o.dma_idx // NDMA_SEM)))
                    for s, v in tg:
                        key = id(s)
                        if waited.get(key, 0) < v:
                            eng.wait_ge(s, v)
                            waited[key] = v
                    ins = o.fn(eng)
                    if o.signals:
                        if o.dma:
                            s, _ = target(o)
                            ins.then_inc(s, 16)
                        else:
                            ins.then_inc(csem[e][o.sig // SEM_CH], 1)
                if self.ndma[e]:
                    n = self.ndma[e]
                    for i in range(max(0, n - NDMA_SEM), n):
                        eng.wait_ge(dsem[e][i % NDMA_SEM], 16 * (i // NDMA_SEM + 1))
            return body

        for e, reg in (("pe", block.tensor), ("act", block.scalar), ("dve", block.vector),
                       ("pool", block.gpsimd), ("sp", block.sync)):
            if self.ops[e]:
                reg(run(e))


class Pool:
    def __init__(self, tiles, name, off=0):
        self.tiles = tiles
        self.name = name
        self.i = 0
        self.off = off

    def get(self):
        j = self.i % len(self.tiles)
        self.i += 1
        return self.tiles[j], (self.name, j + self.off)


def build(S):
    T = NMETA + S
    NCH = S // 512
    NSLOT = NCH // 2
    NKT = 1 + S // 128
    nc = bass.Bass("TRN2", target_bir_lowering=False)

    def din(name, shape, dt=F32):
        return nc.dram_tensor(name, shape, dt, kind="ExternalInput").ap()

    xall = din("xall", [T, D])
    xown = din("xown", [NSLOT, 544, D])
    ropek = din("ropek", [T, 512])
    ropeq = din("ropeq", [NSLOT, 512, 512])
    qrel_d = din("qrel", [128, NSLOT * 8])
    w_in = din("w_in", [D, DIN])
    w_out = din("w_out", [D, D])
    w_up = din("w_up", [D, DFF])
    w_down = din("w_down", [DFF, D])
    attn_g = din("attn_g", [D])
    mlp_g = din("mlp_g", [D])
    final_g = din("final_g", [D])
    conv_w = din("conv_w", [31, 512])
    conv_b = din("conv_b", [512])
    cn_g = din("cn_g", [512])
    cn_b = din("cn_b", [512])
    iota_d = din("iota", [128, 512])
    pow2_d = din("pow2", [128, NBIS])
    e32_d = din("e32", [128, 32])
    g4_d = din("g4", [128, 4])
    dm_d = din("dm", [128, 4 * 128])
    out_d = nc.dram_tensor("out", [NSLOT * 512, D], F32, kind="ExternalOutput").ap()

    k_scr = nc.dram_tensor("k_scr", [4, 128, T], BF16).ap()
    v_scr = nc.dram_tensor("v_scr", [T, 4, 129], BF16).ap()

    import os
    KSTOP = float(os.environ.get('KSTOP', '99'))
    S_ = Sched()
    stage = [0]

    def op(*a, **k):
        if stage[0] <= KSTOP:
            return S_.op(*a, **k)
        return None
    uid = [0]

    def sb(shape, dt, name=None):
        uid[0] += 1
        return nc.alloc_sbuf_tensor(f"{name or 't'}{uid[0]}", shape, dt)

    def mkpool(n, shape, dt, name):
        return Pool([sb(shape, dt, name) for _ in range(n)], name)

    identf = sb([128, 128], F32, "identf")
    ident = sb([128, 128], BF16, "ident")
    op("pool", lambda e: e.memset(identf[:], 0.0), writes=["identf"])
    op("pool", lambda e: e.affine_select(out=identf[:], in_=identf[:], pattern=[[-1, 128]],
                                         compare_op=ALU.not_equal, fill=1.0, base=0, channel_multiplier=1),
       reads=["identf"], writes=["identf"])
    op("dve", lambda e: e.tensor_copy(out=ident[:], in_=identf[:]), reads=["identf"], writes=["ident"])

    def load_const(name, shape, src, dt=F32):
        t = sb(shape, dt, name)
        op("sp", lambda e: e.dma_start(out=t[:], in_=src, allow_slow_non_contiguous=True), writes=[name], dma=True)
        return t

    iota = load_const("iota", [128, 512], iota_d)
    pow2 = load_const("pow2", [128, NBIS], pow2_d)
    e32 = load_const("e32", [128, 32], e32_d)
    g4 = load_const("g4", [128, 4], g4_d)
    qrel = load_const("qrelc", [128, NSLOT * 8], qrel_d)
    gA = load_const("gA", [128, 8], attn_g.rearrange("(c p) -> p c", p=128))
    gM = load_const("gM", [128, 8], mlp_g.rearrange("(c p) -> p c", p=128))
    def bcast_row(name, src, n):
        t = sb([128, n], F32, name)
        op("sp", lambda e: e.dma_start(out=t[:], in_=src.partition_broadcast(128)), writes=[name], dma=True)
        return t
    gF = bcast_row("gF", final_g, D)
    cb_bc = bcast_row("cb_bc", conv_b, 512)
    cg_bc = bcast_row("cg_bc", cn_g, 512)
    cnb_bc = bcast_row("cnb_bc", cn_b, 512)
    cwT = sb([128, 4, 31], F32, "cwT")
    for cc_ in range(4):
        op("sp", lambda e, cc_=cc_: e.dma_start(out=cwT[:, cc_, :], in_=conv_w[:, cc_ * 128:(cc_ + 1) * 128].rearrange("j p -> p j"),
                                                 allow_slow_non_contiguous=True), writes=["cwT"], dma=True)
    dmb = sb([128, 512], BF16, "dmb")
    op("pool", lambda e: e.dma_start(out=dmb[:], in_=dm_d), writes=["dmb"], dma=True)
    wik_t = sb([128, 8, 64], BF16, "wik")
    op("pool", lambda e: e.dma_start(out=wik_t[:, :, :], in_=w_in[:, 2048:2112].rearrange("(c p) n -> p c n", p=128)), writes=["wik"], dma=True)
    wiw_t = sb([128, 8, 8], BF16, "wiw")
    op("pool", lambda e: e.dma_start(out=wiw_t[:, :, :], in_=w_in[:, 2112:2120].rearrange("(c p) n -> p c n", p=128), allow_slow_non_contiguous=True),
       writes=["wiw"], dma=True)

    win_b = nc.dram_tensor("win_b", [D, DIN], BF16).ap()
    wout_b = nc.dram_tensor("wout_b", [D, D], BF16).ap()
    wup_b = nc.dram_tensor("wup_b", [D, DFF], BF16).ap()
    wdown_b = nc.dram_tensor("wdown_b", [DFF, D], BF16).ap()

    ikT = sb([128, T], BF16, "ikT")
    scores = sb([128, T], F32, "scores")
    wbuf = mkpool(4, [128, 8, 512], BF16, "wbuf")
    xt_pool = mkpool(3, [128, D], F32, "xt")
    xn_pool = mkpool(1, [128, D], BF16, "xn")
    xnT_pool = mkpool(2, [128, 8, 128], BF16, "xnT")
    rope_pool = mkpool(2, [128, 256], F32, "rope")
    st_pool = mkpool(4, [128, 8], F32, "stat")
    tm_pool = mkpool(3, [128, 512], BF16, "tm")
    rt_pool = mkpool(2, [128, 256], F32, "rt")
    jk_pool = mkpool(2, [128, 512], F32, "jk")
    bigjunk = sb([128, NMETA + 512 * ((T // 512) // 2 + 1)], mybir.dt.uint8, "bigjunk")
    vb_pool = mkpool(2, [128, 4, 129], BF16, "vb")
    for vbt_ in vb_pool.tiles:
        pass
    pg = Pool([nc.alloc_psum_tensor(f"pg{i}", [128, 512], F32) for i in range(4)], "pg")
    psc = [nc.alloc_psum_tensor(f"psc{i}", [128, 512], F32) for i in range(2)]
    pacc = [nc.alloc_psum_tensor(f"pacc{i}", [128, 512], F32) for i in range(2)]

    def pg_bf16(t):
        return t.bitcast(BF16) if hasattr(t, "bitcast") else None

    def rmsnorm_T(x_t, x_k, rows, gcol, xnT, xnT_k):
        st, st_k = st_pool.get()
        jk, jk_k = jk_pool.get()
        xn, xn_k = xn_pool.get()
        op("act", lambda e: e.activation(out=jk[0:rows, :], in_=x_t[0:rows, 0:512], func=AF.Square,
                                         accum_out=st[0:rows, 0:1]), reads=[x_k], writes=[jk_k, (st_k, 0)])
        op("act", lambda e: e.activation(out=jk[0:rows, :], in_=x_t[0:rows, 512:1024], func=AF.Square,
                                         accum_out=st[0:rows, 1:2]), reads=[x_k], writes=[jk_k, (st_k, 1)])
        op("dve", lambda e: e.tensor_tensor(out=st[0:rows, 2:3], in0=st[0:rows, 0:1], in1=st[0:rows, 1:2], op=ALU.add),
           reads=[(st_k, 0), (st_k, 1)], writes=[(st_k, 2)])
        op("dve", lambda e: e.tensor_scalar(out=st[0:rows, 3:4], in0=st[0:rows, 2:3], scalar1=1.0 / D, scalar2=EPS,
                                            op0=ALU.mult, op1=ALU.add), reads=[(st_k, 2)], writes=[(st_k, 3)])
        op("act", lambda e: e.activation(out=st[0:rows, 4:5], in_=st[0:rows, 3:4], func=AF.Sqrt),
           reads=[(st_k, 3)], writes=[(st_k, 4)])
        op("dve", lambda e: e.reciprocal(out=st[0:rows, 5:6], in_=st[0:rows, 4:5]), reads=[(st_k, 4)], writes=[(st_k, 5)])
        op("dve", lambda e: e.tensor_scalar(out=xn[0:rows, :], in0=x_t[0:rows, :], scalar1=st[0:rows, 5:6], scalar2=None,
                                            op0=ALU.mult), reads=[x_k, (st_k, 5)], writes=[xn_k])
        pt, pt_k = pg.get()
        ptb = pt[:].bitcast(BF16)
        for dc in range(8):
            op("pe", lambda e, dc=dc: e.transpose(out=ptb[:, dc * 128:dc * 128 + rows], in_=xn[0:rows, dc * 128:(dc + 1) * 128],
                                                  identity=ident[0:rows, 0:rows]),
               reads=[xn_k, "ident"], writes=[pt_k])
        op("dve", lambda e: e.tensor_tensor(out=xnT[:, :, 0:rows],
                                            in0=ptb.rearrange("p (c t) -> p c t", c=8)[:, :, 0:rows],
                                            in1=gcol[:, :].unsqueeze(2).to_broadcast([128, 8, rows]), op=ALU.mult),
           reads=[pt_k], writes=[xnT_k])
        return (st, st_k)

    def wviews(kind, i, w):
        if kind == "down":
            v = w[:, :, :].rearrange("p a b -> p (a b)").rearrange("p (f n) -> p f n", f=4)
            r = lambda a: a[i * 512:(i + 1) * 512, :].rearrange("(f p) n -> p f n", p=128)
            return v, r(w_down), r(wdown_b)
        src, dst = {"in": (w_in, win_b), "out": (w_out, wout_b), "up": (w_up, wup_b)}[kind]
        r = lambda a: a[:, i:i + 512].rearrange("(c p) n -> p c n", p=128) if kind == "in" else a[:, i * 512:(i + 1) * 512].rearrange("(c p) n -> p c n", p=128)
        return w[:, :, :], r(src), r(dst)

    def load_w(kind, i, cast=False):
        w, w_k = wbuf.get()
        v, src, scr = wviews(kind, i, w)
        if cast:
            op("pool", lambda e: e.dma_start(out=v, in_=src), writes=[w_k], dma=True)
        else:
            op("sp", lambda e: e.dma_start(out=v, in_=scr), reads=[("wcv", kind, i)], writes=[w_k], dma=True)
        return (v if kind == "down" else w), w_k

    def proj_tm(xnT, xnT_k, rows, w, w_k, ncols, ps, ps_k, pcol=0):
        for dc in range(8):
            op("pe", lambda e, dc=dc: e.matmul(ps[0:rows, pcol:pcol + ncols], lhsT=xnT[:, dc, 0:rows], rhs=w[:, dc, 0:ncols],
                                               start=(dc == 0), stop=(dc == 7)),
               reads=[xnT_k, w_k], writes=[ps_k])


    def rope_tm(ps, ps_k, rows, nh, hd, half, rp, rp_k, roff, dst, dst_k, dcol=0):
        r2 = 2 * half
        n = nh * hd
        xf, xf_k = jk_pool.get()
        op("act", lambda e: e.copy(out=xf[0:rows, 0:n], in_=ps[0:rows, 0:n]), reads=[ps_k], writes=[xf_k])
        op("act", lambda e: e.copy(out=dst[0:rows, dcol:dcol + n], in_=ps[0:rows, 0:n]), reads=[ps_k], writes=[dst_k])
        rt, rt_k = rt_pool.get()
        pv = xf[0:rows, 0:n].rearrange("p (h d) -> p h d", h=nh)
        A = rt[0:rows, 0:nh * r2].rearrange("p (h d) -> p h d", h=nh)
        Bm = rt[0:rows, 128:128 + nh * r2].rearrange("p (h d) -> p h d", h=nh)
        cc_ = rp[0:rows, roff:roff + nh * r2].rearrange("p (h d) -> p h d", h=nh)
        ss_ = rp[0:rows, roff + 128:roff + 128 + nh * r2].rearrange("p (h d) -> p h d", h=nh)
        op("dve", lambda e: e.tensor_tensor(out=A, in0=pv[:, :, 0:r2], in1=cc_, op=ALU.mult), reads=[xf_k, rp_k], writes=[(rt_k, 0)])
        op("dve", lambda e: e.tensor_tensor(out=Bm, in0=pv[:, :, 0:r2], in1=ss_, op=ALU.mult), reads=[xf_k, rp_k], writes=[(rt_k, 1)])
        dv = dst[0:rows, dcol:dcol + n].rearrange("p (h d) -> p h d", h=nh)
        op("dve", lambda e: e.tensor_tensor(out=dv[:, :, 0:half], in0=A[:, :, 0:half], in1=Bm[:, :, half:r2], op=ALU.subtract),
           reads=[(rt_k, 0), (rt_k, 1), dst_k], writes=[dst_k])
        op("dve", lambda e: e.tensor_tensor(out=dv[:, :, half:r2], in0=A[:, :, half:r2], in1=Bm[:, :, 0:half], op=ALU.add),
           reads=[(rt_k, 0), (rt_k, 1), dst_k], writes=[dst_k])

    stage[0] = 1
    for j_, vbt_ in enumerate(vb_pool.tiles):
        op("pool", lambda e, vbt_=vbt_: e.memset(vbt_[:, :, 128:129], 1.0), writes=[("vbones", ("vb", j_))])
    wk, wk_k = load_w("in", 512, cast=True)
    wv, wv_k = load_w("in", 1024, cast=True)
    cvt_pool = Pool(wbuf.tiles[2:4], "wbuf", off=2)
    for kind, idxs in (("in", (0, 1536, 2120, 2632)), ("out", (0, 1)), ("up", tuple(range(8))), ("down", tuple(range(8)))):
        for i in idxs:
            w, w_k = cvt_pool.get()
            v, src, scr = wviews(kind, i, w)
            op("pool", lambda e, v=v, src=src: e.dma_start(out=v, in_=src), writes=[w_k], dma=True)
            op("pool", lambda e, v=v, scr=scr: e.dma_start(out=scr, in_=v), reads=[w_k], writes=[("wcv", kind, i)], dma=True)
    wbuf.i = 2
    wik, wik_k = wik_t, "wik"
    stage[0] = 1
    for kt in range(min(NKT, int(os.environ.get('KT_MAX', '999')))):
        rows = NMETA if kt == 0 else 128
        p0 = 0 if kt == 0 else NMETA + 128 * (kt - 1)
        xt, xt_k = xt_pool.get()
        op("sp", lambda e, xt=xt, p0=p0, rows=rows: e.dma_start(out=xt[0:rows, :], in_=xall[p0:p0 + rows, :]), writes=[xt_k], dma=True)
        rp, rp_k = rope_pool.get()
        op("sp", lambda e, rp=rp, p0=p0, rows=rows: e.dma_start(out=rp[0:rows, :], in_=ropek[p0:p0 + rows, 0:256]), writes=[rp_k], dma=True)
        rpi, rpi_k = rope_pool.get()
        op("sp", lambda e, rpi=rpi, p0=p0, rows=rows: e.dma_start(out=rpi[0:rows, :], in_=ropek[p0:p0 + rows, 256:512]), writes=[rpi_k], dma=True)
        xnT, xnT_k = xnT_pool.get()
        stage[0] = 1.1
        rmsnorm_T(xt, xt_k, rows, gA, xnT, xnT_k)
        stage[0] = 1.2
        ps, ps_k = pg.get()
        proj_tm(xnT, xnT_k, rows, wk, wk_k, 512, ps, ps_k)
        stage[0] = 1.25
        kb, kb_k = tm_pool.get()
        rope_tm(ps, ps_k, rows, 4, 128, 16, rp, rp_k, 0, kb, kb_k)
        stage[0] = 1.3
        pt, pt_k = pg.get()
        ptb = pt[:].bitcast(BF16)
        for h in range(4):
            op("pe", lambda e, h=h, ptb=ptb, kb=kb, rows=rows: e.transpose(out=ptb[:, h * 128:h * 128 + rows], in_=kb[0:rows, h * 128:(h + 1) * 128],
                                                                         identity=ident[0:rows, 0:rows]), reads=[kb_k, "ident"], writes=[pt_k])
        kTt, kTt_k = tm_pool.get()
        op("act", lambda e, kTt=kTt, ptb=ptb: e.copy(out=kTt[:, :], in_=ptb[:, 0:512]), reads=[pt_k], writes=[kTt_k])
        op("sp", lambda e, kTt=kTt, p0=p0, rows=rows: e.dma_start(
            out=k_scr[:, :, p0:p0 + rows].rearrange("h p t -> p h t"),
            in_=kTt[:, :].rearrange("p (h t) -> p h t", h=4)[:, :, 0:rows]),
           reads=[kTt_k], writes=[("k_scr", kt)], dma=True)
        stage[0] = 1.4
        ps, ps_k = pg.get()
        proj_tm(xnT, xnT_k, rows, wv, wv_k, 512, ps, ps_k)
        vb, vb_k = vb_pool.get()
        op("act", lambda e, vb=vb, ps=ps, rows=rows: e.copy(out=vb[0:rows, :, 0:128], in_=ps[0:rows, :].rearrange("p (h d) -> p h d", h=4)),
           reads=[ps_k, ("vbones", vb_k)], writes=[vb_k])
        op("sp", lambda e, vb=vb, p0=p0, rows=rows: e.dma_start(out=v_scr[p0:p0 + rows, :, :], in_=vb[0:rows, :, :]),
           reads=[vb_k, ("vbones", vb_k)], writes=[("v_scr", kt)], dma=True)
        stage[0] = 1.5
        ps, ps_k = pg.get()
        proj_tm(xnT, xnT_k, rows, wik, wik_k, 64, ps, ps_k)
        ib, ib_k = tm_pool.get()
        rope_tm(ps, ps_k, rows, 1, 64, 8, rpi, rpi_k, 0, ib, ib_k)
        op("dve", lambda e, ib=ib, rows=rows: e.tensor_copy(out=ib[0:rows, 64:128], in_=ib[0:rows, 0:64]), reads=[ib_k], writes=[ib_k])
        pt, pt_k = pg.get()
        ptb = pt[:].bitcast(BF16)
        op("pe", lambda e, ptb=ptb, ib=ib, rows=rows: e.transpose(out=ptb[:, 0:rows], in_=ib[0:rows, 0:128], identity=ident[0:rows, 0:rows]),
           reads=[ib_k, "ident"], writes=[pt_k])
        op("act", lambda e, ptb=ptb, p0=p0, rows=rows: e.copy(out=ikT[:, p0:p0 + rows], in_=ptb[:, 0:rows]), reads=[pt_k], writes=[("ikT", kt)])

    stage[0] = 2
    qT = sb([128, 4, 512], BF16, "qT")
    iqT = sb([128, 4, 512], BF16, "iqT")
    iwt = sb([128, 4, 8], F32, "iwt")
    uT = sb([128, 4, 544], BF16, "uT")
    mixed = sb([128, 4, D], BF16, "mixed")
    ysb = sb([128, 4, 512], F32, "ysb")
    Dcc = sb([128, 31, 128], BF16, "Dcc")
    negm_pool = mkpool(2, [128, 512], F32, "negm")
    sel = sb([128, 8, 128], BF16, "sel")
    lw = sb([128, 2, 128], BF16, "lw")
    w2 = sb([128, 8], F32, "w2")
    g4b = sb([128, 4], BF16, "g4b")
    op("dve", lambda e: e.tensor_copy(out=g4b[:], in_=g4[:]), reads=["g4"], writes=["g4b"])
    bis = sb([128, 8 + NBIS], F32, "bis")
    top8 = sb([128, 8], F32, "top8")
    cst = sb([128, 4, 6], F32, "cst")
    cmv = sb([128, 4, 2], F32, "cmv")
    crs = sb([128, 4], F32, "crs")
    R_pool = mkpool(3, [128, 512], BF16, "R")
    PT_pool = mkpool(2, [128, 512], BF16, "PT")
    mb_pool = mkpool(2, [128, 512], BF16, "mb")
    GK = 4
    kv_pool = [(sb([128, 4, GK * 128], BF16, "kbuf"), sb([128, GK, 4, 129], BF16, "vbuf")) for _ in range(2)]
    kmeta = sb([128, 4, NMETA], BF16, "kmeta")
    vmeta = sb([128, 4, 129], BF16, "vmeta")
    op("sp", lambda e: e.dma_start(out=kmeta[:, :, :], in_=k_scr[:, :, 0:NMETA].rearrange("h p t -> p h t")),
       reads=[("k_scr", 0)], writes=["kmeta"], dma=True)
    op("sp", lambda e: e.dma_start(out=vmeta[0:NMETA, :, :], in_=v_scr[0:NMETA, :, :]),
       reads=[("v_scr", 0)], writes=["vmeta"], dma=True)
    mT = sb([128, 8, 128], BF16, "mT")
    hnT = sb([128, 8, 256], BF16, "hnT")
    uf_pool = mkpool(2, [128, 256], BF16, "uf")
    rden = sb([128, 4], F32, "rden")

    SC = 0.125 * (8 ** -0.5)
    kv_ctr = [0]

    for sl in range(NSLOT):
        E = NMETA + 512 * (2 * sl + 2)
        W0 = E - 1024
        NCHK = 2 * sl + 2
        wq, wq_k = load_w("in", 0)
        wiq, wiq_k = load_w("in", 1536)
        wga, wga_k = load_w("in", 2120)
        wgg, wgg_k = load_w("in", 2632)
        plan = []
        for pair_ in range(2):
            plan += [("out", 0), ("out", 1)]
            for fg_ in range(8):
                plan += [("up", fg_), ("down", fg_)]
        wfifo = []

        def issue_next():
            if plan:
                kind_, i_ = plan.pop(0)
                wfifo.append(load_w(kind_, i_))
        for ti in range(5):
            rows = 32 if ti == 0 else 128
            r0 = 0 if ti == 0 else 32 + 128 * (ti - 1)
            xt, xt_k = xt_pool.get()
            op("sp", lambda e, xt=xt, r0=r0, rows=rows, sl=sl: e.dma_start(out=xt[0:rows, :], in_=xown[sl, r0:r0 + rows, :]),
               writes=[xt_k], dma=True)
            xnT, xnT_k = xnT_pool.get()
            rmsnorm_T(xt, xt_k, rows, gA, xnT, xnT_k)
            if ti > 0:
                tb = ti - 1
                rp, rp_k = rope_pool.get()
                op("sp", lambda e, rp=rp, tb=tb, sl=sl: e.dma_start(out=rp[:, :], in_=ropeq[sl, 128 * tb:128 * (tb + 1), 0:256]), writes=[rp_k], dma=True)
                ps, ps_k = pg.get()
                proj_tm(xnT, xnT_k, 128, wq, wq_k, 512, ps, ps_k)
                qb, qb_k = tm_pool.get()
                rope_tm(ps, ps_k, 128, 4, 128, 16, rp, rp_k, 0, qb, qb_k)
                pt, pt_k = pg.get()
                ptb = pt[:].bitcast(BF16)
                for h in range(4):
                    op("pe", lambda e, h=h, ptb=ptb, qb=qb: e.transpose(out=ptb[:, h * 128:(h + 1) * 128], in_=qb[:, h * 128:(h + 1) * 128], identity=ident[:, :]),
                       reads=[qb_k, "ident"], writes=[pt_k])
                op("act", lambda e, ptb=ptb, tb=tb: e.copy(out=qT[:, :, tb * 128:(tb + 1) * 128], in_=ptb[:, 0:512].rearrange("p (h t) -> p h t", h=4)),
                   reads=[pt_k], writes=[("qT", tb)])
                rpi, rpi_k = rope_pool.get()
                op("sp", lambda e, rpi=rpi, tb=tb, sl=sl: e.dma_start(out=rpi[:, :], in_=ropeq[sl, 128 * tb:128 * (tb + 1), 256:512]), writes=[rpi_k], dma=True)
                ps, ps_k = pg.get()
                proj_tm(xnT, xnT_k, 128, wiq, wiq_k, 512, ps, ps_k)
                ib, ib_k = tm_pool.get()
                rope_tm(ps, ps_k, 128, 8, 64, 8, rpi, rpi_k, 0, ib, ib_k)
                pt, pt_k = pg.get()
                ptb = pt[:].bitcast(BF16)
                for hp in range(4):
                    op("pe", lambda e, hp=hp, ptb=ptb, ib=ib: e.transpose(out=ptb[:, hp * 128:(hp + 1) * 128], in_=ib[:, hp * 128:(hp + 1) * 128], identity=ident[:, :]),
                       reads=[ib_k, "ident"], writes=[pt_k])
                op("act", lambda e, ptb=ptb, tb=tb: e.copy(out=iqT[:, tb, :].rearrange("p (g h i) -> p h g i", g=4, h=4, i=32),
                                                           in_=ptb[:, 0:512].rearrange("p (h g i) -> p h g i", h=4, g=4, i=32)),
                   reads=[pt_k], writes=[("iqT", tb)])
                ps, ps_k = pg.get()
                proj_tm(xnT, xnT_k, 128, wiw_t, "wiw", 8, ps, ps_k)
                op("dve", lambda e, ps=ps, tb=tb: e.tensor_scalar(out=iwt[:, tb, :], in0=ps[:, 0:8], scalar1=SC, scalar2=None, op0=ALU.mult),
                   reads=[ps_k], writes=[("iwt", tb)])
            psa, psa_k = pg.get()
            proj_tm(xnT, xnT_k, rows, wga, wga_k, 512, psa, psa_k)
            psg, psg_k = pg.get()
            proj_tm(xnT, xnT_k, rows, wgg, wgg_k, 512, psg, psg_k)
            jk, jk_k = jk_pool.get()
            op("act", lambda e, jk=jk, psg=psg, rows=rows: e.activation(out=jk[0:rows, :], in_=psg[0:rows, :], func=AF.Sigmoid),
               reads=[psg_k], writes=[jk_k])
            ub, ub_k = tm_pool.get()
            op("dve", lambda e, ub=ub, psa=psa, jk=jk, rows=rows: e.tensor_tensor(out=ub[0:rows, :], in0=psa[0:rows, :], in1=jk[0:rows, :], op=ALU.mult),
               reads=[psa_k, jk_k], writes=[ub_k])
            pt, pt_k = pg.get()
            ptb = pt[:].bitcast(BF16)
            for cc in range(4):
                op("pe", lambda e, cc=cc, ptb=ptb, ub=ub, rows=rows: e.transpose(out=ptb[:, cc * 128:cc * 128 + rows], in_=ub[0:rows, cc * 128:(cc + 1) * 128],
                                                                              identity=ident[0:rows, 0:rows]),
                   reads=[ub_k, "ident"], writes=[pt_k])
            op("act", lambda e, ptb=ptb, r0=r0, rows=rows: e.copy(out=uT[:, :, r0:r0 + rows], in_=ptb[:, 0:512].rearrange("p (c t) -> p c t", c=4)[:, :, 0:rows]),
               reads=[pt_k], writes=[("uT", ti)])
        stage[0] = 3
        UTK = [("uT", ti) for ti in range(5)]
        for cc in range(4):
            op("dve", lambda e, cc=cc: e.tensor_tensor(out=Dcc[:, :, :], in0=identf[:, :].unsqueeze(1).to_broadcast([128, 31, 128]),
                                                       in1=cwT[:, cc, :].unsqueeze(2).to_broadcast([128, 31, 128]), op=ALU.mult),
               reads=["identf", "cwT"], writes=["Dcc"])
            for tb in range(4):
                ps, ps_k = pg.get()
                for j in range(31):
                    c0 = 2 + tb * 128 + j
                    op("pe", lambda e, ps=ps, cc=cc, c0=c0, j=j: e.matmul(ps[:, 0:128], lhsT=uT[:, cc, c0:c0 + 128], rhs=Dcc[:, j, :], start=(j == 0), stop=(j == 30)),
                       reads=UTK + ["Dcc"], writes=[ps_k])
                op("act", lambda e, ps=ps, tb=tb, cc=cc: e.copy(out=ysb[:, tb, cc * 128:(cc + 1) * 128], in_=ps[:, 0:128]),
                   reads=[ps_k], writes=[("ysb", tb)])
        for tb in range(4):
            yk = ("ysb", tb)
            y = ysb[:, tb, :]
            op("dve", lambda e, y=y: e.tensor_tensor(out=y, in0=y, in1=cb_bc[:, :], op=ALU.add), reads=[yk, "cb_bc"], writes=[yk])
            for g in range(4):
                op("dve", lambda e, y=y, g=g: e.bn_stats(out=cst[:, g, :], in_=y[:, g * 128:(g + 1) * 128]), reads=[yk], writes=[("cst", g)])
                op("dve", lambda e, g=g: e.bn_aggr(out=cmv[:, g, :], in_=cst[:, g, :]), reads=[("cst", g)], writes=[("cmv", g)])
            CM = [("cmv", g) for g in range(4)]
            op("dve", lambda e: e.tensor_scalar(out=crs[:, :], in0=cmv[:, :, 1], scalar1=EPS, scalar2=None, op0=ALU.add), reads=CM, writes=["crs"])
            op("act", lambda e: e.activation(out=crs[:, :], in_=crs[:, :], func=AF.Sqrt), reads=["crs"], writes=["crs"])
            op("dve", lambda e: e.reciprocal(out=crs[:, :], in_=crs[:, :]), reads=["crs"], writes=["crs"])
            for g in range(4):
                op("dve", lambda e, y=y, g=g: e.tensor_scalar(out=y[:, g * 128:(g + 1) * 128], in0=y[:, g * 128:(g + 1) * 128],
                                                            scalar1=cmv[:, g, 0:1], scalar2=crs[:, g:g + 1], op0=ALU.subtract, op1=ALU.mult),
                   reads=[yk, "crs"] + CM, writes=[yk])
            op("dve", lambda e, y=y: e.tensor_tensor(out=y, in0=y, in1=cg_bc[:, :], op=ALU.mult), reads=[yk, "cg_bc"], writes=[yk])
            op("dve", lambda e, y=y: e.tensor_tensor(out=y, in0=y, in1=cnb_bc[:, :], op=ALU.add), reads=[yk, "cnb_bc"], writes=[yk])
            op("act", lambda e, y=y, tb=tb: e.activation(out=mixed[:, tb, 512:1024], in_=y, func=AF.Silu), reads=[yk], writes=[("mixC", tb)])

        stage[0] = 4
        for tb in range(4):
            if tb == 3:
                for _ in range(4):
                    issue_next()
            for h in range(8):
                par, hp = h % 2, h // 2
                op("pool", lambda e, h=h, par=par, hp=hp, tb=tb: e.tensor_scalar(out=lw[:, par, hp * 32:(hp + 1) * 32], in0=e32[:, :], scalar1=iwt[:, tb, h:h + 1],
                                                                                 scalar2=None, op0=ALU.mult),
                   reads=["e32", ("iwt", tb)], writes=[("lw", h)])
            LWK = [("lw", h) for h in range(8)]
            ps, ps_k = pg.get()
            for par in range(2):
                op("pe", lambda e, ps=ps, par=par: e.matmul(ps[:, par * 4:(par + 1) * 4], lhsT=lw[:, par, :], rhs=g4b[:, :], start=True, stop=True),
                   reads=LWK + ["g4b"], writes=[ps_k])
            op("dve", lambda e, ps=ps: e.tensor_copy(out=w2[:, :], in_=ps[:, 0:8]), reads=[ps_k], writes=["w2"])
            for idx in range(8):
                g = idx % 4
                op("pool", lambda e, idx=idx, g=g: e.tensor_scalar(out=sel[:, idx, :], in0=dmb[:, g * 128:(g + 1) * 128], scalar1=w2[:, idx:idx + 1],
                                                                   scalar2=None, op0=ALU.mult),
                   reads=["dmb", "w2"], writes=[("sel", idx)])
            chunks = [(0, NMETA)] + [(NMETA + 512 * m, 512) for m in range(NCHK)]
            SCK = []
            wcnt = 0
            for ci, (c0, cn) in enumerate(chunks):
                ikk = [("ikT", 0)] if ci == 0 else [("ikT", 1 + 4 * (ci - 1) + q) for q in range(4)]
                sc = psc[ci % 2]
                sc_k = ("psc", ci % 2)
                pend = None
                items = [(g, par) for par in range(2) for g in range(4)]
                for k, (g, par) in enumerate(items):
                    ps, ps_k = pg.get()
                    t0 = tb * 128 + g * 32
                    op("pe", lambda e, ps=ps, par=par, tb=tb, g=g, c0=c0, cn=cn: e.matmul(ps[:, 0:cn], lhsT=iqT[par * 64:(par + 1) * 64, tb, g * 128:(g + 1) * 128],
                                                                                    rhs=ikT[par * 64:(par + 1) * 64, c0:c0 + cn], start=True, stop=True),
                       reads=[("iqT", tb)] + ikk, writes=[ps_k])
                    R, R_k = R_pool.get()
                    if k % 2 == 0:
                        op("act", lambda e, R=R, ps=ps, cn=cn: e.activation(out=R[:, 0:cn], in_=ps[:, 0:cn], func=AF.Relu), reads=[ps_k], writes=[R_k])
                    else:
                        op("dve", lambda e, R=R, ps=ps, cn=cn: e.tensor_scalar(out=R[:, 0:cn], in0=ps[:, 0:cn], scalar1=0.0, scalar2=None, op0=ALU.max),
                           reads=[ps_k], writes=[R_k])
                    if pend is not None:
                        pk, pR, pR_k, pidx = pend
                        op("pe", lambda e, sc=sc, pidx=pidx, pR=pR, cn=cn, pk=pk: e.matmul(sc[:, 0:cn], lhsT=sel[:, pidx, :], rhs=pR[:, 0:cn], start=(pk == 0), stop=False),
                           reads=[("sel", pidx), pR_k], writes=[sc_k])
                    pend = (k, R, R_k, par * 4 + g)
                pk, pR, pR_k, pidx = pend
                op("pe", lambda e, sc=sc, pidx=pidx, pR=pR, cn=cn: e.matmul(sc[:, 0:cn], lhsT=sel[:, pidx, :], rhs=pR[:, 0:cn], start=False, stop=True),
                   reads=[("sel", pidx), pR_k], writes=[sc_k])
                sk = ("sc", ci)
                SCK.append(sk)
                if c0 >= W0 and ci > 0:
                    col = (sl * 4 + tb) * 2 + wcnt
                    negm, negm_k = negm_pool.get()
                    op("dve", lambda e, col=col, negm=negm: e.tensor_scalar(out=negm[:, :], in0=iota[:, :], scalar1=qrel[:, col:col + 1], scalar2=-1e30,
                                                                          op0=ALU.is_gt, op1=ALU.mult), reads=["iota", "qrelc"], writes=[negm_k])
                    op("dve", lambda e, sc=sc, c0=c0, negm=negm: e.tensor_tensor(out=scores[:, c0:c0 + 512], in0=sc[:, :], in1=negm[:, :], op=ALU.add),
                       reads=[sc_k, negm_k], writes=[sk])
                    jk, jk_k = jk_pool.get()
                    op("dve", lambda e, sc=sc, jk=jk, negm=negm: e.tensor_tensor(out=jk[:, :], in0=sc[:, :], in1=negm[:, :], op=ALU.subtract),
                       reads=[sc_k, negm_k], writes=[jk_k])
                    op("dve", lambda e, jk=jk, wcnt=wcnt: e.tensor_reduce(out=bis[:, 5 + wcnt:6 + wcnt], in_=jk[:, :], axis=AX.X, op=ALU.min),
                       reads=[jk_k], writes=[("bis", 5 + wcnt)])
                    wcnt += 1
                else:
                    op("act", lambda e, sc=sc, c0=c0, cn=cn: e.copy(out=scores[:, c0:c0 + cn], in_=sc[:, 0:cn]), reads=[sc_k], writes=[sk])
            assert wcnt == 2
            stage[0] = 5
            nD = max(1, int(round(0.45 * NCHK)))
            ED = NMETA + 512 * nD
            EA = E - ED
            SCK_D = SCK[0:1 + nD]
            SCK_A = SCK[1 + nD:]
            op("dve", lambda e, E=E: e.max(out=top8[:, :], in_=scores[:, 0:E]), reads=SCK, writes=["top8"])
            op("dve", lambda e, W0=W0: e.tensor_reduce(out=bis[:, 7:8], in_=scores[:, 0:W0], axis=AX.X, op=ALU.min), reads=SCK, writes=[("bis", 7)])
            op("dve", lambda e: e.tensor_reduce(out=bis[:, 0:1], in_=bis[:, 5:8], axis=AX.X, op=ALU.min),
               reads=[("bis", 5), ("bis", 6), ("bis", 7)], writes=[("bis", 0)])
            op("dve", lambda e: e.tensor_tensor(out=bis[:, 1:2], in0=top8[:, 0:1], in1=bis[:, 0:1], op=ALU.subtract),
               reads=["top8", ("bis", 0)], writes=[("bis", 1)])
            op("dve", lambda e: e.tensor_scalar(out=bis[:, 8:8 + NBIS], in0=pow2[:, :], scalar1=bis[:, 1:2], scalar2=None, op0=ALU.mult),
               reads=["pow2", ("bis", 1)], writes=["bisW"])
            op("dve", lambda e: e.tensor_tensor(out=bis[:, 2:3], in0=bis[:, 8:9], in1=bis[:, 0:1], op=ALU.add),
               reads=["bisW", ("bis", 0)], writes=[("bis", 2)])
            for k in range(NBIS):
                op("dve", lambda e, ED=ED: e.tensor_scalar(out=bigjunk[:, 0:ED], in0=scores[:, 0:ED], scalar1=bis[:, 2:3], scalar2=None,
                                                          op0=ALU.is_ge, op1=ALU.add, accum_out=bis[:, 3:4]),
                   reads=SCK_D + [("bis", 2)], writes=["bigjunkD", ("bis", 3)])
                op("act", lambda e, ED=ED, E=E, EA=EA: e.activation(out=bigjunk[:, 0:EA].bitcast(mybir.dt.int8), in_=scores[:, ED:E], func=AF.Sign,
                                                            bias=bis[:, 2:3], scale=-1.0, accum_out=bis[:, 5:6]),
                   reads=SCK_A + [("bis", 2)], writes=["bigjunkA", ("bis", 5)])
                op("dve", lambda e: e.scalar_tensor_tensor(out=bis[:, 6:7], in0=bis[:, 3:4], scalar=2.0, in1=bis[:, 5:6], op0=ALU.mult, op1=ALU.subtract),
                   reads=[("bis", 3), ("bis", 5)], writes=[("bis", 6)])
                op("dve", lambda e, k=k, EA=EA: e.tensor_scalar(out=bis[:, 4:5], in0=bis[:, 6:7], scalar1=float(2 * KTOP - 1 - EA), scalar2=bis[:, 8 + k:9 + k],
                                                               op0=ALU.is_ge, op1=ALU.mult), reads=[("bis", 6), "bisW"], writes=[("bis", 4)])
                if k < NBIS - 1:
                    op("dve", lambda e, k=k: e.scalar_tensor_tensor(out=bis[:, 2:3], in0=bis[:, 4:5], scalar=bis[:, 9 + k:10 + k], in1=bis[:, 2:3],
                                                                  op0=ALU.subtract, op1=ALU.add),
                       reads=[("bis", 4), "bisW", ("bis", 2)], writes=[("bis", 2)])
                else:
                    op("dve", lambda e, k=k: e.scalar_tensor_tensor(out=bis[:, 0:1], in0=bis[:, 4:5], scalar=bis[:, 8 + k:9 + k], in1=bis[:, 2:3],
                                                                  op0=ALU.subtract, op1=ALU.add),
                       reads=[("bis", 4), "bisW", ("bis", 2)], writes=[("bis", 0)])
            stage[0] = 6
            nkb_total = 1 + 4 * NCHK
            kblist = []
            for ci, (c0, cn) in enumerate(chunks):
                for kb in range(1 if ci == 0 else 4):
                    kblist.append((ci, c0, cn, kb))
            cstate = {}

            def chunk_setup(ci, c0, cn):
                mbt, mb_k = mb_pool.get()
                op("dve", lambda e, mbt=mbt, c0=c0, cn=cn: e.tensor_scalar(out=mbt[:, 0:cn], in0=scores[:, c0:c0 + cn], scalar1=bis[:, 0:1], scalar2=-30000.0,
                                                                          op0=ALU.is_lt, op1=ALU.mult), reads=[SCK[ci], ("bis", 0)], writes=[mb_k])
                if ci == 0:
                    cstate[ci] = (mbt, mb_k, kmeta, vmeta, "kmeta", "vmeta")
                    return
                kvn = kv_ctr[0] % 2
                kv_ctr[0] += 1
                kbuf, vbuf = kv_pool[kvn]
                kvk, vvk = ("kbuf", kvn), ("vbuf", kvn)
                tl = [1 + 4 * (ci - 1) + q for q in range(4)]
                op("sp", lambda e, kbuf=kbuf, c0=c0: e.dma_start(out=kbuf[:, :, :], in_=k_scr[:, :, c0:c0 + 512].rearrange("h p t -> p h t")),
                   reads=[("k_scr", t) for t in tl], writes=[kvk], dma=True)
                op("sp", lambda e, vbuf=vbuf, c0=c0: e.dma_start(out=vbuf[:, :, :, :].rearrange("p g h e -> p g (h e)"),
                                                                 in_=v_scr[c0:c0 + 512, :, :].rearrange("(g p) h e -> p g (h e)", p=128)),
                   reads=[("v_scr", t) for t in tl], writes=[vvk], dma=True)
                cstate[ci] = (mbt, mb_k, kbuf, vbuf, kvk, vvk)

            def emit_S(i):
                ci, c0, cn, kb = kblist[i]
                if kb == 0:
                    chunk_setup(ci, c0, cn)
                mbt, mb_k, kbuf, vbuf, kvk, vvk = cstate[ci]
                ks = NMETA if ci == 0 else 128
                st, st_k = pg.get()
                for h in range(4):
                    op("pe", lambda e, st=st, h=h, kbuf=kbuf, kb=kb, ks=ks, tb=tb: e.matmul(st[0:ks, h * 128:(h + 1) * 128], lhsT=kbuf[:, h, kb * 128:kb * 128 + ks],
                                                                                         rhs=qT[:, h, tb * 128:(tb + 1) * 128], start=True, stop=False),
                       reads=[kvk, ("qT", tb)], writes=[st_k])
                    op("pe", lambda e, st=st, h=h, mbt=mbt, kb=kb, ks=ks: e.matmul(st[0:ks, h * 128:(h + 1) * 128], lhsT=mbt[:, kb * 128:kb * 128 + ks],
                                                                                 rhs=ident[:, :], start=False, stop=True),
                       reads=[mb_k, "ident"], writes=[st_k])
                PT, PT_k = PT_pool.get()
                op("act", lambda e, PT=PT, st=st, ks=ks: e.activation(out=PT[0:ks, :], in_=st[0:ks, :], func=AF.Exp, scale=128 ** -0.5),
                   reads=[st_k], writes=[PT_k])
                return (PT, PT_k)

            def emit_P(i, PTs):
                ci, c0, cn, kb = kblist[i]
                mbt, mb_k, kbuf, vbuf, kvk, vvk = cstate[ci]
                ks = NMETA if ci == 0 else 128
                PT, PT_k = PTs
                for h in range(4):
                    acc = pacc[h // 2]
                    a0 = (h % 2) * 256
                    rhs = vbuf[0:ks, h, :] if ci == 0 else vbuf[:, kb, h, :]
                    op("pe", lambda e, acc=acc, a0=a0, PT=PT, h=h, ks=ks, rhs=rhs, i=i: e.matmul(acc[:, a0:a0 + 129], lhsT=PT[0:ks, h * 128:(h + 1) * 128], rhs=rhs,
                                                                                           start=(i == 0 and h % 2 == 0), stop=(i == nkb_total - 1),
                                                                                           skip_group_check=True),
                       reads=[PT_k, vvk], writes=[("pacc", h // 2)])

            prev = emit_S(0)
            for i in range(nkb_total):
                nxt = emit_S(i + 1) if i + 1 < nkb_total else None
                emit_P(i, prev)
                prev = nxt
            for h in range(4):
                acc = pacc[h // 2]
                a0 = (h % 2) * 256
                op("dve", lambda e, acc=acc, a0=a0, h=h: e.reciprocal(out=rden[:, h:h + 1], in_=acc[:, a0 + 128:a0 + 129]),
                   reads=[("pacc", h // 2)], writes=[("rden", h)])
                op("dve", lambda e, acc=acc, a0=a0, h=h, tb=tb: e.tensor_scalar(out=mixed[:, tb, h * 128:(h + 1) * 128], in0=acc[:, a0:a0 + 128], scalar1=rden[:, h:h + 1],
                                                                              scalar2=None, op0=ALU.mult),
                   reads=[("pacc", h // 2), ("rden", h)], writes=[("mixA", tb, h)])

        stage[0] = 7
        for pair in range(2):
            h1s = []
            while len(wfifo) < 2:
                issue_next()
            wos = [wfifo.pop(0), wfifo.pop(0)]
            for t2 in range(2):
                tb = 2 * pair + t2
                pt, pt_k = pg.get()
                ptb = pt[:].bitcast(BF16)
                for ec in range(8):
                    op("pe", lambda e, ptb=ptb, ec=ec, tb=tb: e.transpose(out=ptb[:, ec * 128:(ec + 1) * 128], in_=mixed[:, tb, ec * 128:(ec + 1) * 128], identity=ident[:, :]),
                       reads=[("mixA", tb, h) for h in range(4)] + [("mixC", tb), "ident"], writes=[pt_k])
                op("act", lambda e, ptb=ptb: e.copy(out=mT[:, :, :], in_=ptb[:, :].rearrange("p (c t) -> p c t", c=8)), reads=[pt_k], writes=["mT"])
                xt, xt_k = xt_pool.get()
                op("sp", lambda e, xt=xt, sl=sl, tb=tb: e.dma_start(out=xt[:, :], in_=xown[sl, 32 + tb * 128:32 + (tb + 1) * 128, :]), writes=[xt_k], dma=True)
                h1, h1_k = xt, xt_k
                for half in range(2):
                    wo, wo_k = wos[half]
                    for ec in range(8):
                        op("pe", lambda e, half=half, ec=ec, wo=wo: e.matmul(psc[half][:, :], lhsT=mT[:, ec, :], rhs=wo[:, ec, :], start=(ec == 0), stop=(ec == 7)),
                           reads=["mT", wo_k], writes=[("psc", half)])
                    op("dve", lambda e, half=half, h1=h1, xt=xt: e.tensor_tensor(out=h1[:, half * 512:(half + 1) * 512], in0=psc[half][:, :],
                                                                               in1=xt[:, half * 512:(half + 1) * 512], op=ALU.add),
                       reads=[("psc", half), xt_k], writes=[h1_k])
                rmsnorm_T(h1, h1_k, 128, gM, hnT[:, :, t2 * 128:(t2 + 1) * 128], ("hnT", t2))
                h1s.append((h1, h1_k, tb))
            issue_next()
            issue_next()
            accs = [[(psc[0], ("psc", 0)), (psc[1], ("psc", 1))], [(pacc[0], ("pacc", 0)), (pacc[1], ("pacc", 1))]]
            fstate = {}

            def ffn_up(i):
                fg, fc = divmod(i, 4)
                if fc == 0:
                    while len(wfifo) < 2:
                        issue_next()
                    fstate[fg] = (wfifo.pop(0), wfifo.pop(0))
                (wu, wu_k), (wdv, wd_k) = fstate[fg]
                ps, ps_k = pg.get()
                for dc in range(8):
                    op("pe", lambda e, ps=ps, wu=wu, dc=dc, fc=fc: e.matmul(ps[:, 0:256], lhsT=wu[:, dc, fc * 128:(fc + 1) * 128], rhs=hnT[:, dc, :],
                                                                         start=(dc == 0), stop=(dc == 7)),
                       reads=[wu_k, ("hnT", 0), ("hnT", 1)], writes=[ps_k])
                if fc == 3:
                    issue_next()
                uf, uf_k = uf_pool.get()
                op("act", lambda e, uf=uf, ps=ps: e.activation(out=uf[:, :], in_=ps[:, 0:256], func=AF.Relu), reads=[ps_k], writes=[uf_k])
                op("pool", lambda e, uf=uf: e.tensor_tensor(out=uf[:, :], in0=uf[:, :], in1=uf[:, :], op=ALU.mult), reads=[uf_k], writes=[uf_k])
                return uf, uf_k

            def ffn_down(i, ufs):
                fg, fc = divmod(i, 4)
                uf, uf_k = ufs
                (wu, wu_k), (wdv, wd_k) = fstate[fg]
                for t2 in range(2):
                    for half in range(2):
                        acc, acc_k = accs[t2][half]
                        op("pe", lambda e, acc=acc, uf=uf, t2=t2, half=half, wdv=wdv, fc=fc, i=i: e.matmul(
                            acc[:, :], lhsT=uf[:, t2 * 128:(t2 + 1) * 128], rhs=wdv[:, fc, half * 512:(half + 1) * 512], start=(i == 0), stop=(i == 31)),
                           reads=[uf_k, wd_k], writes=[acc_k])
                if fc == 3:
                    issue_next()

            cur = ffn_up(0)
            for i in range(32):
                nxt = ffn_up(i + 1) if i + 1 < 32 else None
                ffn_down(i, cur)
                cur = nxt
            for t2 in range(2):
                h1, h1_k, tb = h1s[t2]
                for half in range(2):
                    acc, acc_k = accs[t2][half]
                    op("dve", lambda e, acc=acc, h1=h1, half=half: e.tensor_tensor(out=h1[:, half * 512:(half + 1) * 512], in0=acc[:, :],
                                                                                 in1=h1[:, half * 512:(half + 1) * 512], op=ALU.add),
                       reads=[acc_k, h1_k], writes=[h1_k])
                st, st_k = st_pool.get()
                jk, jk_k = jk_pool.get()
                op("act", lambda e, jk=jk, h1=h1, st=st: e.activation(out=jk[:, :], in_=h1[:, 0:512], func=AF.Square, accum_out=st[:, 0:1]),
                   reads=[h1_k], writes=[jk_k, (st_k, 0)])
                op("act", lambda e, jk=jk, h1=h1, st=st: e.activation(out=jk[:, :], in_=h1[:, 512:1024], func=AF.Square, accum_out=st[:, 1:2]),
                   reads=[h1_k], writes=[jk_k, (st_k, 1)])
                op("dve", lambda e, st=st: e.tensor_tensor(out=st[:, 2:3], in0=st[:, 0:1], in1=st[:, 1:2], op=ALU.add), reads=[(st_k, 0), (st_k, 1)], writes=[(st_k, 2)])
                op("dve", lambda e, st=st: e.tensor_scalar(out=st[:, 3:4], in0=st[:, 2:3], scalar1=1.0 / D, scalar2=EPS, op0=ALU.mult, op1=ALU.add),
                   reads=[(st_k, 2)], writes=[(st_k, 3)])
                op("act", lambda e, st=st: e.activation(out=st[:, 4:5], in_=st[:, 3:4], func=AF.Sqrt), reads=[(st_k, 3)], writes=[(st_k, 4)])
                op("dve", lambda e, st=st: e.reciprocal(out=st[:, 5:6], in_=st[:, 4:5]), reads=[(st_k, 4)], writes=[(st_k, 5)])
                op("dve", lambda e, h1=h1, st=st: e.scalar_tensor_tensor(out=h1[:, :], in0=h1[:, :], scalar=st[:, 5:6], in1=gF[:, :], op0=ALU.mult, op1=ALU.mult),
                   reads=[h1_k, (st_k, 5), "gF"], writes=[h1_k])
                r0 = sl * 512 + tb * 128
                op("sp", lambda e, h1=h1, r0=r0: e.dma_start(out=out_d[r0:r0 + 128, :], in_=h1[:, :]), reads=[h1_k], writes=[("out", r0)], dma=True)

    if os.environ.get("KDEBUG"):
        print("sbuf remaining", nc.sbuf_bytes_remaining, "ops", {e: len(S_.ops[e]) for e in ENGS})
    with ExitStack() as stack:
        S_.emit(nc, stack)
    return nc


def _rope_tab(pos):
    pos = np.asarray(pos, np.float32)
    out = np.zeros((len(pos), 512), np.float32)
    inv = (np.float32(500000.0) ** (-np.arange(0, 32, 2, dtype=np.float32) / np.float32(32))).astype(np.float32)
    ang = pos[:, None] * inv[None, :]
    c, s = np.cos(ang).astype(np.float32), np.sin(ang).astype(np.float32)
    out[:, 0:128] = np.tile(np.concatenate([c, c], 1), (1, 4))
    out[:, 128:256] = np.tile(np.concatenate([s, s], 1), (1, 4))
    inv = (np.float32(500000.0) ** (-np.arange(0, 16, 2, dtype=np.float32) / np.float32(16))).astype(np.float32)
    ang = pos[:, None] * inv[None, :]
    c, s = np.cos(ang).astype(np.float32), np.sin(ang).astype(np.float32)
    out[:, 256:384] = np.tile(np.concatenate([c, c], 1), (1, 8))
    out[:, 384:512] = np.tile(np.concatenate([s, s], 1), (1, 8))
    return out


def _chunk_of(sl, half):
    return 2 * sl + (sl % 2) if half == 0 else 2 * sl + 1 - (sl % 2)


_NC_CACHE = {}


def kernel(x, meta_tokens, attn_norm_g, w_in, conv_w, conv_b, conv_norm_g, conv_norm_b,
           w_out, mlp_norm_g, w_up, w_down, final_norm_g):
    x = np.asarray(x, np.float32)
    B, S, _ = x.shape
    T = NMETA + S
    NSLOT = S // 1024
    if S not in _NC_CACHE:
        _NC_CACHE[S] = build(S)
    nc = _NC_CACHE[S]
    f = lambda a: np.ascontiguousarray(np.asarray(a, np.float32))
    meta = f(meta_tokens)
    ropek = _rope_tab(np.arange(T))
    p = np.arange(128)
    consts = {
        "iota": np.tile(np.arange(512, dtype=np.float32)[None, :], (128, 1)),
        "pow2": np.tile((2.0 ** -(np.arange(NBIS) + 1.0)).astype(np.float32)[None, :], (128, 1)),
        "e32": (np.arange(32)[None, :] == (p % 32)[:, None]).astype(np.float32),
        "g4": (np.arange(4)[None, :] == (p // 32)[:, None]).astype(np.float32),
        "dm": np.concatenate([(np.arange(128)[None, :] == (32 * g + p % 32)[:, None]).astype(np.float32) for g in range(4)], axis=1),
    }
    shared = {
        "w_in": f(w_in[0]), "w_out": f(w_out[0]), "w_up": f(w_up[0]), "w_down": f(w_down[0]),
        "attn_g": f(attn_norm_g[0]), "mlp_g": f(mlp_norm_g[0]), "final_g": f(final_norm_g),
        "conv_w": f(conv_w[0]), "conv_b": f(conv_b[0]), "cn_g": f(conv_norm_g[0]), "cn_b": f(conv_norm_b[0]),
        "ropek": ropek,
    }
    shared.update(consts)
    in_maps = []
    for core in range(8):
        b, half = core // 2, core % 2
        hall = np.concatenate([meta, x[b]], axis=0)
        xown = np.zeros((NSLOT, 544, D), np.float32)
        ropeq = np.zeros((NSLOT, 512, 512), np.float32)
        qrel = np.zeros((128, NSLOT * 8), np.float32)
        for sl in range(NSLOT):
            c = _chunk_of(sl, half)
            p0 = NMETA + 512 * c
            lo = p0 - 32
            src_lo = max(lo, 0)
            xown[sl, src_lo - lo:, :] = hall[src_lo:p0 + 512]
            ropeq[sl] = ropek[p0:p0 + 512]
            w0 = NMETA + 1024 * sl
            for tb in range(4):
                for wc in range(2):
                    qrel[:, (sl * 4 + tb) * 2 + wc] = (p0 + tb * 128 + p) - w0 - 512 * wc
        m = dict(shared)
        m.update({"xall": hall, "xown": xown, "ropeq": ropeq, "qrel": qrel})
        in_maps.append(m)
    res = run_bass_kernel_spmd(nc, in_maps, core_ids=list(range(8)))
    out = np.zeros((B, S, D), np.float32)
    for core in range(8):
        b, half = core // 2, core % 2
        o = res.results[core]["out"]
        for sl in range(NSLOT):
            c = _chunk_of(sl, half)
            out[b, 512 * c:512 * (c + 1)] = o[sl * 512:(sl + 1) * 512]
    return out
```

```python
import numpy as np
from contextlib import ExitStack
import concourse.bass as bass
import concourse.mybir as mybir
from concourse.bass_utils import run_bass_kernel_spmd

F32 = mybir.dt.float32
BF16 = mybir.dt.bfloat16
ALU = mybir.AluOpType
AF = mybir.ActivationFunctionType
AX = mybir.AxisListType

D = 1024
NMETA = 16
DIN = 3144
DFF = 4096
EPS = 1e-5
KTOP = 256
NBIS = 20
ENGS = ("pe", "act", "dve", "pool", "sp")
SEM_CH = 30000
NDMA_SEM = 12


class Op:
    __slots__ = ("eng", "fn", "deps", "dma", "signals", "sig", "dma_idx")


class Sched:
    def __init__(self):
        self.ops = {e: [] for e in ENGS}
        self.lastw = {}
        self.readers = {}
        self.ndma = {e: 0 for e in ENGS}

    def op(self, eng, fn, reads=(), writes=(), dma=False):
        o = Op()
        o.eng = eng
        o.fn = fn
        o.dma = dma
        o.signals = False
        o.sig = None
        deps = {}
        xr = [k for k in reads if isinstance(k, tuple) and k[0] in ("pg", "psc", "pacc")]
        if xr:
            writes = list(writes) + [k for k in xr if k not in writes]
        for k in reads:
            w = self.lastw.get(k)
            if w is not None:
                deps[id(w)] = (w, True)
        for k in writes:
            w = self.lastw.get(k)
            if w is not None and id(w) not in deps:
                deps[id(w)] = (w, False)
            for r in self.readers.get(k, ()):
                if id(r) not in deps:
                    deps[id(r)] = (r, False)
        o.deps = []
        for d, raw in deps.values():
            if d.eng == eng and not d.dma:
                if eng == "pe":
                    continue
                if not raw and not dma:
                    continue
            d.signals = True
            o.deps.append(d)
        if dma:
            o.dma_idx = self.ndma[eng]
            self.ndma[eng] += 1
            o.signals = True
        for k in reads:
            self.readers.setdefault(k, []).append(o)
        for k in writes:
            self.lastw[k] = o
            self.readers[k] = []
        self.ops[eng].append(o)
        return o

    def emit(self, nc, stack):
        nsig = {}
        for e in ENGS:
            n = 0
            for o in self.ops[e]:
                if o.signals and not o.dma:
                    o.sig = n
                    n += 1
            nsig[e] = n
        csem = {}
        for e in ENGS:
            nch = (nsig[e] + SEM_CH - 1) // SEM_CH
            csem[e] = [stack.enter_context(nc.semaphore(f"c_{e}_{i}")) for i in range(nch)]
        dsem = {}
        for e in ENGS:
            if self.ndma[e]:
                dsem[e] = [stack.enter_context(nc.semaphore(f"d_{e}_{i}")) for i in range(NDMA_SEM)]

        def target(d):
            if d.dma:
                return dsem[d.eng][d.dma_idx % NDMA_SEM], 16 * (d.dma_idx // NDMA_SEM + 1)
            return csem[d.eng][d.sig // SEM_CH], d.sig % SEM_CH + 1

        block = stack.enter_context(nc.Block())

        def run(e):
            def body(eng):
                waited = {}
                for o in self.ops[e]:
                    tg = [target(d) for d in o.deps]
                    if o.dma and o.dma_idx >= NDMA_SEM:
                        tg.append((dsem[e][o.dma_idx % NDMA_SEM], 16 * (o.dma_idx // NDMA_SEM)))
                    for s, v in tg:
                        key = id(s)
                        if waited.get(key, 0) < v:
                            eng.wait_ge(s, v)
                            waited[key] = v
                    ins = o.fn(eng)
                    if o.signals:
                        if o.dma:
                            s, _ = target(o)
                            ins.then_inc(s, 16)
                        else:
                            ins.then_inc(csem[e][o.sig // SEM_CH], 1)
                if self.ndma[e]:
                    n = self.ndma[e]
                    for i in range(max(0, n - NDMA_SEM), n):
                        eng.wait_ge(dsem[e][i % NDMA_SEM], 16 * (i // NDMA_SEM + 1))
            return body

        for e, reg in (("pe", block.tensor), ("act", block.scalar), ("dve", block.vector),
                       ("pool", block.gpsimd), ("sp", block.sync)):
            if self.ops[e]:
                reg(run(e))


class Pool:
    def __init__(self, tiles, name, off=0):
        self.tiles = tiles
        self.name = name
        self.i = 0
        self.off = off

    def get(self):
        j = self.i % len(self.tiles)
        self.i += 1
        return self.tiles[j], (self.name, j + self.off)


def build(S):
    T = NMETA + S
    NCH = S // 512
    NSLOT = NCH // 2
    NKT = 1 + S // 128
    nc = bass.Bass("TRN2", target_bir_lowering=False)

    def din(name, shape, dt=F32):
        return nc.dram_tensor(name, shape, dt, kind="ExternalInput").ap()

    xall = din("xall", [T, D])
    xown = din("xown", [NSLOT, 544, D])
    ropek = din("ropek", [T, 512])
    ropeq = din("ropeq", [NSLOT, 512, 512])
    qrel_d = din("qrel", [128, NSLOT * 8])
    w_in = din("w_in", [D, DIN])
    w_out = din("w_out", [D, D])
    w_up = din("w_up", [D, DFF])
    w_down = din("w_down", [DFF, D])
    attn_g = din("attn_g", [D])
    mlp_g = din("mlp_g", [D])
    final_g = din("final_g", [D])
    conv_w = din("conv_w", [31, 512])
    conv_b = din("conv_b", [512])
    cn_g = din("cn_g", [512])
    cn_b = din("cn_b", [512])
    iota_d = din("iota", [128, 512])
    pow2_d = din("pow2", [128, NBIS])
    e32_d = din("e32", [128, 32])
    g4_d = din("g4", [128, 4])
    dm_d = din("dm", [128, 4 * 128])
    out_d = nc.dram_tensor("out", [NSLOT * 512, D], F32, kind="ExternalOutput").ap()

    k_scr = nc.dram_tensor("k_scr", [4, 128, T], BF16).ap()
    v_scr = nc.dram_tensor("v_scr", [T, 4, 129], BF16).ap()

    import os
    KSTOP = float(os.environ.get('KSTOP', '99'))
    S_ = Sched()
    stage = [0]

    def op(*a, **k):
        if stage[0] <= KSTOP:
            return S_.op(*a, **k)
        return None
    uid = [0]

    def sb(shape, dt, name=None):
        uid[0] += 1
        return nc.alloc_sbuf_tensor(f"{name or 't'}{uid[0]}", shape, dt)

    def mkpool(n, shape, dt, name):
        return Pool([sb(shape, dt, name) for _ in range(n)], name)

    identf = sb([128, 128], F32, "identf")
    ident = sb([128, 128], BF16, "ident")
    op("pool", lambda e: e.memset(identf[:], 0.0), writes=["identf"])
    op("pool", lambda e: e.affine_select(out=identf[:], in_=identf[:], pattern=[[-1, 128]],
                                         compare_op=ALU.not_equal, fill=1.0, base=0, channel_multiplier=1),
       reads=["identf"], writes=["identf"])
    op("dve", lambda e: e.tensor_copy(out=ident[:], in_=identf[:]), reads=["identf"], writes=["ident"])

    def load_const(name, shape, src, dt=F32):
        t = sb(shape, dt, name)
        op("sp", lambda e: e.dma_start(out=t[:], in_=src, allow_slow_non_contiguous=True), writes=[name], dma=True)
        return t

    iota = load_const("iota", [128, 512], iota_d)
    pow2 = load_const("pow2", [128, NBIS], pow2_d)
    e32 = load_const("e32", [128, 32], e32_d)
    g4 = load_const("g4", [128, 4], g4_d)
    qrel = load_const("qrelc", [128, NSLOT * 8], qrel_d)
    gA = load_const("gA", [128, 8], attn_g.rearrange("(c p) -> p c", p=128))
    gM = load_const("gM", [128, 8], mlp_g.rearrange("(c p) -> p c", p=128))
    def bcast_row(name, src, n):
        t = sb([128, n], F32, name)
        op("sp", lambda e: e.dma_start(out=t[:], in_=src.partition_broadcast(128)), writes=[name], dma=True)
        return t
    gF = bcast_row("gF", final_g, D)
    cb_bc = bcast_row("cb_bc", conv_b, 512)
    cg_bc = bcast_row("cg_bc", cn_g, 512)
    cnb_bc = bcast_row("cnb_bc", cn_b, 512)
    cwT = sb([128, 4, 31], F32, "cwT")
    for cc_ in range(4):
        op("sp", lambda e, cc_=cc_: e.dma_start(out=cwT[:, cc_, :], in_=conv_w[:, cc_ * 128:(cc_ + 1) * 128].rearrange("j p -> p j"),
                                                 allow_slow_non_contiguous=True), writes=["cwT"], dma=True)
    dmb = sb([128, 512], BF16, "dmb")
    op("pool", lambda e: e.dma_start(out=dmb[:], in_=dm_d), writes=["dmb"], dma=True)
    wik_t = sb([128, 8, 64], BF16, "wik")
    op("pool", lambda e: e.dma_start(out=wik_t[:, :, :], in_=w_in[:, 2048:2112].rearrange("(c p) n -> p c n", p=128)), writes=["wik"], dma=True)
    wiw_t = sb([128, 8, 8], BF16, "wiw")
    op("pool", lambda e: e.dma_start(out=wiw_t[:, :, :], in_=w_in[:, 2112:2120].rearrange("(c p) n -> p c n", p=128), allow_slow_non_contiguous=True),
       writes=["wiw"], dma=True)

    win_b = nc.dram_tensor("win_b", [D, DIN], BF16).ap()
    wout_b = nc.dram_tensor("wout_b", [D, D], BF16).ap()
    wup_b = nc.dram_tensor("wup_b", [D, DFF], BF16).ap()
    wdown_b = nc.dram_tensor("wdown_b", [DFF, D], BF16).ap()

    ikT = sb([128, T], BF16, "ikT")
    scores = sb([128, T], F32, "scores")
    wbuf = mkpool(4, [128, 8, 512], BF16, "wbuf")
    xt_pool = mkpool(3, [128, D], F32, "xt")
    xn_pool = mkpool(1, [128, D], BF16, "xn")
    xnT_pool = mkpool(2, [128, 8, 128], BF16, "xnT")
    rope_pool = mkpool(2, [128, 256], F32, "rope")
    st_pool = mkpool(4, [128, 8], F32, "stat")
    tm_pool = mkpool(3, [128, 512], BF16, "tm")
    rt_pool = mkpool(2, [128, 256], F32, "rt")
    jk_pool = mkpool(2, [128, 512], F32, "jk")
    bigjunk = sb([128, NMETA + 512 * ((T // 512) // 2 + 1)], mybir.dt.uint8, "bigjunk")
    vb_pool = mkpool(2, [128, 4, 129], BF16, "vb")
    for vbt_ in vb_pool.tiles:
        pass
    pg = Pool([nc.alloc_psum_tensor(f"pg{i}", [128, 512], F32) for i in range(4)], "pg")
    psc = [nc.alloc_psum_tensor(f"psc{i}", [128, 512], F32) for i in range(2)]
    pacc = [nc.alloc_psum_tensor(f"pacc{i}", [128, 512], F32) for i in range(2)]

    def pg_bf16(t):
        return t.bitcast(BF16) if hasattr(t, "bitcast") else None

    def rmsnorm_T(x_t, x_k, rows, gcol, xnT, xnT_k):
        st, st_k = st_pool.get()
        jk, jk_k = jk_pool.get()
        xn, xn_k = xn_pool.get()
        op("act", lambda e: e.activation(out=jk[0:rows, :], in_=x_t[0:rows, 0:512], func=AF.Square,
                                         accum_out=st[0:rows, 0:1]), reads=[x_k], writes=[jk_k, (st_k, 0)])
        op("act", lambda e: e.activation(out=jk[0:rows, :], in_=x_t[0:rows, 512:1024], func=AF.Square,
                                         accum_out=st[0:rows, 1:2]), reads=[x_k], writes=[jk_k, (st_k, 1)])
        op("dve", lambda e: e.tensor_tensor(out=st[0:rows, 2:3], in0=st[0:rows, 0:1], in1=st[0:rows, 1:2], op=ALU.add),
           reads=[(st_k, 0), (st_k, 1)], writes=[(st_k, 2)])
        op("dve", lambda e: e.tensor_scalar(out=st[0:rows, 3:4], in0=st[0:rows, 2:3], scalar1=1.0 / D, scalar2=EPS,
                                            op0=ALU.mult, op1=ALU.add), reads=[(st_k, 2)], writes=[(st_k, 3)])
        op("act", lambda e: e.activation(out=st[0:rows, 4:5], in_=st[0:rows, 3:4], func=AF.Sqrt),
           reads=[(st_k, 3)], writes=[(st_k, 4)])
        op("dve", lambda e: e.reciprocal(out=st[0:rows, 5:6], in_=st[0:rows, 4:5]), reads=[(st_k, 4)], writes=[(st_k, 5)])
        op("dve", lambda e: e.tensor_scalar(out=xn[0:rows, :], in0=x_t[0:rows, :], scalar1=st[0:rows, 5:6], scalar2=None,
                                            op0=ALU.mult), reads=[x_k, (st_k, 5)], writes=[xn_k])
        pt, pt_k = pg.get()
        ptb = pt[:].bitcast(BF16)
        for dc in range(8):
            op("pe", lambda e, dc=dc: e.transpose(out=ptb[:, dc * 128:dc * 128 + rows], in_=xn[0:rows, dc * 128:(dc + 1) * 128],
                                                  identity=ident[0:rows, 0:rows]),
               reads=[xn_k, "ident"], writes=[pt_k])
        op("dve", lambda e: e.tensor_tensor(out=xnT[:, :, 0:rows],
                                            in0=ptb.rearrange("p (c t) -> p c t", c=8)[:, :, 0:rows],
                                            in1=gcol[:, :].unsqueeze(2).to_broadcast([128, 8, rows]), op=ALU.mult),
           reads=[pt_k], writes=[xnT_k])
        return (st, st_k)

    def wviews(kind, i, w):
        if kind == "down":
            v = w[:, :, :].rearrange("p a b -> p (a b)").rearrange("p (f n) -> p f n", f=4)
            r = lambda a: a[i * 512:(i + 1) * 512, :].rearrange("(f p) n -> p f n", p=128)
            return v, r(w_down), r(wdown_b)
        src, dst = {"in": (w_in, win_b), "out": (w_out, wout_b), "up": (w_up, wup_b)}[kind]
        r = lambda a: a[:, i:i + 512].rearrange("(c p) n -> p c n", p=128) if kind == "in" else a[:, i * 512:(i + 1) * 512].rearrange("(c p) n -> p c n", p=128)
        return w[:, :, :], r(src), r(dst)

    def load_w(kind, i, cast=False):
        w, w_k = wbuf.get()
        v, src, scr = wviews(kind, i, w)
        if cast:
            op("pool", lambda e: e.dma_start(out=v, in_=src), writes=[w_k], dma=True)
        else:
            op("sp", lambda e: e.dma_start(out=v, in_=scr), reads=[("wcv", kind, i)], writes=[w_k], dma=True)
        return (v if kind == "down" else w), w_k

    def proj_tm(xnT, xnT_k, rows, w, w_k, ncols, ps, ps_k, pcol=0):
        for dc in range(8):
            op("pe", lambda e, dc=dc: e.matmul(ps[0:rows, pcol:pcol + ncols], lhsT=xnT[:, dc, 0:rows], rhs=w[:, dc, 0:ncols],
                                               start=(dc == 0), stop=(dc == 7)),
               reads=[xnT_k, w_k], writes=[ps_k])


    def rope_tm(ps, ps_k, rows, nh, hd, half, rp, rp_k, roff, dst, dst_k, dcol=0):
        r2 = 2 * half
        n = nh * hd
        xf, xf_k = jk_pool.get()
        op("act", lambda e: e.copy(out=xf[0:rows, 0:n], in_=ps[0:rows, 0:n]), reads=[ps_k], writes=[xf_k])
        op("act", lambda e: e.copy(out=dst[0:rows, dcol:dcol + n], in_=ps[0:rows, 0:n]), reads=[ps_k], writes=[dst_k])
        rt, rt_k = rt_pool.get()
        pv = xf[0:rows, 0:n].rearrange("p (h d) -> p h d", h=nh)
        A = rt[0:rows, 0:nh * r2].rearrange("p (h d) -> p h d", h=nh)
        Bm = rt[0:rows, 128:128 + nh * r2].rearrange("p (h d) -> p h d", h=nh)
        cc_ = rp[0:rows, roff:roff + nh * r2].rearrange("p (h d) -> p h d", h=nh)
        ss_ = rp[0:rows, roff + 128:roff + 128 + nh * r2].rearrange("p (h d) -> p h d", h=nh)
        op("dve", lambda e: e.tensor_tensor(out=A, in0=pv[:, :, 0:r2], in1=cc_, op=ALU.mult), reads=[xf_k, rp_k], writes=[(rt_k, 0)])
        op("dve", lambda e: e.tensor_tensor(out=Bm, in0=pv[:, :, 0:r2], in1=ss_, op=ALU.mult), reads=[xf_k, rp_k], writes=[(rt_k, 1)])
        dv = dst[0:rows, dcol:dcol + n].rearrange("p (h d) -> p h d", h=nh)
        op("dve", lambda e: e.tensor_tensor(out=dv[:, :, 0:half], in0=A[:, :, 0:half], in1=Bm[:, :, half:r2], op=ALU.subtract),
           reads=[(rt_k, 0), (rt_k, 1), dst_k], writes=[dst_k])
        op("dve", lambda e: e.tensor_tensor(out=dv[:, :, half:r2], in0=A[:, :, half:r2], in1=Bm[:, :, 0:half], op=ALU.add),
           reads=[(rt_k, 0), (rt_k, 1), dst_k], writes=[dst_k])

    stage[0] = 1
    for j_, vbt_ in enumerate(vb_pool.tiles):
        op("pool", lambda e, vbt_=vbt_: e.memset(vbt_[:, :, 128:129], 1.0), writes=[("vbones", ("vb", j_))])
    wk, wk_k = load_w("in", 512, cast=True)
    wv, wv_k = load_w("in", 1024, cast=True)
    cvt_pool = Pool(wbuf.tiles[2:4], "wbuf", off=2)
    for kind, idxs in (("in", (0, 1536, 2120, 2632)), ("out", (0, 1)), ("up", tuple(range(8))), ("down", tuple(range(8)))):
        for i in idxs:
            w, w_k = cvt_pool.get()
            v, src, scr = wviews(kind, i, w)
            op("pool", lambda e, v=v, src=src: e.dma_start(out=v, in_=src), writes=[w_k], dma=True)
            op("pool", lambda e, v=v, scr=scr: e.dma_start(out=scr, in_=v), reads=[w_k], writes=[("wcv", kind, i)], dma=True)
    wbuf.i = 2
    wik, wik_k = wik_t, "wik"
    stage[0] = 1
    for kt in range(min(NKT, int(os.environ.get('KT_MAX', '999')))):
        rows = NMETA if kt == 0 else 128
        p0 = 0 if kt == 0 else NMETA + 128 * (kt - 1)
        xt, xt_k = xt_pool.get()
        op("sp", lambda e, xt=xt, p0=p0, rows=rows: e.dma_start(out=xt[0:rows, :], in_=xall[p0:p0 + rows, :]), writes=[xt_k], dma=True)
        rp, rp_k = rope_pool.get()
        op("sp", lambda e, rp=rp, p0=p0, rows=rows: e.dma_start(out=rp[0:rows, :], in_=ropek[p0:p0 + rows, 0:256]), writes=[rp_k], dma=True)
        rpi, rpi_k = rope_pool.get()
        op("sp", lambda e, rpi=rpi, p0=p0, rows=rows: e.dma_start(out=rpi[0:rows, :], in_=ropek[p0:p0 + rows, 256:512]), writes=[rpi_k], dma=True)
        xnT, xnT_k = xnT_pool.get()
        stage[0] = 1.1
        rmsnorm_T(xt, xt_k, rows, gA, xnT, xnT_k)
        stage[0] = 1.2
        ps, ps_k = pg.get()
        proj_tm(xnT, xnT_k, rows, wk, wk_k, 512, ps, ps_k)
        stage[0] = 1.25
        kb, kb_k = tm_pool.get()
        rope_tm(ps, ps_k, rows, 4, 128, 16, rp, rp_k, 0, kb, kb_k)
        stage[0] = 1.3
        pt, pt_k = pg.get()
        ptb = pt[:].bitcast(BF16)
        for h in range(4):
            op("pe", lambda e, h=h, ptb=ptb, kb=kb, rows=rows: e.transpose(out=ptb[:, h * 128:h * 128 + rows], in_=kb[0:rows, h * 128:(h + 1) * 128],
                                                                         identity=ident[0:rows, 0:rows]), reads=[kb_k, "ident"], writes=[pt_k])
        kTt, kTt_k = tm_pool.get()
        op("act", lambda e, kTt=kTt, ptb=ptb: e.copy(out=kTt[:, :], in_=ptb[:, 0:512]), reads=[pt_k], writes=[kTt_k])
        op("sp", lambda e, kTt=kTt, p0=p0, rows=rows: e.dma_start(
            out=k_scr[:, :, p0:p0 + rows].rearrange("h p t -> p h t"),
            in_=kTt[:, :].rearrange("p (h t) -> p h t", h=4)[:, :, 0:rows]),
           reads=[kTt_k], writes=[("k_scr", kt)], dma=True)
        stage[0] = 1.4
        ps, ps_k = pg.get()
        proj_tm(xnT, xnT_k, rows, wv, wv_k, 512, ps, ps_k)
        vb, vb_k = vb_pool.get()
        op("act", lambda e, vb=vb, ps=ps, rows=rows: e.copy(out=vb[0:rows, :, 0:128], in_=ps[0:rows, :].rearrange("p (h d) -> p h d", h=4)),
           reads=[ps_k, ("vbones", vb_k)], writes=[vb_k])
        op("sp", lambda e, vb=vb, p0=p0, rows=rows: e.dma_start(out=v_scr[p0:p0 + rows, :, :], in_=vb[0:rows, :, :]),
           reads=[vb_k, ("vbones", vb_k)], writes=[("v_scr", kt)], dma=True)
        stage[0] = 1.5
        ps, ps_k = pg.get()
        proj_tm(xnT, xnT_k, rows, wik, wik_k, 64, ps, ps_k)
        ib, ib_k = tm_pool.get()
        rope_tm(ps, ps_k, rows, 1, 64, 8, rpi, rpi_k, 0, ib, ib_k)
        op("dve", lambda e, ib=ib, rows=rows: e.tensor_copy(out=ib[0:rows, 64:128], in_=ib[0:rows, 0:64]), reads=[ib_k], writes=[ib_k])
        pt, pt_k = pg.get()
        ptb = pt[:].bitcast(BF16)
        op("pe", lambda e, ptb=ptb, ib=ib, rows=rows: e.transpose(out=ptb[:, 0:rows], in_=ib[0:rows, 0:128], identity=ident[0:rows, 0:rows]),
           reads=[ib_k, "ident"], writes=[pt_k])
        op("act", lambda e, ptb=ptb, p0=p0, rows=rows: e.copy(out=ikT[:, p0:p0 + rows], in_=ptb[:, 0:rows]), reads=[pt_k], writes=[("ikT", kt)])

    stage[0] = 2
    qT = sb([128, 4, 512], BF16, "qT")
    iqT = sb([128, 4, 512], BF16, "iqT")
    iwt = sb([128, 4, 8], F32, "iwt")
    uT = sb([128, 4, 544], BF16, "uT")
    mixed = sb([128, 4, D], BF16, "mixed")
    ysb = sb([128, 4, 512], F32, "ysb")
    Dcc = sb([128, 31, 128], BF16, "Dcc")
    negm_pool = mkpool(2, [128, 512], F32, "negm")
    wabs2 = sb([128, 8], F32, "wabs2")
    wsgn = sb([128, 8], F32, "wsgn")
    bis = sb([128, 8 + NBIS], F32, "bis")
    top8 = sb([128, 8], F32, "top8")
    cst = sb([128, 4, 6], F32, "cst")
    cmv = sb([128, 4, 2], F32, "cmv")
    crs = sb([128, 4], F32, "crs")
    R_pool = mkpool(3, [128, 512], BF16, "R")
    PT_pool = mkpool(2, [128, 512], BF16, "PT")
    mb_pool = mkpool(2, [128, 512], BF16, "mb")
    GK = 4
    kv_pool = [(sb([128, 4, GK * 128], BF16, "kbuf"), sb([128, GK, 4, 129], BF16, "vbuf")) for _ in range(2)]
    kmeta = sb([128, 4, NMETA], BF16, "kmeta")
    vmeta = sb([128, 4, 129], BF16, "vmeta")
    op("sp", lambda e: e.dma_start(out=kmeta[:, :, :], in_=k_scr[:, :, 0:NMETA].rearrange("h p t -> p h t")),
       reads=[("k_scr", 0)], writes=["kmeta"], dma=True)
    op("sp", lambda e: e.dma_start(out=vmeta[0:NMETA, :, :], in_=v_scr[0:NMETA, :, :]),
       reads=[("v_scr", 0)], writes=["vmeta"], dma=True)
    mT = sb([128, 8, 128], BF16, "mT")
    hnT = sb([128, 8, 256], BF16, "hnT")
    uf_pool = mkpool(2, [128, 256], BF16, "uf")
    rden = sb([128, 4], F32, "rden")

    SC = 0.125 * (8 ** -0.5)
    kv_ctr = [0]

    for sl in range(NSLOT):
        E = NMETA + 512 * (2 * sl + 2)
        W0 = E - 1024
        NCHK = 2 * sl + 2
        wq, wq_k = load_w("in", 0)
        wiq, wiq_k = load_w("in", 1536)
        wga, wga_k = load_w("in", 2120)
        wgg, wgg_k = load_w("in", 2632)
        plan = []
        for pair_ in range(2):
            plan += [("out", 0), ("out", 1)]
            for fg_ in range(8):
                plan += [("up", fg_), ("down", fg_)]
        wfifo = []

        def issue_next():
            if plan:
                kind_, i_ = plan.pop(0)
                wfifo.append(load_w(kind_, i_))
        for ti in range(5):
            rows = 32 if ti == 0 else 128
            r0 = 0 if ti == 0 else 32 + 128 * (ti - 1)
            xt, xt_k = xt_pool.get()
            op("sp", lambda e, xt=xt, r0=r0, rows=rows, sl=sl: e.dma_start(out=xt[0:rows, :], in_=xown[sl, r0:r0 + rows, :]),
               writes=[xt_k], dma=True)
            xnT, xnT_k = xnT_pool.get()
            rmsnorm_T(xt, xt_k, rows, gA, xnT, xnT_k)
            if ti > 0:
                tb = ti - 1
                rp, rp_k = rope_pool.get()
                op("sp", lambda e, rp=rp, tb=tb, sl=sl: e.dma_start(out=rp[:, :], in_=ropeq[sl, 128 * tb:128 * (tb + 1), 0:256]), writes=[rp_k], dma=True)
                ps, ps_k = pg.get()
                proj_tm(xnT, xnT_k, 128, wq, wq_k, 512, ps, ps_k)
                qb, qb_k = tm_pool.get()
                rope_tm(ps, ps_k, 128, 4, 128, 16, rp, rp_k, 0, qb, qb_k)
                pt, pt_k = pg.get()
                ptb = pt[:].bitcast(BF16)
                for h in range(4):
                    op("pe", lambda e, h=h, ptb=ptb, qb=qb: e.transpose(out=ptb[:, h * 128:(h + 1) * 128], in_=qb[:, h * 128:(h + 1) * 128], identity=ident[:, :]),
                       reads=[qb_k, "ident"], writes=[pt_k])
                op("act", lambda e, ptb=ptb, tb=tb: e.copy(out=qT[:, :, tb * 128:(tb + 1) * 128], in_=ptb[:, 0:512].rearrange("p (h t) -> p h t", h=4)),
                   reads=[pt_k], writes=[("qT", tb)])
                rpi, rpi_k = rope_pool.get()
                op("sp", lambda e, rpi=rpi, tb=tb, sl=sl: e.dma_start(out=rpi[:, :], in_=ropeq[sl, 128 * tb:128 * (tb + 1), 256:512]), writes=[rpi_k], dma=True)
                ps, ps_k = pg.get()
                proj_tm(xnT, xnT_k, 128, wiq, wiq_k, 512, ps, ps_k)
                ib, ib_k = tm_pool.get()
                rope_tm(ps, ps_k, 128, 8, 64, 8, rpi, rpi_k, 0, ib, ib_k)
                pt, pt_k = pg.get()
                ptb = pt[:].bitcast(BF16)
                for hp in range(4):
                    op("pe", lambda e, hp=hp, ptb=ptb, ib=ib: e.transpose(out=ptb[:, hp * 128:(hp + 1) * 128], in_=ib[:, hp * 128:(hp + 1) * 128], identity=ident[:, :]),
                       reads=[ib_k, "ident"], writes=[pt_k])
                op("act", lambda e, ptb=ptb, tb=tb: e.copy(out=iqT[:, :, tb * 128:(tb + 1) * 128], in_=ptb[:, 0:512].rearrange("p (h t) -> p h t", h=4)),
                   reads=[pt_k], writes=[("iqT", tb)])
                ps, ps_k = pg.get()
                proj_tm(xnT, xnT_k, 128, wiw_t, "wiw", 8, ps, ps_k)
                op("dve", lambda e, ps=ps, tb=tb: e.tensor_scalar(out=iwt[:, tb, :], in0=ps[:, 0:8], scalar1=SC, scalar2=None, op0=ALU.mult),
                   reads=[ps_k], writes=[("iwt", tb)])
            psa, psa_k = pg.get()
            proj_tm(xnT, xnT_k, rows, wga, wga_k, 512, psa, psa_k)
            psg, psg_k = pg.get()
            proj_tm(xnT, xnT_k, rows, wgg, wgg_k, 512, psg, psg_k)
            jk, jk_k = jk_pool.get()
            op("act", lambda e, jk=jk, psg=psg, rows=rows: e.activation(out=jk[0:rows, :], in_=psg[0:rows, :], func=AF.Sigmoid),
               reads=[psg_k], writes=[jk_k])
            ub, ub_k = tm_pool.get()
            op("dve", lambda e, ub=ub, psa=psa, jk=jk, rows=rows: e.tensor_tensor(out=ub[0:rows, :], in0=psa[0:rows, :], in1=jk[0:rows, :], op=ALU.mult),
               reads=[psa_k, jk_k], writes=[ub_k])
            pt, pt_k = pg.get()
            ptb = pt[:].bitcast(BF16)
            for cc in range(4):
                op("pe", lambda e, cc=cc, ptb=ptb, ub=ub, rows=rows: e.transpose(out=ptb[:, cc * 128:cc * 128 + rows], in_=ub[0:rows, cc * 128:(cc + 1) * 128],
                                                                              identity=ident[0:rows, 0:rows]),
                   reads=[ub_k, "ident"], writes=[pt_k])
            op("act", lambda e, ptb=ptb, r0=r0, rows=rows: e.copy(out=uT[:, :, r0:r0 + rows], in_=ptb[:, 0:512].rearrange("p (c t) -> p c t", c=4)[:, :, 0:rows]),
               reads=[pt_k], writes=[("uT", ti)])
        stage[0] = 3
        UTK = [("uT", ti) for ti in range(5)]
        for cc in range(4):
            op("dve", lambda e, cc=cc: e.tensor_tensor(out=Dcc[:, :, :], in0=identf[:, :].unsqueeze(1).to_broadcast([128, 31, 128]),
                                                       in1=cwT[:, cc, :].unsqueeze(2).to_broadcast([128, 31, 128]), op=ALU.mult),
               reads=["identf", "cwT"], writes=["Dcc"])
            for tb in range(4):
                ps, ps_k = pg.get()
                for j in range(31):
                    c0 = 2 + tb * 128 + j
                    op("pe", lambda e, ps=ps, cc=cc, c0=c0, j=j: e.matmul(ps[:, 0:128], lhsT=uT[:, cc, c0:c0 + 128], rhs=Dcc[:, j, :], start=(j == 0), stop=(j == 30)),
                       reads=UTK + ["Dcc"], writes=[ps_k])
                op("act", lambda e, ps=ps, tb=tb, cc=cc: e.copy(out=ysb[:, tb, cc * 128:(cc + 1) * 128], in_=ps[:, 0:128]),
                   reads=[ps_k], writes=[("ysb", tb)])
        for tb in range(4):
            yk = ("ysb", tb)
            y = ysb[:, tb, :]
            op("dve", lambda e, y=y: e.tensor_tensor(out=y, in0=y, in1=cb_bc[:, :], op=ALU.add), reads=[yk, "cb_bc"], writes=[yk])
            for g in range(4):
                op("dve", lambda e, y=y, g=g: e.bn_stats(out=cst[:, g, :], in_=y[:, g * 128:(g + 1) * 128]), reads=[yk], writes=[("cst", g)])
                op("dve", lambda e, g=g: e.bn_aggr(out=cmv[:, g, :], in_=cst[:, g, :]), reads=[("cst", g)], writes=[("cmv", g)])
            CM = [("cmv", g) for g in range(4)]
            op("dve", lambda e: e.tensor_scalar(out=crs[:, :], in0=cmv[:, :, 1], scalar1=EPS, scalar2=None, op0=ALU.add), reads=CM, writes=["crs"])
            op("act", lambda e: e.activation(out=crs[:, :], in_=crs[:, :], func=AF.Sqrt), reads=["crs"], writes=["crs"])
            op("dve", lambda e: e.reciprocal(out=crs[:, :], in_=crs[:, :]), reads=["crs"], writes=["crs"])
            for g in range(4):
                op("dve", lambda e, y=y, g=g: e.tensor_scalar(out=y[:, g * 128:(g + 1) * 128], in0=y[:, g * 128:(g + 1) * 128],
                                                            scalar1=cmv[:, g, 0:1], scalar2=crs[:, g:g + 1], op0=ALU.subtract, op1=ALU.mult),
                   reads=[yk, "crs"] + CM, writes=[yk])
            op("dve", lambda e, y=y: e.tensor_tensor(out=y, in0=y, in1=cg_bc[:, :], op=ALU.mult), reads=[yk, "cg_bc"], writes=[yk])
            op("dve", lambda e, y=y: e.tensor_tensor(out=y, in0=y, in1=cnb_bc[:, :], op=ALU.add), reads=[yk, "cnb_bc"], writes=[yk])
            op("act", lambda e, y=y, tb=tb: e.activation(out=mixed[:, tb, 512:1024], in_=y, func=AF.Silu), reads=[yk], writes=[("mixC", tb)])

        stage[0] = 4
        for tb in range(4):
            if tb == 3:
                for _ in range(4):
                    issue_next()
            op("act", lambda e, tb=tb: e.activation(out=wabs2[:, :], in_=iwt[:, tb, :], func=AF.Abs, scale=2.0),
               reads=[("iwt", tb)], writes=["wabs2"])
            op("dve", lambda e, tb=tb: e.tensor_scalar(out=wsgn[:, :], in0=iwt[:, tb, :], scalar1=0.0, scalar2=0.5, op0=ALU.is_ge, op1=ALU.subtract),
               reads=[("iwt", tb)], writes=["wsgn"])
            chunks = [(0, NMETA)] + [(NMETA + 512 * m, 512) for m in range(NCHK)]
            SCK = []
            wcnt = 0
            for ci, (c0, cn) in enumerate(chunks):
                ikk = [("ikT", 0)] if ci == 0 else [("ikT", 1 + 4 * (ci - 1) + q) for q in range(4)]
                sk = ("sc", ci)
                SCK.append(sk)
                for h in range(8):
                    par, hp = h % 2, h // 2
                    ps, ps_k = pg.get()
                    op("pe", lambda e, ps=ps, par=par, hp=hp, tb=tb, c0=c0, cn=cn: e.matmul(ps[:, 0:cn], lhsT=iqT[par * 64:(par + 1) * 64, hp, tb * 128:(tb + 1) * 128],
                                                                                         rhs=ikT[par * 64:(par + 1) * 64, c0:c0 + cn], start=True, stop=True),
                       reads=[("iqT", tb)] + ikk, writes=[ps_k])
                    R, R_k = R_pool.get()
                    op("act", lambda e, R=R, ps=ps, cn=cn, h=h: e.activation(out=R[:, 0:cn], in_=ps[:, 0:cn], func=AF.Relu, scale=wabs2[:, h:h + 1]),
                       reads=[ps_k, "wabs2"], writes=[R_k])
                    if h == 0:
                        op("dve", lambda e, R=R, c0=c0, cn=cn: e.tensor_scalar(out=scores[:, c0:c0 + cn], in0=R[:, 0:cn], scalar1=wsgn[:, 0:1], scalar2=None, op0=ALU.mult),
                           reads=[R_k, "wsgn"], writes=[sk])
                    else:
                        op("dve", lambda e, R=R, c0=c0, cn=cn, h=h: e.scalar_tensor_tensor(out=scores[:, c0:c0 + cn], in0=R[:, 0:cn], scalar=wsgn[:, h:h + 1],
                                                                                         in1=scores[:, c0:c0 + cn], op0=ALU.mult, op1=ALU.add),
                           reads=[R_k, "wsgn", sk], writes=[sk])
                if c0 >= W0 and ci > 0:
                    col = (sl * 4 + tb) * 2 + wcnt
                    negm, negm_k = negm_pool.get()
                    op("dve", lambda e, col=col, negm=negm: e.tensor_scalar(out=negm[:, :], in0=iota[:, :], scalar1=qrel[:, col:col + 1], scalar2=-1e30,
                                                                          op0=ALU.is_gt, op1=ALU.mult), reads=["iota", "qrelc"], writes=[negm_k])
                    jk, jk_k = jk_pool.get()
                    op("dve", lambda e, jk=jk, c0=c0, negm=negm: e.tensor_tensor(out=jk[:, :], in0=scores[:, c0:c0 + 512], in1=negm[:, :], op=ALU.subtract),
                       reads=[sk, negm_k], writes=[jk_k])
                    op("dve", lambda e, jk=jk, wcnt=wcnt: e.tensor_reduce(out=bis[:, 5 + wcnt:6 + wcnt], in_=jk[:, :], axis=AX.X, op=ALU.min),
                       reads=[jk_k], writes=[("bis", 5 + wcnt)])
                    op("dve", lambda e, c0=c0, negm=negm: e.tensor_tensor(out=scores[:, c0:c0 + 512], in0=scores[:, c0:c0 + 512], in1=negm[:, :], op=ALU.add),
                       reads=[sk, negm_k], writes=[sk])
                    wcnt += 1
            assert wcnt == 2
            stage[0] = 5
            nD = max(1, int(round(0.45 * NCHK)))
            ED = NMETA + 512 * nD
            EA = E - ED
            SCK_D = SCK[0:1 + nD]
            SCK_A = SCK[1 + nD:]
            op("dve", lambda e, E=E: e.max(out=top8[:, :], in_=scores[:, 0:E]), reads=SCK, writes=["top8"])
            op("dve", lambda e, W0=W0: e.tensor_reduce(out=bis[:, 7:8], in_=scores[:, 0:W0], axis=AX.X, op=ALU.min), reads=SCK, writes=[("bis", 7)])
            op("dve", lambda e: e.tensor_reduce(out=bis[:, 0:1], in_=bis[:, 5:8], axis=AX.X, op=ALU.min),
               reads=[("bis", 5), ("bis", 6), ("bis", 7)], writes=[("bis", 0)])
            op("dve", lambda e: e.tensor_tensor(out=bis[:, 1:2], in0=top8[:, 0:1], in1=bis[:, 0:1], op=ALU.subtract),
               reads=["top8", ("bis", 0)], writes=[("bis", 1)])
            op("dve", lambda e: e.tensor_scalar(out=bis[:, 8:8 + NBIS], in0=pow2[:, :], scalar1=bis[:, 1:2], scalar2=None, op0=ALU.mult),
               reads=["pow2", ("bis", 1)], writes=["bisW"])
            op("dve", lambda e: e.tensor_tensor(out=bis[:, 2:3], in0=bis[:, 8:9], in1=bis[:, 0:1], op=ALU.add),
               reads=["bisW", ("bis", 0)], writes=[("bis", 2)])
            for k in range(NBIS):
                op("dve", lambda e, ED=ED: e.tensor_scalar(out=bigjunk[:, 0:ED], in0=scores[:, 0:ED], scalar1=bis[:, 2:3], scalar2=None,
                                                          op0=ALU.is_ge, op1=ALU.add, accum_out=bis[:, 3:4]),
                   reads=SCK_D + [("bis", 2)], writes=["bigjunkD", ("bis", 3)])
                op("act", lambda e, ED=ED, E=E, EA=EA: e.activation(out=bigjunk[:, 0:EA].bitcast(mybir.dt.int8), in_=scores[:, ED:E], func=AF.Sign,
                                                            bias=bis[:, 2:3], scale=-1.0, accum_out=bis[:, 5:6]),
                   reads=SCK_A + [("bis", 2)], writes=["bigjunkA", ("bis", 5)])
                op("dve", lambda e: e.scalar_tensor_tensor(out=bis[:, 6:7], in0=bis[:, 3:4], scalar=2.0, in1=bis[:, 5:6], op0=ALU.mult, op1=ALU.subtract),
                   reads=[("bis", 3), ("bis", 5)], writes=[("bis", 6)])
                op("dve", lambda e, k=k, EA=EA: e.tensor_scalar(out=bis[:, 4:5], in0=bis[:, 6:7], scalar1=float(2 * KTOP - 1 - EA), scalar2=bis[:, 8 + k:9 + k],
                                                               op0=ALU.is_ge, op1=ALU.mult), reads=[("bis", 6), "bisW"], writes=[("bis", 4)])
                if k < NBIS - 1:
                    op("dve", lambda e, k=k: e.scalar_tensor_tensor(out=bis[:, 2:3], in0=bis[:, 4:5], scalar=bis[:, 9 + k:10 + k], in1=bis[:, 2:3],
                                                                  op0=ALU.subtract, op1=ALU.add),
                       reads=[("bis", 4), "bisW", ("bis", 2)], writes=[("bis", 2)])
                else:
                    op("dve", lambda e, k=k: e.scalar_tensor_tensor(out=bis[:, 0:1], in0=bis[:, 4:5], scalar=bis[:, 8 + k:9 + k], in1=bis[:, 2:3],
                                                                  op0=ALU.subtract, op1=ALU.add),
                       reads=[("bis", 4), "bisW", ("bis", 2)], writes=[("bis", 0)])
            stage[0] = 6
            nkb_total = 1 + 4 * NCHK
            kblist = []
            for ci, (c0, cn) in enumerate(chunks):
                for kb in range(1 if ci == 0 else 4):
                    kblist.append((ci, c0, cn, kb))
            cstate = {}

            def chunk_setup(ci, c0, cn):
                mbt, mb_k = mb_pool.get()
                op("dve", lambda e, mbt=mbt, c0=c0, cn=cn: e.tensor_scalar(out=mbt[:, 0:cn], in0=scores[:, c0:c0 + cn], scalar1=bis[:, 0:1], scalar2=-30000.0,
                                                                          op0=ALU.is_lt, op1=ALU.mult), reads=[SCK[ci], ("bis", 0)], writes=[mb_k])
                if ci == 0:
                    cstate[ci] = (mbt, mb_k, kmeta, vmeta, "kmeta", "vmeta")
                    return
                kvn = kv_ctr[0] % 2
                kv_ctr[0] += 1
                kbuf, vbuf = kv_pool[kvn]
                kvk, vvk = ("kbuf", kvn), ("vbuf", kvn)
                tl = [1 + 4 * (ci - 1) + q for q in range(4)]
                op("sp", lambda e, kbuf=kbuf, c0=c0: e.dma_start(out=kbuf[:, :, :], in_=k_scr[:, :, c0:c0 + 512].rearrange("h p t -> p h t")),
                   reads=[("k_scr", t) for t in tl], writes=[kvk], dma=True)
                op("sp", lambda e, vbuf=vbuf, c0=c0: e.dma_start(out=vbuf[:, :, :, :].rearrange("p g h e -> p g (h e)"),
                                                                 in_=v_scr[c0:c0 + 512, :, :].rearrange("(g p) h e -> p g (h e)", p=128)),
                   reads=[("v_scr", t) for t in tl], writes=[vvk], dma=True)
                cstate[ci] = (mbt, mb_k, kbuf, vbuf, kvk, vvk)

            def emit_S(i):
                ci, c0, cn, kb = kblist[i]
                if kb == 0:
                    chunk_setup(ci, c0, cn)
                mbt, mb_k, kbuf, vbuf, kvk, vvk = cstate[ci]
                ks = NMETA if ci == 0 else 128
                st, st_k = pg.get()
                for h in range(4):
                    op("pe", lambda e, st=st, h=h, kbuf=kbuf, kb=kb, ks=ks, tb=tb: e.matmul(st[0:ks, h * 128:(h + 1) * 128], lhsT=kbuf[:, h, kb * 128:kb * 128 + ks],
                                                                                         rhs=qT[:, h, tb * 128:(tb + 1) * 128], start=True, stop=False),
                       reads=[kvk, ("qT", tb)], writes=[st_k])
                    op("pe", lambda e, st=st, h=h, mbt=mbt, kb=kb, ks=ks: e.matmul(st[0:ks, h * 128:(h + 1) * 128], lhsT=mbt[:, kb * 128:kb * 128 + ks],
                                                                                 rhs=ident[:, :], start=False, stop=True),
                       reads=[mb_k, "ident"], writes=[st_k])
                PT, PT_k = PT_pool.get()
                op("act", lambda e, PT=PT, st=st, ks=ks: e.activation(out=PT[0:ks, :], in_=st[0:ks, :], func=AF.Exp, scale=128 ** -0.5),
                   reads=[st_k], writes=[PT_k])
                return (PT, PT_k)

            def emit_P(i, PTs):
                ci, c0, cn, kb = kblist[i]
                mbt, mb_k, kbuf, vbuf, kvk, vvk = cstate[ci]
                ks = NMETA if ci == 0 else 128
                PT, PT_k = PTs
                for h in range(4):
                    acc = pacc[h // 2]
                    a0 = (h % 2) * 256
                    rhs = vbuf[0:ks, h, :] if ci == 0 else vbuf[:, kb, h, :]
                    op("pe", lambda e, acc=acc, a0=a0, PT=PT, h=h, ks=ks, rhs=rhs, i=i: e.matmul(acc[:, a0:a0 + 129], lhsT=PT[0:ks, h * 128:(h + 1) * 128], rhs=rhs,
                                                                                           start=(i == 0 and h % 2 == 0), stop=(i == nkb_total - 1),
                                                                                           skip_group_check=True),
                       reads=[PT_k, vvk], writes=[("pacc", h // 2)])

            prev = emit_S(0)
            for i in range(nkb_total):
                nxt = emit_S(i + 1) if i + 1 < nkb_total else None
                emit_P(i, prev)
                prev = nxt
            for h in range(4):
                acc = pacc[h // 2]
                a0 = (h % 2) * 256
                op("dve", lambda e, acc=acc, a0=a0, h=h: e.reciprocal(out=rden[:, h:h + 1], in_=acc[:, a0 + 128:a0 + 129]),
                   reads=[("pacc", h // 2)], writes=[("rden", h)])
                op("dve", lambda e, acc=acc, a0=a0, h=h, tb=tb: e.tensor_scalar(out=mixed[:, tb, h * 128:(h + 1) * 128], in0=acc[:, a0:a0 + 128], scalar1=rden[:, h:h + 1],
                                                                              scalar2=None, op0=ALU.mult),
                   reads=[("pacc", h // 2), ("rden", h)], writes=[("mixA", tb, h)])

        stage[0] = 7
        for pair in range(2):
            h1s = []
            while len(wfifo) < 2:
                issue_next()
            wos = [wfifo.pop(0), wfifo.pop(0)]
            for t2 in range(2):
                tb = 2 * pair + t2
                pt, pt_k = pg.get()
                ptb = pt[:].bitcast(BF16)
                for ec in range(8):
                    op("pe", lambda e, ptb=ptb, ec=ec, tb=tb: e.transpose(out=ptb[:, ec * 128:(ec + 1) * 128], in_=mixed[:, tb, ec * 128:(ec + 1) * 128], identity=ident[:, :]),
                       reads=[("mixA", tb, h) for h in range(4)] + [("mixC", tb), "ident"], writes=[pt_k])
                op("act", lambda e, ptb=ptb: e.copy(out=mT[:, :, :], in_=ptb[:, :].rearrange("p (c t) -> p c t", c=8)), reads=[pt_k], writes=["mT"])
                xt, xt_k = xt_pool.get()
                op("sp", lambda e, xt=xt, sl=sl, tb=tb: e.dma_start(out=xt[:, :], in_=xown[sl, 32 + tb * 128:32 + (tb + 1) * 128, :]), writes=[xt_k], dma=True)
                h1, h1_k = xt, xt_k
                for half in range(2):
                    wo, wo_k = wos[half]
                    for ec in range(8):
                        op("pe", lambda e, half=half, ec=ec, wo=wo: e.matmul(psc[half][:, :], lhsT=mT[:, ec, :], rhs=wo[:, ec, :], start=(ec == 0), stop=(ec == 7)),
                           reads=["mT", wo_k], writes=[("psc", half)])
                    op("dve", lambda e, half=half, h1=h1, xt=xt: e.tensor_tensor(out=h1[:, half * 512:(half + 1) * 512], in0=psc[half][:, :],
                                                                               in1=xt[:, half * 512:(half + 1) * 512], op=ALU.add),
                       reads=[("psc", half), xt_k], writes=[h1_k])
                rmsnorm_T(h1, h1_k, 128, gM, hnT[:, :, t2 * 128:(t2 + 1) * 128], ("hnT", t2))
                h1s.append((h1, h1_k, tb))
            issue_next()
            issue_next()
            accs = [[(psc[0], ("psc", 0)), (psc[1], ("psc", 1))], [(pacc[0], ("pacc", 0)), (pacc[1], ("pacc", 1))]]
            fstate = {}

            def ffn_up(i):
                fg, fc = divmod(i, 4)
                if fc == 0:
                    while len(wfifo) < 2:
                        issue_next()
                    fstate[fg] = (wfifo.pop(0), wfifo.pop(0))
                (wu, wu_k), (wdv, wd_k) = fstate[fg]
                ps, ps_k = pg.get()
                for dc in range(8):
                    op("pe", lambda e, ps=ps, wu=wu, dc=dc, fc=fc: e.matmul(ps[:, 0:256], lhsT=wu[:, dc, fc * 128:(fc + 1) * 128], rhs=hnT[:, dc, :],
                                                                         start=(dc == 0), stop=(dc == 7)),
                       reads=[wu_k, ("hnT", 0), ("hnT", 1)], writes=[ps_k])
                if fc == 3:
                    issue_next()
                uf, uf_k = uf_pool.get()
                op("act", lambda e, uf=uf, ps=ps: e.activation(out=uf[:, :], in_=ps[:, 0:256], func=AF.Relu), reads=[ps_k], writes=[uf_k])
                op("pool", lambda e, uf=uf: e.tensor_tensor(out=uf[:, :], in0=uf[:, :], in1=uf[:, :], op=ALU.mult), reads=[uf_k], writes=[uf_k])
                return uf, uf_k

            def ffn_down(i, ufs):
                fg, fc = divmod(i, 4)
                uf, uf_k = ufs
                (wu, wu_k), (wdv, wd_k) = fstate[fg]
                for t2 in range(2):
                    for half in range(2):
                        acc, acc_k = accs[t2][half]
                        op("pe", lambda e, acc=acc, uf=uf, t2=t2, half=half, wdv=wdv, fc=fc, i=i: e.matmul(
                            acc[:, :], lhsT=uf[:, t2 * 128:(t2 + 1) * 128], rhs=wdv[:, fc, half * 512:(half + 1) * 512], start=(i == 0), stop=(i == 31)),
                           reads=[uf_k, wd_k], writes=[acc_k])
                if fc == 3:
                    issue_next()

            cur = ffn_up(0)
            for i in range(32):
                nxt = ffn_up(i + 1) if i + 1 < 32 else None
                ffn_down(i, cur)
                cur = nxt
            for t2 in range(2):
                h1, h1_k, tb = h1s[t2]
                for half in range(2):
                    acc, acc_k = accs[t2][half]
                    op("dve", lambda e, acc=acc, h1=h1, half=half: e.tensor_tensor(out=h1[:, half * 512:(half + 1) * 512], in0=acc[:, :],
                                                                                 in1=h1[:, half * 512:(half + 1) * 512], op=ALU.add),
                       reads=[acc_k, h1_k], writes=[h1_k])
                st, st_k = st_pool.get()
                jk, jk_k = jk_pool.get()
                op("act", lambda e, jk=jk, h1=h1, st=st: e.activation(out=jk[:, :], in_=h1[:, 0:512], func=AF.Square, accum_out=st[:, 0:1]),
                   reads=[h1_k], writes=[jk_k, (st_k, 0)])
                op("act", lambda e, jk=jk, h1=h1, st=st: e.activation(out=jk[:, :], in_=h1[:, 512:1024], func=AF.Square, accum_out=st[:, 1:2]),
                   reads=[h1_k], writes=[jk_k, (st_k, 1)])
                op("dve", lambda e, st=st: e.tensor_tensor(out=st[:, 2:3], in0=st[:, 0:1], in1=st[:, 1:2], op=ALU.add), reads=[(st_k, 0), (st_k, 1)], writes=[(st_k, 2)])
                op("dve", lambda e, st=st: e.tensor_scalar(out=st[:, 3:4], in0=st[:, 2:3], scalar1=1.0 / D, scalar2=EPS, op0=ALU.mult, op1=ALU.add),
                   reads=[(st_k, 2)], writes=[(st_k, 3)])
                op("act", lambda e, st=st: e.activation(out=st[:, 4:5], in_=st[:, 3:4], func=AF.Sqrt), reads=[(st_k, 3)], writes=[(st_k, 4)])
                op("dve", lambda e, st=st: e.reciprocal(out=st[:, 5:6], in_=st[:, 4:5]), reads=[(st_k, 4)], writes=[(st_k, 5)])
                op("dve", lambda e, h1=h1, st=st: e.scalar_tensor_tensor(out=h1[:, :], in0=h1[:, :], scalar=st[:, 5:6], in1=gF[:, :], op0=ALU.mult, op1=ALU.mult),
                   reads=[h1_k, (st_k, 5), "gF"], writes=[h1_k])
                r0 = sl * 512 + tb * 128
                op("sp", lambda e, h1=h1, r0=r0: e.dma_start(out=out_d[r0:r0 + 128, :], in_=h1[:, :]), reads=[h1_k], writes=[("out", r0)], dma=True)

    if os.environ.get("KDEBUG"):
        print("sbuf remaining", nc.sbuf_bytes_remaining, "ops", {e: len(S_.ops[e]) for e in ENGS})
    with ExitStack() as stack:
        S_.emit(nc, stack)
    return nc


def _rope_tab(pos):
    pos = np.asarray(pos, np.float32)
    out = np.zeros((len(pos), 512), np.float32)
    inv = (np.float32(500000.0) ** (-np.arange(0, 32, 2, dtype=np.float32) / np.float32(32))).astype(np.float32)
    ang = pos[:, None] * inv[None, :]
    c, s = np.cos(ang).astype(np.float32), np.sin(ang).astype(np.float32)
    out[:, 0:128] = np.tile(np.concatenate([c, c], 1), (1, 4))
    out[:, 128:256] = np.tile(np.concatenate([s, s], 1), (1, 4))
    inv = (np.float32(500000.0) ** (-np.arange(0, 16, 2, dtype=np.float32) / np.float32(16))).astype(np.float32)
    ang = pos[:, None] * inv[None, :]
    c, s = np.cos(ang).astype(np.float32), np.sin(ang).astype(np.float32)
    out[:, 256:384] = np.tile(np.concatenate([c, c], 1), (1, 8))
    out[:, 384:512] = np.tile(np.concatenate([s, s], 1), (1, 8))
    return out


def _chunk_of(sl, half):
    return 2 * sl + (sl % 2) if half == 0 else 2 * sl + 1 - (sl % 2)


_NC_CACHE = {}


def kernel(x, meta_tokens, attn_norm_g, w_in, conv_w, conv_b, conv_norm_g, conv_norm_b,
           w_out, mlp_norm_g, w_up, w_down, final_norm_g):
    x = np.asarray(x, np.float32)
    B, S, _ = x.shape
    T = NMETA + S
    NSLOT = S // 1024
    if S not in _NC_CACHE:
        _NC_CACHE[S] = build(S)
    nc = _NC_CACHE[S]
    f = lambda a: np.ascontiguousarray(np.asarray(a, np.float32))
    meta = f(meta_tokens)
    ropek = _rope_tab(np.arange(T))
    p = np.arange(128)
    consts = {
        "iota": np.tile(np.arange(512, dtype=np.float32)[None, :], (128, 1)),
        "pow2": np.tile((2.0 ** -(np.arange(NBIS) + 1.0)).astype(np.float32)[None, :], (128, 1)),
        "e32": (np.arange(32)[None, :] == (p % 32)[:, None]).astype(np.float32),
        "g4": (np.arange(4)[None, :] == (p // 32)[:, None]).astype(np.float32),
        "dm": np.concatenate([(np.arange(128)[None, :] == (32 * g + p % 32)[:, None]).astype(np.float32) for g in range(4)], axis=1),
    }
    shared = {
        "w_in": f(w_in[0]), "w_out": f(w_out[0]), "w_up": f(w_up[0]), "w_down": f(w_down[0]),
        "attn_g": f(attn_norm_g[0]), "mlp_g": f(mlp_norm_g[0]), "final_g": f(final_norm_g),
        "conv_w": f(conv_w[0]), "conv_b": f(conv_b[0]), "cn_g": f(conv_norm_g[0]), "cn_b": f(conv_norm_b[0]),
        "ropek": ropek,
    }
    shared.update(consts)
    in_maps = []
    for core in range(8):
        b, half = core // 2, core % 2
        hall = np.concatenate([meta, x[b]], axis=0)
        xown = np.zeros((NSLOT, 544, D), np.float32)
        ropeq = np.zeros((NSLOT, 512, 512), np.float32)
        qrel = np.zeros((128, NSLOT * 8), np.float32)
        for sl in range(NSLOT):
            c = _chunk_of(sl, half)
            p0 = NMETA + 512 * c
            lo = p0 - 32
            src_lo = max(lo, 0)
            xown[sl, src_lo - lo:, :] = hall[src_lo:p0 + 512]
            ropeq[sl] = ropek[p0:p0 + 512]
            w0 = NMETA + 1024 * sl
            for tb in range(4):
                for wc in range(2):
                    qrel[:, (sl * 4 + tb) * 2 + wc] = (p0 + tb * 128 + p) - w0 - 512 * wc
        m = dict(shared)
        m.update({"xall": hall, "xown": xown, "ropeq": ropeq, "qrel": qrel})
        in_maps.append(m)
    res = run_bass_kernel_spmd(nc, in_maps, core_ids=list(range(8)))
    out = np.zeros((B, S, D), np.float32)
    for core in range(8):
        b, half = core // 2, core % 2
        o = res.results[core]["out"]
        for sl in range(NSLOT):
            c = _chunk_of(sl, half)
            out[b, 512 * c:512 * (c + 1)] = o[sl * 512:(sl + 1) * 512]
    return out
```

```python
import numpy as np
from contextlib import ExitStack
import concourse.bass as bass
import concourse.mybir as mybir
from concourse.bass_utils import run_bass_kernel_spmd

F32 = mybir.dt.float32
BF16 = mybir.dt.bfloat16
ALU = mybir.AluOpType
AF = mybir.ActivationFunctionType
AX = mybir.AxisListType

D = 1024
NMETA = 16
DIN = 3144
DFF = 4096
EPS = 1e-5
KTOP = 256
NBIS = 20
ENGS = ("pe", "act", "dve", "pool", "sp")
SEM_CH = 30000
NDMA_SEM = 12


class Op:
    __slots__ = ("eng", "fn", "deps", "dma", "signals", "sig", "dma_idx")


class Sched:
    def __init__(self):
        self.ops = {e: [] for e in ENGS}
        self.lastw = {}
        self.readers = {}
        self.ndma = {e: 0 for e in ENGS}

    def op(self, eng, fn, reads=(), writes=(), dma=False):
        o = Op()
        o.eng = eng
        o.fn = fn
        o.dma = dma
        o.signals = False
        o.sig = None
        deps = {}
        xr = [k for k in reads if isinstance(k, tuple) and k[0] in ("pg", "psc", "pacc")]
        if xr:
            writes = list(writes) + [k for k in xr if k not in writes]
        for k in reads:
            w = self.lastw.get(k)
            if w is not None:
                deps[id(w)] = (w, True)
        for k in writes:
            w = self.lastw.get(k)
            if w is not None and id(w) not in deps:
                deps[id(w)] = (w, False)
            for r in self.readers.get(k, ()):
                if id(r) not in deps:
                    deps[id(r)] = (r, False)
        o.deps = []
        for d, raw in deps.values():
            if d.eng == eng and not d.dma:
                if eng == "pe":
                    continue
                if not raw and not dma:
                    continue
            d.signals = True
            o.deps.append(d)
        if dma:
            o.dma_idx = self.ndma[eng]
            self.ndma[eng] += 1
            o.signals = True
        for k in reads:
            self.readers.setdefault(k, []).append(o)
        for k in writes:
            self.lastw[k] = o
            self.readers[k] = []
        self.ops[eng].append(o)
        return o

    def emit(self, nc, stack):
        nsig = {}
        for e in ENGS:
            n = 0
            for o in self.ops[e]:
                if o.signals and not o.dma:
                    o.sig = n
                    n += 1
            nsig[e] = n
        csem = {}
        for e in ENGS:
            nch = (nsig[e] + SEM_CH - 1) // SEM_CH
            csem[e] = [stack.enter_context(nc.semaphore(f"c_{e}_{i}")) for i in range(nch)]
        dsem = {}
        for e in ENGS:
            if self.ndma[e]:
                dsem[e] = [stack.enter_context(nc.semaphore(f"d_{e}_{i}")) for i in range(NDMA_SEM)]

        def target(d):
            if d.dma:
                return dsem[d.eng][d.dma_idx % NDMA_SEM], 16 * (d.dma_idx // NDMA_SEM + 1)
            return csem[d.eng][d.sig // SEM_CH], d.sig % SEM_CH + 1

        block = stack.enter_context(nc.Block())

        def run(e):
            def body(eng):
                waited = {}
                for o in self.ops[e]:
                    tg = [target(d) for d in o.deps]
                    if o.dma and o.dma_idx >= NDMA_SEM:
                        tg.append((dsem[e][o.dma_idx % NDMA_SEM], 16 * (o.dma_idx // NDMA_SEM)))
                    for s, v in tg:
                        key = id(s)
                        if waited.get(key, 0) < v:
                            eng.wait_ge(s, v)
                            waited[key] = v
                    ins = o.fn(eng)
                    if o.signals:
                        if o.dma:
                            s, _ = target(o)
                            ins.then_inc(s, 16)
                        else:
                            ins.then_inc(csem[e][o.sig // SEM_CH], 1)
                if self.ndma[e]:
                    n = self.ndma[e]
                    for i in range(max(0, n - NDMA_SEM), n):
                        eng.wait_ge(dsem[e][i % NDMA_SEM], 16 * (i // NDMA_SEM + 1))
            return body

        for e, reg in (("pe", block.tensor), ("act", block.scalar), ("dve", block.vector),
                       ("pool", block.gpsimd), ("sp", block.sync)):
            if self.ops[e]:
                reg(run(e))


class Pool:
    def __init__(self, tiles, name, off=0):
        self.tiles = tiles
        self.name = name
        self.i = 0
        self.off = off

    def get(self):
        j = self.i % len(self.tiles)
        self.i += 1
        return self.tiles[j], (self.name, j + self.off)


def build(S):
    T = NMETA + S
    NCH = S // 512
    NSLOT = NCH // 2
    NKT = 1 + S // 128
    nc = bass.Bass("TRN2", target_bir_lowering=False)

    def din(name, shape, dt=F32):
        return nc.dram_tensor(name, shape, dt, kind="ExternalInput").ap()

    xall = din("xall", [T, D])
    xown = din("xown", [NSLOT, 544, D])
    ropek = din("ropek", [T, 512])
    ropeq = din("ropeq", [NSLOT, 512, 512])
    qrel_d = din("qrel", [128, NSLOT * 8])
    w_in = din("w_in", [D, DIN])
    w_out = din("w_out", [D, D])
    w_up = din("w_up", [D, DFF])
    w_down = din("w_down", [DFF, D])
    attn_g = din("attn_g", [D])
    mlp_g = din("mlp_g", [D])
    final_g = din("final_g", [D])
    conv_w = din("conv_w", [31, 512])
    conv_b = din("conv_b", [512])
    cn_g = din("cn_g", [512])
    cn_b = din("cn_b", [512])
    iota_d = din("iota", [128, 512])
    pow2_d = din("pow2", [128, NBIS])
    e32_d = din("e32", [128, 32])
    g4_d = din("g4", [128, 4])
    dm_d = din("dm", [128, 4 * 128])
    out_d = nc.dram_tensor("out", [NSLOT * 512, D], F32, kind="ExternalOutput").ap()

    k_scr = nc.dram_tensor("k_scr", [4, 128, T], BF16).ap()
    v_scr = nc.dram_tensor("v_scr", [T, 4, 129], BF16).ap()

    import os
    KSTOP = float(os.environ.get('KSTOP', '99'))
    S_ = Sched()
    stage = [0]

    def op(*a, **k):
        if stage[0] <= KSTOP:
            return S_.op(*a, **k)
        return None
    uid = [0]

    def sb(shape, dt, name=None):
        uid[0] += 1
        return nc.alloc_sbuf_tensor(f"{name or 't'}{uid[0]}", shape, dt)

    def mkpool(n, shape, dt, name):
        return Pool([sb(shape, dt, name) for _ in range(n)], name)

    identf = sb([128, 128], F32, "identf")
    ident = sb([128, 128], BF16, "ident")
    op("pool", lambda e: e.memset(identf[:], 0.0), writes=["identf"])
    op("pool", lambda e: e.affine_select(out=identf[:], in_=identf[:], pattern=[[-1, 128]],
                                         compare_op=ALU.not_equal, fill=1.0, base=0, channel_multiplier=1),
       reads=["identf"], writes=["identf"])
    op("dve", lambda e: e.tensor_copy(out=ident[:], in_=identf[:]), reads=["identf"], writes=["ident"])

    def load_const(name, shape, src, dt=F32):
        t = sb(shape, dt, name)
        op("sp", lambda e: e.dma_start(out=t[:], in_=src, allow_slow_non_contiguous=True), writes=[name], dma=True)
        return t

    iota = load_const("iota", [128, 512], iota_d)
    pow2 = load_const("pow2", [128, NBIS], pow2_d)
    e32 = load_const("e32", [128, 32], e32_d)
    g4 = load_const("g4", [128, 4], g4_d)
    qrel = load_const("qrelc", [128, NSLOT * 8], qrel_d)
    gA = load_const("gA", [128, 8], attn_g.rearrange("(c p) -> p c", p=128))
    gM = load_const("gM", [128, 8], mlp_g.rearrange("(c p) -> p c", p=128))
    def bcast_row(name, src, n):
        t = sb([128, n], F32, name)
        op("sp", lambda e: e.dma_start(out=t[:], in_=src.partition_broadcast(128)), writes=[name], dma=True)
        return t
    gF = bcast_row("gF", final_g, D)
    cb_bc = bcast_row("cb_bc", conv_b, 512)
    cg_bc = bcast_row("cg_bc", cn_g, 512)
    cnb_bc = bcast_row("cnb_bc", cn_b, 512)
    cwT = sb([128, 4, 31], F32, "cwT")
    for cc_ in range(4):
        op("sp", lambda e, cc_=cc_: e.dma_start(out=cwT[:, cc_, :], in_=conv_w[:, cc_ * 128:(cc_ + 1) * 128].rearrange("j p -> p j"),
                                                 allow_slow_non_contiguous=True), writes=["cwT"], dma=True)
    dmb = sb([128, 512], BF16, "dmb")
    op("pool", lambda e: e.dma_start(out=dmb[:], in_=dm_d), writes=["dmb"], dma=True)
    wik_t = sb([128, 8, 64], BF16, "wik")
    op("pool", lambda e: e.dma_start(out=wik_t[:, :, :], in_=w_in[:, 2048:2112].rearrange("(c p) n -> p c n", p=128)), writes=["wik"], dma=True)
    wiw_t = sb([128, 8, 8], BF16, "wiw")
    op("pool", lambda e: e.dma_start(out=wiw_t[:, :, :], in_=w_in[:, 2112:2120].rearrange("(c p) n -> p c n", p=128), allow_slow_non_contiguous=True),
       writes=["wiw"], dma=True)

    win_b = nc.dram_tensor("win_b", [D, DIN], BF16).ap()
    wout_b = nc.dram_tensor("wout_b", [D, D], BF16).ap()
    wup_b = nc.dram_tensor("wup_b", [D, DFF], BF16).ap()
    wdown_b = nc.dram_tensor("wdown_b", [DFF, D], BF16).ap()

    ikT = sb([128, T], BF16, "ikT")
    scores = sb([128, T], F32, "scores")
    wbuf = mkpool(4, [128, 8, 512], BF16, "wbuf")
    xt_pool = mkpool(3, [128, D], F32, "xt")
    xn_pool = mkpool(1, [128, D], BF16, "xn")
    xnT_pool = mkpool(2, [128, 8, 128], BF16, "xnT")
    rope_pool = mkpool(4, [128, 256], F32, "rope")
    st_pool = mkpool(4, [128, 8], F32, "stat")
    tm_pool = mkpool(3, [128, 512], BF16, "tm")
    rt_pool = mkpool(2, [128, 256], F32, "rt")
    jk_pool = mkpool(2, [128, 512], F32, "jk")
    bigjunk = sb([128, NMETA + 512 * ((T // 512) // 2 + 1)], mybir.dt.uint8, "bigjunk")
    vb_pool = mkpool(2, [128, 4, 129], BF16, "vb")
    for vbt_ in vb_pool.tiles:
        pass
    pg = Pool([nc.alloc_psum_tensor(f"pg{i}", [128, 512], F32) for i in range(4)], "pg")
    psc = [nc.alloc_psum_tensor(f"psc{i}", [128, 512], F32) for i in range(2)]
    pacc = [nc.alloc_psum_tensor(f"pacc{i}", [128, 512], F32) for i in range(2)]

    def pg_bf16(t):
        return t.bitcast(BF16) if hasattr(t, "bitcast") else None

    def rmsnorm_T(x_t, x_k, rows, gcol, xnT, xnT_k):
        st, st_k = st_pool.get()
        jk, jk_k = jk_pool.get()
        xn, xn_k = xn_pool.get()
        op("act", lambda e: e.activation(out=jk[0:rows, :], in_=x_t[0:rows, 0:512], func=AF.Square,
                                         accum_out=st[0:rows, 0:1]), reads=[x_k], writes=[jk_k, (st_k, 0)])
        op("act", lambda e: e.activation(out=jk[0:rows, :], in_=x_t[0:rows, 512:1024], func=AF.Square,
                                         accum_out=st[0:rows, 1:2]), reads=[x_k], writes=[jk_k, (st_k, 1)])
        op("dve", lambda e: e.tensor_tensor(out=st[0:rows, 2:3], in0=st[0:rows, 0:1], in1=st[0:rows, 1:2], op=ALU.add),
           reads=[(st_k, 0), (st_k, 1)], writes=[(st_k, 2)])
        op("dve", lambda e: e.tensor_scalar(out=st[0:rows, 3:4], in0=st[0:rows, 2:3], scalar1=1.0 / D, scalar2=EPS,
                                            op0=ALU.mult, op1=ALU.add), reads=[(st_k, 2)], writes=[(st_k, 3)])
        op("act", lambda e: e.activation(out=st[0:rows, 4:5], in_=st[0:rows, 3:4], func=AF.Sqrt),
           reads=[(st_k, 3)], writes=[(st_k, 4)])
        op("dve", lambda e: e.reciprocal(out=st[0:rows, 5:6], in_=st[0:rows, 4:5]), reads=[(st_k, 4)], writes=[(st_k, 5)])
        op("dve", lambda e: e.tensor_scalar(out=xn[0:rows, :], in0=x_t[0:rows, :], scalar1=st[0:rows, 5:6], scalar2=None,
                                            op0=ALU.mult), reads=[x_k, (st_k, 5)], writes=[xn_k])
        pt, pt_k = pg.get()
        ptb = pt[:].bitcast(BF16)
        for dc in range(8):
            op("pe", lambda e, dc=dc: e.transpose(out=ptb[:, dc * 128:dc * 128 + rows], in_=xn[0:rows, dc * 128:(dc + 1) * 128],
                                                  identity=ident[0:rows, 0:rows]),
               reads=[xn_k, "ident"], writes=[pt_k])
        op("dve", lambda e: e.tensor_tensor(out=xnT[:, :, 0:rows],
                                            in0=ptb.rearrange("p (c t) -> p c t", c=8)[:, :, 0:rows],
                                            in1=gcol[:, :].unsqueeze(2).to_broadcast([128, 8, rows]), op=ALU.mult),
           reads=[pt_k], writes=[xnT_k])
        return (st, st_k)

    def wviews(kind, i, w):
        if kind == "down":
            v = w[:, :, :].rearrange("p a b -> p (a b)").rearrange("p (f n) -> p f n", f=4)
            r = lambda a: a[i * 512:(i + 1) * 512, :].rearrange("(f p) n -> p f n", p=128)
            return v, r(w_down), r(wdown_b)
        src, dst = {"in": (w_in, win_b), "out": (w_out, wout_b), "up": (w_up, wup_b)}[kind]
        r = lambda a: a[:, i:i + 512].rearrange("(c p) n -> p c n", p=128) if kind == "in" else a[:, i * 512:(i + 1) * 512].rearrange("(c p) n -> p c n", p=128)
        return w[:, :, :], r(src), r(dst)

    def load_w(kind, i, cast=False):
        w, w_k = wbuf.get()
        v, src, scr = wviews(kind, i, w)
        if cast:
            op("pool", lambda e: e.dma_start(out=v, in_=src), writes=[w_k], dma=True)
        else:
            op("sp", lambda e: e.dma_start(out=v, in_=scr), reads=[("wcv", kind, i)], writes=[w_k], dma=True)
        return (v if kind == "down" else w), w_k

    def proj_tm(xnT, xnT_k, rows, w, w_k, ncols, ps, ps_k, pcol=0):
        for dc in range(8):
            op("pe", lambda e, dc=dc: e.matmul(ps[0:rows, pcol:pcol + ncols], lhsT=xnT[:, dc, 0:rows], rhs=w[:, dc, 0:ncols],
                                               start=(dc == 0), stop=(dc == 7)),
               reads=[xnT_k, w_k], writes=[ps_k])


    def rope_tm(ps, ps_k, rows, nh, hd, half, rp, rp_k, roff, dst, dst_k, dcol=0):
        r2 = 2 * half
        n = nh * hd
        xf, xf_k = jk_pool.get()
        op("act", lambda e: e.copy(out=xf[0:rows, 0:n], in_=ps[0:rows, 0:n]), reads=[ps_k], writes=[xf_k])
        op("act", lambda e: e.copy(out=dst[0:rows, dcol:dcol + n], in_=ps[0:rows, 0:n]), reads=[ps_k], writes=[dst_k])
        rt, rt_k = rt_pool.get()
        pv = xf[0:rows, 0:n].rearrange("p (h d) -> p h d", h=nh)
        A = rt[0:rows, 0:nh * r2].rearrange("p (h d) -> p h d", h=nh)
        Bm = rt[0:rows, 128:128 + nh * r2].rearrange("p (h d) -> p h d", h=nh)
        cc_ = rp[0:rows, roff:roff + nh * r2].rearrange("p (h d) -> p h d", h=nh)
        ss_ = rp[0:rows, roff + 128:roff + 128 + nh * r2].rearrange("p (h d) -> p h d", h=nh)
        op("dve", lambda e: e.tensor_tensor(out=A, in0=pv[:, :, 0:r2], in1=cc_, op=ALU.mult), reads=[xf_k, rp_k], writes=[(rt_k, 0)])
        op("dve", lambda e: e.tensor_tensor(out=Bm, in0=pv[:, :, 0:r2], in1=ss_, op=ALU.mult), reads=[xf_k, rp_k], writes=[(rt_k, 1)])
        dv = dst[0:rows, dcol:dcol + n].rearrange("p (h d) -> p h d", h=nh)
        op("dve", lambda e: e.tensor_tensor(out=dv[:, :, 0:half], in0=A[:, :, 0:half], in1=Bm[:, :, half:r2], op=ALU.subtract),
           reads=[(rt_k, 0), (rt_k, 1), dst_k], writes=[dst_k])
        op("dve", lambda e: e.tensor_tensor(out=dv[:, :, half:r2], in0=A[:, :, half:r2], in1=Bm[:, :, 0:half], op=ALU.add),
           reads=[(rt_k, 0), (rt_k, 1), dst_k], writes=[dst_k])

    stage[0] = 1
    for j_, vbt_ in enumerate(vb_pool.tiles):
        op("pool", lambda e, vbt_=vbt_: e.memset(vbt_[:, :, 128:129], 1.0), writes=[("vbones", ("vb", j_))])
    wk, wk_k = load_w("in", 512, cast=True)
    wv, wv_k = load_w("in", 1024, cast=True)
    cvt_pool = Pool(wbuf.tiles[2:4], "wbuf", off=2)
    for kind, idxs in (("in", (0, 1536, 2120, 2632)), ("out", (0, 1)), ("up", tuple(range(8))), ("down", tuple(range(8)))):
        for i in idxs:
            w, w_k = cvt_pool.get()
            v, src, scr = wviews(kind, i, w)
            op("pool", lambda e, v=v, src=src: e.dma_start(out=v, in_=src), writes=[w_k], dma=True)
            op("pool", lambda e, v=v, scr=scr: e.dma_start(out=scr, in_=v), reads=[w_k], writes=[("wcv", kind, i)], dma=True)
    wbuf.i = 2
    wik, wik_k = wik_t, "wik"
    stage[0] = 1
    def p1_tile(kt):
        rows = NMETA if kt == 0 else 128
        p0 = 0 if kt == 0 else NMETA + 128 * (kt - 1)
        xt, xt_k = xt_pool.get()
        op("sp", lambda e, xt=xt, p0=p0, rows=rows: e.dma_start(out=xt[0:rows, :], in_=xall[p0:p0 + rows, :]), writes=[xt_k], dma=True)
        rp, rp_k = rope_pool.get()
        op("sp", lambda e, rp=rp, p0=p0, rows=rows: e.dma_start(out=rp[0:rows, :], in_=ropek[p0:p0 + rows, 0:256]), writes=[rp_k], dma=True)
        rpi, rpi_k = rope_pool.get()
        op("sp", lambda e, rpi=rpi, p0=p0, rows=rows: e.dma_start(out=rpi[0:rows, :], in_=ropek[p0:p0 + rows, 256:512]), writes=[rpi_k], dma=True)
        xnT, xnT_k = xnT_pool.get()
        rmsnorm_T(xt, xt_k, rows, gA, xnT, xnT_k)
        yield
        ps, ps_k = pg.get()
        proj_tm(xnT, xnT_k, rows, wk, wk_k, 512, ps, ps_k)
        kb, kb_k = tm_pool.get()
        rope_tm(ps, ps_k, rows, 4, 128, 16, rp, rp_k, 0, kb, kb_k)
        pt, pt_k = pg.get()
        ptb = pt[:].bitcast(BF16)
        for h in range(4):
            op("pe", lambda e, h=h, ptb=ptb, kb=kb, rows=rows: e.transpose(out=ptb[:, h * 128:h * 128 + rows], in_=kb[0:rows, h * 128:(h + 1) * 128],
                                                                         identity=ident[0:rows, 0:rows]), reads=[kb_k, "ident"], writes=[pt_k])
        kTt, kTt_k = tm_pool.get()
        op("act", lambda e, kTt=kTt, ptb=ptb: e.copy(out=kTt[:, :], in_=ptb[:, 0:512]), reads=[pt_k], writes=[kTt_k])
        op("sp", lambda e, kTt=kTt, p0=p0, rows=rows: e.dma_start(
            out=k_scr[:, :, p0:p0 + rows].rearrange("h p t -> p h t"),
            in_=kTt[:, :].rearrange("p (h t) -> p h t", h=4)[:, :, 0:rows]),
           reads=[kTt_k], writes=[("k_scr", kt)], dma=True)
        ps, ps_k = pg.get()
        proj_tm(xnT, xnT_k, rows, wv, wv_k, 512, ps, ps_k)
        vb, vb_k = vb_pool.get()
        op("act", lambda e, vb=vb, ps=ps, rows=rows: e.copy(out=vb[0:rows, :, 0:128], in_=ps[0:rows, :].rearrange("p (h d) -> p h d", h=4)),
           reads=[ps_k, ("vbones", vb_k)], writes=[vb_k])
        op("sp", lambda e, vb=vb, p0=p0, rows=rows: e.dma_start(out=v_scr[p0:p0 + rows, :, :], in_=vb[0:rows, :, :]),
           reads=[vb_k, ("vbones", vb_k)], writes=[("v_scr", kt)], dma=True)
        ps, ps_k = pg.get()
        proj_tm(xnT, xnT_k, rows, wik, wik_k, 64, ps, ps_k)
        ib, ib_k = tm_pool.get()
        rope_tm(ps, ps_k, rows, 1, 64, 8, rpi, rpi_k, 0, ib, ib_k)
        op("dve", lambda e, ib=ib, rows=rows: e.tensor_copy(out=ib[0:rows, 64:128], in_=ib[0:rows, 0:64]), reads=[ib_k], writes=[ib_k])
        pt, pt_k = pg.get()
        ptb = pt[:].bitcast(BF16)
        op("pe", lambda e, ptb=ptb, ib=ib, rows=rows: e.transpose(out=ptb[:, 0:rows], in_=ib[0:rows, 0:128], identity=ident[0:rows, 0:rows]),
           reads=[ib_k, "ident"], writes=[pt_k])
        op("act", lambda e, ptb=ptb, p0=p0, rows=rows: e.copy(out=ikT[:, p0:p0 + rows], in_=ptb[:, 0:rows]), reads=[pt_k], writes=[("ikT", kt)])

    def run_pipelined(gens):
        if gens:
            next(gens[0])
        for i in range(len(gens)):
            if i + 1 < len(gens):
                next(gens[i + 1])
            for _ in gens[i]:
                pass

    run_pipelined([p1_tile(kt) for kt in range(NKT)])

    stage[0] = 2
    qT = sb([128, 4, 512], BF16, "qT")
    iqT = sb([128, 4, 512], BF16, "iqT")
    iwt = sb([128, 4, 8], F32, "iwt")
    uT = sb([128, 4, 544], BF16, "uT")
    mixed = sb([128, 4, D], BF16, "mixed")
    ysb = sb([128, 4, 512], F32, "ysb")
    Dcc = sb([128, 31, 128], BF16, "Dcc")
    negm_pool = mkpool(2, [128, 512], F32, "negm")
    wabs2 = sb([128, 8], F32, "wabs2")
    wsgn = sb([128, 8], F32, "wsgn")
    bis = sb([128, 8 + NBIS], F32, "bis")
    top8 = sb([128, 8], F32, "top8")
    cst = sb([128, 4, 6], F32, "cst")
    cmv = sb([128, 4, 2], F32, "cmv")
    crs = sb([128, 4], F32, "crs")
    R_pool = mkpool(3, [128, 512], BF16, "R")
    PT_pool = mkpool(2, [128, 512], BF16, "PT")
    mb_pool = mkpool(2, [128, 512], BF16, "mb")
    GK = 4
    kv_pool = [(sb([128, 4, GK * 128], BF16, "kbuf"), sb([128, GK, 4, 129], BF16, "vbuf")) for _ in range(2)]
    kmeta = sb([128, 4, NMETA], BF16, "kmeta")
    vmeta = sb([128, 4, 129], BF16, "vmeta")
    op("sp", lambda e: e.dma_start(out=kmeta[:, :, :], in_=k_scr[:, :, 0:NMETA].rearrange("h p t -> p h t")),
       reads=[("k_scr", 0)], writes=["kmeta"], dma=True)
    op("sp", lambda e: e.dma_start(out=vmeta[0:NMETA, :, :], in_=v_scr[0:NMETA, :, :]),
       reads=[("v_scr", 0)], writes=["vmeta"], dma=True)
    mT = sb([128, 8, 128], BF16, "mT")
    hnT = sb([128, 8, 256], BF16, "hnT")
    uf_pool = mkpool(2, [128, 256], BF16, "uf")
    rden = sb([128, 4], F32, "rden")

    SC = 0.125 * (8 ** -0.5)
    kv_ctr = [0]

    for sl in range(NSLOT):
        E = NMETA + 512 * (2 * sl + 2)
        W0 = E - 1024
        NCHK = 2 * sl + 2
        wq, wq_k = load_w("in", 0)
        wiq, wiq_k = load_w("in", 1536)
        wga, wga_k = load_w("in", 2120)
        wgg, wgg_k = load_w("in", 2632)
        plan = []
        for pair_ in range(2):
            plan += [("out", 0), ("out", 1)]
            for fg_ in range(8):
                plan += [("up", fg_), ("down", fg_)]
        wfifo = []

        def issue_next():
            if plan:
                kind_, i_ = plan.pop(0)
                wfifo.append(load_w(kind_, i_))
        def sa_tile(ti, sl=sl, wq=wq, wq_k=wq_k, wiq=wiq, wiq_k=wiq_k, wga=wga, wga_k=wga_k, wgg=wgg, wgg_k=wgg_k):
            rows = 32 if ti == 0 else 128
            r0 = 0 if ti == 0 else 32 + 128 * (ti - 1)
            xt, xt_k = xt_pool.get()
            op("sp", lambda e, xt=xt, r0=r0, rows=rows, sl=sl: e.dma_start(out=xt[0:rows, :], in_=xown[sl, r0:r0 + rows, :]),
               writes=[xt_k], dma=True)
            xnT, xnT_k = xnT_pool.get()
            rmsnorm_T(xt, xt_k, rows, gA, xnT, xnT_k)
            if ti > 0:
                tb = ti - 1
                rp, rp_k = rope_pool.get()
                op("sp", lambda e, rp=rp, tb=tb, sl=sl: e.dma_start(out=rp[:, :], in_=ropeq[sl, 128 * tb:128 * (tb + 1), 0:256]), writes=[rp_k], dma=True)
                rpi, rpi_k = rope_pool.get()
                op("sp", lambda e, rpi=rpi, tb=tb, sl=sl: e.dma_start(out=rpi[:, :], in_=ropeq[sl, 128 * tb:128 * (tb + 1), 256:512]), writes=[rpi_k], dma=True)
            yield
            if ti > 0:
                ps, ps_k = pg.get()
                proj_tm(xnT, xnT_k, 128, wq, wq_k, 512, ps, ps_k)
                qb, qb_k = tm_pool.get()
                rope_tm(ps, ps_k, 128, 4, 128, 16, rp, rp_k, 0, qb, qb_k)
                pt, pt_k = pg.get()
                ptb = pt[:].bitcast(BF16)
                for h in range(4):
                    op("pe", lambda e, h=h, ptb=ptb, qb=qb: e.transpose(out=ptb[:, h * 128:(h + 1) * 128], in_=qb[:, h * 128:(h + 1) * 128], identity=ident[:, :]),
                       reads=[qb_k, "ident"], writes=[pt_k])
                op("act", lambda e, ptb=ptb, tb=tb: e.copy(out=qT[:, :, tb * 128:(tb + 1) * 128], in_=ptb[:, 0:512].rearrange("p (h t) -> p h t", h=4)),
                   reads=[pt_k], writes=[("qT", tb)])
                ps, ps_k = pg.get()
                proj_tm(xnT, xnT_k, 128, wiq, wiq_k, 512, ps, ps_k)
                ib, ib_k = tm_pool.get()
                rope_tm(ps, ps_k, 128, 8, 64, 8, rpi, rpi_k, 0, ib, ib_k)
                pt, pt_k = pg.get()
                ptb = pt[:].bitcast(BF16)
                for hp in range(4):
                    op("pe", lambda e, hp=hp, ptb=ptb, ib=ib: e.transpose(out=ptb[:, hp * 128:(hp + 1) * 128], in_=ib[:, hp * 128:(hp + 1) * 128], identity=ident[:, :]),
                       reads=[ib_k, "ident"], writes=[pt_k])
                op("act", lambda e, ptb=ptb, tb=tb: e.copy(out=iqT[:, :, tb * 128:(tb + 1) * 128], in_=ptb[:, 0:512].rearrange("p (h t) -> p h t", h=4)),
                   reads=[pt_k], writes=[("iqT", tb)])
                ps, ps_k = pg.get()
                proj_tm(xnT, xnT_k, 128, wiw_t, "wiw", 8, ps, ps_k)
                op("dve", lambda e, ps=ps, tb=tb: e.tensor_scalar(out=iwt[:, tb, :], in0=ps[:, 0:8], scalar1=SC, scalar2=None, op0=ALU.mult),
                   reads=[ps_k], writes=[("iwt", tb)])
            psa, psa_k = pg.get()
            proj_tm(xnT, xnT_k, rows, wga, wga_k, 512, psa, psa_k)
            psg, psg_k = pg.get()
            proj_tm(xnT, xnT_k, rows, wgg, wgg_k, 512, psg, psg_k)
            jk, jk_k = jk_pool.get()
            op("act", lambda e, jk=jk, psg=psg, rows=rows: e.activation(out=jk[0:rows, :], in_=psg[0:rows, :], func=AF.Sigmoid),
               reads=[psg_k], writes=[jk_k])
            ub, ub_k = tm_pool.get()
            op("dve", lambda e, ub=ub, psa=psa, jk=jk, rows=rows: e.tensor_tensor(out=ub[0:rows, :], in0=psa[0:rows, :], in1=jk[0:rows, :], op=ALU.mult),
               reads=[psa_k, jk_k], writes=[ub_k])
            pt, pt_k = pg.get()
            ptb = pt[:].bitcast(BF16)
            for cc in range(4):
                op("pe", lambda e, cc=cc, ptb=ptb, ub=ub, rows=rows: e.transpose(out=ptb[:, cc * 128:cc * 128 + rows], in_=ub[0:rows, cc * 128:(cc + 1) * 128],
                                                                              identity=ident[0:rows, 0:rows]),
                   reads=[ub_k, "ident"], writes=[pt_k])
            op("act", lambda e, ptb=ptb, r0=r0, rows=rows: e.copy(out=uT[:, :, r0:r0 + rows], in_=ptb[:, 0:512].rearrange("p (c t) -> p c t", c=4)[:, :, 0:rows]),
               reads=[pt_k], writes=[("uT", ti)])

        run_pipelined([sa_tile(ti) for ti in range(5)])
        stage[0] = 3
        UTK = [("uT", ti) for ti in range(5)]
        for cc in range(4):
            op("dve", lambda e, cc=cc: e.tensor_tensor(out=Dcc[:, :, :], in0=identf[:, :].unsqueeze(1).to_broadcast([128, 31, 128]),
                                                       in1=cwT[:, cc, :].unsqueeze(2).to_broadcast([128, 31, 128]), op=ALU.mult),
               reads=["identf", "cwT"], writes=["Dcc"])
            for tb in range(4):
                ps, ps_k = pg.get()
                for j in range(31):
                    c0 = 2 + tb * 128 + j
                    op("pe", lambda e, ps=ps, cc=cc, c0=c0, j=j: e.matmul(ps[:, 0:128], lhsT=uT[:, cc, c0:c0 + 128], rhs=Dcc[:, j, :], start=(j == 0), stop=(j == 30)),
                       reads=UTK + ["Dcc"], writes=[ps_k])
                op("act", lambda e, ps=ps, tb=tb, cc=cc: e.copy(out=ysb[:, tb, cc * 128:(cc + 1) * 128], in_=ps[:, 0:128]),
                   reads=[ps_k], writes=[("ysb", tb)])
        for tb in range(4):
            yk = ("ysb", tb)
            y = ysb[:, tb, :]
            op("dve", lambda e, y=y: e.tensor_tensor(out=y, in0=y, in1=cb_bc[:, :], op=ALU.add), reads=[yk, "cb_bc"], writes=[yk])
            for g in range(4):
                op("dve", lambda e, y=y, g=g: e.bn_stats(out=cst[:, g, :], in_=y[:, g * 128:(g + 1) * 128]), reads=[yk], writes=[("cst", g)])
                op("dve", lambda e, g=g: e.bn_aggr(out=cmv[:, g, :], in_=cst[:, g, :]), reads=[("cst", g)], writes=[("cmv", g)])
            CM = [("cmv", g) for g in range(4)]
            op("dve", lambda e: e.tensor_scalar(out=crs[:, :], in0=cmv[:, :, 1], scalar1=EPS, scalar2=None, op0=ALU.add), reads=CM, writes=["crs"])
            op("act", lambda e: e.activation(out=crs[:, :], in_=crs[:, :], func=AF.Sqrt), reads=["crs"], writes=["crs"])
            op("dve", lambda e: e.reciprocal(out=crs[:, :], in_=crs[:, :]), reads=["crs"], writes=["crs"])
            for g in range(4):
                op("dve", lambda e, y=y, g=g: e.tensor_scalar(out=y[:, g * 128:(g + 1) * 128], in0=y[:, g * 128:(g + 1) * 128],
                                                            scalar1=cmv[:, g, 0:1], scalar2=crs[:, g:g + 1], op0=ALU.subtract, op1=ALU.mult),
                   reads=[yk, "crs"] + CM, writes=[yk])
            op("dve", lambda e, y=y: e.tensor_tensor(out=y, in0=y, in1=cg_bc[:, :], op=ALU.mult), reads=[yk, "cg_bc"], writes=[yk])
            op("dve", lambda e, y=y: e.tensor_tensor(out=y, in0=y, in1=cnb_bc[:, :], op=ALU.add), reads=[yk, "cnb_bc"], writes=[yk])
            op("act", lambda e, y=y, tb=tb: e.activation(out=mixed[:, tb, 512:1024], in_=y, func=AF.Silu), reads=[yk], writes=[("mixC", tb)])

        stage[0] = 4
        for tb in range(4):
            if tb == 3:
                for _ in range(4):
                    issue_next()
            op("act", lambda e, tb=tb: e.activation(out=wabs2[:, :], in_=iwt[:, tb, :], func=AF.Abs, scale=2.0),
               reads=[("iwt", tb)], writes=["wabs2"])
            op("dve", lambda e, tb=tb: e.tensor_scalar(out=wsgn[:, :], in0=iwt[:, tb, :], scalar1=0.0, scalar2=0.5, op0=ALU.is_ge, op1=ALU.subtract),
               reads=[("iwt", tb)], writes=["wsgn"])
            chunks = [(0, NMETA)] + [(NMETA + 512 * m, 512) for m in range(NCHK)]
            SCK = []
            wcnt = 0
            for ci, (c0, cn) in enumerate(chunks):
                ikk = [("ikT", 0)] if ci == 0 else [("ikT", 1 + 4 * (ci - 1) + q) for q in range(4)]
                sk = ("sc", ci)
                SCK.append(sk)
                for h in range(8):
                    par, hp = h % 2, h // 2
                    ps, ps_k = pg.get()
                    op("pe", lambda e, ps=ps, par=par, hp=hp, tb=tb, c0=c0, cn=cn: e.matmul(ps[:, 0:cn], lhsT=iqT[par * 64:(par + 1) * 64, hp, tb * 128:(tb + 1) * 128],
                                                                                         rhs=ikT[par * 64:(par + 1) * 64, c0:c0 + cn], start=True, stop=True),
                       reads=[("iqT", tb)] + ikk, writes=[ps_k])
                    R, R_k = R_pool.get()
                    op("act", lambda e, R=R, ps=ps, cn=cn, h=h: e.activation(out=R[:, 0:cn], in_=ps[:, 0:cn], func=AF.Relu, scale=wabs2[:, h:h + 1]),
                       reads=[ps_k, "wabs2"], writes=[R_k])
                    if h == 0:
                        op("dve", lambda e, R=R, c0=c0, cn=cn: e.tensor_scalar(out=scores[:, c0:c0 + cn], in0=R[:, 0:cn], scalar1=wsgn[:, 0:1], scalar2=None, op0=ALU.mult),
                           reads=[R_k, "wsgn"], writes=[sk])
                    else:
                        op("dve", lambda e, R=R, c0=c0, cn=cn, h=h: e.scalar_tensor_tensor(out=scores[:, c0:c0 + cn], in0=R[:, 0:cn], scalar=wsgn[:, h:h + 1],
                                                                                         in1=scores[:, c0:c0 + cn], op0=ALU.mult, op1=ALU.add),
                           reads=[R_k, "wsgn", sk], writes=[sk])
                if c0 >= W0 and ci > 0:
                    col = (sl * 4 + tb) * 2 + wcnt
                    negm, negm_k = negm_pool.get()
                    op("dve", lambda e, col=col, negm=negm: e.tensor_scalar(out=negm[:, :], in0=iota[:, :], scalar1=qrel[:, col:col + 1], scalar2=-1e30,
                                                                          op0=ALU.is_gt, op1=ALU.mult), reads=["iota", "qrelc"], writes=[negm_k])
                    jk, jk_k = jk_pool.get()
                    op("dve", lambda e, jk=jk, c0=c0, negm=negm: e.tensor_tensor(out=jk[:, :], in0=scores[:, c0:c0 + 512], in1=negm[:, :], op=ALU.subtract),
                       reads=[sk, negm_k], writes=[jk_k])
                    op("dve", lambda e, jk=jk, wcnt=wcnt: e.tensor_reduce(out=bis[:, 5 + wcnt:6 + wcnt], in_=jk[:, :], axis=AX.X, op=ALU.min),
                       reads=[jk_k], writes=[("bis", 5 + wcnt)])
                    op("dve", lambda e, c0=c0, negm=negm: e.tensor_tensor(out=scores[:, c0:c0 + 512], in0=scores[:, c0:c0 + 512], in1=negm[:, :], op=ALU.add),
                       reads=[sk, negm_k], writes=[sk])
                    wcnt += 1
            assert wcnt == 2
            stage[0] = 5
            nD = max(1, int(round(0.45 * NCHK)))
            ED = NMETA + 512 * nD
            EA = E - ED
            SCK_D = SCK[0:1 + nD]
            SCK_A = SCK[1 + nD:]
            op("dve", lambda e, E=E: e.max(out=top8[:, :], in_=scores[:, 0:E]), reads=SCK, writes=["top8"])
            op("dve", lambda e, W0=W0: e.tensor_reduce(out=bis[:, 7:8], in_=scores[:, 0:W0], axis=AX.X, op=ALU.min), reads=SCK, writes=[("bis", 7)])
            op("dve", lambda e: e.tensor_reduce(out=bis[:, 0:1], in_=bis[:, 5:8], axis=AX.X, op=ALU.min),
               reads=[("bis", 5), ("bis", 6), ("bis", 7)], writes=[("bis", 0)])
            op("dve", lambda e: e.tensor_tensor(out=bis[:, 1:2], in0=top8[:, 0:1], in1=bis[:, 0:1], op=ALU.subtract),
               reads=["top8", ("bis", 0)], writes=[("bis", 1)])
            op("dve", lambda e: e.tensor_scalar(out=bis[:, 8:8 + NBIS], in0=pow2[:, :], scalar1=bis[:, 1:2], scalar2=None, op0=ALU.mult),
               reads=["pow2", ("bis", 1)], writes=["bisW"])
            op("dve", lambda e: e.tensor_tensor(out=bis[:, 2:3], in0=bis[:, 8:9], in1=bis[:, 0:1], op=ALU.add),
               reads=["bisW", ("bis", 0)], writes=[("bis", 2)])
            for k in range(NBIS):
                op("dve", lambda e, ED=ED: e.tensor_scalar(out=bigjunk[:, 0:ED], in0=scores[:, 0:ED], scalar1=bis[:, 2:3], scalar2=None,
                                                          op0=ALU.is_ge, op1=ALU.add, accum_out=bis[:, 3:4]),
                   reads=SCK_D + [("bis", 2)], writes=["bigjunkD", ("bis", 3)])
                op("act", lambda e, ED=ED, E=E, EA=EA: e.activation(out=bigjunk[:, 0:EA].bitcast(mybir.dt.int8), in_=scores[:, ED:E], func=AF.Sign,
                                                            bias=bis[:, 2:3], scale=-1.0, accum_out=bis[:, 5:6]),
                   reads=SCK_A + [("bis", 2)], writes=["bigjunkA", ("bis", 5)])
                op("dve", lambda e: e.scalar_tensor_tensor(out=bis[:, 6:7], in0=bis[:, 3:4], scalar=2.0, in1=bis[:, 5:6], op0=ALU.mult, op1=ALU.subtract),
                   reads=[("bis", 3), ("bis", 5)], writes=[("bis", 6)])
                op("dve", lambda e, k=k, EA=EA: e.tensor_scalar(out=bis[:, 4:5], in0=bis[:, 6:7], scalar1=float(2 * KTOP - 1 - EA), scalar2=bis[:, 8 + k:9 + k],
                                                               op0=ALU.is_ge, op1=ALU.mult), reads=[("bis", 6), "bisW"], writes=[("bis", 4)])
                if k < NBIS - 1:
                    op("dve", lambda e, k=k: e.scalar_tensor_tensor(out=bis[:, 2:3], in0=bis[:, 4:5], scalar=bis[:, 9 + k:10 + k], in1=bis[:, 2:3],
                                                                  op0=ALU.subtract, op1=ALU.add),
                       reads=[("bis", 4), "bisW", ("bis", 2)], writes=[("bis", 2)])
                else:
                    op("dve", lambda e, k=k: e.scalar_tensor_tensor(out=bis[:, 0:1], in0=bis[:, 4:5], scalar=bis[:, 8 + k:9 + k], in1=bis[:, 2:3],
                                                                  op0=ALU.subtract, op1=ALU.add),
                       reads=[("bis", 4), "bisW", ("bis", 2)], writes=[("bis", 0)])
            stage[0] = 6
            nkb_total = 1 + 4 * NCHK
            kblist = []
            for ci, (c0, cn) in enumerate(chunks):
                for kb in range(1 if ci == 0 else 4):
                    kblist.append((ci, c0, cn, kb))
            cstate = {}

            def chunk_setup(ci, c0, cn):
                mbt, mb_k = mb_pool.get()
                op("dve", lambda e, mbt=mbt, c0=c0, cn=cn: e.tensor_scalar(out=mbt[:, 0:cn], in0=scores[:, c0:c0 + cn], scalar1=bis[:, 0:1], scalar2=-30000.0,
                                                                          op0=ALU.is_lt, op1=ALU.mult), reads=[SCK[ci], ("bis", 0)], writes=[mb_k])
                if ci == 0:
                    cstate[ci] = (mbt, mb_k, kmeta, vmeta, "kmeta", "vmeta")
                    return
                kvn = kv_ctr[0] % 2
                kv_ctr[0] += 1
                kbuf, vbuf = kv_pool[kvn]
                kvk, vvk = ("kbuf", kvn), ("vbuf", kvn)
                tl = [1 + 4 * (ci - 1) + q for q in range(4)]
                op("sp", lambda e, kbuf=kbuf, c0=c0: e.dma_start(out=kbuf[:, :, :], in_=k_scr[:, :, c0:c0 + 512].rearrange("h p t -> p h t")),
                   reads=[("k_scr", t) for t in tl], writes=[kvk], dma=True)
                op("sp", lambda e, vbuf=vbuf, c0=c0: e.dma_start(out=vbuf[:, :, :, :].rearrange("p g h e -> p g (h e)"),
                                                                 in_=v_scr[c0:c0 + 512, :, :].rearrange("(g p) h e -> p g (h e)", p=128)),
                   reads=[("v_scr", t) for t in tl], writes=[vvk], dma=True)
                cstate[ci] = (mbt, mb_k, kbuf, vbuf, kvk, vvk)

            def emit_S(i):
                ci, c0, cn, kb = kblist[i]
                if kb == 0:
                    chunk_setup(ci, c0, cn)
                mbt, mb_k, kbuf, vbuf, kvk, vvk = cstate[ci]
                ks = NMETA if ci == 0 else 128
                st, st_k = pg.get()
                for h in range(4):
                    op("pe", lambda e, st=st, h=h, kbuf=kbuf, kb=kb, ks=ks, tb=tb: e.matmul(st[0:ks, h * 128:(h + 1) * 128], lhsT=kbuf[:, h, kb * 128:kb * 128 + ks],
                                                                                         rhs=qT[:, h, tb * 128:(tb + 1) * 128], start=True, stop=False),
                       reads=[kvk, ("qT", tb)], writes=[st_k])
                    op("pe", lambda e, st=st, h=h, mbt=mbt, kb=kb, ks=ks: e.matmul(st[0:ks, h * 128:(h + 1) * 128], lhsT=mbt[:, kb * 128:kb * 128 + ks],
                                                                                 rhs=ident[:, :], start=False, stop=True),
                       reads=[mb_k, "ident"], writes=[st_k])
                PT, PT_k = PT_pool.get()
                op("act", lambda e, PT=PT, st=st, ks=ks: e.activation(out=PT[0:ks, :], in_=st[0:ks, :], func=AF.Exp, scale=128 ** -0.5),
                   reads=[st_k], writes=[PT_k])
                return (PT, PT_k)

            def emit_P(i, PTs):
                ci, c0, cn, kb = kblist[i]
                mbt, mb_k, kbuf, vbuf, kvk, vvk = cstate[ci]
                ks = NMETA if ci == 0 else 128
                PT, PT_k = PTs
                for h in range(4):
                    acc = pacc[h // 2]
                    a0 = (h % 2) * 256
                    rhs = vbuf[0:ks, h, :] if ci == 0 else vbuf[:, kb, h, :]
                    op("pe", lambda e, acc=acc, a0=a0, PT=PT, h=h, ks=ks, rhs=rhs, i=i: e.matmul(acc[:, a0:a0 + 129], lhsT=PT[0:ks, h * 128:(h + 1) * 128], rhs=rhs,
                                                                                           start=(i == 0 and h % 2 == 0), stop=(i == nkb_total - 1),
                                                                                           skip_group_check=True),
                       reads=[PT_k, vvk], writes=[("pacc", h // 2)])

            prev = emit_S(0)
            for i in range(nkb_total):
                nxt = emit_S(i + 1) if i + 1 < nkb_total else None
                emit_P(i, prev)
                prev = nxt
            for h in range(4):
                acc = pacc[h // 2]
                a0 = (h % 2) * 256
                op("dve", lambda e, acc=acc, a0=a0, h=h: e.reciprocal(out=rden[:, h:h + 1], in_=acc[:, a0 + 128:a0 + 129]),
                   reads=[("pacc", h // 2)], writes=[("rden", h)])
                op("dve", lambda e, acc=acc, a0=a0, h=h, tb=tb: e.tensor_scalar(out=mixed[:, tb, h * 128:(h + 1) * 128], in0=acc[:, a0:a0 + 128], scalar1=rden[:, h:h + 1],
                                                                              scalar2=None, op0=ALU.mult),
                   reads=[("pacc", h // 2), ("rden", h)], writes=[("mixA", tb, h)])

        stage[0] = 7
        for pair in range(2):
            h1s = []
            while len(wfifo) < 2:
                issue_next()
            wos = [wfifo.pop(0), wfifo.pop(0)]
            for t2 in range(2):
                tb = 2 * pair + t2
                pt, pt_k = pg.get()
                ptb = pt[:].bitcast(BF16)
                for ec in range(8):
                    op("pe", lambda e, ptb=ptb, ec=ec, tb=tb: e.transpose(out=ptb[:, ec * 128:(ec + 1) * 128], in_=mixed[:, tb, ec * 128:(ec + 1) * 128], identity=ident[:, :]),
                       reads=[("mixA", tb, h) for h in range(4)] + [("mixC", tb), "ident"], writes=[pt_k])
                op("act", lambda e, ptb=ptb: e.copy(out=mT[:, :, :], in_=ptb[:, :].rearrange("p (c t) -> p c t", c=8)), reads=[pt_k], writes=["mT"])
                xt, xt_k = xt_pool.get()
                op("sp", lambda e, xt=xt, sl=sl, tb=tb: e.dma_start(out=xt[:, :], in_=xown[sl, 32 + tb * 128:32 + (tb + 1) * 128, :]), writes=[xt_k], dma=True)
                h1, h1_k = xt, xt_k
                for half in range(2):
                    wo, wo_k = wos[half]
                    for ec in range(8):
                        op("pe", lambda e, half=half, ec=ec, wo=wo: e.matmul(psc[half][:, :], lhsT=mT[:, ec, :], rhs=wo[:, ec, :], start=(ec == 0), stop=(ec == 7)),
                           reads=["mT", wo_k], writes=[("psc", half)])
                    op("dve", lambda e, half=half, h1=h1, xt=xt: e.tensor_tensor(out=h1[:, half * 512:(half + 1) * 512], in0=psc[half][:, :],
                                                                               in1=xt[:, half * 512:(half + 1) * 512], op=ALU.add),
                       reads=[("psc", half), xt_k], writes=[h1_k])
                rmsnorm_T(h1, h1_k, 128, gM, hnT[:, :, t2 * 128:(t2 + 1) * 128], ("hnT", t2))
                h1s.append((h1, h1_k, tb))
            issue_next()
            issue_next()
            accs = [[(psc[0], ("psc", 0)), (psc[1], ("psc", 1))], [(pacc[0], ("pacc", 0)), (pacc[1], ("pacc", 1))]]
            fstate = {}

            def ffn_up(i):
                fg, fc = divmod(i, 4)
                if fc == 0:
                    while len(wfifo) < 2:
                        issue_next()
                    fstate[fg] = (wfifo.pop(0), wfifo.pop(0))
                (wu, wu_k), (wdv, wd_k) = fstate[fg]
                ps, ps_k = pg.get()
                for dc in range(8):
                    op("pe", lambda e, ps=ps, wu=wu, dc=dc, fc=fc: e.matmul(ps[:, 0:256], lhsT=wu[:, dc, fc * 128:(fc + 1) * 128], rhs=hnT[:, dc, :],
                                                                         start=(dc == 0), stop=(dc == 7)),
                       reads=[wu_k, ("hnT", 0), ("hnT", 1)], writes=[ps_k])
                if fc == 3:
                    issue_next()
                uf, uf_k = uf_pool.get()
                op("act", lambda e, uf=uf, ps=ps: e.activation(out=uf[:, :], in_=ps[:, 0:256], func=AF.Relu), reads=[ps_k], writes=[uf_k])
                op("pool", lambda e, uf=uf: e.tensor_tensor(out=uf[:, :], in0=uf[:, :], in1=uf[:, :], op=ALU.mult), reads=[uf_k], writes=[uf_k])
                return uf, uf_k

            def ffn_down(i, ufs):
                fg, fc = divmod(i, 4)
                uf, uf_k = ufs
                (wu, wu_k), (wdv, wd_k) = fstate[fg]
                for t2 in range(2):
                    for half in range(2):
                        acc, acc_k = accs[t2][half]
                        op("pe", lambda e, acc=acc, uf=uf, t2=t2, half=half, wdv=wdv, fc=fc, i=i: e.matmul(
                            acc[:, :], lhsT=uf[:, t2 * 128:(t2 + 1) * 128], rhs=wdv[:, fc, half * 512:(half + 1) * 512], start=(i == 0), stop=(i == 31)),
                           reads=[uf_k, wd_k], writes=[acc_k])
                if fc == 3:
                    issue_next()

            cur = ffn_up(0)
            for i in range(32):
                nxt = ffn_up(i + 1) if i + 1 < 32 else None
                ffn_down(i, cur)
                cur = nxt
            for t2 in range(2):
                h1, h1_k, tb = h1s[t2]
                for half in range(2):
                    acc, acc_k = accs[t2][half]
                    op("dve", lambda e, acc=acc, h1=h1, half=half: e.tensor_tensor(out=h1[:, half * 512:(half + 1) * 512], in0=acc[:, :],
                                                                                 in1=h1[:, half * 512:(half + 1) * 512], op=ALU.add),
                       reads=[acc_k, h1_k], writes=[h1_k])
                st, st_k = st_pool.get()
                jk, jk_k = jk_pool.get()
                op("act", lambda e, jk=jk, h1=h1, st=st: e.activation(out=jk[:, :], in_=h1[:, 0:512], func=AF.Square, accum_out=st[:, 0:1]),
                   reads=[h1_k], writes=[jk_k, (st_k, 0)])
                op("act", lambda e, jk=jk, h1=h1, st=st: e.activation(out=jk[:, :], in_=h1[:, 512:1024], func=AF.Square, accum_out=st[:, 1:2]),
                   reads=[h1_k], writes=[jk_k, (st_k, 1)])
                op("dve", lambda e, st=st: e.tensor_tensor(out=st[:, 2:3], in0=st[:, 0:1], in1=st[:, 1:2], op=ALU.add), reads=[(st_k, 0), (st_k, 1)], writes=[(st_k, 2)])
                op("dve", lambda e, st=st: e.tensor_scalar(out=st[:, 3:4], in0=st[:, 2:3], scalar1=1.0 / D, scalar2=EPS, op0=ALU.mult, op1=ALU.add),
                   reads=[(st_k, 2)], writes=[(st_k, 3)])
                op("act", lambda e, st=st: e.activation(out=st[:, 4:5], in_=st[:, 3:4], func=AF.Sqrt), reads=[(st_k, 3)], writes=[(st_k, 4)])
                op("dve", lambda e, st=st: e.reciprocal(out=st[:, 5:6], in_=st[:, 4:5]), reads=[(st_k, 4)], writes=[(st_k, 5)])
                op("dve", lambda e, h1=h1, st=st: e.scalar_tensor_tensor(out=h1[:, :], in0=h1[:, :], scalar=st[:, 5:6], in1=gF[:, :], op0=ALU.mult, op1=ALU.mult),
                   reads=[h1_k, (st_k, 5), "gF"], writes=[h1_k])
                r0 = sl * 512 + tb * 128
                op("sp", lambda e, h1=h1, r0=r0: e.dma_start(out=out_d[r0:r0 + 128, :], in_=h1[:, :]), reads=[h1_k], writes=[("out", r0)], dma=True)

    if os.environ.get("KDEBUG"):
        print("sbuf remaining", nc.sbuf_bytes_remaining, "ops", {e: len(S_.ops[e]) for e in ENGS})
    with ExitStack() as stack:
        S_.emit(nc, stack)
    return nc


def _rope_tab(pos):
    pos = np.asarray(pos, np.float32)
    out = np.zeros((len(pos), 512), np.float32)
    inv = (np.float32(500000.0) ** (-np.arange(0, 32, 2, dtype=np.float32) / np.float32(32))).astype(np.float32)
    ang = pos[:, None] * inv[None, :]
    c, s = np.cos(ang).astype(np.float32), np.sin(ang).astype(np.float32)
    out[:, 0:128] = np.tile(np.concatenate([c, c], 1), (1, 4))
    out[:, 128:256] = np.tile(np.concatenate([s, s], 1), (1, 4))
    inv = (np.float32(500000.0) ** (-np.arange(0, 16, 2, dtype=np.float32) / np.float32(16))).astype(np.float32)
    ang = pos[:, None] * inv[None, :]
    c, s = np.cos(ang).astype(np.float32), np.sin(ang).astype(np.float32)
    out[:, 256:384] = np.tile(np.concatenate([c, c], 1), (1, 8))
    out[:, 384:512] = np.tile(np.concatenate([s, s], 1), (1, 8))
    return out


def _chunk_of(sl, half):
    return 2 * sl + (sl % 2) if half == 0 else 2 * sl + 1 - (sl % 2)


_NC_CACHE = {}


def kernel(x, meta_tokens, attn_norm_g, w_in, conv_w, conv_b, conv_norm_g, conv_norm_b,
           w_out, mlp_norm_g, w_up, w_down, final_norm_g):
    x = np.asarray(x, np.float32)
    B, S, _ = x.shape
    T = NMETA + S
    NSLOT = S // 1024
    if S not in _NC_CACHE:
        _NC_CACHE[S] = build(S)
    nc = _NC_CACHE[S]
    f = lambda a: np.ascontiguousarray(np.asarray(a, np.float32))
    meta = f(meta_tokens)
    ropek = _rope_tab(np.arange(T))
    p = np.arange(128)
    consts = {
        "iota": np.tile(np.arange(512, dtype=np.float32)[None, :], (128, 1)),
        "pow2": np.tile((2.0 ** -(np.arange(NBIS) + 1.0)).astype(np.float32)[None, :], (128, 1)),
        "e32": (np.arange(32)[None, :] == (p % 32)[:, None]).astype(np.float32),
        "g4": (np.arange(4)[None, :] == (p // 32)[:, None]).astype(np.float32),
        "dm": np.concatenate([(np.arange(128)[None, :] == (32 * g + p % 32)[:, None]).astype(np.float32) for g in range(4)], axis=1),
    }
    shared = {
        "w_in": f(w_in[0]), "w_out": f(w_out[0]), "w_up": f(w_up[0]), "w_down": f(w_down[0]),
        "attn_g": f(attn_norm_g[0]), "mlp_g": f(mlp_norm_g[0]), "final_g": f(final_norm_g),
        "conv_w": f(conv_w[0]), "conv_b": f(conv_b[0]), "cn_g": f(conv_norm_g[0]), "cn_b": f(conv_norm_b[0]),
        "ropek": ropek,
    }
    shared.update(consts)
    in_maps = []
    for core in range(8):
        b, half = core // 2, core % 2
        hall = np.concatenate([meta, x[b]], axis=0)
        xown = np.zeros((NSLOT, 544, D), np.float32)
        ropeq = np.zeros((NSLOT, 512, 512), np.float32)
        qrel = np.zeros((128, NSLOT * 8), np.float32)
        for sl in range(NSLOT):
            c = _chunk_of(sl, half)
            p0 = NMETA + 512 * c
            lo = p0 - 32
            src_lo = max(lo, 0)
            xown[sl, src_lo - lo:, :] = hall[src_lo:p0 + 512]
            ropeq[sl] = ropek[p0:p0 + 512]
            w0 = NMETA + 1024 * sl
            for tb in range(4):
                for wc in range(2):
                    qrel[:, (sl * 4 + tb) * 2 + wc] = (p0 + tb * 128 + p) - w0 - 512 * wc
        m = dict(shared)
        m.update({"xall": hall, "xown": xown, "ropeq": ropeq, "qrel": qrel})
        in_maps.append(m)
    res = run_bass_kernel_spmd(nc, in_maps, core_ids=list(range(8)))
    out = np.zeros((B, S, D), np.float32)
    for core in range(8):
        b, half = core // 2, core % 2
        o = res.results[core]["out"]
        for sl in range(NSLOT):
            c = _chunk_of(sl, half)
            out[b, 512 * c:512 * (c + 1)] = o[sl * 512:(sl + 1) * 512]
    return out
```

```python
import numpy as np
from contextlib import ExitStack
import concourse.bass as bass
import concourse.mybir as mybir
from concourse.bass_utils import run_bass_kernel_spmd

F32 = mybir.dt.float32
BF16 = mybir.dt.bfloat16
ALU = mybir.AluOpType
AF = mybir.ActivationFunctionType
AX = mybir.AxisListType

D = 1024
NMETA = 16
DIN = 3144
DFF = 4096
EPS = 1e-5
KTOP = 256
NBIS = 20
ENGS = ("pe", "act", "dve", "pool", "sp")
SEM_CH = 30000
NDMA_SEM = 12


class Op:
    __slots__ = ("eng", "fn", "deps", "dma", "signals", "sig", "dma_idx")


class Sched:
    def __init__(self):
        self.ops = {e: [] for e in ENGS}
        self.lastw = {}
        self.readers = {}
        self.ndma = {e: 0 for e in ENGS}

    def op(self, eng, fn, reads=(), writes=(), dma=False):
        o = Op()
        o.eng = eng
        o.fn = fn
        o.dma = dma
        o.signals = False
        o.sig = None
        deps = {}
        xr = [k for k in reads if isinstance(k, tuple) and k[0] in ("pg", "psc", "pacc")]
        if xr:
            writes = list(writes) + [k for k in xr if k not in writes]
        for k in reads:
            w = self.lastw.get(k)
            if w is not None:
                deps[id(w)] = (w, True)
        for k in writes:
            w = self.lastw.get(k)
            if w is not None and id(w) not in deps:
                deps[id(w)] = (w, False)
            for r in self.readers.get(k, ()):
                if id(r) not in deps:
                    deps[id(r)] = (r, False)
        o.deps = []
        for d, raw in deps.values():
            if d.eng == eng and not d.dma:
                if eng == "pe":
                    continue
                if not raw and not dma:
                    continue
            d.signals = True
            o.deps.append(d)
        if dma:
            o.dma_idx = self.ndma[eng]
            self.ndma[eng] += 1
            o.signals = True
        for k in reads:
            self.readers.setdefault(k, []).append(o)
        for k in writes:
            self.lastw[k] = o
            self.readers[k] = []
        self.ops[eng].append(o)
        return o

    def emit(self, nc, stack):
        nsig = {}
        for e in ENGS:
            n = 0
            for o in self.ops[e]:
                if o.signals and not o.dma:
                    o.sig = n
                    n += 1
            nsig[e] = n
        csem = {}
        for e in ENGS:
            nch = (nsig[e] + SEM_CH - 1) // SEM_CH
            csem[e] = [stack.enter_context(nc.semaphore(f"c_{e}_{i}")) for i in range(nch)]
        dsem = {}
        for e in ENGS:
            if self.ndma[e]:
                dsem[e] = [stack.enter_context(nc.semaphore(f"d_{e}_{i}")) for i in range(NDMA_SEM)]

        def target(d):
            if d.dma:
                return dsem[d.eng][d.dma_idx % NDMA_SEM], 16 * (d.dma_idx // NDMA_SEM + 1)
            return csem[d.eng][d.sig // SEM_CH], d.sig % SEM_CH + 1

        block = stack.enter_context(nc.Block())

        def run(e):
            def body(eng):
                waited = {}
                for o in self.ops[e]:
                    tg = [target(d) for d in o.deps]
                    if o.dma and o.dma_idx >= NDMA_SEM:
                        tg.append((dsem[e][o.dma_idx % NDMA_SEM], 16 * (o.dma_idx // NDMA_SEM)))
                    for s, v in tg:
                        key = id(s)
                        if waited.get(key, 0) < v:
                            eng.wait_ge(s, v)
                            waited[key] = v
                    ins = o.fn(eng)
                    if o.signals:
                        if o.dma:
                            s, _ = target(o)
                            ins.then_inc(s, 16)
                        else:
                            ins.then_inc(csem[e][o.sig // SEM_CH], 1)
                if self.ndma[e]:
                    n = self.ndma[e]
                    for i in range(max(0, n - NDMA_SEM), n):
                        eng.wait_ge(dsem[e][i % NDMA_SEM], 16 * (i // NDMA_SEM + 1))
            return body

        for e, reg in (("pe", block.tensor), ("act", block.scalar), ("dve", block.vector),
                       ("pool", block.gpsimd), ("sp", block.sync)):
            if self.ops[e]:
                reg(run(e))


class Pool:
    def __init__(self, tiles, name, off=0):
        self.tiles = tiles
        self.name = name
        self.i = 0
        self.off = off

    def get(self):
        j = self.i % len(self.tiles)
        self.i += 1
        return self.tiles[j], (self.name, j + self.off)


def build(S):
    T = NMETA + S
    NCH = S // 512
    NSLOT = NCH // 2
    NKT = 1 + S // 128
    nc = bass.Bass("TRN2", target_bir_lowering=False)

    def din(name, shape, dt=F32):
        return nc.dram_tensor(name, shape, dt, kind="ExternalInput").ap()

    xall = din("xall", [T, D])
    xown = din("xown", [NSLOT, 544, D])
    ropek = din("ropek", [T, 512])
    ropeq = din("ropeq", [NSLOT, 512, 512])
    qrel_d = din("qrel", [128, NSLOT * 8])
    w_in = din("w_in", [D, DIN])
    w_out = din("w_out", [D, D])
    w_up = din("w_up", [D, DFF])
    w_down = din("w_down", [DFF, D])
    attn_g = din("attn_g", [D])
    mlp_g = din("mlp_g", [D])
    final_g = din("final_g", [D])
    conv_w = din("conv_w", [31, 512])
    conv_b = din("conv_b", [512])
    cn_g = din("cn_g", [512])
    cn_b = din("cn_b", [512])
    iota_d = din("iota", [128, 512])
    pow2_d = din("pow2", [128, NBIS])
    e32_d = din("e32", [128, 32])
    g4_d = din("g4", [128, 4])
    dm_d = din("dm", [128, 4 * 128])
    out_d = nc.dram_tensor("out", [NSLOT * 512, D], F32, kind="ExternalOutput").ap()

    k_scr = nc.dram_tensor("k_scr", [4, 128, T], BF16).ap()
    v_scr = nc.dram_tensor("v_scr", [T, 4, 129], BF16).ap()

    import os
    KSTOP = float(os.environ.get('KSTOP', '99'))
    S_ = Sched()
    stage = [0]

    def op(*a, **k):
        if stage[0] <= KSTOP:
            return S_.op(*a, **k)
        return None
    uid = [0]

    def sb(shape, dt, name=None):
        uid[0] += 1
        return nc.alloc_sbuf_tensor(f"{name or 't'}{uid[0]}", shape, dt)

    def mkpool(n, shape, dt, name):
        return Pool([sb(shape, dt, name) for _ in range(n)], name)

    identf = sb([128, 128], F32, "identf")
    ident = sb([128, 128], BF16, "ident")
    op("pool", lambda e: e.memset(identf[:], 0.0), writes=["identf"])
    op("pool", lambda e: e.affine_select(out=identf[:], in_=identf[:], pattern=[[-1, 128]],
                                         compare_op=ALU.not_equal, fill=1.0, base=0, channel_multiplier=1),
       reads=["identf"], writes=["identf"])
    op("dve", lambda e: e.tensor_copy(out=ident[:], in_=identf[:]), reads=["identf"], writes=["ident"])

    def load_const(name, shape, src, dt=F32):
        t = sb(shape, dt, name)
        op("sp", lambda e: e.dma_start(out=t[:], in_=src, allow_slow_non_contiguous=True), writes=[name], dma=True)
        return t

    iota = load_const("iota", [128, 512], iota_d)
    pow2 = load_const("pow2", [128, NBIS], pow2_d)
    e32 = load_const("e32", [128, 32], e32_d)
    g4 = load_const("g4", [128, 4], g4_d)
    qrel = load_const("qrelc", [128, NSLOT * 8], qrel_d)
    gA = load_const("gA", [128, 8], attn_g.rearrange("(c p) -> p c", p=128))
    gM = load_const("gM", [128, 8], mlp_g.rearrange("(c p) -> p c", p=128))
    def bcast_row(name, src, n):
        t = sb([128, n], F32, name)
        op("sp", lambda e: e.dma_start(out=t[:], in_=src.partition_broadcast(128)), writes=[name], dma=True)
        return t
    gF = bcast_row("gF", final_g, D)
    cb_bc = bcast_row("cb_bc", conv_b, 512)
    cg_bc = bcast_row("cg_bc", cn_g, 512)
    cnb_bc = bcast_row("cnb_bc", cn_b, 512)
    cwT = sb([128, 4, 31], F32, "cwT")
    for cc_ in range(4):
        op("sp", lambda e, cc_=cc_: e.dma_start(out=cwT[:, cc_, :], in_=conv_w[:, cc_ * 128:(cc_ + 1) * 128].rearrange("j p -> p j"),
                                                 allow_slow_non_contiguous=True), writes=["cwT"], dma=True)
    dmb = sb([128, 512], BF16, "dmb")
    op("pool", lambda e: e.dma_start(out=dmb[:], in_=dm_d), writes=["dmb"], dma=True)
    wik_t = sb([128, 8, 64], BF16, "wik")
    op("pool", lambda e: e.dma_start(out=wik_t[:, :, :], in_=w_in[:, 2048:2112].rearrange("(c p) n -> p c n", p=128)), writes=["wik"], dma=True)
    wiw_t = sb([128, 8, 8], BF16, "wiw")
    op("pool", lambda e: e.dma_start(out=wiw_t[:, :, :], in_=w_in[:, 2112:2120].rearrange("(c p) n -> p c n", p=128), allow_slow_non_contiguous=True),
       writes=["wiw"], dma=True)

    win_b = nc.dram_tensor("win_b", [D, DIN], BF16).ap()
    wout_b = nc.dram_tensor("wout_b", [D, D], BF16).ap()
    wup_b = nc.dram_tensor("wup_b", [D, DFF], BF16).ap()
    wdown_b = nc.dram_tensor("wdown_b", [DFF, D], BF16).ap()

    ikT = sb([128, T], BF16, "ikT")
    scores = sb([128, T], F32, "scores")
    wbuf = mkpool(4, [128, 8, 512], BF16, "wbuf")
    xt_pool = mkpool(3, [128, D], F32, "xt")
    xn_pool = mkpool(1, [128, D], BF16, "xn")
    xnT_pool = mkpool(2, [128, 8, 128], BF16, "xnT")
    rope_pool = mkpool(4, [128, 256], F32, "rope")
    st_pool = mkpool(4, [128, 8], F32, "stat")
    tm_pool = mkpool(3, [128, 512], BF16, "tm")
    rt_pool = mkpool(2, [128, 256], F32, "rt")
    jk_pool = mkpool(2, [128, 512], F32, "jk")
    bigjunk = sb([128, NMETA + 512 * ((T // 512) // 2 + 1)], mybir.dt.uint8, "bigjunk")
    vb_pool = mkpool(2, [128, 4, 129], BF16, "vb")
    for vbt_ in vb_pool.tiles:
        pass
    pg = Pool([nc.alloc_psum_tensor(f"pg{i}", [128, 512], F32) for i in range(4)], "pg")
    psc = [nc.alloc_psum_tensor(f"psc{i}", [128, 512], F32) for i in range(2)]
    pacc = [nc.alloc_psum_tensor(f"pacc{i}", [128, 512], F32) for i in range(2)]

    def pg_bf16(t):
        return t.bitcast(BF16) if hasattr(t, "bitcast") else None

    def rmsnorm_T(x_t, x_k, rows, gcol, xnT, xnT_k):
        st, st_k = st_pool.get()
        jk, jk_k = jk_pool.get()
        xn, xn_k = xn_pool.get()
        op("act", lambda e: e.activation(out=jk[0:rows, :], in_=x_t[0:rows, 0:512], func=AF.Square,
                                         accum_out=st[0:rows, 0:1]), reads=[x_k], writes=[jk_k, (st_k, 0)])
        op("act", lambda e: e.activation(out=jk[0:rows, :], in_=x_t[0:rows, 512:1024], func=AF.Square,
                                         accum_out=st[0:rows, 1:2]), reads=[x_k], writes=[jk_k, (st_k, 1)])
        op("dve", lambda e: e.tensor_tensor(out=st[0:rows, 2:3], in0=st[0:rows, 0:1], in1=st[0:rows, 1:2], op=ALU.add),
           reads=[(st_k, 0), (st_k, 1)], writes=[(st_k, 2)])
        op("dve", lambda e: e.tensor_scalar(out=st[0:rows, 3:4], in0=st[0:rows, 2:3], scalar1=1.0 / D, scalar2=EPS,
                                            op0=ALU.mult, op1=ALU.add), reads=[(st_k, 2)], writes=[(st_k, 3)])
        op("act", lambda e: e.activation(out=st[0:rows, 4:5], in_=st[0:rows, 3:4], func=AF.Sqrt),
           reads=[(st_k, 3)], writes=[(st_k, 4)])
        op("dve", lambda e: e.reciprocal(out=st[0:rows, 5:6], in_=st[0:rows, 4:5]), reads=[(st_k, 4)], writes=[(st_k, 5)])
        op("dve", lambda e: e.tensor_scalar(out=xn[0:rows, :], in0=x_t[0:rows, :], scalar1=st[0:rows, 5:6], scalar2=None,
                                            op0=ALU.mult), reads=[x_k, (st_k, 5)], writes=[xn_k])
        pt, pt_k = pg.get()
        ptb = pt[:].bitcast(BF16)
        for dc in range(8):
            op("pe", lambda e, dc=dc: e.transpose(out=ptb[:, dc * 128:dc * 128 + rows], in_=xn[0:rows, dc * 128:(dc + 1) * 128],
                                                  identity=ident[0:rows, 0:rows]),
               reads=[xn_k, "ident"], writes=[pt_k])
        op("dve", lambda e: e.tensor_tensor(out=xnT[:, :, 0:rows],
                                            in0=ptb.rearrange("p (c t) -> p c t", c=8)[:, :, 0:rows],
                                            in1=gcol[:, :].unsqueeze(2).to_broadcast([128, 8, rows]), op=ALU.mult),
           reads=[pt_k], writes=[xnT_k])
        return (st, st_k)

    def wviews(kind, i, w):
        if kind == "down":
            v = w[:, :, :].rearrange("p a b -> p (a b)").rearrange("p (f n) -> p f n", f=4)
            r = lambda a: a[i * 512:(i + 1) * 512, :].rearrange("(f p) n -> p f n", p=128)
            return v, r(w_down), r(wdown_b)
        src, dst = {"in": (w_in, win_b), "out": (w_out, wout_b), "up": (w_up, wup_b)}[kind]
        r = lambda a: a[:, i:i + 512].rearrange("(c p) n -> p c n", p=128) if kind == "in" else a[:, i * 512:(i + 1) * 512].rearrange("(c p) n -> p c n", p=128)
        return w[:, :, :], r(src), r(dst)

    def load_w(kind, i, cast=False):
        w, w_k = wbuf.get()
        v, src, scr = wviews(kind, i, w)
        if cast:
            op("pool", lambda e: e.dma_start(out=v, in_=src), writes=[w_k], dma=True)
        else:
            op("sp", lambda e: e.dma_start(out=v, in_=scr), reads=[("wcv", kind, i)], writes=[w_k], dma=True)
        return (v if kind == "down" else w), w_k

    def proj_tm(xnT, xnT_k, rows, w, w_k, ncols, ps, ps_k, pcol=0):
        for dc in range(8):
            op("pe", lambda e, dc=dc: e.matmul(ps[0:rows, pcol:pcol + ncols], lhsT=xnT[:, dc, 0:rows], rhs=w[:, dc, 0:ncols],
                                               start=(dc == 0), stop=(dc == 7)),
               reads=[xnT_k, w_k], writes=[ps_k])


    def rope_tm(ps, ps_k, rows, nh, hd, half, rp, rp_k, roff, dst, dst_k, dcol=0):
        r2 = 2 * half
        n = nh * hd
        xf, xf_k = jk_pool.get()
        op("act", lambda e: e.copy(out=xf[0:rows, 0:n], in_=ps[0:rows, 0:n]), reads=[ps_k], writes=[xf_k])
        op("act", lambda e: e.copy(out=dst[0:rows, dcol:dcol + n], in_=ps[0:rows, 0:n]), reads=[ps_k], writes=[dst_k])
        rt, rt_k = rt_pool.get()
        pv = xf[0:rows, 0:n].rearrange("p (h d) -> p h d", h=nh)
        A = rt[0:rows, 0:nh * r2].rearrange("p (h d) -> p h d", h=nh)
        Bm = rt[0:rows, 128:128 + nh * r2].rearrange("p (h d) -> p h d", h=nh)
        cc_ = rp[0:rows, roff:roff + nh * r2].rearrange("p (h d) -> p h d", h=nh)
        ss_ = rp[0:rows, roff + 128:roff + 128 + nh * r2].rearrange("p (h d) -> p h d", h=nh)
        op("dve", lambda e: e.tensor_tensor(out=A, in0=pv[:, :, 0:r2], in1=cc_, op=ALU.mult), reads=[xf_k, rp_k], writes=[(rt_k, 0)])
        op("dve", lambda e: e.tensor_tensor(out=Bm, in0=pv[:, :, 0:r2], in1=ss_, op=ALU.mult), reads=[xf_k, rp_k], writes=[(rt_k, 1)])
        dv = dst[0:rows, dcol:dcol + n].rearrange("p (h d) -> p h d", h=nh)
        op("dve", lambda e: e.tensor_tensor(out=dv[:, :, 0:half], in0=A[:, :, 0:half], in1=Bm[:, :, half:r2], op=ALU.subtract),
           reads=[(rt_k, 0), (rt_k, 1), dst_k], writes=[dst_k])
        op("dve", lambda e: e.tensor_tensor(out=dv[:, :, half:r2], in0=A[:, :, half:r2], in1=Bm[:, :, 0:half], op=ALU.add),
           reads=[(rt_k, 0), (rt_k, 1), dst_k], writes=[dst_k])

    stage[0] = 1
    for j_, vbt_ in enumerate(vb_pool.tiles):
        op("pool", lambda e, vbt_=vbt_: e.memset(vbt_[:, :, 128:129], 1.0), writes=[("vbones", ("vb", j_))])
    wk, wk_k = load_w("in", 512, cast=True)
    wv, wv_k = load_w("in", 1024, cast=True)
    cvt_pool = Pool(wbuf.tiles[2:4], "wbuf", off=2)
    for kind, idxs in (("in", (0, 1536, 2120, 2632)), ("out", (0, 1)), ("up", tuple(range(8))), ("down", tuple(range(8)))):
        for i in idxs:
            w, w_k = cvt_pool.get()
            v, src, scr = wviews(kind, i, w)
            op("pool", lambda e, v=v, src=src: e.dma_start(out=v, in_=src), writes=[w_k], dma=True)
            op("pool", lambda e, v=v, scr=scr: e.dma_start(out=scr, in_=v), reads=[w_k], writes=[("wcv", kind, i)], dma=True)
    wbuf.i = 2
    wik, wik_k = wik_t, "wik"
    stage[0] = 1
    def p1_tile(kt):
        rows = NMETA if kt == 0 else 128
        p0 = 0 if kt == 0 else NMETA + 128 * (kt - 1)
        xt, xt_k = xt_pool.get()
        op("sp", lambda e, xt=xt, p0=p0, rows=rows: e.dma_start(out=xt[0:rows, :], in_=xall[p0:p0 + rows, :]), writes=[xt_k], dma=True)
        rp, rp_k = rope_pool.get()
        op("sp", lambda e, rp=rp, p0=p0, rows=rows: e.dma_start(out=rp[0:rows, :], in_=ropek[p0:p0 + rows, 0:256]), writes=[rp_k], dma=True)
        rpi, rpi_k = rope_pool.get()
        op("sp", lambda e, rpi=rpi, p0=p0, rows=rows: e.dma_start(out=rpi[0:rows, :], in_=ropek[p0:p0 + rows, 256:512]), writes=[rpi_k], dma=True)
        xnT, xnT_k = xnT_pool.get()
        rmsnorm_T(xt, xt_k, rows, gA, xnT, xnT_k)
        yield
        psK, psK_k = pg.get()
        proj_tm(xnT, xnT_k, rows, wk, wk_k, 512, psK, psK_k)
        psV, psV_k = pg.get()
        proj_tm(xnT, xnT_k, rows, wv, wv_k, 512, psV, psV_k)
        psI, psI_k = pg.get()
        proj_tm(xnT, xnT_k, rows, wik, wik_k, 64, psI, psI_k)
        kb, kb_k = tm_pool.get()
        rope_tm(psK, psK_k, rows, 4, 128, 16, rp, rp_k, 0, kb, kb_k)
        vb, vb_k = vb_pool.get()
        op("act", lambda e, vb=vb, ps=psV, rows=rows: e.copy(out=vb[0:rows, :, 0:128], in_=ps[0:rows, :].rearrange("p (h d) -> p h d", h=4)),
           reads=[psV_k, ("vbones", vb_k)], writes=[vb_k])
        op("act", lambda e, vb=vb, p0=p0, rows=rows: e.dma_start(out=v_scr[p0:p0 + rows, :, :], in_=vb[0:rows, :, :]),
           reads=[vb_k, ("vbones", vb_k)], writes=[("v_scr", kt)], dma=True)
        ib, ib_k = tm_pool.get()
        rope_tm(psI, psI_k, rows, 1, 64, 8, rpi, rpi_k, 0, ib, ib_k)
        op("dve", lambda e, ib=ib, rows=rows: e.tensor_copy(out=ib[0:rows, 64:128], in_=ib[0:rows, 0:64]), reads=[ib_k], writes=[ib_k])
        pt, pt_k = pg.get()
        ptb = pt[:].bitcast(BF16)
        for h in range(4):
            op("pe", lambda e, h=h, ptb=ptb, kb=kb, rows=rows: e.transpose(out=ptb[:, h * 128:h * 128 + rows], in_=kb[0:rows, h * 128:(h + 1) * 128],
                                                                         identity=ident[0:rows, 0:rows]), reads=[kb_k, "ident"], writes=[pt_k])
        kTt, kTt_k = tm_pool.get()
        op("act", lambda e, kTt=kTt, ptb=ptb: e.copy(out=kTt[:, :], in_=ptb[:, 0:512]), reads=[pt_k], writes=[kTt_k])
        op("act", lambda e, kTt=kTt, p0=p0, rows=rows: e.dma_start(
            out=k_scr[:, :, p0:p0 + rows].rearrange("h p t -> p h t"),
            in_=kTt[:, :].rearrange("p (h t) -> p h t", h=4)[:, :, 0:rows]),
           reads=[kTt_k], writes=[("k_scr", kt)], dma=True)
        pt2, pt2_k = pg.get()
        ptb2 = pt2[:].bitcast(BF16)
        op("pe", lambda e, ptb2=ptb2, ib=ib, rows=rows: e.transpose(out=ptb2[:, 0:rows], in_=ib[0:rows, 0:128], identity=ident[0:rows, 0:rows]),
           reads=[ib_k, "ident"], writes=[pt2_k])
        op("act", lambda e, ptb2=ptb2, p0=p0, rows=rows: e.copy(out=ikT[:, p0:p0 + rows], in_=ptb2[:, 0:rows]), reads=[pt2_k], writes=[("ikT", kt)])

    def run_pipelined(gens):
        if gens:
            next(gens[0])
        for i in range(len(gens)):
            if i + 1 < len(gens):
                next(gens[i + 1])
            for _ in gens[i]:
                pass

    run_pipelined([p1_tile(kt) for kt in range(NKT)])

    stage[0] = 2
    qT = sb([128, 4, 512], BF16, "qT")
    iqT = sb([128, 4, 512], BF16, "iqT")
    iwt = sb([128, 4, 8], F32, "iwt")
    uT = sb([128, 4, 544], BF16, "uT")
    mixed = sb([128, 4, D], BF16, "mixed")
    ysb = sb([128, 4, 512], F32, "ysb")
    Dcc = sb([128, 31, 128], BF16, "Dcc")
    negm_pool = mkpool(2, [128, 512], F32, "negm")
    wabs2 = sb([128, 8], F32, "wabs2")
    wsgn = sb([128, 8], F32, "wsgn")
    bis = sb([128, 8 + NBIS], F32, "bis")
    top8 = sb([128, 8], F32, "top8")
    cst = sb([128, 4, 6], F32, "cst")
    cmv = sb([128, 4, 2], F32, "cmv")
    crs = sb([128, 4], F32, "crs")
    R_pool = mkpool(3, [128, 512], BF16, "R")
    PT_pool = mkpool(2, [128, 512], BF16, "PT")
    mb_pool = mkpool(2, [128, 512], BF16, "mb")
    GK = 4
    kv_pool = [(sb([128, 4, GK * 128], BF16, "kbuf"), sb([128, GK, 4, 129], BF16, "vbuf")) for _ in range(2)]
    kmeta = sb([128, 4, NMETA], BF16, "kmeta")
    vmeta = sb([128, 4, 129], BF16, "vmeta")
    op("sp", lambda e: e.dma_start(out=kmeta[:, :, :], in_=k_scr[:, :, 0:NMETA].rearrange("h p t -> p h t")),
       reads=[("k_scr", 0)], writes=["kmeta"], dma=True)
    op("sp", lambda e: e.dma_start(out=vmeta[0:NMETA, :, :], in_=v_scr[0:NMETA, :, :]),
       reads=[("v_scr", 0)], writes=["vmeta"], dma=True)
    mT = sb([128, 8, 128], BF16, "mT")
    hnT = sb([128, 8, 256], BF16, "hnT")
    uf_pool = mkpool(2, [128, 256], BF16, "uf")
    rden = sb([128, 4], F32, "rden")

    SC = 0.125 * (8 ** -0.5)
    kv_ctr = [0]

    for sl in range(NSLOT):
        E = NMETA + 512 * (2 * sl + 2)
        W0 = E - 1024
        NCHK = 2 * sl + 2
        wq, wq_k = load_w("in", 0)
        wiq, wiq_k = load_w("in", 1536)
        wga, wga_k = load_w("in", 2120)
        wgg, wgg_k = load_w("in", 2632)
        plan = []
        for pair_ in range(2):
            plan += [("out", 0), ("out", 1)]
            for fg_ in range(8):
                plan += [("up", fg_), ("down", fg_)]
        wfifo = []

        def issue_next():
            if plan:
                kind_, i_ = plan.pop(0)
                wfifo.append(load_w(kind_, i_))
        def sa_tile(ti, sl=sl, wq=wq, wq_k=wq_k, wiq=wiq, wiq_k=wiq_k, wga=wga, wga_k=wga_k, wgg=wgg, wgg_k=wgg_k):
            rows = 32 if ti == 0 else 128
            r0 = 0 if ti == 0 else 32 + 128 * (ti - 1)
            xt, xt_k = xt_pool.get()
            op("sp", lambda e, xt=xt, r0=r0, rows=rows, sl=sl: e.dma_start(out=xt[0:rows, :], in_=xown[sl, r0:r0 + rows, :]),
               writes=[xt_k], dma=True)
            xnT, xnT_k = xnT_pool.get()
            rmsnorm_T(xt, xt_k, rows, gA, xnT, xnT_k)
            if ti > 0:
                tb = ti - 1
                rp, rp_k = rope_pool.get()
                op("sp", lambda e, rp=rp, tb=tb, sl=sl: e.dma_start(out=rp[:, :], in_=ropeq[sl, 128 * tb:128 * (tb + 1), 0:256]), writes=[rp_k], dma=True)
                rpi, rpi_k = rope_pool.get()
                op("sp", lambda e, rpi=rpi, tb=tb, sl=sl: e.dma_start(out=rpi[:, :], in_=ropeq[sl, 128 * tb:128 * (tb + 1), 256:512]), writes=[rpi_k], dma=True)
            yield
            if ti > 0:
                ps, ps_k = pg.get()
                proj_tm(xnT, xnT_k, 128, wq, wq_k, 512, ps, ps_k)
                qb, qb_k = tm_pool.get()
                rope_tm(ps, ps_k, 128, 4, 128, 16, rp, rp_k, 0, qb, qb_k)
                pt, pt_k = pg.get()
                ptb = pt[:].bitcast(BF16)
                for h in range(4):
                    op("pe", lambda e, h=h, ptb=ptb, qb=qb: e.transpose(out=ptb[:, h * 128:(h + 1) * 128], in_=qb[:, h * 128:(h + 1) * 128], identity=ident[:, :]),
                       reads=[qb_k, "ident"], writes=[pt_k])
                op("act", lambda e, ptb=ptb, tb=tb: e.copy(out=qT[:, :, tb * 128:(tb + 1) * 128], in_=ptb[:, 0:512].rearrange("p (h t) -> p h t", h=4)),
                   reads=[pt_k], writes=[("qT", tb)])
                ps, ps_k = pg.get()
                proj_tm(xnT, xnT_k, 128, wiq, wiq_k, 512, ps, ps_k)
                ib, ib_k = tm_pool.get()
                rope_tm(ps, ps_k, 128, 8, 64, 8, rpi, rpi_k, 0, ib, ib_k)
                pt, pt_k = pg.get()
                ptb = pt[:].bitcast(BF16)
                for hp in range(4):
                    op("pe", lambda e, hp=hp, ptb=ptb, ib=ib: e.transpose(out=ptb[:, hp * 128:(hp + 1) * 128], in_=ib[:, hp * 128:(hp + 1) * 128], identity=ident[:, :]),
                       reads=[ib_k, "ident"], writes=[pt_k])
                op("act", lambda e, ptb=ptb, tb=tb: e.copy(out=iqT[:, :, tb * 128:(tb + 1) * 128], in_=ptb[:, 0:512].rearrange("p (h t) -> p h t", h=4)),
                   reads=[pt_k], writes=[("iqT", tb)])
                ps, ps_k = pg.get()
                proj_tm(xnT, xnT_k, 128, wiw_t, "wiw", 8, ps, ps_k)
                op("dve", lambda e, ps=ps, tb=tb: e.tensor_scalar(out=iwt[:, tb, :], in0=ps[:, 0:8], scalar1=SC, scalar2=None, op0=ALU.mult),
                   reads=[ps_k], writes=[("iwt", tb)])
            psa, psa_k = pg.get()
            proj_tm(xnT, xnT_k, rows, wga, wga_k, 512, psa, psa_k)
            psg, psg_k = pg.get()
            proj_tm(xnT, xnT_k, rows, wgg, wgg_k, 512, psg, psg_k)
            jk, jk_k = jk_pool.get()
            op("act", lambda e, jk=jk, psg=psg, rows=rows: e.activation(out=jk[0:rows, :], in_=psg[0:rows, :], func=AF.Sigmoid),
               reads=[psg_k], writes=[jk_k])
            ub, ub_k = tm_pool.get()
            op("dve", lambda e, ub=ub, psa=psa, jk=jk, rows=rows: e.tensor_tensor(out=ub[0:rows, :], in0=psa[0:rows, :], in1=jk[0:rows, :], op=ALU.mult),
               reads=[psa_k, jk_k], writes=[ub_k])
            pt, pt_k = pg.get()
            ptb = pt[:].bitcast(BF16)
            for cc in range(4):
                op("pe", lambda e, cc=cc, ptb=ptb, ub=ub, rows=rows: e.transpose(out=ptb[:, cc * 128:cc * 128 + rows], in_=ub[0:rows, cc * 128:(cc + 1) * 128],
                                                                              identity=ident[0:rows, 0:rows]),
                   reads=[ub_k, "ident"], writes=[pt_k])
            op("act", lambda e, ptb=ptb, r0=r0, rows=rows: e.copy(out=uT[:, :, r0:r0 + rows], in_=ptb[:, 0:512].rearrange("p (c t) -> p c t", c=4)[:, :, 0:rows]),
               reads=[pt_k], writes=[("uT", ti)])

        run_pipelined([sa_tile(ti) for ti in range(5)])
        stage[0] = 3
        UTK = [("uT", ti) for ti in range(5)]
        for cc in range(4):
            op("dve", lambda e, cc=cc: e.tensor_tensor(out=Dcc[:, :, :], in0=identf[:, :].unsqueeze(1).to_broadcast([128, 31, 128]),
                                                       in1=cwT[:, cc, :].unsqueeze(2).to_broadcast([128, 31, 128]), op=ALU.mult),
               reads=["identf", "cwT"], writes=["Dcc"])
            for tb in range(4):
                ps, ps_k = pg.get()
                for j in range(31):
                    c0 = 2 + tb * 128 + j
                    op("pe", lambda e, ps=ps, cc=cc, c0=c0, j=j: e.matmul(ps[:, 0:128], lhsT=uT[:, cc, c0:c0 + 128], rhs=Dcc[:, j, :], start=(j == 0), stop=(j == 30)),
                       reads=UTK + ["Dcc"], writes=[ps_k])
                op("act", lambda e, ps=ps, tb=tb, cc=cc: e.copy(out=ysb[:, tb, cc * 128:(cc + 1) * 128], in_=ps[:, 0:128]),
                   reads=[ps_k], writes=[("ysb", tb)])
        for tb in range(4):
            yk = ("ysb", tb)
            y = ysb[:, tb, :]
            op("dve", lambda e, y=y: e.tensor_tensor(out=y, in0=y, in1=cb_bc[:, :], op=ALU.add), reads=[yk, "cb_bc"], writes=[yk])
            for g in range(4):
                op("dve", lambda e, y=y, g=g: e.bn_stats(out=cst[:, g, :], in_=y[:, g * 128:(g + 1) * 128]), reads=[yk], writes=[("cst", g)])
                op("dve", lambda e, g=g: e.bn_aggr(out=cmv[:, g, :], in_=cst[:, g, :]), reads=[("cst", g)], writes=[("cmv", g)])
            CM = [("cmv", g) for g in range(4)]
            op("dve", lambda e: e.tensor_scalar(out=crs[:, :], in0=cmv[:, :, 1], scalar1=EPS, scalar2=None, op0=ALU.add), reads=CM, writes=["crs"])
            op("act", lambda e: e.activation(out=crs[:, :], in_=crs[:, :], func=AF.Sqrt), reads=["crs"], writes=["crs"])
            op("dve", lambda e: e.reciprocal(out=crs[:, :], in_=crs[:, :]), reads=["crs"], writes=["crs"])
            for g in range(4):
                op("dve", lambda e, y=y, g=g: e.tensor_scalar(out=y[:, g * 128:(g + 1) * 128], in0=y[:, g * 128:(g + 1) * 128],
                                                            scalar1=cmv[:, g, 0:1], scalar2=crs[:, g:g + 1], op0=ALU.subtract, op1=ALU.mult),
                   reads=[yk, "crs"] + CM, writes=[yk])
            op("dve", lambda e, y=y: e.tensor_tensor(out=y, in0=y, in1=cg_bc[:, :], op=ALU.mult), reads=[yk, "cg_bc"], writes=[yk])
            op("dve", lambda e, y=y: e.tensor_tensor(out=y, in0=y, in1=cnb_bc[:, :], op=ALU.add), reads=[yk, "cnb_bc"], writes=[yk])
            op("act", lambda e, y=y, tb=tb: e.activation(out=mixed[:, tb, 512:1024], in_=y, func=AF.Silu), reads=[yk], writes=[("mixC", tb)])

        stage[0] = 4
        for tb in range(4):
            if tb == 3:
                for _ in range(4):
                    issue_next()
            op("act", lambda e, tb=tb: e.activation(out=wabs2[:, :], in_=iwt[:, tb, :], func=AF.Abs, scale=2.0),
               reads=[("iwt", tb)], writes=["wabs2"])
            op("dve", lambda e, tb=tb: e.tensor_scalar(out=wsgn[:, :], in0=iwt[:, tb, :], scalar1=0.0, scalar2=0.5, op0=ALU.is_ge, op1=ALU.subtract),
               reads=[("iwt", tb)], writes=["wsgn"])
            chunks = [(0, NMETA)] + [(NMETA + 512 * m, 512) for m in range(NCHK)]
            SCK = []
            wcnt = 0
            for ci, (c0, cn) in enumerate(chunks):
                ikk = [("ikT", 0)] if ci == 0 else [("ikT", 1 + 4 * (ci - 1) + q) for q in range(4)]
                sk = ("sc", ci)
                SCK.append(sk)
                for h in range(8):
                    par, hp = h % 2, h // 2
                    ps, ps_k = pg.get()
                    op("pe", lambda e, ps=ps, par=par, hp=hp, tb=tb, c0=c0, cn=cn: e.matmul(ps[:, 0:cn], lhsT=iqT[par * 64:(par + 1) * 64, hp, tb * 128:(tb + 1) * 128],
                                                                                         rhs=ikT[par * 64:(par + 1) * 64, c0:c0 + cn], start=True, stop=True),
                       reads=[("iqT", tb)] + ikk, writes=[ps_k])
                    R, R_k = R_pool.get()
                    op("act", lambda e, R=R, ps=ps, cn=cn, h=h: e.activation(out=R[:, 0:cn], in_=ps[:, 0:cn], func=AF.Relu, scale=wabs2[:, h:h + 1]),
                       reads=[ps_k, "wabs2"], writes=[R_k])
                    if h == 0:
                        op("dve", lambda e, R=R, c0=c0, cn=cn: e.tensor_scalar(out=scores[:, c0:c0 + cn], in0=R[:, 0:cn], scalar1=wsgn[:, 0:1], scalar2=None, op0=ALU.mult),
                           reads=[R_k, "wsgn"], writes=[sk])
                    else:
                        op("dve", lambda e, R=R, c0=c0, cn=cn, h=h: e.scalar_tensor_tensor(out=scores[:, c0:c0 + cn], in0=R[:, 0:cn], scalar=wsgn[:, h:h + 1],
                                                                                         in1=scores[:, c0:c0 + cn], op0=ALU.mult, op1=ALU.add),
                           reads=[R_k, "wsgn", sk], writes=[sk])
                if c0 >= W0 and ci > 0:
                    col = (sl * 4 + tb) * 2 + wcnt
                    negm, negm_k = negm_pool.get()
                    op("dve", lambda e, col=col, negm=negm: e.tensor_scalar(out=negm[:, :], in0=iota[:, :], scalar1=qrel[:, col:col + 1], scalar2=-1e30,
                                                                          op0=ALU.is_gt, op1=ALU.mult), reads=["iota", "qrelc"], writes=[negm_k])
                    jk, jk_k = jk_pool.get()
                    op("dve", lambda e, jk=jk, c0=c0, negm=negm: e.tensor_tensor(out=jk[:, :], in0=scores[:, c0:c0 + 512], in1=negm[:, :], op=ALU.subtract),
                       reads=[sk, negm_k], writes=[jk_k])
                    op("dve", lambda e, jk=jk, wcnt=wcnt: e.tensor_reduce(out=bis[:, 5 + wcnt:6 + wcnt], in_=jk[:, :], axis=AX.X, op=ALU.min),
                       reads=[jk_k], writes=[("bis", 5 + wcnt)])
                    op("dve", lambda e, c0=c0, negm=negm: e.tensor_tensor(out=scores[:, c0:c0 + 512], in0=scores[:, c0:c0 + 512], in1=negm[:, :], op=ALU.add),
                       reads=[sk, negm_k], writes=[sk])
                    wcnt += 1
            assert wcnt == 2
            stage[0] = 5
            nD = max(1, int(round(0.45 * NCHK)))
            ED = NMETA + 512 * nD
            EA = E - ED
            SCK_D = SCK[0:1 + nD]
            SCK_A = SCK[1 + nD:]
            op("dve", lambda e, E=E: e.max(out=top8[:, :], in_=scores[:, 0:E]), reads=SCK, writes=["top8"])
            op("dve", lambda e, W0=W0: e.tensor_reduce(out=bis[:, 7:8], in_=scores[:, 0:W0], axis=AX.X, op=ALU.min), reads=SCK, writes=[("bis", 7)])
            op("dve", lambda e: e.tensor_reduce(out=bis[:, 0:1], in_=bis[:, 5:8], axis=AX.X, op=ALU.min),
               reads=[("bis", 5), ("bis", 6), ("bis", 7)], writes=[("bis", 0)])
            op("dve", lambda e: e.tensor_tensor(out=bis[:, 1:2], in0=top8[:, 0:1], in1=bis[:, 0:1], op=ALU.subtract),
               reads=["top8", ("bis", 0)], writes=[("bis", 1)])
            op("dve", lambda e: e.tensor_scalar(out=bis[:, 8:8 + NBIS], in0=pow2[:, :], scalar1=bis[:, 1:2], scalar2=None, op0=ALU.mult),
               reads=["pow2", ("bis", 1)], writes=["bisW"])
            op("dve", lambda e: e.tensor_tensor(out=bis[:, 2:3], in0=bis[:, 8:9], in1=bis[:, 0:1], op=ALU.add),
               reads=["bisW", ("bis", 0)], writes=[("bis", 2)])
            for k in range(NBIS):
                op("dve", lambda e, ED=ED: e.tensor_scalar(out=bigjunk[:, 0:ED], in0=scores[:, 0:ED], scalar1=bis[:, 2:3], scalar2=None,
                                                          op0=ALU.is_ge, op1=ALU.add, accum_out=bis[:, 3:4]),
                   reads=SCK_D + [("bis", 2)], writes=["bigjunkD", ("bis", 3)])
                op("act", lambda e, ED=ED, E=E, EA=EA: e.activation(out=bigjunk[:, 0:EA].bitcast(mybir.dt.int8), in_=scores[:, ED:E], func=AF.Sign,
                                                            bias=bis[:, 2:3], scale=-1.0, accum_out=bis[:, 5:6]),
                   reads=SCK_A + [("bis", 2)], writes=["bigjunkA", ("bis", 5)])
                op("dve", lambda e: e.scalar_tensor_tensor(out=bis[:, 6:7], in0=bis[:, 3:4], scalar=2.0, in1=bis[:, 5:6], op0=ALU.mult, op1=ALU.subtract),
                   reads=[("bis", 3), ("bis", 5)], writes=[("bis", 6)])
                op("dve", lambda e, k=k, EA=EA: e.tensor_scalar(out=bis[:, 4:5], in0=bis[:, 6:7], scalar1=float(2 * KTOP - 1 - EA), scalar2=bis[:, 8 + k:9 + k],
                                                               op0=ALU.is_ge, op1=ALU.mult), reads=[("bis", 6), "bisW"], writes=[("bis", 4)])
                if k < NBIS - 1:
                    op("dve", lambda e, k=k: e.scalar_tensor_tensor(out=bis[:, 2:3], in0=bis[:, 4:5], scalar=bis[:, 9 + k:10 + k], in1=bis[:, 2:3],
                                                                  op0=ALU.subtract, op1=ALU.add),
                       reads=[("bis", 4), "bisW", ("bis", 2)], writes=[("bis", 2)])
                else:
                    op("dve", lambda e, k=k: e.scalar_tensor_tensor(out=bis[:, 0:1], in0=bis[:, 4:5], scalar=bis[:, 8 + k:9 + k], in1=bis[:, 2:3],
                                                                  op0=ALU.subtract, op1=ALU.add),
                       reads=[("bis", 4), "bisW", ("bis", 2)], writes=[("bis", 0)])
            stage[0] = 6
            nkb_total = 1 + 4 * NCHK
            kblist = []
            for ci, (c0, cn) in enumerate(chunks):
                for kb in range(1 if ci == 0 else 4):
                    kblist.append((ci, c0, cn, kb))
            cstate = {}

            def chunk_setup(ci, c0, cn):
                mbt, mb_k = mb_pool.get()
                op("dve", lambda e, mbt=mbt, c0=c0, cn=cn: e.tensor_scalar(out=mbt[:, 0:cn], in0=scores[:, c0:c0 + cn], scalar1=bis[:, 0:1], scalar2=-30000.0,
                                                                          op0=ALU.is_lt, op1=ALU.mult), reads=[SCK[ci], ("bis", 0)], writes=[mb_k])
                if ci == 0:
                    cstate[ci] = (mbt, mb_k, kmeta, vmeta, "kmeta", "vmeta")
                    return
                kvn = kv_ctr[0] % 2
                kv_ctr[0] += 1
                kbuf, vbuf = kv_pool[kvn]
                kvk, vvk = ("kbuf", kvn), ("vbuf", kvn)
                tl = [1 + 4 * (ci - 1) + q for q in range(4)]
                op("sp", lambda e, kbuf=kbuf, c0=c0: e.dma_start(out=kbuf[:, :, :], in_=k_scr[:, :, c0:c0 + 512].rearrange("h p t -> p h t")),
                   reads=[("k_scr", t) for t in tl], writes=[kvk], dma=True)
                op("sp", lambda e, vbuf=vbuf, c0=c0: e.dma_start(out=vbuf[:, :, :, :].rearrange("p g h e -> p g (h e)"),
                                                                 in_=v_scr[c0:c0 + 512, :, :].rearrange("(g p) h e -> p g (h e)", p=128)),
                   reads=[("v_scr", t) for t in tl], writes=[vvk], dma=True)
                cstate[ci] = (mbt, mb_k, kbuf, vbuf, kvk, vvk)

            def emit_S(i):
                ci, c0, cn, kb = kblist[i]
                if kb == 0:
                    chunk_setup(ci, c0, cn)
                mbt, mb_k, kbuf, vbuf, kvk, vvk = cstate[ci]
                ks = NMETA if ci == 0 else 128
                st, st_k = pg.get()
                for h in range(4):
                    op("pe", lambda e, st=st, h=h, kbuf=kbuf, kb=kb, ks=ks, tb=tb: e.matmul(st[0:ks, h * 128:(h + 1) * 128], lhsT=kbuf[:, h, kb * 128:kb * 128 + ks],
                                                                                         rhs=qT[:, h, tb * 128:(tb + 1) * 128], start=True, stop=False),
                       reads=[kvk, ("qT", tb)], writes=[st_k])
                    op("pe", lambda e, st=st, h=h, mbt=mbt, kb=kb, ks=ks: e.matmul(st[0:ks, h * 128:(h + 1) * 128], lhsT=mbt[:, kb * 128:kb * 128 + ks],
                                                                                 rhs=ident[:, :], start=False, stop=True),
                       reads=[mb_k, "ident"], writes=[st_k])
                PT, PT_k = PT_pool.get()
                op("act", lambda e, PT=PT, st=st, ks=ks: e.activation(out=PT[0:ks, :], in_=st[0:ks, :], func=AF.Exp, scale=128 ** -0.5),
                   reads=[st_k], writes=[PT_k])
                return (PT, PT_k)

            def emit_P(i, PTs):
                ci, c0, cn, kb = kblist[i]
                mbt, mb_k, kbuf, vbuf, kvk, vvk = cstate[ci]
                ks = NMETA if ci == 0 else 128
                PT, PT_k = PTs
                for h in range(4):
                    acc = pacc[h // 2]
                    a0 = (h % 2) * 256
                    rhs = vbuf[0:ks, h, :] if ci == 0 else vbuf[:, kb, h, :]
                    op("pe", lambda e, acc=acc, a0=a0, PT=PT, h=h, ks=ks, rhs=rhs, i=i: e.matmul(acc[:, a0:a0 + 129], lhsT=PT[0:ks, h * 128:(h + 1) * 128], rhs=rhs,
                                                                                           start=(i == 0 and h % 2 == 0), stop=(i == nkb_total - 1),
                                                                                           skip_group_check=True),
                       reads=[PT_k, vvk], writes=[("pacc", h // 2)])

            prev = emit_S(0)
            for i in range(nkb_total):
                nxt = emit_S(i + 1) if i + 1 < nkb_total else None
                emit_P(i, prev)
                prev = nxt
            for h in range(4):
                acc = pacc[h // 2]
                a0 = (h % 2) * 256
                op("dve", lambda e, acc=acc, a0=a0, h=h: e.reciprocal(out=rden[:, h:h + 1], in_=acc[:, a0 + 128:a0 + 129]),
                   reads=[("pacc", h // 2)], writes=[("rden", h)])
                op("dve", lambda e, acc=acc, a0=a0, h=h, tb=tb: e.tensor_scalar(out=mixed[:, tb, h * 128:(h + 1) * 128], in0=acc[:, a0:a0 + 128], scalar1=rden[:, h:h + 1],
                                                                              scalar2=None, op0=ALU.mult),
                   reads=[("pacc", h // 2), ("rden", h)], writes=[("mixA", tb, h)])

        stage[0] = 7
        for pair in range(2):
            h1s = []
            while len(wfifo) < 2:
                issue_next()
            wos = [wfifo.pop(0), wfifo.pop(0)]
            for t2 in range(2):
                tb = 2 * pair + t2
                pt, pt_k = pg.get()
                ptb = pt[:].bitcast(BF16)
                for ec in range(8):
                    op("pe", lambda e, ptb=ptb, ec=ec, tb=tb: e.transpose(out=ptb[:, ec * 128:(ec + 1) * 128], in_=mixed[:, tb, ec * 128:(ec + 1) * 128], identity=ident[:, :]),
                       reads=[("mixA", tb, h) for h in range(4)] + [("mixC", tb), "ident"], writes=[pt_k])
                op("act", lambda e, ptb=ptb: e.copy(out=mT[:, :, :], in_=ptb[:, :].rearrange("p (c t) -> p c t", c=8)), reads=[pt_k], writes=["mT"])
                xt, xt_k = xt_pool.get()
                op("sp", lambda e, xt=xt, sl=sl, tb=tb: e.dma_start(out=xt[:, :], in_=xown[sl, 32 + tb * 128:32 + (tb + 1) * 128, :]), writes=[xt_k], dma=True)
                h1, h1_k = xt, xt_k
                for half in range(2):
                    wo, wo_k = wos[half]
                    for ec in range(8):
                        op("pe", lambda e, half=half, ec=ec, wo=wo: e.matmul(psc[half][:, :], lhsT=mT[:, ec, :], rhs=wo[:, ec, :], start=(ec == 0), stop=(ec == 7)),
                           reads=["mT", wo_k], writes=[("psc", half)])
                    op("dve", lambda e, half=half, h1=h1, xt=xt: e.tensor_tensor(out=h1[:, half * 512:(half + 1) * 512], in0=psc[half][:, :],
                                                                               in1=xt[:, half * 512:(half + 1) * 512], op=ALU.add),
                       reads=[("psc", half), xt_k], writes=[h1_k])
                rmsnorm_T(h1, h1_k, 128, gM, hnT[:, :, t2 * 128:(t2 + 1) * 128], ("hnT", t2))
                h1s.append((h1, h1_k, tb))
            issue_next()
            issue_next()
            accs = [[(psc[0], ("psc", 0)), (psc[1], ("psc", 1))], [(pacc[0], ("pacc", 0)), (pacc[1], ("pacc", 1))]]
            fstate = {}

            def ffn_up(i):
                fg, fc = divmod(i, 4)
                if fc == 0:
                    while len(wfifo) < 2:
                        issue_next()
                    fstate[fg] = (wfifo.pop(0), wfifo.pop(0))
                (wu, wu_k), (wdv, wd_k) = fstate[fg]
                ps, ps_k = pg.get()
                for dc in range(8):
                    op("pe", lambda e, ps=ps, wu=wu, dc=dc, fc=fc: e.matmul(ps[:, 0:256], lhsT=wu[:, dc, fc * 128:(fc + 1) * 128], rhs=hnT[:, dc, :],
                                                                         start=(dc == 0), stop=(dc == 7)),
                       reads=[wu_k, ("hnT", 0), ("hnT", 1)], writes=[ps_k])
                if fc == 3:
                    issue_next()
                uf, uf_k = uf_pool.get()
                op("act", lambda e, uf=uf, ps=ps: e.activation(out=uf[:, :], in_=ps[:, 0:256], func=AF.Relu), reads=[ps_k], writes=[uf_k])
                op("pool", lambda e, uf=uf: e.tensor_tensor(out=uf[:, :], in0=uf[:, :], in1=uf[:, :], op=ALU.mult), reads=[uf_k], writes=[uf_k])
                return uf, uf_k

            def ffn_down(i, ufs):
                fg, fc = divmod(i, 4)
                uf, uf_k = ufs
                (wu, wu_k), (wdv, wd_k) = fstate[fg]
                for t2 in range(2):
                    for half in range(2):
                        acc, acc_k = accs[t2][half]
                        op("pe", lambda e, acc=acc, uf=uf, t2=t2, half=half, wdv=wdv, fc=fc, i=i: e.matmul(
                            acc[:, :], lhsT=uf[:, t2 * 128:(t2 + 1) * 128], rhs=wdv[:, fc, half * 512:(half + 1) * 512], start=(i == 0), stop=(i == 31)),
                           reads=[uf_k, wd_k], writes=[acc_k])
                if fc == 3:
                    issue_next()

            cur = ffn_up(0)
            for i in range(32):
                nxt = ffn_up(i + 1) if i + 1 < 32 else None
                ffn_down(i, cur)
                cur = nxt
            for t2 in range(2):
                h1, h1_k, tb = h1s[t2]
                for half in range(2):
                    acc, acc_k = accs[t2][half]
                    op("dve", lambda e, acc=acc, h1=h1, half=half: e.tensor_tensor(out=h1[:, half * 512:(half + 1) * 512], in0=acc[:, :],
                                                                                 in1=h1[:, half * 512:(half + 1) * 512], op=ALU.add),
                       reads=[acc_k, h1_k], writes=[h1_k])
                st, st_k = st_pool.get()
                jk, jk_k = jk_pool.get()
                op("act", lambda e, jk=jk, h1=h1, st=st: e.activation(out=jk[:, :], in_=h1[:, 0:512], func=AF.Square, accum_out=st[:, 0:1]),
                   reads=[h1_k], writes=[jk_k, (st_k, 0)])
                op("act", lambda e, jk=jk, h1=h1, st=st: e.activation(out=jk[:, :], in_=h1[:, 512:1024], func=AF.Square, accum_out=st[:, 1:2]),
                   reads=[h1_k], writes=[jk_k, (st_k, 1)])
                op("dve", lambda e, st=st: e.tensor_tensor(out=st[:, 2:3], in0=st[:, 0:1], in1=st[:, 1:2], op=ALU.add), reads=[(st_k, 0), (st_k, 1)], writes=[(st_k, 2)])
                op("dve", lambda e, st=st: e.tensor_scalar(out=st[:, 3:4], in0=st[:, 2:3], scalar1=1.0 / D, scalar2=EPS, op0=ALU.mult, op1=ALU.add),
                   reads=[(st_k, 2)], writes=[(st_k, 3)])
                op("act", lambda e, st=st: e.activation(out=st[:, 4:5], in_=st[:, 3:4], func=AF.Sqrt), reads=[(st_k, 3)], writes=[(st_k, 4)])
                op("dve", lambda e, st=st: e.reciprocal(out=st[:, 5:6], in_=st[:, 4:5]), reads=[(st_k, 4)], writes=[(st_k, 5)])
                op("dve", lambda e, h1=h1, st=st: e.scalar_tensor_tensor(out=h1[:, :], in0=h1[:, :], scalar=st[:, 5:6], in1=gF[:, :], op0=ALU.mult, op1=ALU.mult),
                   reads=[h1_k, (st_k, 5), "gF"], writes=[h1_k])
                r0 = sl * 512 + tb * 128
                op("act", lambda e, h1=h1, r0=r0: e.dma_start(out=out_d[r0:r0 + 128, :], in_=h1[:, :]), reads=[h1_k], writes=[("out", r0)], dma=True)

    if os.environ.get("KDEBUG"):
        print("sbuf remaining", nc.sbuf_bytes_remaining, "ops", {e: len(S_.ops[e]) for e in ENGS})
    with ExitStack() as stack:
        S_.emit(nc, stack)
    return nc


def _rope_tab(pos):
    pos = np.asarray(pos, np.float32)
    out = np.zeros((len(pos), 512), np.float32)
    inv = (np.float32(500000.0) ** (-np.arange(0, 32, 2, dtype=np.float32) / np.float32(32))).astype(np.float32)
    ang = pos[:, None] * inv[None, :]
    c, s = np.cos(ang).astype(np.float32), np.sin(ang).astype(np.float32)
    out[:, 0:128] = np.tile(np.concatenate([c, c], 1), (1, 4))
    out[:, 128:256] = np.tile(np.concatenate([s, s], 1), (1, 4))
    inv = (np.float32(500000.0) ** (-np.arange(0, 16, 2, dtype=np.float32) / np.float32(16))).astype(np.float32)
    ang = pos[:, None] * inv[None, :]
    c, s = np.cos(ang).astype(np.float32), np.sin(ang).astype(np.float32)
    out[:, 256:384] = np.tile(np.concatenate([c, c], 1), (1, 8))
    out[:, 384:512] = np.tile(np.concatenate([s, s], 1), (1, 8))
    return out


def _chunk_of(sl, half):
    return 2 * sl + (sl % 2) if half == 0 else 2 * sl + 1 - (sl % 2)


_NC_CACHE = {}


def kernel(x, meta_tokens, attn_norm_g, w_in, conv_w, conv_b, conv_norm_g, conv_norm_b,
           w_out, mlp_norm_g, w_up, w_down, final_norm_g):
    x = np.asarray(x, np.float32)
    B, S, _ = x.shape
    T = NMETA + S
    NSLOT = S // 1024
    if S not in _NC_CACHE:
        _NC_CACHE[S] = build(S)
    nc = _NC_CACHE[S]
    f = lambda a: np.ascontiguousarray(np.asarray(a, np.float32))
    meta = f(meta_tokens)
    ropek = _rope_tab(np.arange(T))
    p = np.arange(128)
    consts = {
        "iota": np.tile(np.arange(512, dtype=np.float32)[None, :], (128, 1)),
        "pow2": np.tile((2.0 ** -(np.arange(NBIS) + 1.0)).astype(np.float32)[None, :], (128, 1)),
        "e32": (np.arange(32)[None, :] == (p % 32)[:, None]).astype(np.float32),
        "g4": (np.arange(4)[None, :] == (p // 32)[:, None]).astype(np.float32),
        "dm": np.concatenate([(np.arange(128)[None, :] == (32 * g + p % 32)[:, None]).astype(np.float32) for g in range(4)], axis=1),
    }
    shared = {
        "w_in": f(w_in[0]), "w_out": f(w_out[0]), "w_up": f(w_up[0]), "w_down": f(w_down[0]),
        "attn_g": f(attn_norm_g[0]), "mlp_g": f(mlp_norm_g[0]), "final_g": f(final_norm_g),
        "conv_w": f(conv_w[0]), "conv_b": f(conv_b[0]), "cn_g": f(conv_norm_g[0]), "cn_b": f(conv_norm_b[0]),
        "ropek": ropek,
    }
    shared.update(consts)
    in_maps = []
    for core in range(8):
        b, half = core // 2, core % 2
        hall = np.concatenate([meta, x[b]], axis=0)
        xown = np.zeros((NSLOT, 544, D), np.float32)
        ropeq = np.zeros((NSLOT, 512, 512), np.float32)
        qrel = np.zeros((128, NSLOT * 8), np.float32)
        for sl in range(NSLOT):
            c = _chunk_of(sl, half)
            p0 = NMETA + 512 * c
            lo = p0 - 32
            src_lo = max(lo, 0)
            xown[sl, src_lo - lo:, :] = hall[src_lo:p0 + 512]
            ropeq[sl] = ropek[p0:p0 + 512]
            w0 = NMETA + 1024 * sl
            for tb in range(4):
                for wc in range(2):
                    qrel[:, (sl * 4 + tb) * 2 + wc] = (p0 + tb * 128 + p) - w0 - 512 * wc
        m = dict(shared)
        m.update({"xall": hall, "xown": xown, "ropeq": ropeq, "qrel": qrel})
        in_maps.append(m)
    res = run_bass_kernel_spmd(nc, in_maps, core_ids=list(range(8)))
    out = np.zeros((B, S, D), np.float32)
    for core in range(8):
        b, half = core // 2, core % 2
        o = res.results[core]["out"]
        for sl in range(NSLOT):
            c = _chunk_of(sl, half)
            out[b, 512 * c:512 * (c + 1)] = o[sl * 512:(sl + 1) * 512]
    return out
```

```python
import numpy as np
from contextlib import ExitStack
import concourse.bass as bass
import concourse.mybir as mybir
from concourse.bass_utils import run_bass_kernel_spmd

F32 = mybir.dt.float32
BF16 = mybir.dt.bfloat16
ALU = mybir.AluOpType
AF = mybir.ActivationFunctionType
AX = mybir.AxisListType

D = 1024
NMETA = 16
DIN = 3144
DFF = 4096
EPS = 1e-5
KTOP = 256
NBIS = 20
ENGS = ("pe", "act", "dve", "pool", "sp")
SEM_CH = 30000
NDMA_SEM = 12


class Op:
    __slots__ = ("eng", "fn", "deps", "dma", "signals", "sig", "dma_idx")


class Sched:
    def __init__(self):
        self.ops = {e: [] for e in ENGS}
        self.lastw = {}
        self.readers = {}
        self.ndma = {e: 0 for e in ENGS}

    def op(self, eng, fn, reads=(), writes=(), dma=False):
        o = Op()
        o.eng = eng
        o.fn = fn
        o.dma = dma
        o.signals = False
        o.sig = None
        deps = {}
        xr = [k for k in reads if isinstance(k, tuple) and k[0] in ("pg", "psc", "pacc")]
        if xr:
            writes = list(writes) + [k for k in xr if k not in writes]
        for k in reads:
            w = self.lastw.get(k)
            if w is not None:
                deps[id(w)] = (w, True)
        for k in writes:
            w = self.lastw.get(k)
            if w is not None and id(w) not in deps:
                deps[id(w)] = (w, False)
            for r in self.readers.get(k, ()):
                if id(r) not in deps:
                    deps[id(r)] = (r, False)
        o.deps = []
        for d, raw in deps.values():
            if d.eng == eng and not d.dma:
                if eng == "pe":
                    continue
                if not raw and not dma:
                    continue
            d.signals = True
            o.deps.append(d)
        if dma:
            o.dma_idx = self.ndma[eng]
            self.ndma[eng] += 1
            o.signals = True
        for k in reads:
            self.readers.setdefault(k, []).append(o)
        for k in writes:
            self.lastw[k] = o
            self.readers[k] = []
        self.ops[eng].append(o)
        return o

    def emit(self, nc, stack):
        nsig = {}
        for e in ENGS:
            n = 0
            for o in self.ops[e]:
                if o.signals and not o.dma:
                    o.sig = n
                    n += 1
            nsig[e] = n
        csem = {}
        for e in ENGS:
            nch = (nsig[e] + SEM_CH - 1) // SEM_CH
            csem[e] = [stack.enter_context(nc.semaphore(f"c_{e}_{i}")) for i in range(nch)]
        dsem = {}
        for e in ENGS:
            if self.ndma[e]:
                dsem[e] = [stack.enter_context(nc.semaphore(f"d_{e}_{i}")) for i in range(NDMA_SEM)]

        def target(d):
            if d.dma:
                return dsem[d.eng][d.dma_idx % NDMA_SEM], 16 * (d.dma_idx // NDMA_SEM + 1)
            return csem[d.eng][d.sig // SEM_CH], d.sig % SEM_CH + 1

        block = stack.enter_context(nc.Block())

        def run(e):
            def body(eng):
                waited = {}
                for o in self.ops[e]:
                    tg = [target(d) for d in o.deps]
                    if o.dma and o.dma_idx >= NDMA_SEM:
                        tg.append((dsem[e][o.dma_idx % NDMA_SEM], 16 * (o.dma_idx // NDMA_SEM)))
                    for s, v in tg:
                        key = id(s)
                        if waited.get(key, 0) < v:
                            eng.wait_ge(s, v)
                            waited[key] = v
                    ins = o.fn(eng)
                    if o.signals:
                        if o.dma:
                            s, _ = target(o)
                            ins.then_inc(s, 16)
                        else:
                            ins.then_inc(csem[e][o.sig // SEM_CH], 1)
                if self.ndma[e]:
                    n = self.ndma[e]
                    for i in range(max(0, n - NDMA_SEM), n):
                        eng.wait_ge(dsem[e][i % NDMA_SEM], 16 * (i // NDMA_SEM + 1))
            return body

        for e, reg in (("pe", block.tensor), ("act", block.scalar), ("dve", block.vector),
                       ("pool", block.gpsimd), ("sp", block.sync)):
            if self.ops[e]:
                reg(run(e))


class Pool:
    def __init__(self, tiles, name, off=0):
        self.tiles = tiles
        self.name = name
        self.i = 0
        self.off = off

    def get(self):
        j = self.i % len(self.tiles)
        self.i += 1
        return self.tiles[j], (self.name, j + self.off)


def build(S):
    T = NMETA + S
    NCH = S // 512
    NSLOT = NCH // 2
    NKT = 1 + S // 128
    nc = bass.Bass("TRN2", target_bir_lowering=False)

    def din(name, shape, dt=F32):
        return nc.dram_tensor(name, shape, dt, kind="ExternalInput").ap()

    xall = din("xall", [T, D])
    xown = din("xown", [NSLOT, 544, D])
    ropek = din("ropek", [T, 512])
    ropeq = din("ropeq", [NSLOT, 512, 512])
    qrel_d = din("qrel", [128, NSLOT * 8])
    w_in = din("w_in", [D, DIN])
    w_out = din("w_out", [D, D])
    w_up = din("w_up", [D, DFF])
    w_down = din("w_down", [DFF, D])
    attn_g = din("attn_g", [D])
    mlp_g = din("mlp_g", [D])
    final_g = din("final_g", [D])
    conv_w = din("conv_w", [31, 512])
    conv_b = din("conv_b", [512])
    cn_g = din("cn_g", [512])
    cn_b = din("cn_b", [512])
    iota_d = din("iota", [128, 512])
    pow2_d = din("pow2", [128, NBIS])
    e32_d = din("e32", [128, 32])
    g4_d = din("g4", [128, 4])
    dm_d = din("dm", [128, 4 * 128])
    out_d = nc.dram_tensor("out", [NSLOT * 512, D], F32, kind="ExternalOutput").ap()

    k_scr = nc.dram_tensor("k_scr", [4, 128, T], BF16).ap()
    v_scr = nc.dram_tensor("v_scr", [T, 4, 129], BF16).ap()

    import os
    KSTOP = float(os.environ.get('KSTOP', '99'))
    S_ = Sched()
    stage = [0]

    def op(*a, **k):
        if stage[0] <= KSTOP:
            return S_.op(*a, **k)
        return None
    uid = [0]

    def sb(shape, dt, name=None):
        uid[0] += 1
        return nc.alloc_sbuf_tensor(f"{name or 't'}{uid[0]}", shape, dt)

    def mkpool(n, shape, dt, name):
        return Pool([sb(shape, dt, name) for _ in range(n)], name)

    identf = sb([128, 128], F32, "identf")
    ident = sb([128, 128], BF16, "ident")
    op("pool", lambda e: e.memset(identf[:], 0.0), writes=["identf"])
    op("pool", lambda e: e.affine_select(out=identf[:], in_=identf[:], pattern=[[-1, 128]],
                                         compare_op=ALU.not_equal, fill=1.0, base=0, channel_multiplier=1),
       reads=["identf"], writes=["identf"])
    op("dve", lambda e: e.tensor_copy(out=ident[:], in_=identf[:]), reads=["identf"], writes=["ident"])

    def load_const(name, shape, src, dt=F32):
        t = sb(shape, dt, name)
        op("sp", lambda e: e.dma_start(out=t[:], in_=src, allow_slow_non_contiguous=True), writes=[name], dma=True)
        return t

    iota = load_const("iota", [128, 512], iota_d)
    pow2 = load_const("pow2", [128, NBIS], pow2_d)
    e32 = load_const("e32", [128, 32], e32_d)
    g4 = load_const("g4", [128, 4], g4_d)
    qrel = load_const("qrelc", [128, NSLOT * 8], qrel_d)
    gA = load_const("gA", [128, 8], attn_g.rearrange("(c p) -> p c", p=128))
    gM = load_const("gM", [128, 8], mlp_g.rearrange("(c p) -> p c", p=128))
    def bcast_row(name, src, n):
        t = sb([128, n], F32, name)
        op("sp", lambda e: e.dma_start(out=t[:], in_=src.partition_broadcast(128)), writes=[name], dma=True)
        return t
    gF = bcast_row("gF", final_g, D)
    cb_bc = bcast_row("cb_bc", conv_b, 512)
    cg_bc = bcast_row("cg_bc", cn_g, 512)
    cnb_bc = bcast_row("cnb_bc", cn_b, 512)
    cwT = sb([128, 4, 31], F32, "cwT")
    for cc_ in range(4):
        op("sp", lambda e, cc_=cc_: e.dma_start(out=cwT[:, cc_, :], in_=conv_w[:, cc_ * 128:(cc_ + 1) * 128].rearrange("j p -> p j"),
                                                 allow_slow_non_contiguous=True), writes=["cwT"], dma=True)
    dmb = sb([128, 512], BF16, "dmb")
    op("pool", lambda e: e.dma_start(out=dmb[:], in_=dm_d), writes=["dmb"], dma=True)
    wik_t = sb([128, 8, 64], BF16, "wik")
    op("pool", lambda e: e.dma_start(out=wik_t[:, :, :], in_=w_in[:, 2048:2112].rearrange("(c p) n -> p c n", p=128)), writes=["wik"], dma=True)
    wiw_t = sb([128, 8, 8], BF16, "wiw")
    op("pool", lambda e: e.dma_start(out=wiw_t[:, :, :], in_=w_in[:, 2112:2120].rearrange("(c p) n -> p c n", p=128), allow_slow_non_contiguous=True),
       writes=["wiw"], dma=True)

    win_b = nc.dram_tensor("win_b", [D, DIN], BF16).ap()
    wout_b = nc.dram_tensor("wout_b", [D, D], BF16).ap()
    wup_b = nc.dram_tensor("wup_b", [D, DFF], BF16).ap()
    wdown_b = nc.dram_tensor("wdown_b", [DFF, D], BF16).ap()

    ikT = sb([128, T], BF16, "ikT")
    scores = sb([128, T], F32, "scores")
    wbuf = mkpool(4, [128, 8, 512], BF16, "wbuf")
    xt_pool = mkpool(3, [128, D], F32, "xt")
    xn_pool = mkpool(1, [128, D], BF16, "xn")
    xnT_pool = mkpool(2, [128, 8, 128], BF16, "xnT")
    rope_pool = mkpool(4, [128, 256], F32, "rope")
    st_pool = mkpool(4, [128, 8], F32, "stat")
    tm_pool = mkpool(3, [128, 512], BF16, "tm")
    rt_pool = mkpool(2, [128, 256], F32, "rt")
    jk_pool = mkpool(2, [128, 512], F32, "jk")
    bigjunk = sb([128, NMETA + 512 * ((T // 512) // 2 + 1)], mybir.dt.uint8, "bigjunk")
    vb_pool = mkpool(2, [128, 4, 129], BF16, "vb")
    for vbt_ in vb_pool.tiles:
        pass
    pg = Pool([nc.alloc_psum_tensor(f"pg{i}", [128, 512], F32) for i in range(4)], "pg")
    psc = [nc.alloc_psum_tensor(f"psc{i}", [128, 512], F32) for i in range(2)]
    pacc = [nc.alloc_psum_tensor(f"pacc{i}", [128, 512], F32) for i in range(2)]

    def pg_bf16(t):
        return t.bitcast(BF16) if hasattr(t, "bitcast") else None

    def rmsnorm_T(x_t, x_k, rows, gcol, xnT, xnT_k):
        st, st_k = st_pool.get()
        jk, jk_k = jk_pool.get()
        xn, xn_k = xn_pool.get()
        op("act", lambda e: e.activation(out=jk[0:rows, :], in_=x_t[0:rows, 0:512], func=AF.Square,
                                         accum_out=st[0:rows, 0:1]), reads=[x_k], writes=[jk_k, (st_k, 0)])
        op("act", lambda e: e.activation(out=jk[0:rows, :], in_=x_t[0:rows, 512:1024], func=AF.Square,
                                         accum_out=st[0:rows, 1:2]), reads=[x_k], writes=[jk_k, (st_k, 1)])
        op("dve", lambda e: e.tensor_tensor(out=st[0:rows, 2:3], in0=st[0:rows, 0:1], in1=st[0:rows, 1:2], op=ALU.add),
           reads=[(st_k, 0), (st_k, 1)], writes=[(st_k, 2)])
        op("dve", lambda e: e.tensor_scalar(out=st[0:rows, 3:4], in0=st[0:rows, 2:3], scalar1=1.0 / D, scalar2=EPS,
                                            op0=ALU.mult, op1=ALU.add), reads=[(st_k, 2)], writes=[(st_k, 3)])
        op("act", lambda e: e.activation(out=st[0:rows, 4:5], in_=st[0:rows, 3:4], func=AF.Sqrt),
           reads=[(st_k, 3)], writes=[(st_k, 4)])
        op("dve", lambda e: e.reciprocal(out=st[0:rows, 5:6], in_=st[0:rows, 4:5]), reads=[(st_k, 4)], writes=[(st_k, 5)])
        op("dve", lambda e: e.tensor_scalar(out=xn[0:rows, :], in0=x_t[0:rows, :], scalar1=st[0:rows, 5:6], scalar2=None,
                                            op0=ALU.mult), reads=[x_k, (st_k, 5)], writes=[xn_k])
        pt, pt_k = pg.get()
        ptb = pt[:].bitcast(BF16)
        for dc in range(8):
            op("pe", lambda e, dc=dc: e.transpose(out=ptb[:, dc * 128:dc * 128 + rows], in_=xn[0:rows, dc * 128:(dc + 1) * 128],
                                                  identity=ident[0:rows, 0:rows]),
               reads=[xn_k, "ident"], writes=[pt_k])
        op("dve", lambda e: e.tensor_tensor(out=xnT[:, :, 0:rows],
                                            in0=ptb.rearrange("p (c t) -> p c t", c=8)[:, :, 0:rows],
                                            in1=gcol[:, :].unsqueeze(2).to_broadcast([128, 8, rows]), op=ALU.mult),
           reads=[pt_k], writes=[xnT_k])
        return (st, st_k)

    def wviews(kind, i, w):
        if kind == "down":
            v = w[:, :, :].rearrange("p a b -> p (a b)").rearrange("p (f n) -> p f n", f=4)
            r = lambda a: a[i * 512:(i + 1) * 512, :].rearrange("(f p) n -> p f n", p=128)
            return v, r(w_down), r(wdown_b)
        src, dst = {"in": (w_in, win_b), "out": (w_out, wout_b), "up": (w_up, wup_b)}[kind]
        r = lambda a: a[:, i:i + 512].rearrange("(c p) n -> p c n", p=128) if kind == "in" else a[:, i * 512:(i + 1) * 512].rearrange("(c p) n -> p c n", p=128)
        return w[:, :, :], r(src), r(dst)

    def load_w(kind, i, cast=False):
        w, w_k = wbuf.get()
        v, src, scr = wviews(kind, i, w)
        if cast:
            op("pool", lambda e: e.dma_start(out=v, in_=src), writes=[w_k], dma=True)
        else:
            op("sp", lambda e: e.dma_start(out=v, in_=scr), reads=[("wcv", kind, i)], writes=[w_k], dma=True)
        return (v if kind == "down" else w), w_k

    def proj_tm(xnT, xnT_k, rows, w, w_k, ncols, ps, ps_k, pcol=0):
        for dc in range(8):
            op("pe", lambda e, dc=dc: e.matmul(ps[0:rows, pcol:pcol + ncols], lhsT=xnT[:, dc, 0:rows], rhs=w[:, dc, 0:ncols],
                                               start=(dc == 0), stop=(dc == 7)),
               reads=[xnT_k, w_k], writes=[ps_k])


    def rope_tm(ps, ps_k, rows, nh, hd, half, rp, rp_k, roff, dst, dst_k, dcol=0):
        r2 = 2 * half
        n = nh * hd
        xf, xf_k = jk_pool.get()
        op("act", lambda e: e.copy(out=xf[0:rows, 0:n], in_=ps[0:rows, 0:n]), reads=[ps_k], writes=[xf_k])
        op("act", lambda e: e.copy(out=dst[0:rows, dcol:dcol + n], in_=ps[0:rows, 0:n]), reads=[ps_k], writes=[dst_k])
        rt, rt_k = rt_pool.get()
        pv = xf[0:rows, 0:n].rearrange("p (h d) -> p h d", h=nh)
        A = rt[0:rows, 0:nh * r2].rearrange("p (h d) -> p h d", h=nh)
        Bm = rt[0:rows, 128:128 + nh * r2].rearrange("p (h d) -> p h d", h=nh)
        cc_ = rp[0:rows, roff:roff + nh * r2].rearrange("p (h d) -> p h d", h=nh)
        ss_ = rp[0:rows, roff + 128:roff + 128 + nh * r2].rearrange("p (h d) -> p h d", h=nh)
        op("dve", lambda e: e.tensor_tensor(out=A, in0=pv[:, :, 0:r2], in1=cc_, op=ALU.mult), reads=[xf_k, rp_k], writes=[(rt_k, 0)])
        op("dve", lambda e: e.tensor_tensor(out=Bm, in0=pv[:, :, 0:r2], in1=ss_, op=ALU.mult), reads=[xf_k, rp_k], writes=[(rt_k, 1)])
        dv = dst[0:rows, dcol:dcol + n].rearrange("p (h d) -> p h d", h=nh)
        op("dve", lambda e: e.tensor_tensor(out=dv[:, :, 0:half], in0=A[:, :, 0:half], in1=Bm[:, :, half:r2], op=ALU.subtract),
           reads=[(rt_k, 0), (rt_k, 1), dst_k], writes=[dst_k])
        op("dve", lambda e: e.tensor_tensor(out=dv[:, :, half:r2], in0=A[:, :, half:r2], in1=Bm[:, :, 0:half], op=ALU.add),
           reads=[(rt_k, 0), (rt_k, 1), dst_k], writes=[dst_k])

    stage[0] = 1
    for j_, vbt_ in enumerate(vb_pool.tiles):
        op("pool", lambda e, vbt_=vbt_: e.memset(vbt_[:, :, 128:129], 1.0), writes=[("vbones", ("vb", j_))])
    wk, wk_k = load_w("in", 512, cast=True)
    wv, wv_k = load_w("in", 1024, cast=True)
    cvt_pool = Pool(wbuf.tiles[2:4], "wbuf", off=2)
    for kind, idxs in (("in", (0, 1536, 2120, 2632)), ("out", (0, 1)), ("up", tuple(range(8))), ("down", tuple(range(8)))):
        for i in idxs:
            w, w_k = cvt_pool.get()
            v, src, scr = wviews(kind, i, w)
            op("pool", lambda e, v=v, src=src: e.dma_start(out=v, in_=src), writes=[w_k], dma=True)
            op("pool", lambda e, v=v, scr=scr: e.dma_start(out=scr, in_=v), reads=[w_k], writes=[("wcv", kind, i)], dma=True)
    wbuf.i = 2
    wik, wik_k = wik_t, "wik"
    stage[0] = 1
    def p1_tile(kt):
        rows = NMETA if kt == 0 else 128
        p0 = 0 if kt == 0 else NMETA + 128 * (kt - 1)
        xt, xt_k = xt_pool.get()
        op("sp", lambda e, xt=xt, p0=p0, rows=rows: e.dma_start(out=xt[0:rows, :], in_=xall[p0:p0 + rows, :]), writes=[xt_k], dma=True)
        rp, rp_k = rope_pool.get()
        op("sp", lambda e, rp=rp, p0=p0, rows=rows: e.dma_start(out=rp[0:rows, :], in_=ropek[p0:p0 + rows, 0:256]), writes=[rp_k], dma=True)
        rpi, rpi_k = rope_pool.get()
        op("sp", lambda e, rpi=rpi, p0=p0, rows=rows: e.dma_start(out=rpi[0:rows, :], in_=ropek[p0:p0 + rows, 256:512]), writes=[rpi_k], dma=True)
        xnT, xnT_k = xnT_pool.get()
        rmsnorm_T(xt, xt_k, rows, gA, xnT, xnT_k)
        yield
        psK, psK_k = pg.get()
        proj_tm(xnT, xnT_k, rows, wk, wk_k, 512, psK, psK_k)
        psV, psV_k = pg.get()
        proj_tm(xnT, xnT_k, rows, wv, wv_k, 512, psV, psV_k)
        psI, psI_k = pg.get()
        proj_tm(xnT, xnT_k, rows, wik, wik_k, 64, psI, psI_k)
        kb, kb_k = tm_pool.get()
        rope_tm(psK, psK_k, rows, 4, 128, 16, rp, rp_k, 0, kb, kb_k)
        vb, vb_k = vb_pool.get()
        op("act", lambda e, vb=vb, ps=psV, rows=rows: e.copy(out=vb[0:rows, :, 0:128], in_=ps[0:rows, :].rearrange("p (h d) -> p h d", h=4)),
           reads=[psV_k, ("vbones", vb_k)], writes=[vb_k])
        op("act", lambda e, vb=vb, p0=p0, rows=rows: e.dma_start(out=v_scr[p0:p0 + rows, :, :], in_=vb[0:rows, :, :]),
           reads=[vb_k, ("vbones", vb_k)], writes=[("v_scr", kt)], dma=True)
        ib, ib_k = tm_pool.get()
        rope_tm(psI, psI_k, rows, 1, 64, 8, rpi, rpi_k, 0, ib, ib_k)
        op("dve", lambda e, ib=ib, rows=rows: e.tensor_copy(out=ib[0:rows, 64:128], in_=ib[0:rows, 0:64]), reads=[ib_k], writes=[ib_k])
        pt, pt_k = pg.get()
        ptb = pt[:].bitcast(BF16)
        for h in range(4):
            op("pe", lambda e, h=h, ptb=ptb, kb=kb, rows=rows: e.transpose(out=ptb[:, h * 128:h * 128 + rows], in_=kb[0:rows, h * 128:(h + 1) * 128],
                                                                         identity=ident[0:rows, 0:rows]), reads=[kb_k, "ident"], writes=[pt_k])
        kTt, kTt_k = tm_pool.get()
        op("act", lambda e, kTt=kTt, ptb=ptb: e.copy(out=kTt[:, :], in_=ptb[:, 0:512]), reads=[pt_k], writes=[kTt_k])
        op("act", lambda e, kTt=kTt, p0=p0, rows=rows: e.dma_start(
            out=k_scr[:, :, p0:p0 + rows].rearrange("h p t -> p h t"),
            in_=kTt[:, :].rearrange("p (h t) -> p h t", h=4)[:, :, 0:rows]),
           reads=[kTt_k], writes=[("k_scr", kt)], dma=True)
        pt2, pt2_k = pg.get()
        ptb2 = pt2[:].bitcast(BF16)
        op("pe", lambda e, ptb2=ptb2, ib=ib, rows=rows: e.transpose(out=ptb2[:, 0:rows], in_=ib[0:rows, 0:128], identity=ident[0:rows, 0:rows]),
           reads=[ib_k, "ident"], writes=[pt2_k])
        op("act", lambda e, ptb2=ptb2, p0=p0, rows=rows: e.copy(out=ikT[:, p0:p0 + rows], in_=ptb2[:, 0:rows]), reads=[pt2_k], writes=[("ikT", kt)])

    def run_pipelined(gens):
        if gens:
            next(gens[0])
        for i in range(len(gens)):
            if i + 1 < len(gens):
                next(gens[i + 1])
            for _ in gens[i]:
                pass

    run_pipelined([p1_tile(kt) for kt in range(NKT)])

    stage[0] = 2
    qT = sb([128, 4, 512], BF16, "qT")
    iqT = sb([128, 4, 512], BF16, "iqT")
    iwt = sb([128, 4, 8], F32, "iwt")
    uT = sb([128, 4, 544], BF16, "uT")
    mixed = sb([128, 4, D], BF16, "mixed")
    ysb = sb([128, 4, 512], F32, "ysb")
    Dcc = sb([128, 31, 128], BF16, "Dcc")
    negm_pool = mkpool(2, [128, 512], F32, "negm")
    wabs2 = sb([128, 8], F32, "wabs2")
    wsgn = sb([128, 8], F32, "wsgn")
    bis = sb([128, 8 + NBIS], F32, "bis")
    top8 = sb([128, 8], F32, "top8")
    cst = sb([128, 4, 6], F32, "cst")
    cmv = sb([128, 4, 2], F32, "cmv")
    crs = sb([128, 4], F32, "crs")
    R_pool = mkpool(3, [128, 512], BF16, "R")
    PT_pool = mkpool(3, [128, 512], BF16, "PT")
    mb_pool = mkpool(2, [128, 512], BF16, "mb")
    GK = 4
    kv_pool = [(sb([128, 4, GK * 128], BF16, "kbuf"), sb([128, GK, 4, 129], BF16, "vbuf")) for _ in range(2)]
    kmeta = sb([128, 4, NMETA], BF16, "kmeta")
    vmeta = sb([128, 4, 129], BF16, "vmeta")
    op("sp", lambda e: e.dma_start(out=kmeta[:, :, :], in_=k_scr[:, :, 0:NMETA].rearrange("h p t -> p h t")),
       reads=[("k_scr", 0)], writes=["kmeta"], dma=True)
    op("sp", lambda e: e.dma_start(out=vmeta[0:NMETA, :, :], in_=v_scr[0:NMETA, :, :]),
       reads=[("v_scr", 0)], writes=["vmeta"], dma=True)
    mT = sb([128, 8, 128], BF16, "mT")
    hnT = sb([128, 8, 256], BF16, "hnT")
    uf_pool = mkpool(2, [128, 256], BF16, "uf")
    rden = sb([128, 4], F32, "rden")

    SC = 0.125 * (8 ** -0.5)
    kv_ctr = [0]

    for sl in range(NSLOT):
        E = NMETA + 512 * (2 * sl + 2)
        W0 = E - 1024
        NCHK = 2 * sl + 2
        wq, wq_k = load_w("in", 0)
        wiq, wiq_k = load_w("in", 1536)
        wga, wga_k = load_w("in", 2120)
        wgg, wgg_k = load_w("in", 2632)
        plan = []
        for pair_ in range(2):
            plan += [("out", 0), ("out", 1)]
            for fg_ in range(8):
                plan += [("up", fg_), ("down", fg_)]
        wfifo = []

        def issue_next():
            if plan:
                kind_, i_ = plan.pop(0)
                wfifo.append(load_w(kind_, i_))
        def sa_tile(ti, sl=sl, wq=wq, wq_k=wq_k, wiq=wiq, wiq_k=wiq_k, wga=wga, wga_k=wga_k, wgg=wgg, wgg_k=wgg_k):
            rows = 32 if ti == 0 else 128
            r0 = 0 if ti == 0 else 32 + 128 * (ti - 1)
            xt, xt_k = xt_pool.get()
            op("sp", lambda e, xt=xt, r0=r0, rows=rows, sl=sl: e.dma_start(out=xt[0:rows, :], in_=xown[sl, r0:r0 + rows, :]),
               writes=[xt_k], dma=True)
            xnT, xnT_k = xnT_pool.get()
            rmsnorm_T(xt, xt_k, rows, gA, xnT, xnT_k)
            if ti > 0:
                tb = ti - 1
                rp, rp_k = rope_pool.get()
                op("sp", lambda e, rp=rp, tb=tb, sl=sl: e.dma_start(out=rp[:, :], in_=ropeq[sl, 128 * tb:128 * (tb + 1), 0:256]), writes=[rp_k], dma=True)
                rpi, rpi_k = rope_pool.get()
                op("sp", lambda e, rpi=rpi, tb=tb, sl=sl: e.dma_start(out=rpi[:, :], in_=ropeq[sl, 128 * tb:128 * (tb + 1), 256:512]), writes=[rpi_k], dma=True)
            yield
            if ti > 0:
                ps, ps_k = pg.get()
                proj_tm(xnT, xnT_k, 128, wq, wq_k, 512, ps, ps_k)
                qb, qb_k = tm_pool.get()
                rope_tm(ps, ps_k, 128, 4, 128, 16, rp, rp_k, 0, qb, qb_k)
                pt, pt_k = pg.get()
                ptb = pt[:].bitcast(BF16)
                for h in range(4):
                    op("pe", lambda e, h=h, ptb=ptb, qb=qb: e.transpose(out=ptb[:, h * 128:(h + 1) * 128], in_=qb[:, h * 128:(h + 1) * 128], identity=ident[:, :]),
                       reads=[qb_k, "ident"], writes=[pt_k])
                op("act", lambda e, ptb=ptb, tb=tb: e.copy(out=qT[:, :, tb * 128:(tb + 1) * 128], in_=ptb[:, 0:512].rearrange("p (h t) -> p h t", h=4)),
                   reads=[pt_k], writes=[("qT", tb)])
                ps, ps_k = pg.get()
                proj_tm(xnT, xnT_k, 128, wiq, wiq_k, 512, ps, ps_k)
                ib, ib_k = tm_pool.get()
                rope_tm(ps, ps_k, 128, 8, 64, 8, rpi, rpi_k, 0, ib, ib_k)
                pt, pt_k = pg.get()
                ptb = pt[:].bitcast(BF16)
                for hp in range(4):
                    op("pe", lambda e, hp=hp, ptb=ptb, ib=ib: e.transpose(out=ptb[:, hp * 128:(hp + 1) * 128], in_=ib[:, hp * 128:(hp + 1) * 128], identity=ident[:, :]),
                       reads=[ib_k, "ident"], writes=[pt_k])
                op("act", lambda e, ptb=ptb, tb=tb: e.copy(out=iqT[:, :, tb * 128:(tb + 1) * 128], in_=ptb[:, 0:512].rearrange("p (h t) -> p h t", h=4)),
                   reads=[pt_k], writes=[("iqT", tb)])
                ps, ps_k = pg.get()
                proj_tm(xnT, xnT_k, 128, wiw_t, "wiw", 8, ps, ps_k)
                op("dve", lambda e, ps=ps, tb=tb: e.tensor_scalar(out=iwt[:, tb, :], in0=ps[:, 0:8], scalar1=SC, scalar2=None, op0=ALU.mult),
                   reads=[ps_k], writes=[("iwt", tb)])
            psa, psa_k = pg.get()
            proj_tm(xnT, xnT_k, rows, wga, wga_k, 512, psa, psa_k)
            psg, psg_k = pg.get()
            proj_tm(xnT, xnT_k, rows, wgg, wgg_k, 512, psg, psg_k)
            jk, jk_k = jk_pool.get()
            op("act", lambda e, jk=jk, psg=psg, rows=rows: e.activation(out=jk[0:rows, :], in_=psg[0:rows, :], func=AF.Sigmoid),
               reads=[psg_k], writes=[jk_k])
            ub, ub_k = tm_pool.get()
            op("dve", lambda e, ub=ub, psa=psa, jk=jk, rows=rows: e.tensor_tensor(out=ub[0:rows, :], in0=psa[0:rows, :], in1=jk[0:rows, :], op=ALU.mult),
               reads=[psa_k, jk_k], writes=[ub_k])
            pt, pt_k = pg.get()
            ptb = pt[:].bitcast(BF16)
            for cc in range(4):
                op("pe", lambda e, cc=cc, ptb=ptb, ub=ub, rows=rows: e.transpose(out=ptb[:, cc * 128:cc * 128 + rows], in_=ub[0:rows, cc * 128:(cc + 1) * 128],
                                                                              identity=ident[0:rows, 0:rows]),
                   reads=[ub_k, "ident"], writes=[pt_k])
            op("act", lambda e, ptb=ptb, r0=r0, rows=rows: e.copy(out=uT[:, :, r0:r0 + rows], in_=ptb[:, 0:512].rearrange("p (c t) -> p c t", c=4)[:, :, 0:rows]),
               reads=[pt_k], writes=[("uT", ti)])

        run_pipelined([sa_tile(ti) for ti in range(5)])
        stage[0] = 3
        UTK = [("uT", ti) for ti in range(5)]
        for cc in range(4):
            op("dve", lambda e, cc=cc: e.tensor_tensor(out=Dcc[:, :, :], in0=identf[:, :].unsqueeze(1).to_broadcast([128, 31, 128]),
                                                       in1=cwT[:, cc, :].unsqueeze(2).to_broadcast([128, 31, 128]), op=ALU.mult),
               reads=["identf", "cwT"], writes=["Dcc"])
            for tb in range(4):
                ps, ps_k = pg.get()
                for j in range(31):
                    c0 = 2 + tb * 128 + j
                    op("pe", lambda e, ps=ps, cc=cc, c0=c0, j=j: e.matmul(ps[:, 0:128], lhsT=uT[:, cc, c0:c0 + 128], rhs=Dcc[:, j, :], start=(j == 0), stop=(j == 30)),
                       reads=UTK + ["Dcc"], writes=[ps_k])
                op("act", lambda e, ps=ps, tb=tb, cc=cc: e.copy(out=ysb[:, tb, cc * 128:(cc + 1) * 128], in_=ps[:, 0:128]),
                   reads=[ps_k], writes=[("ysb", tb)])
        for tb in range(4):
            yk = ("ysb", tb)
            y = ysb[:, tb, :]
            op("dve", lambda e, y=y: e.tensor_tensor(out=y, in0=y, in1=cb_bc[:, :], op=ALU.add), reads=[yk, "cb_bc"], writes=[yk])
            for g in range(4):
                op("dve", lambda e, y=y, g=g: e.bn_stats(out=cst[:, g, :], in_=y[:, g * 128:(g + 1) * 128]), reads=[yk], writes=[("cst", g)])
                op("dve", lambda e, g=g: e.bn_aggr(out=cmv[:, g, :], in_=cst[:, g, :]), reads=[("cst", g)], writes=[("cmv", g)])
            CM = [("cmv", g) for g in range(4)]
            op("dve", lambda e: e.tensor_scalar(out=crs[:, :], in0=cmv[:, :, 1], scalar1=EPS, scalar2=None, op0=ALU.add), reads=CM, writes=["crs"])
            op("act", lambda e: e.activation(out=crs[:, :], in_=crs[:, :], func=AF.Sqrt), reads=["crs"], writes=["crs"])
            op("dve", lambda e: e.reciprocal(out=crs[:, :], in_=crs[:, :]), reads=["crs"], writes=["crs"])
            for g in range(4):
                op("dve", lambda e, y=y, g=g: e.tensor_scalar(out=y[:, g * 128:(g + 1) * 128], in0=y[:, g * 128:(g + 1) * 128],
                                                            scalar1=cmv[:, g, 0:1], scalar2=crs[:, g:g + 1], op0=ALU.subtract, op1=ALU.mult),
                   reads=[yk, "crs"] + CM, writes=[yk])
            op("dve", lambda e, y=y: e.tensor_tensor(out=y, in0=y, in1=cg_bc[:, :], op=ALU.mult), reads=[yk, "cg_bc"], writes=[yk])
            op("dve", lambda e, y=y: e.tensor_tensor(out=y, in0=y, in1=cnb_bc[:, :], op=ALU.add), reads=[yk, "cnb_bc"], writes=[yk])
            op("act", lambda e, y=y, tb=tb: e.activation(out=mixed[:, tb, 512:1024], in_=y, func=AF.Silu), reads=[yk], writes=[("mixC", tb)])

        stage[0] = 4
        for tb in range(4):
            if tb == 3:
                for _ in range(4):
                    issue_next()
            op("act", lambda e, tb=tb: e.activation(out=wabs2[:, :], in_=iwt[:, tb, :], func=AF.Abs, scale=2.0),
               reads=[("iwt", tb)], writes=["wabs2"])
            op("dve", lambda e, tb=tb: e.tensor_scalar(out=wsgn[:, :], in0=iwt[:, tb, :], scalar1=0.0, scalar2=0.5, op0=ALU.is_ge, op1=ALU.subtract),
               reads=[("iwt", tb)], writes=["wsgn"])
            chunks = [(0, NMETA)] + [(NMETA + 512 * m, 512) for m in range(NCHK)]
            SCK = []
            wcnt = 0
            for ci, (c0, cn) in enumerate(chunks):
                ikk = [("ikT", 0)] if ci == 0 else [("ikT", 1 + 4 * (ci - 1) + q) for q in range(4)]
                sk = ("sc", ci)
                SCK.append(sk)
                for h in range(8):
                    par, hp = h % 2, h // 2
                    ps, ps_k = pg.get()
                    op("pe", lambda e, ps=ps, par=par, hp=hp, tb=tb, c0=c0, cn=cn: e.matmul(ps[:, 0:cn], lhsT=iqT[par * 64:(par + 1) * 64, hp, tb * 128:(tb + 1) * 128],
                                                                                         rhs=ikT[par * 64:(par + 1) * 64, c0:c0 + cn], start=True, stop=True),
                       reads=[("iqT", tb)] + ikk, writes=[ps_k])
                    R, R_k = R_pool.get()
                    op("act", lambda e, R=R, ps=ps, cn=cn, h=h: e.activation(out=R[:, 0:cn], in_=ps[:, 0:cn], func=AF.Relu, scale=wabs2[:, h:h + 1]),
                       reads=[ps_k, "wabs2"], writes=[R_k])
                    if h == 0:
                        op("dve", lambda e, R=R, c0=c0, cn=cn: e.tensor_scalar(out=scores[:, c0:c0 + cn], in0=R[:, 0:cn], scalar1=wsgn[:, 0:1], scalar2=None, op0=ALU.mult),
                           reads=[R_k, "wsgn"], writes=[sk])
                    else:
                        op("dve", lambda e, R=R, c0=c0, cn=cn, h=h: e.scalar_tensor_tensor(out=scores[:, c0:c0 + cn], in0=R[:, 0:cn], scalar=wsgn[:, h:h + 1],
                                                                                         in1=scores[:, c0:c0 + cn], op0=ALU.mult, op1=ALU.add),
                           reads=[R_k, "wsgn", sk], writes=[sk])
                if c0 >= W0 and ci > 0:
                    col = (sl * 4 + tb) * 2 + wcnt
                    negm, negm_k = negm_pool.get()
                    op("dve", lambda e, col=col, negm=negm: e.tensor_scalar(out=negm[:, :], in0=iota[:, :], scalar1=qrel[:, col:col + 1], scalar2=-1e30,
                                                                          op0=ALU.is_gt, op1=ALU.mult), reads=["iota", "qrelc"], writes=[negm_k])
                    jk, jk_k = jk_pool.get()
                    op("dve", lambda e, jk=jk, c0=c0, negm=negm: e.tensor_tensor(out=jk[:, :], in0=scores[:, c0:c0 + 512], in1=negm[:, :], op=ALU.subtract),
                       reads=[sk, negm_k], writes=[jk_k])
                    op("dve", lambda e, jk=jk, wcnt=wcnt: e.tensor_reduce(out=bis[:, 5 + wcnt:6 + wcnt], in_=jk[:, :], axis=AX.X, op=ALU.min),
                       reads=[jk_k], writes=[("bis", 5 + wcnt)])
                    op("dve", lambda e, c0=c0, negm=negm: e.tensor_tensor(out=scores[:, c0:c0 + 512], in0=scores[:, c0:c0 + 512], in1=negm[:, :], op=ALU.add),
                       reads=[sk, negm_k], writes=[sk])
                    wcnt += 1
            assert wcnt == 2
            stage[0] = 5
            nD = max(1, int(round(0.45 * NCHK)))
            ED = NMETA + 512 * nD
            EA = E - ED
            SCK_D = SCK[0:1 + nD]
            SCK_A = SCK[1 + nD:]
            op("dve", lambda e, E=E: e.max(out=top8[:, :], in_=scores[:, 0:E]), reads=SCK, writes=["top8"])
            op("dve", lambda e, W0=W0: e.tensor_reduce(out=bis[:, 7:8], in_=scores[:, 0:W0], axis=AX.X, op=ALU.min), reads=SCK, writes=[("bis", 7)])
            op("dve", lambda e: e.tensor_reduce(out=bis[:, 0:1], in_=bis[:, 5:8], axis=AX.X, op=ALU.min),
               reads=[("bis", 5), ("bis", 6), ("bis", 7)], writes=[("bis", 0)])
            op("dve", lambda e: e.tensor_tensor(out=bis[:, 1:2], in0=top8[:, 0:1], in1=bis[:, 0:1], op=ALU.subtract),
               reads=["top8", ("bis", 0)], writes=[("bis", 1)])
            op("dve", lambda e: e.tensor_scalar(out=bis[:, 8:8 + NBIS], in0=pow2[:, :], scalar1=bis[:, 1:2], scalar2=None, op0=ALU.mult),
               reads=["pow2", ("bis", 1)], writes=["bisW"])
            op("dve", lambda e: e.tensor_tensor(out=bis[:, 2:3], in0=bis[:, 8:9], in1=bis[:, 0:1], op=ALU.add),
               reads=["bisW", ("bis", 0)], writes=[("bis", 2)])
            for k in range(NBIS):
                op("dve", lambda e, ED=ED: e.tensor_scalar(out=bigjunk[:, 0:ED], in0=scores[:, 0:ED], scalar1=bis[:, 2:3], scalar2=None,
                                                          op0=ALU.is_ge, op1=ALU.add, accum_out=bis[:, 3:4]),
                   reads=SCK_D + [("bis", 2)], writes=["bigjunkD", ("bis", 3)])
                op("act", lambda e, ED=ED, E=E, EA=EA: e.activation(out=bigjunk[:, 0:EA].bitcast(mybir.dt.int8), in_=scores[:, ED:E], func=AF.Sign,
                                                            bias=bis[:, 2:3], scale=-1.0, accum_out=bis[:, 5:6]),
                   reads=SCK_A + [("bis", 2)], writes=["bigjunkA", ("bis", 5)])
                op("dve", lambda e: e.scalar_tensor_tensor(out=bis[:, 6:7], in0=bis[:, 3:4], scalar=2.0, in1=bis[:, 5:6], op0=ALU.mult, op1=ALU.subtract),
                   reads=[("bis", 3), ("bis", 5)], writes=[("bis", 6)])
                op("dve", lambda e, k=k, EA=EA: e.tensor_scalar(out=bis[:, 4:5], in0=bis[:, 6:7], scalar1=float(2 * KTOP - 1 - EA), scalar2=bis[:, 8 + k:9 + k],
                                                               op0=ALU.is_ge, op1=ALU.mult), reads=[("bis", 6), "bisW"], writes=[("bis", 4)])
                if k < NBIS - 1:
                    op("dve", lambda e, k=k: e.scalar_tensor_tensor(out=bis[:, 2:3], in0=bis[:, 4:5], scalar=bis[:, 9 + k:10 + k], in1=bis[:, 2:3],
                                                                  op0=ALU.subtract, op1=ALU.add),
                       reads=[("bis", 4), "bisW", ("bis", 2)], writes=[("bis", 2)])
                else:
                    op("dve", lambda e, k=k: e.scalar_tensor_tensor(out=bis[:, 0:1], in0=bis[:, 4:5], scalar=bis[:, 8 + k:9 + k], in1=bis[:, 2:3],
                                                                  op0=ALU.subtract, op1=ALU.add),
                       reads=[("bis", 4), "bisW", ("bis", 2)], writes=[("bis", 0)])
            stage[0] = 6
            nkb_total = 1 + 4 * NCHK
            kblist = []
            for ci, (c0, cn) in enumerate(chunks):
                for kb in range(1 if ci == 0 else 4):
                    kblist.append((ci, c0, cn, kb))
            cstate = {}

            def chunk_setup(ci, c0, cn):
                mbt, mb_k = mb_pool.get()
                op("dve", lambda e, mbt=mbt, c0=c0, cn=cn: e.tensor_scalar(out=mbt[:, 0:cn], in0=scores[:, c0:c0 + cn], scalar1=bis[:, 0:1], scalar2=-30000.0,
                                                                          op0=ALU.is_lt, op1=ALU.mult), reads=[SCK[ci], ("bis", 0)], writes=[mb_k])
                if ci == 0:
                    cstate[ci] = (mbt, mb_k, kmeta, vmeta, "kmeta", "vmeta")
                    return
                kvn = kv_ctr[0] % 2
                kv_ctr[0] += 1
                kbuf, vbuf = kv_pool[kvn]
                kvk, vvk = ("kbuf", kvn), ("vbuf", kvn)
                tl = [1 + 4 * (ci - 1) + q for q in range(4)]
                op("sp", lambda e, kbuf=kbuf, c0=c0: e.dma_start(out=kbuf[:, :, :], in_=k_scr[:, :, c0:c0 + 512].rearrange("h p t -> p h t")),
                   reads=[("k_scr", t) for t in tl], writes=[kvk], dma=True)
                op("sp", lambda e, vbuf=vbuf, c0=c0: e.dma_start(out=vbuf[:, :, :, :].rearrange("p g h e -> p g (h e)"),
                                                                 in_=v_scr[c0:c0 + 512, :, :].rearrange("(g p) h e -> p g (h e)", p=128)),
                   reads=[("v_scr", t) for t in tl], writes=[vvk], dma=True)
                cstate[ci] = (mbt, mb_k, kbuf, vbuf, kvk, vvk)

            def emit_S(i):
                ci, c0, cn, kb = kblist[i]
                if kb == 0:
                    chunk_setup(ci, c0, cn)
                mbt, mb_k, kbuf, vbuf, kvk, vvk = cstate[ci]
                ks = NMETA if ci == 0 else 128
                st, st_k = pg.get()
                for h in range(4):
                    op("pe", lambda e, st=st, h=h, kbuf=kbuf, kb=kb, ks=ks, tb=tb: e.matmul(st[0:ks, h * 128:(h + 1) * 128], lhsT=kbuf[:, h, kb * 128:kb * 128 + ks],
                                                                                         rhs=qT[:, h, tb * 128:(tb + 1) * 128], start=True, stop=False),
                       reads=[kvk, ("qT", tb)], writes=[st_k])
                    op("pe", lambda e, st=st, h=h, mbt=mbt, kb=kb, ks=ks: e.matmul(st[0:ks, h * 128:(h + 1) * 128], lhsT=mbt[:, kb * 128:kb * 128 + ks],
                                                                                 rhs=ident[:, :], start=False, stop=True),
                       reads=[mb_k, "ident"], writes=[st_k])
                PT, PT_k = PT_pool.get()
                op("act", lambda e, PT=PT, st=st, ks=ks: e.activation(out=PT[0:ks, :], in_=st[0:ks, :], func=AF.Exp, scale=128 ** -0.5),
                   reads=[st_k], writes=[PT_k])
                return (PT, PT_k)

            def emit_P(i, PTs):
                ci, c0, cn, kb = kblist[i]
                mbt, mb_k, kbuf, vbuf, kvk, vvk = cstate[ci]
                ks = NMETA if ci == 0 else 128
                PT, PT_k = PTs
                for h in range(4):
                    acc = pacc[h // 2]
                    a0 = (h % 2) * 256
                    rhs = vbuf[0:ks, h, :] if ci == 0 else vbuf[:, kb, h, :]
                    op("pe", lambda e, acc=acc, a0=a0, PT=PT, h=h, ks=ks, rhs=rhs, i=i: e.matmul(acc[:, a0:a0 + 129], lhsT=PT[0:ks, h * 128:(h + 1) * 128], rhs=rhs,
                                                                                           start=(i == 0 and h % 2 == 0), stop=(i == nkb_total - 1),
                                                                                           skip_group_check=True),
                       reads=[PT_k, vvk], writes=[("pacc", h // 2)])

            DEPTH = 2
            pend = [emit_S(j) for j in range(min(DEPTH, nkb_total))]
            for i in range(nkb_total):
                if i + DEPTH < nkb_total:
                    pend.append(emit_S(i + DEPTH))
                emit_P(i, pend.pop(0))
            for h in range(4):
                acc = pacc[h // 2]
                a0 = (h % 2) * 256
                op("dve", lambda e, acc=acc, a0=a0, h=h: e.reciprocal(out=rden[:, h:h + 1], in_=acc[:, a0 + 128:a0 + 129]),
                   reads=[("pacc", h // 2)], writes=[("rden", h)])
                op("dve", lambda e, acc=acc, a0=a0, h=h, tb=tb: e.tensor_scalar(out=mixed[:, tb, h * 128:(h + 1) * 128], in0=acc[:, a0:a0 + 128], scalar1=rden[:, h:h + 1],
                                                                              scalar2=None, op0=ALU.mult),
                   reads=[("pacc", h // 2), ("rden", h)], writes=[("mixA", tb, h)])

        stage[0] = 7
        for pair in range(2):
            h1s = []
            while len(wfifo) < 2:
                issue_next()
            wos = [wfifo.pop(0), wfifo.pop(0)]
            for t2 in range(2):
                tb = 2 * pair + t2
                pt, pt_k = pg.get()
                ptb = pt[:].bitcast(BF16)
                for ec in range(8):
                    op("pe", lambda e, ptb=ptb, ec=ec, tb=tb: e.transpose(out=ptb[:, ec * 128:(ec + 1) * 128], in_=mixed[:, tb, ec * 128:(ec + 1) * 128], identity=ident[:, :]),
                       reads=[("mixA", tb, h) for h in range(4)] + [("mixC", tb), "ident"], writes=[pt_k])
                op("act", lambda e, ptb=ptb: e.copy(out=mT[:, :, :], in_=ptb[:, :].rearrange("p (c t) -> p c t", c=8)), reads=[pt_k], writes=["mT"])
                xt, xt_k = xt_pool.get()
                op("sp", lambda e, xt=xt, sl=sl, tb=tb: e.dma_start(out=xt[:, :], in_=xown[sl, 32 + tb * 128:32 + (tb + 1) * 128, :]), writes=[xt_k], dma=True)
                h1, h1_k = xt, xt_k
                for half in range(2):
                    wo, wo_k = wos[half]
                    for ec in range(8):
                        op("pe", lambda e, half=half, ec=ec, wo=wo: e.matmul(psc[half][:, :], lhsT=mT[:, ec, :], rhs=wo[:, ec, :], start=(ec == 0), stop=(ec == 7)),
                           reads=["mT", wo_k], writes=[("psc", half)])
                    op("dve", lambda e, half=half, h1=h1, xt=xt: e.tensor_tensor(out=h1[:, half * 512:(half + 1) * 512], in0=psc[half][:, :],
                                                                               in1=xt[:, half * 512:(half + 1) * 512], op=ALU.add),
                       reads=[("psc", half), xt_k], writes=[h1_k])
                rmsnorm_T(h1, h1_k, 128, gM, hnT[:, :, t2 * 128:(t2 + 1) * 128], ("hnT", t2))
                h1s.append((h1, h1_k, tb))
            issue_next()
            issue_next()
            accs = [[(psc[0], ("psc", 0)), (psc[1], ("psc", 1))], [(pacc[0], ("pacc", 0)), (pacc[1], ("pacc", 1))]]
            fstate = {}

            def ffn_up(i):
                fg, fc = divmod(i, 4)
                if fc == 0:
                    while len(wfifo) < 2:
                        issue_next()
                    fstate[fg] = (wfifo.pop(0), wfifo.pop(0))
                (wu, wu_k), (wdv, wd_k) = fstate[fg]
                ps, ps_k = pg.get()
                for dc in range(8):
                    op("pe", lambda e, ps=ps, wu=wu, dc=dc, fc=fc: e.matmul(ps[:, 0:256], lhsT=wu[:, dc, fc * 128:(fc + 1) * 128], rhs=hnT[:, dc, :],
                                                                         start=(dc == 0), stop=(dc == 7)),
                       reads=[wu_k, ("hnT", 0), ("hnT", 1)], writes=[ps_k])
                if fc == 3:
                    issue_next()
                uf, uf_k = uf_pool.get()
                op("act", lambda e, uf=uf, ps=ps: e.activation(out=uf[:, :], in_=ps[:, 0:256], func=AF.Relu), reads=[ps_k], writes=[uf_k])
                op("pool", lambda e, uf=uf: e.tensor_tensor(out=uf[:, :], in0=uf[:, :], in1=uf[:, :], op=ALU.mult), reads=[uf_k], writes=[uf_k])
                return uf, uf_k

            def ffn_down(i, ufs):
                fg, fc = divmod(i, 4)
                uf, uf_k = ufs
                (wu, wu_k), (wdv, wd_k) = fstate[fg]
                for t2 in range(2):
                    for half in range(2):
                        acc, acc_k = accs[t2][half]
                        op("pe", lambda e, acc=acc, uf=uf, t2=t2, half=half, wdv=wdv, fc=fc, i=i: e.matmul(
                            acc[:, :], lhsT=uf[:, t2 * 128:(t2 + 1) * 128], rhs=wdv[:, fc, half * 512:(half + 1) * 512], start=(i == 0), stop=(i == 31)),
                           reads=[uf_k, wd_k], writes=[acc_k])
                if fc == 3:
                    issue_next()

            cur = ffn_up(0)
            for i in range(32):
                nxt = ffn_up(i + 1) if i + 1 < 32 else None
                ffn_down(i, cur)
                cur = nxt
            for t2 in range(2):
                h1, h1_k, tb = h1s[t2]
                for half in range(2):
                    acc, acc_k = accs[t2][half]
                    op("dve", lambda e, acc=acc, h1=h1, half=half: e.tensor_tensor(out=h1[:, half * 512:(half + 1) * 512], in0=acc[:, :],
                                                                                 in1=h1[:, half * 512:(half + 1) * 512], op=ALU.add),
                       reads=[acc_k, h1_k], writes=[h1_k])
                st, st_k = st_pool.get()
                jk, jk_k = jk_pool.get()
                op("act", lambda e, jk=jk, h1=h1, st=st: e.activation(out=jk[:, :], in_=h1[:, 0:512], func=AF.Square, accum_out=st[:, 0:1]),
                   reads=[h1_k], writes=[jk_k, (st_k, 0)])
                op("act", lambda e, jk=jk, h1=h1, st=st: e.activation(out=jk[:, :], in_=h1[:, 512:1024], func=AF.Square, accum_out=st[:, 1:2]),
                   reads=[h1_k], writes=[jk_k, (st_k, 1)])
                op("dve", lambda e, st=st: e.tensor_tensor(out=st[:, 2:3], in0=st[:, 0:1], in1=st[:, 1:2], op=ALU.add), reads=[(st_k, 0), (st_k, 1)], writes=[(st_k, 2)])
                op("dve", lambda e, st=st: e.tensor_scalar(out=st[:, 3:4], in0=st[:, 2:3], scalar1=1.0 / D, scalar2=EPS, op0=ALU.mult, op1=ALU.add),
                   reads=[(st_k, 2)], writes=[(st_k, 3)])
                op("act", lambda e, st=st: e.activation(out=st[:, 4:5], in_=st[:, 3:4], func=AF.Sqrt), reads=[(st_k, 3)], writes=[(st_k, 4)])
                op("dve", lambda e, st=st: e.reciprocal(out=st[:, 5:6], in_=st[:, 4:5]), reads=[(st_k, 4)], writes=[(st_k, 5)])
                op("dve", lambda e, h1=h1, st=st: e.scalar_tensor_tensor(out=h1[:, :], in0=h1[:, :], scalar=st[:, 5:6], in1=gF[:, :], op0=ALU.mult, op1=ALU.mult),
                   reads=[h1_k, (st_k, 5), "gF"], writes=[h1_k])
                r0 = sl * 512 + tb * 128
                op("act", lambda e, h1=h1, r0=r0: e.dma_start(out=out_d[r0:r0 + 128, :], in_=h1[:, :]), reads=[h1_k], writes=[("out", r0)], dma=True)

    if os.environ.get("KDEBUG"):
        print("sbuf remaining", nc.sbuf_bytes_remaining, "ops", {e: len(S_.ops[e]) for e in ENGS})
    with ExitStack() as stack:
        S_.emit(nc, stack)
    return nc


def _rope_tab(pos):
    pos = np.asarray(pos, np.float32)
    out = np.zeros((len(pos), 512), np.float32)
    inv = (np.float32(500000.0) ** (-np.arange(0, 32, 2, dtype=np.float32) / np.float32(32))).astype(np.float32)
    ang = pos[:, None] * inv[None, :]
    c, s = np.cos(ang).astype(np.float32), np.sin(ang).astype(np.float32)
    out[:, 0:128] = np.tile(np.concatenate([c, c], 1), (1, 4))
    out[:, 128:256] = np.tile(np.concatenate([s, s], 1), (1, 4))
    inv = (np.float32(500000.0) ** (-np.arange(0, 16, 2, dtype=np.float32) / np.float32(16))).astype(np.float32)
    ang = pos[:, None] * inv[None, :]
    c, s = np.cos(ang).astype(np.float32), np.sin(ang).astype(np.float32)
    out[:, 256:384] = np.tile(np.concatenate([c, c], 1), (1, 8))
    out[:, 384:512] = np.tile(np.concatenate([s, s], 1), (1, 8))
    return out


def _chunk_of(sl, half):
    return 2 * sl + (sl % 2) if half == 0 else 2 * sl + 1 - (sl % 2)


_NC_CACHE = {}


def kernel(x, meta_tokens, attn_norm_g, w_in, conv_w, conv_b, conv_norm_g, conv_norm_b,
           w_out, mlp_norm_g, w_up, w_down, final_norm_g):
    x = np.asarray(x, np.float32)
    B, S, _ = x.shape
    T = NMETA + S
    NSLOT = S // 1024
    if S not in _NC_CACHE:
        _NC_CACHE[S] = build(S)
    nc = _NC_CACHE[S]
    f = lambda a: np.ascontiguousarray(np.asarray(a, np.float32))
    meta = f(meta_tokens)
    ropek = _rope_tab(np.arange(T))
    p = np.arange(128)
    consts = {
        "iota": np.tile(np.arange(512, dtype=np.float32)[None, :], (128, 1)),
        "pow2": np.tile((2.0 ** -(np.arange(NBIS) + 1.0)).astype(np.float32)[None, :], (128, 1)),
        "e32": (np.arange(32)[None, :] == (p % 32)[:, None]).astype(np.float32),
        "g4": (np.arange(4)[None, :] == (p // 32)[:, None]).astype(np.float32),
        "dm": np.concatenate([(np.arange(128)[None, :] == (32 * g + p % 32)[:, None]).astype(np.float32) for g in range(4)], axis=1),
    }
    shared = {
        "w_in": f(w_in[0]), "w_out": f(w_out[0]), "w_up": f(w_up[0]), "w_down": f(w_down[0]),
        "attn_g": f(attn_norm_g[0]), "mlp_g": f(mlp_norm_g[0]), "final_g": f(final_norm_g),
        "conv_w": f(conv_w[0]), "conv_b": f(conv_b[0]), "cn_g": f(conv_norm_g[0]), "cn_b": f(conv_norm_b[0]),
        "ropek": ropek,
    }
    shared.update(consts)
    in_maps = []
    for core in range(8):
        b, half = core // 2, core % 2
        hall = np.concatenate([meta, x[b]], axis=0)
        xown = np.zeros((NSLOT, 544, D), np.float32)
        ropeq = np.zeros((NSLOT, 512, 512), np.float32)
        qrel = np.zeros((128, NSLOT * 8), np.float32)
        for sl in range(NSLOT):
            c = _chunk_of(sl, half)
            p0 = NMETA + 512 * c
            lo = p0 - 32
            src_lo = max(lo, 0)
            xown[sl, src_lo - lo:, :] = hall[src_lo:p0 + 512]
            ropeq[sl] = ropek[p0:p0 + 512]
            w0 = NMETA + 1024 * sl
            for tb in range(4):
                for wc in range(2):
                    qrel[:, (sl * 4 + tb) * 2 + wc] = (p0 + tb * 128 + p) - w0 - 512 * wc
        m = dict(shared)
        m.update({"xall": hall, "xown": xown, "ropeq": ropeq, "qrel": qrel})
        in_maps.append(m)
    res = run_bass_kernel_spmd(nc, in_maps, core_ids=list(range(8)))
    out = np.zeros((B, S, D), np.float32)
    for core in range(8):
        b, half = core // 2, core % 2
        o = res.results[core]["out"]
        for sl in range(NSLOT):
            c = _chunk_of(sl, half)
            out[b, 512 * c:512 * (c + 1)] = o[sl * 512:(sl + 1) * 512]
    return out
```

```python
import numpy as np
from contextlib import ExitStack
import concourse.bass as bass
import concourse.mybir as mybir
from concourse.bass_utils import run_bass_kernel_spmd

F32 = mybir.dt.float32
BF16 = mybir.dt.bfloat16
ALU = mybir.AluOpType
AF = mybir.ActivationFunctionType
AX = mybir.AxisListType

D = 1024
NMETA = 16
DIN = 3144
DFF = 4096
EPS = 1e-5
KTOP = 256
NBIS = 20
ENGS = ("pe", "act", "dve", "pool", "sp")
SEM_CH = 30000
NDMA_SEM = 12


class Op:
    __slots__ = ("eng", "fn", "deps", "dma", "signals", "sig", "dma_idx")


class Sched:
    def __init__(self):
        self.ops = {e: [] for e in ENGS}
        self.lastw = {}
        self.readers = {}
        self.ndma = {e: 0 for e in ENGS}

    def op(self, eng, fn, reads=(), writes=(), dma=False):
        o = Op()
        o.eng = eng
        o.fn = fn
        o.dma = dma
        o.signals = False
        o.sig = None
        deps = {}
        xr = [k for k in reads if isinstance(k, tuple) and k[0] in ("pg", "psc", "pacc")]
        if xr:
            writes = list(writes) + [k for k in xr if k not in writes]
        for k in reads:
            w = self.lastw.get(k)
            if w is not None:
                deps[id(w)] = (w, True)
        for k in writes:
            w = self.lastw.get(k)
            if w is not None and id(w) not in deps:
                deps[id(w)] = (w, False)
            for r in self.readers.get(k, ()):
                if id(r) not in deps:
                    deps[id(r)] = (r, False)
        o.deps = []
        for d, raw in deps.values():
            if d.eng == eng and not d.dma:
                if eng == "pe":
                    continue
                if not raw and not dma:
                    continue
            d.signals = True
            o.deps.append(d)
        if dma:
            o.dma_idx = self.ndma[eng]
            self.ndma[eng] += 1
            o.signals = True
        for k in reads:
            self.readers.setdefault(k, []).append(o)
        for k in writes:
            self.lastw[k] = o
            self.readers[k] = []
        self.ops[eng].append(o)
        return o

    def emit(self, nc, stack):
        nsig = {}
        for e in ENGS:
            n = 0
            for o in self.ops[e]:
                if o.signals and not o.dma:
                    o.sig = n
                    n += 1
            nsig[e] = n
        csem = {}
        for e in ENGS:
            nch = (nsig[e] + SEM_CH - 1) // SEM_CH
            csem[e] = [stack.enter_context(nc.semaphore(f"c_{e}_{i}")) for i in range(nch)]
        dsem = {}
        for e in ENGS:
            if self.ndma[e]:
                dsem[e] = [stack.enter_context(nc.semaphore(f"d_{e}_{i}")) for i in range(NDMA_SEM)]

        def target(d):
            if d.dma:
                return dsem[d.eng][d.dma_idx % NDMA_SEM], 16 * (d.dma_idx // NDMA_SEM + 1)
            return csem[d.eng][d.sig // SEM_CH], d.sig % SEM_CH + 1

        block = stack.enter_context(nc.Block())

        def run(e):
            def body(eng):
                waited = {}
                for o in self.ops[e]:
                    tg = [target(d) for d in o.deps]
                    if o.dma and o.dma_idx >= NDMA_SEM:
                        tg.append((dsem[e][o.dma_idx % NDMA_SEM], 16 * (o.dma_idx // NDMA_SEM)))
                    for s, v in tg:
                        key = id(s)
                        if waited.get(key, 0) < v:
                            eng.wait_ge(s, v)
                            waited[key] = v
                    ins = o.fn(eng)
                    if o.signals:
                        if o.dma:
                            s, _ = target(o)
                            ins.then_inc(s, 16)
                        else:
                            ins.then_inc(csem[e][o.sig // SEM_CH], 1)
                if self.ndma[e]:
                    n = self.ndma[e]
                    for i in range(max(0, n - NDMA_SEM), n):
                        eng.wait_ge(dsem[e][i % NDMA_SEM], 16 * (i // NDMA_SEM + 1))
            return body

        for e, reg in (("pe", block.tensor), ("act", block.scalar), ("dve", block.vector),
                       ("pool", block.gpsimd), ("sp", block.sync)):
            if self.ops[e]:
                reg(run(e))


class Pool:
    def __init__(self, tiles, name, off=0):
        self.tiles = tiles
        self.name = name
        self.i = 0
        self.off = off

    def get(self):
        j = self.i % len(self.tiles)
        self.i += 1
        return self.tiles[j], (self.name, j + self.off)


def build(S):
    T = NMETA + S
    NCH = S // 512
    NSLOT = NCH // 2
    NKT = 1 + S // 128
    nc = bass.Bass("TRN2", target_bir_lowering=False)

    def din(name, shape, dt=F32):
        return nc.dram_tensor(name, shape, dt, kind="ExternalInput").ap()

    xall = din("xall", [T, D])
    xown = din("xown", [NSLOT, 544, D])
    ropek = din("ropek", [T, 512])
    ropeq = din("ropeq", [NSLOT, 512, 512])
    qrel_d = din("qrel", [128, NSLOT * 8])
    w_in = din("w_in", [D, DIN])
    w_out = din("w_out", [D, D])
    w_up = din("w_up", [D, DFF])
    w_down = din("w_down", [DFF, D])
    attn_g = din("attn_g", [D])
    mlp_g = din("mlp_g", [D])
    final_g = din("final_g", [D])
    conv_w = din("conv_w", [31, 512])
    conv_b = din("conv_b", [512])
    cn_g = din("cn_g", [512])
    cn_b = din("cn_b", [512])
    iota_d = din("iota", [128, 512])
    pow2_d = din("pow2", [128, NBIS])
    e32_d = din("e32", [128, 32])
    g4_d = din("g4", [128, 4])
    dm_d = din("dm", [128, 4 * 128])
    out_d = nc.dram_tensor("out", [NSLOT * 512, D], F32, kind="ExternalOutput").ap()

    k_scr = nc.dram_tensor("k_scr", [4, 128, T], BF16).ap()
    v_scr = nc.dram_tensor("v_scr", [T, 4, 129], BF16).ap()

    import os
    KSTOP = float(os.environ.get('KSTOP', '99'))
    S_ = Sched()
    stage = [0]

    def op(*a, **k):
        if stage[0] <= KSTOP:
            return S_.op(*a, **k)
        return None
    uid = [0]

    def sb(shape, dt, name=None):
        uid[0] += 1
        return nc.alloc_sbuf_tensor(f"{name or 't'}{uid[0]}", shape, dt)

    def mkpool(n, shape, dt, name):
        return Pool([sb(shape, dt, name) for _ in range(n)], name)

    identf = sb([128, 128], F32, "identf")
    ident = sb([128, 128], BF16, "ident")
    op("pool", lambda e: e.memset(identf[:], 0.0), writes=["identf"])
    op("pool", lambda e: e.affine_select(out=identf[:], in_=identf[:], pattern=[[-1, 128]],
                                         compare_op=ALU.not_equal, fill=1.0, base=0, channel_multiplier=1),
       reads=["identf"], writes=["identf"])
    op("dve", lambda e: e.tensor_copy(out=ident[:], in_=identf[:]), reads=["identf"], writes=["ident"])

    def load_const(name, shape, src, dt=F32):
        t = sb(shape, dt, name)
        op("sp", lambda e: e.dma_start(out=t[:], in_=src, allow_slow_non_contiguous=True), writes=[name], dma=True)
        return t

    iota = load_const("iota", [128, 512], iota_d)
    pow2 = load_const("pow2", [128, NBIS], pow2_d)
    e32 = load_const("e32", [128, 32], e32_d)
    g4 = load_const("g4", [128, 4], g4_d)
    qrel = load_const("qrelc", [128, NSLOT * 8], qrel_d)
    gA = load_const("gA", [128, 8], attn_g.rearrange("(c p) -> p c", p=128))
    gM = load_const("gM", [128, 8], mlp_g.rearrange("(c p) -> p c", p=128))
    def bcast_row(name, src, n):
        t = sb([128, n], F32, name)
        op("sp", lambda e: e.dma_start(out=t[:], in_=src.partition_broadcast(128)), writes=[name], dma=True)
        return t
    gF = bcast_row("gF", final_g, D)
    cb_bc = bcast_row("cb_bc", conv_b, 512)
    cg_bc = bcast_row("cg_bc", cn_g, 512)
    cnb_bc = bcast_row("cnb_bc", cn_b, 512)
    cwT = sb([128, 4, 31], F32, "cwT")
    for cc_ in range(4):
        op("sp", lambda e, cc_=cc_: e.dma_start(out=cwT[:, cc_, :], in_=conv_w[:, cc_ * 128:(cc_ + 1) * 128].rearrange("j p -> p j"),
                                                 allow_slow_non_contiguous=True), writes=["cwT"], dma=True)
    dmb = sb([128, 512], BF16, "dmb")
    op("pool", lambda e: e.dma_start(out=dmb[:], in_=dm_d), writes=["dmb"], dma=True)
    wik_t = sb([128, 8, 64], BF16, "wik")
    op("pool", lambda e: e.dma_start(out=wik_t[:, :, :], in_=w_in[:, 2048:2112].rearrange("(c p) n -> p c n", p=128)), writes=["wik"], dma=True)
    wiw_t = sb([128, 8, 8], BF16, "wiw")
    op("pool", lambda e: e.dma_start(out=wiw_t[:, :, :], in_=w_in[:, 2112:2120].rearrange("(c p) n -> p c n", p=128), allow_slow_non_contiguous=True),
       writes=["wiw"], dma=True)

    win_b = nc.dram_tensor("win_b", [D, DIN], BF16).ap()
    wout_b = nc.dram_tensor("wout_b", [D, D], BF16).ap()
    wup_b = nc.dram_tensor("wup_b", [D, DFF], BF16).ap()
    wdown_b = nc.dram_tensor("wdown_b", [DFF, D], BF16).ap()

    ikT = sb([128, T], BF16, "ikT")
    scores = sb([128, T], F32, "scores")
    wbuf = mkpool(4, [128, 8, 512], BF16, "wbuf")
    xt_pool = mkpool(3, [128, D], F32, "xt")
    xn_pool = mkpool(1, [128, D], BF16, "xn")
    xnT_pool = mkpool(2, [128, 8, 128], BF16, "xnT")
    rope_pool = mkpool(4, [128, 256], F32, "rope")
    st_pool = mkpool(4, [128, 8], F32, "stat")
    tm_pool = mkpool(3, [128, 512], BF16, "tm")
    rt_pool = mkpool(2, [128, 256], F32, "rt")
    jk_pool = mkpool(2, [128, 512], F32, "jk")
    bigjunk = sb([128, NMETA + 512 * ((T // 512) // 2 + 1)], mybir.dt.uint8, "bigjunk")
    vb_pool = mkpool(2, [128, 4, 129], BF16, "vb")
    for vbt_ in vb_pool.tiles:
        pass
    pg = Pool([nc.alloc_psum_tensor(f"pg{i}", [128, 512], F32) for i in range(4)], "pg")
    psc = [nc.alloc_psum_tensor(f"psc{i}", [128, 512], F32) for i in range(2)]
    pacc = [nc.alloc_psum_tensor(f"pacc{i}", [128, 512], F32) for i in range(2)]

    def pg_bf16(t):
        return t.bitcast(BF16) if hasattr(t, "bitcast") else None

    def rmsnorm_T(x_t, x_k, rows, gcol, xnT, xnT_k):
        st, st_k = st_pool.get()
        jk, jk_k = jk_pool.get()
        xn, xn_k = xn_pool.get()
        op("act", lambda e: e.activation(out=jk[0:rows, :], in_=x_t[0:rows, 0:512], func=AF.Square,
                                         accum_out=st[0:rows, 0:1]), reads=[x_k], writes=[jk_k, (st_k, 0)])
        op("act", lambda e: e.activation(out=jk[0:rows, :], in_=x_t[0:rows, 512:1024], func=AF.Square,
                                         accum_out=st[0:rows, 1:2]), reads=[x_k], writes=[jk_k, (st_k, 1)])
        op("dve", lambda e: e.tensor_tensor(out=st[0:rows, 2:3], in0=st[0:rows, 0:1], in1=st[0:rows, 1:2], op=ALU.add),
           reads=[(st_k, 0), (st_k, 1)], writes=[(st_k, 2)])
        op("dve", lambda e: e.tensor_scalar(out=st[0:rows, 3:4], in0=st[0:rows, 2:3], scalar1=1.0 / D, scalar2=EPS,
                                            op0=ALU.mult, op1=ALU.add), reads=[(st_k, 2)], writes=[(st_k, 3)])
        op("act", lambda e: e.activation(out=st[0:rows, 4:5], in_=st[0:rows, 3:4], func=AF.Sqrt),
           reads=[(st_k, 3)], writes=[(st_k, 4)])
        op("dve", lambda e: e.reciprocal(out=st[0:rows, 5:6], in_=st[0:rows, 4:5]), reads=[(st_k, 4)], writes=[(st_k, 5)])
        op("dve", lambda e: e.tensor_scalar(out=xn[0:rows, :], in0=x_t[0:rows, :], scalar1=st[0:rows, 5:6], scalar2=None,
                                            op0=ALU.mult), reads=[x_k, (st_k, 5)], writes=[xn_k])
        pt, pt_k = pg.get()
        ptb = pt[:].bitcast(BF16)
        for dc in range(8):
            op("pe", lambda e, dc=dc: e.transpose(out=ptb[:, dc * 128:dc * 128 + rows], in_=xn[0:rows, dc * 128:(dc + 1) * 128],
                                                  identity=ident[0:rows, 0:rows]),
               reads=[xn_k, "ident"], writes=[pt_k])
        op("dve", lambda e: e.tensor_tensor(out=xnT[:, :, 0:rows],
                                            in0=ptb.rearrange("p (c t) -> p c t", c=8)[:, :, 0:rows],
                                            in1=gcol[:, :].unsqueeze(2).to_broadcast([128, 8, rows]), op=ALU.mult),
           reads=[pt_k], writes=[xnT_k])
        return (st, st_k)

    def wviews(kind, i, w):
        if kind == "down":
            v = w[:, :, :].rearrange("p a b -> p (a b)").rearrange("p (f n) -> p f n", f=4)
            r = lambda a: a[i * 512:(i + 1) * 512, :].rearrange("(f p) n -> p f n", p=128)
            return v, r(w_down), r(wdown_b)
        src, dst = {"in": (w_in, win_b), "out": (w_out, wout_b), "up": (w_up, wup_b)}[kind]
        r = lambda a: a[:, i:i + 512].rearrange("(c p) n -> p c n", p=128) if kind == "in" else a[:, i * 512:(i + 1) * 512].rearrange("(c p) n -> p c n", p=128)
        return w[:, :, :], r(src), r(dst)

    def load_w(kind, i, cast=False):
        w, w_k = wbuf.get()
        v, src, scr = wviews(kind, i, w)
        if cast:
            op("pool", lambda e: e.dma_start(out=v, in_=src), writes=[w_k], dma=True)
        else:
            op("sp", lambda e: e.dma_start(out=v, in_=scr), reads=[("wcv", kind, i)], writes=[w_k], dma=True)
        return (v if kind == "down" else w), w_k

    def proj_tm(xnT, xnT_k, rows, w, w_k, ncols, ps, ps_k, pcol=0):
        for dc in range(8):
            op("pe", lambda e, dc=dc: e.matmul(ps[0:rows, pcol:pcol + ncols], lhsT=xnT[:, dc, 0:rows], rhs=w[:, dc, 0:ncols],
                                               start=(dc == 0), stop=(dc == 7)),
               reads=[xnT_k, w_k], writes=[ps_k])


    def rope_tm(ps, ps_k, rows, nh, hd, half, rp, rp_k, roff, dst, dst_k, dcol=0):
        r2 = 2 * half
        n = nh * hd
        xf, xf_k = jk_pool.get()
        op("act", lambda e: e.copy(out=xf[0:rows, 0:n], in_=ps[0:rows, 0:n]), reads=[ps_k], writes=[xf_k])
        op("act", lambda e: e.copy(out=dst[0:rows, dcol:dcol + n], in_=ps[0:rows, 0:n]), reads=[ps_k], writes=[dst_k])
        rt, rt_k = rt_pool.get()
        pv = xf[0:rows, 0:n].rearrange("p (h d) -> p h d", h=nh)
        A = rt[0:rows, 0:nh * r2].rearrange("p (h d) -> p h d", h=nh)
        Bm = rt[0:rows, 128:128 + nh * r2].rearrange("p (h d) -> p h d", h=nh)
        cc_ = rp[0:rows, roff:roff + nh * r2].rearrange("p (h d) -> p h d", h=nh)
        ss_ = rp[0:rows, roff + 128:roff + 128 + nh * r2].rearrange("p (h d) -> p h d", h=nh)
        op("dve", lambda e: e.tensor_tensor(out=A, in0=pv[:, :, 0:r2], in1=cc_, op=ALU.mult), reads=[xf_k, rp_k], writes=[(rt_k, 0)])
        op("dve", lambda e: e.tensor_tensor(out=Bm, in0=pv[:, :, 0:r2], in1=ss_, op=ALU.mult), reads=[xf_k, rp_k], writes=[(rt_k, 1)])
        dv = dst[0:rows, dcol:dcol + n].rearrange("p (h d) -> p h d", h=nh)
        op("dve", lambda e: e.tensor_tensor(out=dv[:, :, 0:half], in0=A[:, :, 0:half], in1=Bm[:, :, half:r2], op=ALU.subtract),
           reads=[(rt_k, 0), (rt_k, 1), dst_k], writes=[dst_k])
        op("dve", lambda e: e.tensor_tensor(out=dv[:, :, half:r2], in0=A[:, :, half:r2], in1=Bm[:, :, 0:half], op=ALU.add),
           reads=[(rt_k, 0), (rt_k, 1), dst_k], writes=[dst_k])

    stage[0] = 1
    for j_, vbt_ in enumerate(vb_pool.tiles):
        op("pool", lambda e, vbt_=vbt_: e.memset(vbt_[:, :, 128:129], 1.0), writes=[("vbones", ("vb", j_))])
    wk, wk_k = load_w("in", 512, cast=True)
    wv, wv_k = load_w("in", 1024, cast=True)
    cvt_pool = Pool(wbuf.tiles[2:4], "wbuf", off=2)
    for kind, idxs in (("in", (0, 1536, 2120, 2632)), ("out", (0, 1)), ("up", tuple(range(8))), ("down", tuple(range(8)))):
        for i in idxs:
            w, w_k = cvt_pool.get()
            v, src, scr = wviews(kind, i, w)
            op("pool", lambda e, v=v, src=src: e.dma_start(out=v, in_=src), writes=[w_k], dma=True)
            op("pool", lambda e, v=v, scr=scr: e.dma_start(out=scr, in_=v), reads=[w_k], writes=[("wcv", kind, i)], dma=True)
    wbuf.i = 2
    wik, wik_k = wik_t, "wik"
    stage[0] = 1
    def p1_tile(kt):
        rows = NMETA if kt == 0 else 128
        p0 = 0 if kt == 0 else NMETA + 128 * (kt - 1)
        xt, xt_k = xt_pool.get()
        op("sp", lambda e, xt=xt, p0=p0, rows=rows: e.dma_start(out=xt[0:rows, :], in_=xall[p0:p0 + rows, :]), writes=[xt_k], dma=True)
        rp, rp_k = rope_pool.get()
        op("sp", lambda e, rp=rp, p0=p0, rows=rows: e.dma_start(out=rp[0:rows, :], in_=ropek[p0:p0 + rows, 0:256]), writes=[rp_k], dma=True)
        rpi, rpi_k = rope_pool.get()
        op("sp", lambda e, rpi=rpi, p0=p0, rows=rows: e.dma_start(out=rpi[0:rows, :], in_=ropek[p0:p0 + rows, 256:512]), writes=[rpi_k], dma=True)
        xnT, xnT_k = xnT_pool.get()
        rmsnorm_T(xt, xt_k, rows, gA, xnT, xnT_k)
        yield
        psK, psK_k = pg.get()
        proj_tm(xnT, xnT_k, rows, wk, wk_k, 512, psK, psK_k)
        psV, psV_k = pg.get()
        proj_tm(xnT, xnT_k, rows, wv, wv_k, 512, psV, psV_k)
        psI, psI_k = pg.get()
        proj_tm(xnT, xnT_k, rows, wik, wik_k, 64, psI, psI_k)
        kb, kb_k = tm_pool.get()
        rope_tm(psK, psK_k, rows, 4, 128, 16, rp, rp_k, 0, kb, kb_k)
        vb, vb_k = vb_pool.get()
        op("act", lambda e, vb=vb, ps=psV, rows=rows: e.copy(out=vb[0:rows, :, 0:128], in_=ps[0:rows, :].rearrange("p (h d) -> p h d", h=4)),
           reads=[psV_k, ("vbones", vb_k)], writes=[vb_k])
        op("act", lambda e, vb=vb, p0=p0, rows=rows: e.dma_start(out=v_scr[p0:p0 + rows, :, :], in_=vb[0:rows, :, :]),
           reads=[vb_k, ("vbones", vb_k)], writes=[("v_scr", kt)], dma=True)
        ib, ib_k = tm_pool.get()
        rope_tm(psI, psI_k, rows, 1, 64, 8, rpi, rpi_k, 0, ib, ib_k)
        op("dve", lambda e, ib=ib, rows=rows: e.tensor_copy(out=ib[0:rows, 64:128], in_=ib[0:rows, 0:64]), reads=[ib_k], writes=[ib_k])
        pt, pt_k = pg.get()
        ptb = pt[:].bitcast(BF16)
        for h in range(4):
            op("pe", lambda e, h=h, ptb=ptb, kb=kb, rows=rows: e.transpose(out=ptb[:, h * 128:h * 128 + rows], in_=kb[0:rows, h * 128:(h + 1) * 128],
                                                                         identity=ident[0:rows, 0:rows]), reads=[kb_k, "ident"], writes=[pt_k])
        kTt, kTt_k = tm_pool.get()
        op("act", lambda e, kTt=kTt, ptb=ptb: e.copy(out=kTt[:, :], in_=ptb[:, 0:512]), reads=[pt_k], writes=[kTt_k])
        op("act", lambda e, kTt=kTt, p0=p0, rows=rows: e.dma_start(
            out=k_scr[:, :, p0:p0 + rows].rearrange("h p t -> p h t"),
            in_=kTt[:, :].rearrange("p (h t) -> p h t", h=4)[:, :, 0:rows]),
           reads=[kTt_k], writes=[("k_scr", kt)], dma=True)
        pt2, pt2_k = pg.get()
        ptb2 = pt2[:].bitcast(BF16)
        op("pe", lambda e, ptb2=ptb2, ib=ib, rows=rows: e.transpose(out=ptb2[:, 0:rows], in_=ib[0:rows, 0:128], identity=ident[0:rows, 0:rows]),
           reads=[ib_k, "ident"], writes=[pt2_k])
        op("act", lambda e, ptb2=ptb2, p0=p0, rows=rows: e.copy(out=ikT[:, p0:p0 + rows], in_=ptb2[:, 0:rows]), reads=[pt2_k], writes=[("ikT", kt)])

    def run_pipelined(gens):
        if gens:
            next(gens[0])
        for i in range(len(gens)):
            if i + 1 < len(gens):
                next(gens[i + 1])
            for _ in gens[i]:
                pass

    run_pipelined([p1_tile(kt) for kt in range(NKT)])

    stage[0] = 2
    qT = sb([128, 4, 512], BF16, "qT")
    iqT = sb([128, 4, 512], BF16, "iqT")
    iwt = sb([128, 4, 8], F32, "iwt")
    uT = sb([128, 4, 544], BF16, "uT")
    mixed = sb([128, 4, D], BF16, "mixed")
    ysb = sb([128, 4, 512], F32, "ysb")
    Dcc = sb([128, 31, 128], BF16, "Dcc")
    negm_pool = mkpool(2, [128, 512], F32, "negm")
    wabs2 = sb([128, 8], F32, "wabs2")
    wsgn = sb([128, 8], F32, "wsgn")
    bis = sb([128, 8 + NBIS], F32, "bis")
    top8 = sb([128, 8], F32, "top8")
    cst = sb([128, 4, 6], F32, "cst")
    cmv = sb([128, 4, 2], F32, "cmv")
    crs = sb([128, 4], F32, "crs")
    R_pool = mkpool(3, [128, 512], BF16, "R")
    PT_pool = mkpool(2, [128, 512], BF16, "PT")
    mb_pool = mkpool(2, [128, 512], BF16, "mb")
    GK = 4
    kv_pool = [(sb([128, 4, GK * 128], BF16, "kbuf"), sb([128, GK, 4, 129], BF16, "vbuf")) for _ in range(2)]
    kmeta = sb([128, 4, NMETA], BF16, "kmeta")
    vmeta = sb([128, 4, 129], BF16, "vmeta")
    op("sp", lambda e: e.dma_start(out=kmeta[:, :, :], in_=k_scr[:, :, 0:NMETA].rearrange("h p t -> p h t")),
       reads=[("k_scr", 0)], writes=["kmeta"], dma=True)
    op("sp", lambda e: e.dma_start(out=vmeta[0:NMETA, :, :], in_=v_scr[0:NMETA, :, :]),
       reads=[("v_scr", 0)], writes=["vmeta"], dma=True)
    mT = sb([128, 8, 128], BF16, "mT")
    hnT = sb([128, 8, 256], BF16, "hnT")
    uf_pool = mkpool(2, [128, 256], BF16, "uf")
    rden = sb([128, 4], F32, "rden")

    SC = 0.125 * (8 ** -0.5)
    kv_ctr = [0]

    for sl in range(NSLOT):
        E = NMETA + 512 * (2 * sl + 2)
        W0 = E - 1024
        NCHK = 2 * sl + 2
        wq, wq_k = load_w("in", 0)
        wiq, wiq_k = load_w("in", 1536)
        wga, wga_k = load_w("in", 2120)
        wgg, wgg_k = load_w("in", 2632)
        plan = []
        for pair_ in range(2):
            plan += [("out", 0), ("out", 1)]
            for fg_ in range(8):
                plan += [("up", fg_), ("down", fg_)]
        wfifo = []

        def issue_next():
            if plan:
                kind_, i_ = plan.pop(0)
                wfifo.append(load_w(kind_, i_))
        def sa_tile(ti, sl=sl, wq=wq, wq_k=wq_k, wiq=wiq, wiq_k=wiq_k, wga=wga, wga_k=wga_k, wgg=wgg, wgg_k=wgg_k):
            rows = 32 if ti == 0 else 128
            r0 = 0 if ti == 0 else 32 + 128 * (ti - 1)
            xt, xt_k = xt_pool.get()
            op("sp", lambda e, xt=xt, r0=r0, rows=rows, sl=sl: e.dma_start(out=xt[0:rows, :], in_=xown[sl, r0:r0 + rows, :]),
               writes=[xt_k], dma=True)
            xnT, xnT_k = xnT_pool.get()
            rmsnorm_T(xt, xt_k, rows, gA, xnT, xnT_k)
            if ti > 0:
                tb = ti - 1
                rp, rp_k = rope_pool.get()
                op("sp", lambda e, rp=rp, tb=tb, sl=sl: e.dma_start(out=rp[:, :], in_=ropeq[sl, 128 * tb:128 * (tb + 1), 0:256]), writes=[rp_k], dma=True)
                rpi, rpi_k = rope_pool.get()
                op("sp", lambda e, rpi=rpi, tb=tb, sl=sl: e.dma_start(out=rpi[:, :], in_=ropeq[sl, 128 * tb:128 * (tb + 1), 256:512]), writes=[rpi_k], dma=True)
            yield
            if ti > 0:
                psq, psq_k = pg.get()
                proj_tm(xnT, xnT_k, 128, wq, wq_k, 512, psq, psq_k)
                psi, psi_k = pg.get()
                proj_tm(xnT, xnT_k, 128, wiq, wiq_k, 512, psi, psi_k)
                psw, psw_k = pg.get()
                proj_tm(xnT, xnT_k, 128, wiw_t, "wiw", 8, psw, psw_k)
                op("dve", lambda e, ps=psw, tb=tb: e.tensor_scalar(out=iwt[:, tb, :], in0=ps[:, 0:8], scalar1=SC, scalar2=None, op0=ALU.mult),
                   reads=[psw_k], writes=[("iwt", tb)])
                qb, qb_k = tm_pool.get()
                rope_tm(psq, psq_k, 128, 4, 128, 16, rp, rp_k, 0, qb, qb_k)
                ib, ib_k = tm_pool.get()
                rope_tm(psi, psi_k, 128, 8, 64, 8, rpi, rpi_k, 0, ib, ib_k)
            psa, psa_k = pg.get()
            proj_tm(xnT, xnT_k, rows, wga, wga_k, 512, psa, psa_k)
            psg, psg_k = pg.get()
            proj_tm(xnT, xnT_k, rows, wgg, wgg_k, 512, psg, psg_k)
            if ti > 0:
                pt, pt_k = pg.get()
                ptb = pt[:].bitcast(BF16)
                for h in range(4):
                    op("pe", lambda e, h=h, ptb=ptb, qb=qb: e.transpose(out=ptb[:, h * 128:(h + 1) * 128], in_=qb[:, h * 128:(h + 1) * 128], identity=ident[:, :]),
                       reads=[qb_k, "ident"], writes=[pt_k])
                op("act", lambda e, ptb=ptb, tb=tb: e.copy(out=qT[:, :, tb * 128:(tb + 1) * 128], in_=ptb[:, 0:512].rearrange("p (h t) -> p h t", h=4)),
                   reads=[pt_k], writes=[("qT", tb)])
                pt2, pt2_k = pg.get()
                ptb2 = pt2[:].bitcast(BF16)
                for hp in range(4):
                    op("pe", lambda e, hp=hp, ptb2=ptb2, ib=ib: e.transpose(out=ptb2[:, hp * 128:(hp + 1) * 128], in_=ib[:, hp * 128:(hp + 1) * 128], identity=ident[:, :]),
                       reads=[ib_k, "ident"], writes=[pt2_k])
                op("act", lambda e, ptb2=ptb2, tb=tb: e.copy(out=iqT[:, :, tb * 128:(tb + 1) * 128], in_=ptb2[:, 0:512].rearrange("p (h t) -> p h t", h=4)),
                   reads=[pt2_k], writes=[("iqT", tb)])
            jk, jk_k = jk_pool.get()
            op("act", lambda e, jk=jk, psg=psg, rows=rows: e.activation(out=jk[0:rows, :], in_=psg[0:rows, :], func=AF.Sigmoid),
               reads=[psg_k], writes=[jk_k])
            ub, ub_k = tm_pool.get()
            op("dve", lambda e, ub=ub, psa=psa, jk=jk, rows=rows: e.tensor_tensor(out=ub[0:rows, :], in0=psa[0:rows, :], in1=jk[0:rows, :], op=ALU.mult),
               reads=[psa_k, jk_k], writes=[ub_k])
            pt3, pt3_k = pg.get()
            ptb3 = pt3[:].bitcast(BF16)
            for cc in range(4):
                op("pe", lambda e, cc=cc, ptb3=ptb3, ub=ub, rows=rows: e.transpose(out=ptb3[:, cc * 128:cc * 128 + rows], in_=ub[0:rows, cc * 128:(cc + 1) * 128],
                                                                               identity=ident[0:rows, 0:rows]),
                   reads=[ub_k, "ident"], writes=[pt3_k])
            op("act", lambda e, ptb3=ptb3, r0=r0, rows=rows: e.copy(out=uT[:, :, r0:r0 + rows], in_=ptb3[:, 0:512].rearrange("p (c t) -> p c t", c=4)[:, :, 0:rows]),
               reads=[pt3_k], writes=[("uT", ti)])

        run_pipelined([sa_tile(ti) for ti in range(5)])
        stage[0] = 3
        UTK = [("uT", ti) for ti in range(5)]
        for cc in range(4):
            op("dve", lambda e, cc=cc: e.tensor_tensor(out=Dcc[:, :, :], in0=identf[:, :].unsqueeze(1).to_broadcast([128, 31, 128]),
                                                       in1=cwT[:, cc, :].unsqueeze(2).to_broadcast([128, 31, 128]), op=ALU.mult),
               reads=["identf", "cwT"], writes=["Dcc"])
            for tb in range(4):
                ps, ps_k = pg.get()
                for j in range(31):
                    c0 = 2 + tb * 128 + j
                    op("pe", lambda e, ps=ps, cc=cc, c0=c0, j=j: e.matmul(ps[:, 0:128], lhsT=uT[:, cc, c0:c0 + 128], rhs=Dcc[:, j, :], start=(j == 0), stop=(j == 30)),
                       reads=UTK + ["Dcc"], writes=[ps_k])
                op("act", lambda e, ps=ps, tb=tb, cc=cc: e.copy(out=ysb[:, tb, cc * 128:(cc + 1) * 128], in_=ps[:, 0:128]),
                   reads=[ps_k], writes=[("ysb", tb)])
        for tb in range(4):
            yk = ("ysb", tb)
            y = ysb[:, tb, :]
            op("dve", lambda e, y=y: e.tensor_tensor(out=y, in0=y, in1=cb_bc[:, :], op=ALU.add), reads=[yk, "cb_bc"], writes=[yk])
            for g in range(4):
                op("dve", lambda e, y=y, g=g: e.bn_stats(out=cst[:, g, :], in_=y[:, g * 128:(g + 1) * 128]), reads=[yk], writes=[("cst", g)])
                op("dve", lambda e, g=g: e.bn_aggr(out=cmv[:, g, :], in_=cst[:, g, :]), reads=[("cst", g)], writes=[("cmv", g)])
            CM = [("cmv", g) for g in range(4)]
            op("dve", lambda e: e.tensor_scalar(out=crs[:, :], in0=cmv[:, :, 1], scalar1=EPS, scalar2=None, op0=ALU.add), reads=CM, writes=["crs"])
            op("act", lambda e: e.activation(out=crs[:, :], in_=crs[:, :], func=AF.Sqrt), reads=["crs"], writes=["crs"])
            op("dve", lambda e: e.reciprocal(out=crs[:, :], in_=crs[:, :]), reads=["crs"], writes=["crs"])
            for g in range(4):
                op("dve", lambda e, y=y, g=g: e.tensor_scalar(out=y[:, g * 128:(g + 1) * 128], in0=y[:, g * 128:(g + 1) * 128],
                                                            scalar1=cmv[:, g, 0:1], scalar2=crs[:, g:g + 1], op0=ALU.subtract, op1=ALU.mult),
                   reads=[yk, "crs"] + CM, writes=[yk])
            op("dve", lambda e, y=y: e.tensor_tensor(out=y, in0=y, in1=cg_bc[:, :], op=ALU.mult), reads=[yk, "cg_bc"], writes=[yk])
            op("dve", lambda e, y=y: e.tensor_tensor(out=y, in0=y, in1=cnb_bc[:, :], op=ALU.add), reads=[yk, "cnb_bc"], writes=[yk])
            op("act", lambda e, y=y, tb=tb: e.activation(out=mixed[:, tb, 512:1024], in_=y, func=AF.Silu), reads=[yk], writes=[("mixC", tb)])

        stage[0] = 4
        for tb in range(4):
            if tb == 3:
                for _ in range(4):
                    issue_next()
            op("act", lambda e, tb=tb: e.activation(out=wabs2[:, :], in_=iwt[:, tb, :], func=AF.Abs, scale=2.0),
               reads=[("iwt", tb)], writes=["wabs2"])
            op("dve", lambda e, tb=tb: e.tensor_scalar(out=wsgn[:, :], in0=iwt[:, tb, :], scalar1=0.0, scalar2=0.5, op0=ALU.is_ge, op1=ALU.subtract),
               reads=[("iwt", tb)], writes=["wsgn"])
            chunks = [(0, NMETA)] + [(NMETA + 512 * m, 512) for m in range(NCHK)]
            SCK = []
            wcnt = 0
            for ci, (c0, cn) in enumerate(chunks):
                ikk = [("ikT", 0)] if ci == 0 else [("ikT", 1 + 4 * (ci - 1) + q) for q in range(4)]
                sk = ("sc", ci)
                SCK.append(sk)
                for h in range(8):
                    par, hp = h % 2, h // 2
                    ps, ps_k = pg.get()
                    op("pe", lambda e, ps=ps, par=par, hp=hp, tb=tb, c0=c0, cn=cn: e.matmul(ps[:, 0:cn], lhsT=iqT[par * 64:(par + 1) * 64, hp, tb * 128:(tb + 1) * 128],
                                                                                         rhs=ikT[par * 64:(par + 1) * 64, c0:c0 + cn], start=True, stop=True),
                       reads=[("iqT", tb)] + ikk, writes=[ps_k])
                    R, R_k = R_pool.get()
                    op("act", lambda e, R=R, ps=ps, cn=cn, h=h: e.activation(out=R[:, 0:cn], in_=ps[:, 0:cn], func=AF.Relu, scale=wabs2[:, h:h + 1]),
                       reads=[ps_k, "wabs2"], writes=[R_k])
                    if h == 0:
                        op("dve", lambda e, R=R, c0=c0, cn=cn: e.tensor_scalar(out=scores[:, c0:c0 + cn], in0=R[:, 0:cn], scalar1=wsgn[:, 0:1], scalar2=None, op0=ALU.mult),
                           reads=[R_k, "wsgn"], writes=[sk])
                    else:
                        op("dve", lambda e, R=R, c0=c0, cn=cn, h=h: e.scalar_tensor_tensor(out=scores[:, c0:c0 + cn], in0=R[:, 0:cn], scalar=wsgn[:, h:h + 1],
                                                                                         in1=scores[:, c0:c0 + cn], op0=ALU.mult, op1=ALU.add),
                           reads=[R_k, "wsgn", sk], writes=[sk])
                if c0 >= W0 and ci > 0:
                    col = (sl * 4 + tb) * 2 + wcnt
                    negm, negm_k = negm_pool.get()
                    op("dve", lambda e, col=col, negm=negm: e.tensor_scalar(out=negm[:, :], in0=iota[:, :], scalar1=qrel[:, col:col + 1], scalar2=-1e30,
                                                                          op0=ALU.is_gt, op1=ALU.mult), reads=["iota", "qrelc"], writes=[negm_k])
                    jk, jk_k = jk_pool.get()
                    op("dve", lambda e, jk=jk, c0=c0, negm=negm: e.tensor_tensor(out=jk[:, :], in0=scores[:, c0:c0 + 512], in1=negm[:, :], op=ALU.subtract),
                       reads=[sk, negm_k], writes=[jk_k])
                    op("dve", lambda e, jk=jk, wcnt=wcnt: e.tensor_reduce(out=bis[:, 5 + wcnt:6 + wcnt], in_=jk[:, :], axis=AX.X, op=ALU.min),
                       reads=[jk_k], writes=[("bis", 5 + wcnt)])
                    op("dve", lambda e, c0=c0, negm=negm: e.tensor_tensor(out=scores[:, c0:c0 + 512], in0=scores[:, c0:c0 + 512], in1=negm[:, :], op=ALU.add),
                       reads=[sk, negm_k], writes=[sk])
                    wcnt += 1
            assert wcnt == 2
            stage[0] = 5
            nD = max(1, int(round(0.45 * NCHK)))
            ED = NMETA + 512 * nD
            EA = E - ED
            SCK_D = SCK[0:1 + nD]
            SCK_A = SCK[1 + nD:]
            op("dve", lambda e, E=E: e.max(out=top8[:, :], in_=scores[:, 0:E]), reads=SCK, writes=["top8"])
            op("dve", lambda e, W0=W0: e.tensor_reduce(out=bis[:, 7:8], in_=scores[:, 0:W0], axis=AX.X, op=ALU.min), reads=SCK, writes=[("bis", 7)])
            op("dve", lambda e: e.tensor_reduce(out=bis[:, 0:1], in_=bis[:, 5:8], axis=AX.X, op=ALU.min),
               reads=[("bis", 5), ("bis", 6), ("bis", 7)], writes=[("bis", 0)])
            op("dve", lambda e: e.tensor_tensor(out=bis[:, 1:2], in0=top8[:, 0:1], in1=bis[:, 0:1], op=ALU.subtract),
               reads=["top8", ("bis", 0)], writes=[("bis", 1)])
            op("dve", lambda e: e.tensor_scalar(out=bis[:, 8:8 + NBIS], in0=pow2[:, :], scalar1=bis[:, 1:2], scalar2=None, op0=ALU.mult),
               reads=["pow2", ("bis", 1)], writes=["bisW"])
            op("dve", lambda e: e.tensor_tensor(out=bis[:, 2:3], in0=bis[:, 8:9], in1=bis[:, 0:1], op=ALU.add),
               reads=["bisW", ("bis", 0)], writes=[("bis", 2)])
            for k in range(NBIS):
                op("dve", lambda e, ED=ED: e.tensor_scalar(out=bigjunk[:, 0:ED], in0=scores[:, 0:ED], scalar1=bis[:, 2:3], scalar2=None,
                                                          op0=ALU.is_ge, op1=ALU.add, accum_out=bis[:, 3:4]),
                   reads=SCK_D + [("bis", 2)], writes=["bigjunkD", ("bis", 3)])
                op("act", lambda e, ED=ED, E=E, EA=EA: e.activation(out=bigjunk[:, 0:EA].bitcast(mybir.dt.int8), in_=scores[:, ED:E], func=AF.Sign,
                                                            bias=bis[:, 2:3], scale=-1.0, accum_out=bis[:, 5:6]),
                   reads=SCK_A + [("bis", 2)], writes=["bigjunkA", ("bis", 5)])
                op("dve", lambda e: e.scalar_tensor_tensor(out=bis[:, 6:7], in0=bis[:, 3:4], scalar=2.0, in1=bis[:, 5:6], op0=ALU.mult, op1=ALU.subtract),
                   reads=[("bis", 3), ("bis", 5)], writes=[("bis", 6)])
                op("dve", lambda e, k=k, EA=EA: e.tensor_scalar(out=bis[:, 4:5], in0=bis[:, 6:7], scalar1=float(2 * KTOP - 1 - EA), scalar2=bis[:, 8 + k:9 + k],
                                                               op0=ALU.is_ge, op1=ALU.mult), reads=[("bis", 6), "bisW"], writes=[("bis", 4)])
                if k < NBIS - 1:
                    op("dve", lambda e, k=k: e.scalar_tensor_tensor(out=bis[:, 2:3], in0=bis[:, 4:5], scalar=bis[:, 9 + k:10 + k], in1=bis[:, 2:3],
                                                                  op0=ALU.subtract, op1=ALU.add),
                       reads=[("bis", 4), "bisW", ("bis", 2)], writes=[("bis", 2)])
                else:
                    op("dve", lambda e, k=k: e.scalar_tensor_tensor(out=bis[:, 0:1], in0=bis[:, 4:5], scalar=bis[:, 8 + k:9 + k], in1=bis[:, 2:3],
                                                                  op0=ALU.subtract, op1=ALU.add),
                       reads=[("bis", 4), "bisW", ("bis", 2)], writes=[("bis", 0)])
            stage[0] = 6
            nkb_total = 1 + 4 * NCHK
            kblist = []
            for ci, (c0, cn) in enumerate(chunks):
                for kb in range(1 if ci == 0 else 4):
                    kblist.append((ci, c0, cn, kb))
            cstate = {}

            def chunk_setup(ci, c0, cn):
                mbt, mb_k = mb_pool.get()
                op("dve", lambda e, mbt=mbt, c0=c0, cn=cn: e.tensor_scalar(out=mbt[:, 0:cn], in0=scores[:, c0:c0 + cn], scalar1=bis[:, 0:1], scalar2=-30000.0,
                                                                          op0=ALU.is_lt, op1=ALU.mult), reads=[SCK[ci], ("bis", 0)], writes=[mb_k])
                if ci == 0:
                    cstate[ci] = (mbt, mb_k, kmeta, vmeta, "kmeta", "vmeta")
                    return
                kvn = kv_ctr[0] % 2
                kv_ctr[0] += 1
                kbuf, vbuf = kv_pool[kvn]
                kvk, vvk = ("kbuf", kvn), ("vbuf", kvn)
                tl = [1 + 4 * (ci - 1) + q for q in range(4)]
                op("sp", lambda e, kbuf=kbuf, c0=c0: e.dma_start(out=kbuf[:, :, :], in_=k_scr[:, :, c0:c0 + 512].rearrange("h p t -> p h t")),
                   reads=[("k_scr", t) for t in tl], writes=[kvk], dma=True)
                op("sp", lambda e, vbuf=vbuf, c0=c0: e.dma_start(out=vbuf[:, :, :, :].rearrange("p g h e -> p g (h e)"),
                                                                 in_=v_scr[c0:c0 + 512, :, :].rearrange("(g p) h e -> p g (h e)", p=128)),
                   reads=[("v_scr", t) for t in tl], writes=[vvk], dma=True)
                cstate[ci] = (mbt, mb_k, kbuf, vbuf, kvk, vvk)

            def emit_S(i):
                ci, c0, cn, kb = kblist[i]
                if kb == 0:
                    chunk_setup(ci, c0, cn)
                mbt, mb_k, kbuf, vbuf, kvk, vvk = cstate[ci]
                ks = NMETA if ci == 0 else 128
                st, st_k = pg.get()
                for h in range(4):
                    op("pe", lambda e, st=st, h=h, kbuf=kbuf, kb=kb, ks=ks, tb=tb: e.matmul(st[0:ks, h * 128:(h + 1) * 128], lhsT=kbuf[:, h, kb * 128:kb * 128 + ks],
                                                                                         rhs=qT[:, h, tb * 128:(tb + 1) * 128], start=True, stop=False),
                       reads=[kvk, ("qT", tb)], writes=[st_k])
                    op("pe", lambda e, st=st, h=h, mbt=mbt, kb=kb, ks=ks: e.matmul(st[0:ks, h * 128:(h + 1) * 128], lhsT=mbt[:, kb * 128:kb * 128 + ks],
                                                                                 rhs=ident[:, :], start=False, stop=True),
                       reads=[mb_k, "ident"], writes=[st_k])
                PT, PT_k = PT_pool.get()
                op("act", lambda e, PT=PT, st=st, ks=ks: e.activation(out=PT[0:ks, :], in_=st[0:ks, :], func=AF.Exp, scale=128 ** -0.5),
                   reads=[st_k], writes=[PT_k])
                return (PT, PT_k)

            def emit_P(i, PTs):
                ci, c0, cn, kb = kblist[i]
                mbt, mb_k, kbuf, vbuf, kvk, vvk = cstate[ci]
                ks = NMETA if ci == 0 else 128
                PT, PT_k = PTs
                for h in range(4):
                    acc = pacc[h // 2]
                    a0 = (h % 2) * 256
                    rhs = vbuf[0:ks, h, :] if ci == 0 else vbuf[:, kb, h, :]
                    op("pe", lambda e, acc=acc, a0=a0, PT=PT, h=h, ks=ks, rhs=rhs, i=i: e.matmul(acc[:, a0:a0 + 129], lhsT=PT[0:ks, h * 128:(h + 1) * 128], rhs=rhs,
                                                                                           start=(i == 0 and h % 2 == 0), stop=(i == nkb_total - 1),
                                                                                           skip_group_check=True),
                       reads=[PT_k, vvk], writes=[("pacc", h // 2)])

            prev = emit_S(0)
            for i in range(nkb_total):
                nxt = emit_S(i + 1) if i + 1 < nkb_total else None
                emit_P(i, prev)
                prev = nxt
            for h in range(4):
                acc = pacc[h // 2]
                a0 = (h % 2) * 256
                op("dve", lambda e, acc=acc, a0=a0, h=h: e.reciprocal(out=rden[:, h:h + 1], in_=acc[:, a0 + 128:a0 + 129]),
                   reads=[("pacc", h // 2)], writes=[("rden", h)])
                op("dve", lambda e, acc=acc, a0=a0, h=h, tb=tb: e.tensor_scalar(out=mixed[:, tb, h * 128:(h + 1) * 128], in0=acc[:, a0:a0 + 128], scalar1=rden[:, h:h + 1],
                                                                              scalar2=None, op0=ALU.mult),
                   reads=[("pacc", h // 2), ("rden", h)], writes=[("mixA", tb, h)])

        stage[0] = 7
        for pair in range(2):
            h1s = []
            while len(wfifo) < 2:
                issue_next()
            wos = [wfifo.pop(0), wfifo.pop(0)]
            for t2 in range(2):
                tb = 2 * pair + t2
                pt, pt_k = pg.get()
                ptb = pt[:].bitcast(BF16)
                for ec in range(8):
                    op("pe", lambda e, ptb=ptb, ec=ec, tb=tb: e.transpose(out=ptb[:, ec * 128:(ec + 1) * 128], in_=mixed[:, tb, ec * 128:(ec + 1) * 128], identity=ident[:, :]),
                       reads=[("mixA", tb, h) for h in range(4)] + [("mixC", tb), "ident"], writes=[pt_k])
                op("act", lambda e, ptb=ptb: e.copy(out=mT[:, :, :], in_=ptb[:, :].rearrange("p (c t) -> p c t", c=8)), reads=[pt_k], writes=["mT"])
                xt, xt_k = xt_pool.get()
                op("sp", lambda e, xt=xt, sl=sl, tb=tb: e.dma_start(out=xt[:, :], in_=xown[sl, 32 + tb * 128:32 + (tb + 1) * 128, :]), writes=[xt_k], dma=True)
                h1, h1_k = xt, xt_k
                for half in range(2):
                    wo, wo_k = wos[half]
                    for ec in range(8):
                        op("pe", lambda e, half=half, ec=ec, wo=wo: e.matmul(psc[half][:, :], lhsT=mT[:, ec, :], rhs=wo[:, ec, :], start=(ec == 0), stop=(ec == 7)),
                           reads=["mT", wo_k], writes=[("psc", half)])
                    op("dve", lambda e, half=half, h1=h1, xt=xt: e.tensor_tensor(out=h1[:, half * 512:(half + 1) * 512], in0=psc[half][:, :],
                                                                               in1=xt[:, half * 512:(half + 1) * 512], op=ALU.add),
                       reads=[("psc", half), xt_k], writes=[h1_k])
                rmsnorm_T(h1, h1_k, 128, gM, hnT[:, :, t2 * 128:(t2 + 1) * 128], ("hnT", t2))
                h1s.append((h1, h1_k, tb))
            issue_next()
            issue_next()
            accs = [[(psc[0], ("psc", 0)), (psc[1], ("psc", 1))], [(pacc[0], ("pacc", 0)), (pacc[1], ("pacc", 1))]]
            fstate = {}

            def ffn_up(i):
                fg, fc = divmod(i, 4)
                if fc == 0:
                    while len(wfifo) < 2:
                        issue_next()
                    fstate[fg] = (wfifo.pop(0), wfifo.pop(0))
                (wu, wu_k), (wdv, wd_k) = fstate[fg]
                ps, ps_k = pg.get()
                for dc in range(8):
                    op("pe", lambda e, ps=ps, wu=wu, dc=dc, fc=fc: e.matmul(ps[:, 0:256], lhsT=wu[:, dc, fc * 128:(fc + 1) * 128], rhs=hnT[:, dc, :],
                                                                         start=(dc == 0), stop=(dc == 7)),
                       reads=[wu_k, ("hnT", 0), ("hnT", 1)], writes=[ps_k])
                if fc == 3:
                    issue_next()
                uf, uf_k = uf_pool.get()
                op("act", lambda e, uf=uf, ps=ps: e.activation(out=uf[:, :], in_=ps[:, 0:256], func=AF.Relu), reads=[ps_k], writes=[uf_k])
                op("pool", lambda e, uf=uf: e.tensor_tensor(out=uf[:, :], in0=uf[:, :], in1=uf[:, :], op=ALU.mult), reads=[uf_k], writes=[uf_k])
                return uf, uf_k

            def ffn_down(i, ufs):
                fg, fc = divmod(i, 4)
                uf, uf_k = ufs
                (wu, wu_k), (wdv, wd_k) = fstate[fg]
                for t2 in range(2):
                    for half in range(2):
                        acc, acc_k = accs[t2][half]
                        op("pe", lambda e, acc=acc, uf=uf, t2=t2, half=half, wdv=wdv, fc=fc, i=i: e.matmul(
                            acc[:, :], lhsT=uf[:, t2 * 128:(t2 + 1) * 128], rhs=wdv[:, fc, half * 512:(half + 1) * 512], start=(i == 0), stop=(i == 31)),
                           reads=[uf_k, wd_k], writes=[acc_k])
                if fc == 3:
                    issue_next()

            cur = ffn_up(0)
            for i in range(32):
                nxt = ffn_up(i + 1) if i + 1 < 32 else None
                ffn_down(i, cur)
                cur = nxt
            for t2 in range(2):
                h1, h1_k, tb = h1s[t2]
                for half in range(2):
                    acc, acc_k = accs[t2][half]
                    op("dve", lambda e, acc=acc, h1=h1, half=half: e.tensor_tensor(out=h1[:, half * 512:(half + 1) * 512], in0=acc[:, :],
                                                                                 in1=h1[:, half * 512:(half + 1) * 512], op=ALU.add),
                       reads=[acc_k, h1_k], writes=[h1_k])
                st, st_k = st_pool.get()
                jk, jk_k = jk_pool.get()
                op("act", lambda e, jk=jk, h1=h1, st=st: e.activation(out=jk[:, :], in_=h1[:, 0:512], func=AF.Square, accum_out=st[:, 0:1]),
                   reads=[h1_k], writes=[jk_k, (st_k, 0)])
                op("act", lambda e, jk=jk, h1=h1, st=st: e.activation(out=jk[:, :], in_=h1[:, 512:1024], func=AF.Square, accum_out=st[:, 1:2]),
                   reads=[h1_k], writes=[jk_k, (st_k, 1)])
                op("dve", lambda e, st=st: e.tensor_tensor(out=st[:, 2:3], in0=st[:, 0:1], in1=st[:, 1:2], op=ALU.add), reads=[(st_k, 0), (st_k, 1)], writes=[(st_k, 2)])
                op("dve", lambda e, st=st: e.tensor_scalar(out=st[:, 3:4], in0=st[:, 2:3], scalar1=1.0 / D, scalar2=EPS, op0=ALU.mult, op1=ALU.add),
                   reads=[(st_k, 2)], writes=[(st_k, 3)])
                op("act", lambda e, st=st: e.activation(out=st[:, 4:5], in_=st[:, 3:4], func=AF.Sqrt), reads=[(st_k, 3)], writes=[(st_k, 4)])
                op("dve", lambda e, st=st: e.reciprocal(out=st[:, 5:6], in_=st[:, 4:5]), reads=[(st_k, 4)], writes=[(st_k, 5)])
                op("dve", lambda e, h1=h1, st=st: e.scalar_tensor_tensor(out=h1[:, :], in0=h1[:, :], scalar=st[:, 5:6], in1=gF[:, :], op0=ALU.mult, op1=ALU.mult),
                   reads=[h1_k, (st_k, 5), "gF"], writes=[h1_k])
                r0 = sl * 512 + tb * 128
                op("act", lambda e, h1=h1, r0=r0: e.dma_start(out=out_d[r0:r0 + 128, :], in_=h1[:, :]), reads=[h1_k], writes=[("out", r0)], dma=True)

    if os.environ.get("KDEBUG"):
        print("sbuf remaining", nc.sbuf_bytes_remaining, "ops", {e: len(S_.ops[e]) for e in ENGS})
    with ExitStack() as stack:
        S_.emit(nc, stack)
    return nc


def _rope_tab(pos):
    pos = np.asarray(pos, np.float32)
    out = np.zeros((len(pos), 512), np.float32)
    inv = (np.float32(500000.0) ** (-np.arange(0, 32, 2, dtype=np.float32) / np.float32(32))).astype(np.float32)
    ang = pos[:, None] * inv[None, :]
    c, s = np.cos(ang).astype(np.float32), np.sin(ang).astype(np.float32)
    out[:, 0:128] = np.tile(np.concatenate([c, c], 1), (1, 4))
    out[:, 128:256] = np.tile(np.concatenate([s, s], 1), (1, 4))
    inv = (np.float32(500000.0) ** (-np.arange(0, 16, 2, dtype=np.float32) / np.float32(16))).astype(np.float32)
    ang = pos[:, None] * inv[None, :]
    c, s = np.cos(ang).astype(np.float32), np.sin(ang).astype(np.float32)
    out[:, 256:384] = np.tile(np.concatenate([c, c], 1), (1, 8))
    out[:, 384:512] = np.tile(np.concatenate([s, s], 1), (1, 8))
    return out


def _chunk_of(sl, half):
    return 2 * sl + (sl % 2) if half == 0 else 2 * sl + 1 - (sl % 2)


_NC_CACHE = {}


def kernel(x, meta_tokens, attn_norm_g, w_in, conv_w, conv_b, conv_norm_g, conv_norm_b,
           w_out, mlp_norm_g, w_up, w_down, final_norm_g):
    x = np.asarray(x, np.float32)
    B, S, _ = x.shape
    T = NMETA + S
    NSLOT = S // 1024
    if S not in _NC_CACHE:
        _NC_CACHE[S] = build(S)
    nc = _NC_CACHE[S]
    f = lambda a: np.ascontiguousarray(np.asarray(a, np.float32))
    meta = f(meta_tokens)
    ropek = _rope_tab(np.arange(T))
    p = np.arange(128)
    consts = {
        "iota": np.tile(np.arange(512, dtype=np.float32)[None, :], (128, 1)),
        "pow2": np.tile((2.0 ** -(np.arange(NBIS) + 1.0)).astype(np.float32)[None, :], (128, 1)),
        "e32": (np.arange(32)[None, :] == (p % 32)[:, None]).astype(np.float32),
        "g4": (np.arange(4)[None, :] == (p // 32)[:, None]).astype(np.float32),
        "dm": np.concatenate([(np.arange(128)[None, :] == (32 * g + p % 32)[:, None]).astype(np.float32) for g in range(4)], axis=1),
    }
    shared = {
        "w_in": f(w_in[0]), "w_out": f(w_out[0]), "w_up": f(w_up[0]), "w_down": f(w_down[0]),
        "attn_g": f(attn_norm_g[0]), "mlp_g": f(mlp_norm_g[0]), "final_g": f(final_norm_g),
        "conv_w": f(conv_w[0]), "conv_b": f(conv_b[0]), "cn_g": f(conv_norm_g[0]), "cn_b": f(conv_norm_b[0]),
        "ropek": ropek,
    }
    shared.update(consts)
    in_maps = []
    for core in range(8):
        b, half = core // 2, core % 2
        hall = np.concatenate([meta, x[b]], axis=0)
        xown = np.zeros((NSLOT, 544, D), np.float32)
        ropeq = np.zeros((NSLOT, 512, 512), np.float32)
        qrel = np.zeros((128, NSLOT * 8), np.float32)
        for sl in range(NSLOT):
            c = _chunk_of(sl, half)
            p0 = NMETA + 512 * c
            lo = p0 - 32
            src_lo = max(lo, 0)
            xown[sl, src_lo - lo:, :] = hall[src_lo:p0 + 512]
            ropeq[sl] = ropek[p0:p0 + 512]
            w0 = NMETA + 1024 * sl
            for tb in range(4):
                for wc in range(2):
                    qrel[:, (sl * 4 + tb) * 2 + wc] = (p0 + tb * 128 + p) - w0 - 512 * wc
        m = dict(shared)
        m.update({"xall": hall, "xown": xown, "ropeq": ropeq, "qrel": qrel})
        in_maps.append(m)
    res = run_bass_kernel_spmd(nc, in_maps, core_ids=list(range(8)))
    out = np.zeros((B, S, D), np.float32)
    for core in range(8):
        b, half = core // 2, core % 2
        o = res.results[core]["out"]
        for sl in range(NSLOT):
            c = _chunk_of(sl, half)
            out[b, 512 * c:512 * (c + 1)] = o[sl * 512:(sl + 1) * 512]
    return out
```
